# Optimizing a Trainium2 kernel written in Bass

```python
import jax, jax.numpy as jnp
from jax import lax
import numpy as np

D_MODEL = 1024
BATCH = 2
SEQ = 8192
DEPTH = 2

MEM_LEN = 256
HEAD_DIM = D_MODEL // 16
H_NA = 6
H_DIL = 6
H_MEM = 4
W_NA = H_NA * HEAD_DIM
W_DIL = H_DIL * HEAD_DIM
W_MEM = H_MEM * HEAD_DIM
MIX_WIDTH = W_NA + W_DIL + W_MEM
IN_WIDTH = 3 * W_NA + 3 * W_DIL + W_MEM
GRID_W = 64
NA_KH = 8
NA_KW = 16
NA_QROWS = 2
RPB_H = 2 * NA_KH - 1
RPB_W = 2 * NA_KW - 1
DIL_CFG = ((128, 1), (512, 4), (2048, 16))
DIL_QBLOCK = 128
ROPE_THETA = 10000.0
D_FF = 2816
N_EXPERTS = 8
TOP_K = 2
D_FF_EXPERT = 3584
MOE_BLOCK = 256
N_DENSE = (DEPTH + 1) // 2
N_MOE = DEPTH // 2
RMS_EPS = 1e-6
NEG_INF = -1e30

kernel_name = "hybrid_na_dilated_mem_moe_encoder"


def rms_norm(x, g):
    xf = x.astype(jnp.float32)
    y = xf * lax.rsqrt(jnp.mean(xf * xf, axis=-1, keepdims=True) + RMS_EPS)
    return (y * g.astype(jnp.float32)).astype(x.dtype)


def apply_rope(x):
    T = x.shape[1]
    half = HEAD_DIM // 2
    inv_freq = jnp.power(ROPE_THETA, -(2.0 / HEAD_DIM) * jnp.arange(half, dtype=jnp.float32))
    ang = jnp.arange(T, dtype=jnp.float32)[:, None] * inv_freq[None, :]
    cos = jnp.cos(ang)[None, :, None, :]
    sin = jnp.sin(ang)[None, :, None, :]
    xf = x.astype(jnp.float32)
    x1, x2 = xf[..., :half], xf[..., half:]
    return jnp.concatenate([x1 * cos - x2 * sin, x1 * sin + x2 * cos], axis=-1).astype(x.dtype)


def neighborhood_attention(q, k, v, rpb):
    B, T, H, Dh = q.shape
    rows = T // GRID_W
    kh = min(NA_KH, rows)
    kw = NA_KW
    r = jnp.arange(rows, dtype=jnp.int32)
    row_idx = jnp.clip(r - kh // 2, 0, rows - kh)[:, None] + jnp.arange(kh, dtype=jnp.int32)[None, :]
    c = jnp.arange(GRID_W, dtype=jnp.int32)
    col_idx = jnp.clip(c - kw // 2, 0, GRID_W - kw)[:, None] + jnp.arange(kw, dtype=jnp.int32)[None, :]
    row_rel = row_idx - r[:, None] + (NA_KH - 1)
    col_rel = col_idx - c[:, None] + (NA_KW - 1)
    tok_idx = row_idx[:, None, :, None] * GRID_W + col_idx[None, :, None, :]
    bias_idx = row_rel[:, None, :, None] * RPB_W + col_rel[None, :, None, :]
    rpb_flat = rpb.reshape(H, RPB_H * RPB_W)
    qg = q.reshape(B, rows, GRID_W, H, Dh)
    scale = Dh ** -0.5
    n_blk = rows // NA_QROWS
    tok_blocks = tok_idx.reshape(n_blk, NA_QROWS, GRID_W, kh, kw)
    bias_blocks = bias_idx.reshape(n_blk, NA_QROWS, GRID_W, kh, kw)
    q_blocks = jnp.moveaxis(qg.reshape(B, n_blk, NA_QROWS, GRID_W, H, Dh), 1, 0)

    def row_block(args):
        q_blk, ti, bi = args
        k_nb = jnp.take(k, ti, axis=1)
        v_nb = jnp.take(v, ti, axis=1)
        logits = jnp.einsum('brchd,brcijhd->bhrcij', q_blk, k_nb).astype(jnp.float32) * scale
        bias = jnp.take(rpb_flat, bi, axis=1)
        logits = logits + bias[None].astype(jnp.float32)
        shp = logits.shape
        p = jax.nn.softmax(logits.reshape(shp[:4] + (kh * kw,)), axis=-1).reshape(shp)
        return jnp.einsum('bhrcij,brcijhd->brchd', p.astype(v.dtype), v_nb)

    outs = lax.map(row_block, (q_blocks, tok_blocks, bias_blocks))
    return jnp.moveaxis(outs, 0, 1).reshape(B, T, H, Dh)


def dilated_mixture_attention(q, k, v):
    B, T, H, Dh = q.shape
    scale = Dh ** -0.5
    offsets = []
    for w, d in DIL_CFG:
        n_side = (w // 2) // d
        offsets.append(d * jnp.arange(-n_side, n_side + 1, dtype=jnp.int32))
    n_blk = T // DIL_QBLOCK
    q_blocks = jnp.moveaxis(q.reshape(B, n_blk, DIL_QBLOCK, H, Dh), 1, 0)
    starts = jnp.arange(n_blk, dtype=jnp.int32) * DIL_QBLOCK

    def q_block(args):
        q_blk, t0 = args
        t = t0 + jnp.arange(DIL_QBLOCK, dtype=jnp.int32)
        outs, lses = [], []
        for off in offsets:
            pos = t[:, None] + off[None, :]
            valid = (pos >= 0) & (pos < T)
            pc = jnp.clip(pos, 0, T - 1)
            kk = jnp.take(k, pc, axis=1)
            vv = jnp.take(v, pc, axis=1)
            s = jnp.einsum('bqhd,bqkhd->bhqk', q_blk, kk).astype(jnp.float32) * scale
            s = jnp.where(valid[None, None], s, NEG_INF)
            m = jnp.max(s, axis=-1, keepdims=True)
            p = jnp.exp(s - m)
            l = jnp.sum(p, axis=-1, keepdims=True)
            o = jnp.einsum('bhqk,bqkhd->bhqd', (p / l).astype(v.dtype), vv)
            outs.append(o.astype(jnp.float32))
            lses.append((m + jnp.log(l))[..., 0])
        wts = jax.nn.softmax(jnp.stack(lses), axis=0)
        o = jnp.sum(wts[..., None] * jnp.stack(outs), axis=0)
        return jnp.transpose(o, (0, 2, 1, 3)).astype(q.dtype)

    outs = lax.map(q_block, (q_blocks, starts))
    return jnp.moveaxis(outs, 0, 1).reshape(B, T, H, Dh)


def memory_attention(q, km, vm):
    s = jnp.einsum('bthd,bmhd->bhtm', q, km).astype(jnp.float32) * (HEAD_DIM ** -0.5)
    p = jax.nn.softmax(s, axis=-1)
    return jnp.einsum('bhtm,bmhd->bthd', p.astype(vm.dtype), vm)


def swiglu(x, w_gate, w_up, w_down):
    return (jax.nn.silu(x @ w_gate) * (x @ w_up)) @ w_down


def moe_swiglu(x, w_router, w_gate, w_up, w_down):
    B, T, D = x.shape
    N = B * T
    NK = N * TOP_K
    xf = x.reshape(N, D)
    logits = (xf @ w_router).astype(jnp.float32)
    top_logit, top_idx = lax.top_k(logits, TOP_K)
    gates = jax.nn.softmax(top_logit, axis=-1)
    e_flat = top_idx.reshape(NK).astype(jnp.int32)
    tok_flat = jnp.broadcast_to(jnp.arange(N, dtype=jnp.int32)[:, None], (N, TOP_K)).reshape(NK)
    g_flat = gates.reshape(NK)
    order = jnp.argsort(e_flat)
    e_s = e_flat[order]
    tok_s = tok_flat[order]
    g_s = g_flat[order]
    counts = jnp.sum((e_flat[:, None] == jnp.arange(N_EXPERTS, dtype=jnp.int32)[None, :]).astype(jnp.int32), axis=0)
    padded = (counts + MOE_BLOCK - 1) // MOE_BLOCK * MOE_BLOCK
    start = jnp.cumsum(counts) - counts
    pend = jnp.cumsum(padded)
    pstart = pend - padded
    dest = pstart[e_s] + (jnp.arange(NK, dtype=jnp.int32) - start[e_s])
    P = NK + N_EXPERTS * MOE_BLOCK
    row_tok = jnp.full((P,), N, jnp.int32).at[dest].set(tok_s)
    row_gate = jnp.zeros((P,), jnp.float32).at[dest].set(g_s)
    n_blk = P // MOE_BLOCK
    blk_start = jnp.arange(n_blk, dtype=jnp.int32) * MOE_BLOCK
    blk_expert = jnp.minimum(
        jnp.sum((blk_start[:, None] >= pend[None, :]).astype(jnp.int32), axis=1),
        N_EXPERTS - 1)
    x_pad = jnp.concatenate([xf, jnp.zeros((1, D), xf.dtype)], axis=0)
    x_rows = jnp.take(x_pad, row_tok, axis=0).reshape(n_blk, MOE_BLOCK, D)

    def expert_block(args):
        xb, e = args
        return swiglu(xb, w_gate[e], w_up[e], w_down[e])

    y = lax.map(expert_block, (x_rows, blk_expert)).reshape(P, D)
    y = y * row_gate[:, None].astype(y.dtype)
    out = jax.ops.segment_sum(y, row_tok, num_segments=N + 1)[:N]
    return out.reshape(B, T, D)


def setup_inputs(seed: int = 0) -> dict:
    key = jax.random.key(seed)
    ks = jax.random.split(key, 24)
    f32 = jnp.float32

    def nrm(k, shape, scale):
        return jax.random.normal(k, shape, f32) * scale

    def gain(k, shape):
        return 1.0 + 0.05 * jax.random.normal(k, shape, f32)

    return {
        "x": nrm(ks[0], (BATCH, SEQ, D_MODEL), 1.0),
        "mem": nrm(ks[1], (BATCH, MEM_LEN, D_MODEL), 1.0),
        "g_attn": gain(ks[2], (DEPTH, D_MODEL)),
        "w_in": nrm(ks[3], (DEPTH, D_MODEL, IN_WIDTH), D_MODEL ** -0.5),
        "g_qk_na": gain(ks[4], (DEPTH, 2, HEAD_DIM)),
        "rpb_na": nrm(ks[5], (DEPTH, H_NA, RPB_H, RPB_W), 0.1),
        "g_qk_dil": gain(ks[6], (DEPTH, 2, HEAD_DIM)),
        "g_mem": gain(ks[7], (DEPTH, D_MODEL)),
        "w_mem_kv": nrm(ks[8], (DEPTH, D_MODEL, 2 * W_MEM), D_MODEL ** -0.5),
        "g_qk_mem": gain(ks[9], (DEPTH, 2, HEAD_DIM)),
        "g_out": gain(ks[10], (DEPTH, MIX_WIDTH)),
        "w_out": nrm(ks[11], (DEPTH, MIX_WIDTH, D_MODEL), MIX_WIDTH ** -0.5),
        "g_ffn": gain(ks[12], (DEPTH, D_MODEL)),
        "w_gate_dense": nrm(ks[13], (N_DENSE, D_MODEL, D_FF), D_MODEL ** -0.5),
        "w_up_dense": nrm(ks[14], (N_DENSE, D_MODEL, D_FF), D_MODEL ** -0.5),
        "w_down_dense": nrm(ks[15], (N_DENSE, D_FF, D_MODEL), D_FF ** -0.5),
        "w_router": nrm(ks[16], (N_MOE, D_MODEL, N_EXPERTS), D_MODEL ** -0.5),
        "w_gate_moe": nrm(ks[17], (N_MOE, N_EXPERTS, D_MODEL, D_FF_EXPERT), D_MODEL ** -0.5),
        "w_up_moe": nrm(ks[18], (N_MOE, N_EXPERTS, D_MODEL, D_FF_EXPERT), D_MODEL ** -0.5),
        "w_down_moe": nrm(ks[19], (N_MOE, N_EXPERTS, D_FF_EXPERT, D_MODEL), D_FF_EXPERT ** -0.5),
    }


def reference(x, mem, g_attn, w_in, g_qk_na, rpb_na, g_qk_dil, g_mem, w_mem_kv, g_qk_mem,
              g_out, w_out, g_ffn, w_gate_dense, w_up_dense, w_down_dense, w_router,
              w_gate_moe, w_up_moe, w_down_moe):
    B, T, _ = x.shape
    M = mem.shape[1]
    o1 = W_NA
    o2 = 2 * W_NA
    o3 = 3 * W_NA
    o4 = o3 + W_DIL
    o5 = o3 + 2 * W_DIL
    o6 = o3 + 3 * W_DIL
    h = x
    for layer in range(DEPTH):
        u = rms_norm(h, g_attn[layer])
        proj = u @ w_in[layer]
        qa = proj[..., :o1]
        ka = proj[..., o1:o2]
        va = proj[..., o2:o3]
        qb = proj[..., o3:o4]
        kb = proj[..., o4:o5]
        vb = proj[..., o5:o6]
        qm = proj[..., o6:]

        qa = rms_norm(qa.reshape(B, T, H_NA, HEAD_DIM), g_qk_na[layer, 0])
        ka = rms_norm(ka.reshape(B, T, H_NA, HEAD_DIM), g_qk_na[layer, 1])
        va = va.reshape(B, T, H_NA, HEAD_DIM)
        o_na = neighborhood_attention(qa, ka, va, rpb_na[layer])

        qb = apply_rope(rms_norm(qb.reshape(B, T, H_DIL, HEAD_DIM), g_qk_dil[layer, 0]))
        kb = apply_rope(rms_norm(kb.reshape(B, T, H_DIL, HEAD_DIM), g_qk_dil[layer, 1]))
        vb = vb.reshape(B, T, H_DIL, HEAD_DIM)
        o_dil = dilated_mixture_attention(qb, kb, vb)

        mem_n = rms_norm(mem, g_mem[layer])
        kv_m = mem_n @ w_mem_kv[layer]
        km = rms_norm(kv_m[..., :W_MEM].reshape(B, M, H_MEM, HEAD_DIM), g_qk_mem[layer, 1])
        vm = kv_m[..., W_MEM:].reshape(B, M, H_MEM, HEAD_DIM)
        qm = rms_norm(qm.reshape(B, T, H_MEM, HEAD_DIM), g_qk_mem[layer, 0])
        o_mem = memory_attention(qm, km, vm)

        go = g_out[layer]
        mixed = jnp.concatenate([
            rms_norm(o_na.reshape(B, T, W_NA), go[:W_NA]),
            rms_norm(o_dil.reshape(B, T, W_DIL), go[W_NA:W_NA + W_DIL]),
            rms_norm(o_mem.reshape(B, T, W_MEM), go[W_NA + W_DIL:]),
        ], axis=-1)
        h = h + mixed @ w_out[layer]

        u = rms_norm(h, g_ffn[layer])
        i = layer // 2
        if layer % 2 == 0:
            f = swiglu(u, w_gate_dense[i], w_up_dense[i], w_down_dense[i])
        else:
            f = moe_swiglu(u, w_router[i], w_gate_moe[i], w_up_moe[i], w_down_moe[i])
        h = h + f
    return h
```

```python
import numpy as np
import ml_dtypes
import numpy as np
from contextlib import ExitStack
import concourse.bass as bass
import concourse.mybir as mybir

F32 = mybir.dt.float32
BF16 = mybir.dt.bfloat16
AF = mybir.ActivationFunctionType
ALU = mybir.AluOpType
AX = mybir.AxisListType


class Buf:
    __slots__ = ("name", "w", "r", "t", "dsem")

    def __init__(self, name, t=None):
        self.name = name
        self.w = None
        self.r = []
        self.t = t
        self.dsem = None

    def __getitem__(self, idx):
        return self.t[idx]


class MK:
    SAME_ENGINE_SYNC = False

    def __init__(self, nc, es):
        self.nc = nc
        self.es = es
        self.eng = {"pe": nc.tensor, "act": nc.scalar, "dve": nc.vector, "pool": nc.gpsimd, "sp": nc.sync}
        self.esem = {k: es.enter_context(nc.semaphore("s_" + k)) for k in self.eng}
        self.ecnt = {k: 0 for k in self.eng}
        self.waited = {k: {} for k in self.eng}
        self.dsems = []
        self.dcnt = {}
        self.ndma = 0
        self.n_ins = 0

    def sb(self, name, shape, dt):
        t = self.es.enter_context(self.nc.sbuf_tensor(name, list(shape), dt))
        return Buf(name, t)

    def ps(self, name, shape, dt=F32):
        t = self.es.enter_context(self.nc.psum_tensor(name, list(shape), dt))
        return Buf(name, t)

    def new_dsem(self, name):
        s = self.es.enter_context(self.nc.semaphore(name))
        self.dcnt[id(s)] = [s, 0]
        return s

    def _wait(self, E, raw, other, strict=False):
        eng = self.eng[E]
        best = {}
        own = self.esem[E]
        for lst, is_raw in ((raw, True), (other, False)):
            for ev in lst:
                if ev is None:
                    continue
                sem, val = ev
                if sem is own and not self.SAME_ENGINE_SYNC:
                    if not (is_raw and (E != "pe" or strict)):
                        continue
                k = id(sem)
                if k not in best or best[k][1] < val:
                    best[k] = (sem, val)
        for k, (sem, val) in best.items():
            if self.waited[E].get(k, 0) >= val:
                continue
            eng.wait_ge(sem, val)
            self.waited[E][k] = val

    def _deps(self, reads, writes):
        raw = [b.w for b in reads]
        other = []
        for b in writes:
            other.append(b.w)
            other.extend(b.r)
        return raw, other

    def _commit(self, ev, reads, writes):
        for b in reads:
            b.r.append(ev)
            if len(b.r) > 64:
                best = {}
                for s, v in b.r:
                    if id(s) not in best or best[id(s)][1] < v:
                        best[id(s)] = (s, v)
                b.r = list(best.values())
        for b in writes:
            b.w = ev
            b.r = []

    def op(self, E, fn, reads=(), writes=(), strict=False):
        self._wait(E, *self._deps(reads, writes), strict=strict)
        ins = fn(self.eng[E])
        self.ecnt[E] += 1
        ins.then_inc(self.esem[E], 1)
        ev = (self.esem[E], self.ecnt[E])
        self._commit(ev, reads, writes)
        self.n_ins += 1
        return ins

    def dma(self, E, out, in_, reads=(), writes=(), dsem=None, **kw):
        self._wait(E, *self._deps(reads, writes))
        ins = self.eng[E].dma_start(out=out, in_=in_, **kw)
        wb = writes[0]
        if wb.dsem is None:
            wb.dsem = self.new_dsem("d_" + wb.name)
        dsem = wb.dsem
        rec = self.dcnt[id(dsem)]
        rec[1] += 16
        ins.then_inc(dsem, 16)
        ev = (dsem, rec[1])
        self._commit(ev, reads, writes)
        self.ndma += 1
        return ins

    def final_wait(self, E, bufs):
        self._wait(E, [b.w for b in bufs], [])


NT = 2048
CH = 512
NCH = NT // CH
EPS = 1e-6

QK_GROUPS = ([("q", j, 0 + 128 * j, 0, False) for j in range(3)]
             + [("k", j, 384 + 128 * j, 1, False) for j in range(3)]
             + [("q", 3 + j, 1152 + 128 * j, 2, True) for j in range(3)]
             + [("k", 3 + j, 1536 + 128 * j, 3, True) for j in range(3)]
             + [("q", 6 + j, 2304 + 128 * j, 4, False) for j in range(2)])


def rms_chunk(m, src, n, gvec, ones, epsb, ps_ss, sq, rs, uT, u32=None):
    m.op("act", lambda e: e.activation(out=sq[:, :, 0:n], in_=src[:, :, 0:n], func=AF.Square), reads=[src], writes=[sq])
    for c in range(8):
        m.op("pe", lambda e: e.matmul(ps_ss[:, 0:n], lhsT=ones[:, 0, :], rhs=sq[:, c, 0:n], start=(c == 0), stop=(c == 7)),
             reads=[sq, ones], writes=[ps_ss])
    m.op("act", lambda e: e.activation(out=rs[:, 0:n], in_=ps_ss[:, 0:n], func=AF.Ln, scale=1.0 / 1024, bias=epsb[:, 0:1]),
         reads=[ps_ss, epsb], writes=[rs])
    m.op("act", lambda e: e.activation(out=rs[:, 0:n], in_=rs[:, 0:n], func=AF.Exp, scale=-0.5), reads=[rs], writes=[rs])
    for c in range(8):
        m.op("dve", lambda e: e.scalar_tensor_tensor(out=uT[:, c, 0:n], in0=src[:, c, 0:n], scalar=gvec[:, c:c + 1],
                                                     in1=rs[:, 0:n], op0=ALU.mult, op1=ALU.mult),
             reads=[src, gvec, rs], writes=[uT])
        if u32 is not None:
            m.op("pool", lambda e: e.scalar_tensor_tensor(out=u32[:, c, 0:n], in0=src[:, c, 0:n], scalar=gvec[:, c:c + 1],
                                                          in1=rs[:, 0:n], op0=ALU.mult, op1=ALU.mult),
                 reads=[src, gvec, rs], writes=[u32])


def build_pa():
    nc = bass.Bass("TRN2", target_bir_lowering=False)
    D = lambda n, s, dt, k: nc.dram_tensor(n, s, dt, kind=k).ap()
    hT = D("hT", [128, 8, NT], F32, "ExternalInput")
    w_in = D("w_in", [128, 8, 2560], F32, "ExternalInput")
    gattn = D("gattn", [128, 8], F32, "ExternalInput")
    gqk = D("gqk", [128, 8], F32, "ExternalInput")
    cmat = D("cmat", [128, 3, 128], F32, "ExternalInput")
    cs = D("cs", [128, 2, NT], F32, "ExternalInput")
    memT = D("memT", [128, 8, 256], F32, "ExternalInput")
    gmem = D("gmem", [128, 8], F32, "ExternalInput")
    w_kv = D("w_kv", [128, 8, 512], F32, "ExternalInput")
    qT_o = D("qT", [128, 8, NT], BF16, "ExternalOutput")
    kT_o = D("kT", [128, 6, NT], BF16, "ExternalOutput")
    v_o = D("v", [128, 16, 12 * 65], BF16, "ExternalOutput")
    kmT_o = D("kmT", [128, 2, 256], BF16, "ExternalOutput")
    vm_o = D("vm", [128, 2, 4 * 65], BF16, "ExternalOutput")

    with ExitStack() as es:
        m = MK(nc, es)
        W = m.sb("W", [128, 8, 2560], BF16)
        Wkv = m.sb("Wkv", [128, 8, 512], BF16)
        cm = m.sb("cm", [128, 3, 128], BF16)
        ga = m.sb("ga", [128, 8], F32)
        gm = m.sb("gm", [128, 8], F32)
        gq = m.sb("gq", [128, 8], F32)
        epsb = m.sb("epsb", [128, 1], F32)
        cst = [m.sb("cst%d" % i, [128, 2, CH], F32) for i in range(2)]
        hc = [m.sb("hc%d" % i, [128, 8, CH], F32) for i in range(2)]
        sq = m.sb("sq", [128, 8, CH], BF16)
        rs = m.sb("rs", [128, CH], F32)
        uT = [m.sb("uT%d" % i, [128, 8, CH], BF16) for i in range(2)]
        sq2 = [m.sb("sq2%d" % i, [128, CH], BF16) for i in range(2)]
        r2 = [m.sb("r2%d" % i, [128, CH], F32) for i in range(2)]
        qn = [m.sb("qn%d" % i, [128, CH], BF16) for i in range(2)]
        t1 = [m.sb("t1%d" % i, [128, CH], F32) for i in range(2)]
        t2 = [m.sb("t2%d" % i, [128, CH], F32) for i in range(2)]
        qTs2 = [m.sb("qTs%d" % i, [128, 8, CH], BF16) for i in range(2)]
        kTs2 = [m.sb("kTs%d" % i, [128, 6, CH], BF16) for i in range(2)]
        vs2 = [m.sb("vs%d" % i, [128, 4, 12 * 65], BF16) for i in range(2)]
        kms = m.sb("kms", [128, 2, 256], BF16)
        vms = m.sb("vms", [128, 2, 4 * 65], BF16)
        ps_ss = m.ps("ps_ss", [128, 512])
        ps_y = [m.ps("ps_y%d" % i, [128, 512]) for i in range(2)]
        ps_s2 = [m.ps("ps_s2%d" % i, [128, 512]) for i in range(2)]
        ps_qp = m.ps("ps_qp", [128, 512])
        ps_v = [m.ps("ps_v%d" % i, [128, 512]) for i in range(2)]

        dW = [m.new_dsem("dW%d" % i) for i in range(4)]
        dsm = m.new_dsem("dsm")
        dh = [m.new_dsem("dh%d" % i) for i in range(2)]
        dc = [m.new_dsem("dc%d" % i) for i in range(2)]
        dout = m.new_dsem("dout")

        m.dma("sp", ga[:], gattn, writes=[ga], dsem=dsm)
        m.dma("sp", gm[:], gmem, writes=[gm], dsem=dsm)
        m.dma("sp", gq[:], gqk, writes=[gq], dsem=dsm)
        m.dma("pool", cm[:], cmat, writes=[cm], dsem=dW[0])
        m.op("dve", lambda e: e.memset(epsb[:], EPS), writes=[epsb])
        for i in range(2):
            m.op("dve", lambda e: e.memset(vs2[i][:], 1.0), writes=[vs2[i]])
        m.op("dve", lambda e: e.memset(vms[:], 1.0), writes=[vms])
        Wb = [Buf("Wb%d" % i) for i in range(4)]
        for i in range(4):
            m.dma("pool", W[:, :, 640 * i:640 * (i + 1)], w_in[:, :, 640 * i:640 * (i + 1)], writes=[Wb[i]], dsem=dW[i])
        m.dma("pool", Wkv[:], w_kv, writes=[Wkv], dsem=dW[0])

        def wbufs(c0, c1):
            return [Wb[i] for i in range(4) if c0 < 640 * (i + 1) and c1 > 640 * i]

        cnt = {"y": 0, "v": 0}

        def qk_group(u, n, Wt, wdeps, col0, gcolumn, rope, dst, dsti, tok0, csb):
            i = cnt["y"] % 2
            cnt["y"] += 1
            y, s2, sq2b, r2b, qnb = ps_y[i], ps_s2[i], sq2[i], r2[i], qn[i]
            for c in range(8):
                m.op("pe", lambda e: e.matmul(y[:, 0:n], lhsT=Wt[:, c, col0:col0 + 128], rhs=u[:, c, 0:n], start=(c == 0), stop=(c == 7)),
                     reads=[u] + wdeps, writes=[y])
            m.op("act", lambda e: e.activation(out=sq2b[:, 0:n], in_=y[:, 0:n], func=AF.Square), reads=[y], writes=[sq2b])
            m.op("pe", lambda e: e.matmul(s2[:, 0:n], lhsT=cm[:, 1, :], rhs=sq2b[:, 0:n], start=True, stop=True), reads=[sq2b, cm], writes=[s2])
            m.op("act", lambda e: e.activation(out=r2b[:, 0:n], in_=s2[:, 0:n], func=AF.Ln, scale=1.0 / 64, bias=epsb[:, 0:1]),
                 reads=[s2, epsb], writes=[r2b])
            m.op("act", lambda e: e.activation(out=r2b[:, 0:n], in_=r2b[:, 0:n], func=AF.Exp, scale=-0.5), reads=[r2b], writes=[r2b])
            d = dst[:, dsti, tok0:tok0 + n]
            if not rope:
                m.op("dve", lambda e: e.scalar_tensor_tensor(out=d, in0=y[:, 0:n], scalar=gq[:, gcolumn:gcolumn + 1], in1=r2b[:, 0:n],
                                                             op0=ALU.mult, op1=ALU.mult), reads=[y, gq, r2b], writes=[dst])
            else:
                t1b, t2b = t1[i], t2[i]
                m.op("dve", lambda e: e.scalar_tensor_tensor(out=qnb[:, 0:n], in0=y[:, 0:n], scalar=gq[:, gcolumn:gcolumn + 1], in1=r2b[:, 0:n],
                                                             op0=ALU.mult, op1=ALU.mult), reads=[y, gq, r2b], writes=[qnb])
                m.op("pe", lambda e: e.matmul(ps_qp[:, 0:n], lhsT=cm[:, 2, :], rhs=qnb[:, 0:n], start=True, stop=True), reads=[qnb, cm], writes=[ps_qp])
                m.op("pool", lambda e: e.tensor_tensor(out=t1b[:, 0:n], in0=qnb[:, 0:n], in1=csb[:, 0, 0:n], op=ALU.mult), reads=[qnb, csb], writes=[t1b])
                m.op("dve", lambda e: e.tensor_tensor(out=t2b[:, 0:n], in0=ps_qp[:, 0:n], in1=csb[:, 1, 0:n], op=ALU.mult), reads=[ps_qp, csb], writes=[t2b])
                m.op("pool", lambda e: e.tensor_tensor(out=d, in0=t1b[:, 0:n], in1=t2b[:, 0:n], op=ALU.add), reads=[t1b, t2b], writes=[dst])

        def v_tile(u, Wt, wdeps, col0, ncols, sub, dst, tile, head0, nheads):
            i = cnt["v"] % 2
            cnt["v"] += 1
            pv = ps_v[i]
            for c in range(8):
                m.op("pe", lambda e: e.matmul(pv[:, 0:ncols], lhsT=u[:, c, sub * 128:(sub + 1) * 128], rhs=Wt[:, c, col0:col0 + ncols],
                                              start=(c == 0), stop=(c == 7)), reads=[u] + wdeps, writes=[pv])
            dv = dst[:, tile, head0 * 65:(head0 + nheads) * 65].rearrange("p (h d) -> p h d", d=65)[:, :, 0:64]
            sv = pv[:, 0:ncols].rearrange("p (h d) -> p h d", d=64)
            m.op("act", lambda e: e.activation(out=dv, in_=sv, func=AF.Copy), reads=[pv], writes=[dst])

        m.dma("sp", hc[0][:, :, 0:256], memT, writes=[hc[0]], dsem=dh[0])
        rms_chunk(m, hc[0], 256, gm, cm, epsb, ps_ss, sq, rs, uT[0])
        for j in range(2):
            qk_group(uT[0], 256, Wkv, [Wkv], 128 * j, 5, False, kms, j, 0, None)
        for sub in range(2):
            v_tile(uT[0], Wkv, [Wkv], 256, 256, sub, vms, sub, 0, 4)
        obm = [Buf("o1"), Buf("o2")]
        m.dma("sp", kmT_o, kms[:], reads=[kms], writes=[obm[0]], dsem=dout)
        m.dma("sp", vm_o, vms[:], reads=[vms], writes=[obm[1]], dsem=dout)

        ob = [Buf("oq"), Buf("ok"), Buf("ov")]
        for t in range(NCH):
            b = (t + 1) % 2
            tok0 = t * CH
            m.dma("sp", hc[b][:], hT[:, :, tok0:tok0 + CH], writes=[hc[b]], dsem=dh[b])
            m.dma("sp", cst[b][:], cs[:, :, tok0:tok0 + CH], writes=[cst[b]], dsem=dc[b])
            rms_chunk(m, hc[b], CH, ga, cm, epsb, ps_ss, sq, rs, uT[b])
            qTs, kTs, vs = qTs2[b], kTs2[b], vs2[b]
            for (dn, di, col0, gc, rope) in QK_GROUPS:
                qk_group(uT[b], CH, W, wbufs(col0, col0 + 128), col0, gc, rope, qTs if dn == "q" else kTs, di, 0, cst[b])
            for sub in range(4):
                v_tile(uT[b], W, wbufs(768, 1152), 768, 384, sub, vs, sub, 0, 6)
                v_tile(uT[b], W, wbufs(1920, 2304), 1920, 384, sub, vs, sub, 6, 6)
            m.dma("sp", qT_o[:, :, tok0:tok0 + CH], qTs[:], reads=[qTs], writes=[ob[0]], dsem=dout)
            m.dma("sp", kT_o[:, :, tok0:tok0 + CH], kTs[:], reads=[kTs], writes=[ob[1]], dsem=dout)
            m.dma("sp", v_o[:, 4 * t:4 * t + 4, :], vs[:], reads=[vs], writes=[ob[2]], dsem=dout)
        m.final_wait("sp", ob + obm)
        print("P_A instrs", m.n_ins, "dmas", m.ndma)
    return nc


NT = 2048
EPS = 1e-6
SCALE = 0.125
DEBUG = False


def build_pb():
    nc = bass.Bass("TRN2", target_bir_lowering=False)
    D = lambda n, s, dt, k: nc.dram_tensor(n, s, dt, kind=k).ap()
    qT = D("qT", [128, 8, NT], BF16, "ExternalInput")
    kA = D("kA", [128, 3, 2560], BF16, "ExternalInput")
    vA = D("vA", [128, 3, 20, 130], BF16, "ExternalInput")
    kAe = D("kAe", [128, 3, 4, 640], BF16, "ExternalInput")
    vAe = D("vAe", [128, 3, 4, 650], BF16, "ExternalInput")
    kB = D("kB", [128, 3, 4096], BF16, "ExternalInput")
    vB = D("vB", [128, 3, 32, 130], BF16, "ExternalInput")
    kM = D("kM", [128, 2, 256], BF16, "ExternalInput")
    vM = D("vM", [128, 2, 2, 130], BF16, "ExternalInput")
    nab = D("nab", [128, 3, 5, 1280], F32, "ExternalInput")
    cmask = D("cmask", [128, 17 * 128], F32, "ExternalInput")
    ident = D("ident", [128, 128], F32, "ExternalInput")
    gout = D("gout", [128, 1024], F32, "ExternalInput")
    w_out = D("w_out", [128, 8, 1024], F32, "ExternalInput")
    hT = D("hT", [128, 8, NT], F32, "ExternalInput")
    hmid = D("hmidT", [128, 8, NT], F32, "ExternalOutput")
    o_dbg = D("o_dbg", [128, 16, 1024], BF16, "ExternalOutput") if DEBUG else None
    dbg2 = D("dbg2", [128, 1024], BF16, "ExternalOutput") if DEBUG else None
    dbg3 = D("dbg3", [128, 4], F32, "ExternalOutput") if DEBUG else None
    dbg4 = D("dbg4", [128, 8, 512], BF16, "ExternalOutput") if DEBUG else None

    with ExitStack() as es:
        m = MK(nc, es)
        qp = [m.sb("qp%d" % i, [128, NT], BF16) for i in range(2)]
        kb = [m.sb("kb%d" % i, [128, 4096], BF16) for i in range(2)]
        vb = [m.sb("vb%d" % i, [128, 32, 130], BF16) for i in range(2)]
        ke = [m.sb("ke0", [128, 4, 640], BF16)] * 2
        ve = [m.sb("ve0", [128, 4, 650], BF16)] * 2
        stage = [m.sb("stage%d" % i, [128, 1280], F32) for i in range(2)]
        Mna = [m.sb("Mna0", [128, 5, 1280], BF16)] * 2
        Cm = m.sb("Cm", [128, 17 * 128], BF16)
        idn = m.sb("idn", [128, 128], BF16)
        go = m.sb("go", [128, 1024], F32)
        Wo = m.sb("Wo", [128, 8, 1024], BF16)
        epsb = m.sb("epsb", [128, 1], F32)
        o_all = m.sb("o_all", [128, 16, 1024], BF16)
        Eb = [m.sb("E%d" % i, [128, 512], BF16) for i in range(3)]
        Pb = [m.sb("P%d" % i, [128, 512], BF16) for i in range(3)]
        rec = [m.sb("rec%d" % i, [128, 2], F32) for i in range(2)]
        sqf = m.sb("sqf", [128, 1024], F32)
        ms = [m.sb("ms%d" % i, [128, 4], F32) for i in range(2)]
        mixed = [m.sb("mixed%d" % i, [128, 1024], BF16) for i in range(2)]
        mixedT = [m.sb("mixedT%d" % i, [128, 8, 512], BF16) for i in range(2)]
        hc = [m.sb("hc0", [128, 8, 512], F32)] * 2
        ps_s = [m.ps("ps_s%d" % i, [128, 512]) for i in range(3)]
        ps_o = [m.ps("ps_o%d" % i, [128, 512]) for i in range(2)]
        ps_T = m.ps("ps_T", [128, 1024], BF16)
        ps_y = [m.ps("ps_y%d" % i, [128, 512]) for i in range(2)]

        dl = [m.new_dsem("dl%d" % i) for i in range(2)]
        dst_ = [m.new_dsem("dst%d" % i) for i in range(2)]
        dcst = m.new_dsem("dcst")
        dh = [m.new_dsem("dh%d" % i) for i in range(2)]
        dout = m.new_dsem("dout")

        m.dma("pool", Cm[:], cmask, writes=[Cm], dsem=dcst)
        m.dma("pool", idn[:], ident, writes=[idn], dsem=dcst)
        m.dma("sp", go[:], gout, writes=[go], dsem=dcst)
        m.op("dve", lambda e: e.memset(epsb[:], EPS), writes=[epsb])
        Wo_loaded = [False]

        gcnt = [0]
        stc = [0]
        SLOT = {0: 0, 1: 1, 14: 2, 15: 3}
        VAR = {0: 0, 1: 1, 14: 3, 15: 4}

        for p in range(8):
            kind = "A" if p < 3 else ("B" if p < 6 else "M")
            b = p % 2
            m.dma("sp", qp[b][:], qT[:, p, :], writes=[qp[b]], dsem=dl[b])
            if kind == "A":
                m.dma("sp", kb[b][:, 0:2560], kA[:, p, :], writes=[kb[b]], dsem=dl[b])
                m.dma("sp", vb[b][:, 0:20, :], vA[:, p], writes=[vb[b]], dsem=dl[b])
                m.dma("sp", ke[b][:], kAe[:, p], writes=[ke[b]], dsem=dl[b])
                m.dma("sp", ve[b][:], vAe[:, p], writes=[ve[b]], dsem=dl[b])
                for v in range(5):
                    sb_ = stc[0] % 2
                    stc[0] += 1
                    m.dma("sp", stage[sb_][:], nab[:, p, v, :], writes=[stage[sb_]], dsem=dst_[sb_])
                    m.op("act", lambda e: e.activation(out=Mna[b][:, v, :], in_=stage[sb_][:], func=AF.Exp), reads=[stage[sb_]], writes=[Mna[b]])
            elif kind == "B":
                m.dma("sp", kb[b][:], kB[:, p - 3, :], writes=[kb[b]], dsem=dl[b])
                m.dma("sp", vb[b][:], vB[:, p - 3], writes=[vb[b]], dsem=dl[b])
            else:
                m.dma("sp", kb[b][:, 0:256], kM[:, p - 6, :], writes=[kb[b]], dsem=dl[b])
                m.dma("sp", vb[b][:, 0:2, :], vM[:, p - 6], writes=[vb[b]], dsem=dl[b])
            if p == 2 and not Wo_loaded[0]:
                for cc in range(0, 8, 2):
                    m.dma("pool", Wo[:, cc:cc + 2, :], w_out[:, cc:cc + 2, :], writes=[Wo], dsem=dcst)
                Wo_loaded[0] = True

            for i in range(16):
                O = ps_o[i % 2]
                for hh in range(2):
                    pbs = 64 * hh
                    tiles = []
                    if kind == "A":
                        slot = SLOT.get(i)
                        var = VAR.get(i, 2)
                        for j in range(5):
                            if slot is None:
                                k_ap = kb[b][pbs:pbs + 64, (i + j) * 128:(i + j + 1) * 128]
                                v_ap = vb[b][:, i + j, hh * 65:(hh + 1) * 65]
                            else:
                                k_ap = ke[b][pbs:pbs + 64, slot, j * 128:(j + 1) * 128]
                                v_ap = ve[b][:, slot, j * 130 + hh * 65:j * 130 + (hh + 1) * 65]
                            tiles.append((k_ap, v_ap, (Mna[b], var, hh * 640 + j * 128)))
                        kdeps = [kb[b], ke[b]]
                        vdeps = [vb[b], ve[b]]
                    elif kind == "B":
                        for j in range(17):
                            k_ap = kb[b][pbs:pbs + 64, (i + j) * 128:(i + j + 1) * 128]
                            v_ap = vb[b][:, i + j, hh * 65:(hh + 1) * 65]
                            tiles.append((k_ap, v_ap, (Cm, None, j * 128)))
                        kdeps = [kb[b]]
                        vdeps = [vb[b]]
                    else:
                        for j in range(2):
                            k_ap = kb[b][pbs:pbs + 64, j * 128:(j + 1) * 128]
                            v_ap = vb[b][:, j, hh * 65:(hh + 1) * 65]
                            tiles.append((k_ap, v_ap, None))
                        kdeps = [kb[b]]
                        vdeps = [vb[b]]
                    q_ap = qp[b][pbs:pbs + 64, i * 128:(i + 1) * 128]
                    nt = len(tiles)
                    for g0 in range(0, nt, 4):
                        grp = tiles[g0:g0 + 4]
                        n = len(grp) * 128
                        gi = gcnt[0] % 3
                        gcnt[0] += 1
                        S, E, P = ps_s[gi], Eb[gi], Pb[gi]
                        for t, (k_ap, v_ap, mk_) in enumerate(grp):
                            m.op("pe", lambda e: e.matmul(S[:, t * 128:(t + 1) * 128], lhsT=k_ap, rhs=q_ap, start=True, stop=True),
                                 reads=kdeps + [qp[b]], writes=[S])
                        m.op("act", lambda e: e.activation(out=E[:, 0:n], in_=S[:, 0:n], func=AF.Exp, scale=SCALE), reads=[S], writes=[E])
                        mk0 = grp[0][2]
                        if mk0 is not None:
                            mb, var, c0 = mk0
                            m_ap = mb[:, c0:c0 + n] if var is None else mb[:, var, c0:c0 + n]
                            m.op("dve", lambda e: e.tensor_tensor(out=P[:, 0:n], in0=E[:, 0:n], in1=m_ap, op=ALU.mult), reads=[E, mb], writes=[P])
                            src = P
                        else:
                            src = E
                        for t, (k_ap, v_ap, mk_) in enumerate(grp):
                            first = (g0 + t == 0)
                            last = (g0 + t == nt - 1)
                            m.op("pe", lambda e: e.matmul(O[:, hh * 65:(hh + 1) * 65], lhsT=src[:, t * 128:(t + 1) * 128], rhs=v_ap, start=first, stop=last),
                                 reads=vdeps + [src], writes=[O])
                rc = rec[i % 2]
                Ov = O[:, 0:130].rearrange("p (h d) -> p h d", d=65)
                m.op("dve", lambda e: e.reciprocal(out=rc[:, 0:2], in_=Ov[:, :, 64]), reads=[O], writes=[rc])
                for hh in range(2):
                    m.op("dve", lambda e: e.tensor_scalar(out=o_all[:, i, p * 128 + hh * 64:p * 128 + hh * 64 + 64], in0=O[:, hh * 65:hh * 65 + 64],
                                                          scalar1=rc[:, hh:hh + 1], scalar2=None, op0=ALU.mult), reads=[O, rc], writes=[o_all], strict=(hh == 0))

        obd = Buf("odbg_out")
        if DEBUG:
            m.dma("sp", o_dbg, o_all[:], reads=[o_all], writes=[obd], dsem=dout)
        GR = [(0, 384), (384, 768), (768, 1024)]
        ob = Buf("hmid_out")
        for i in range(16):
            b = i % 2
            msb = ms[b]
            m.op("dve", lambda e: e.tensor_tensor(out=sqf[:, :], in0=o_all[:, i, :], in1=o_all[:, i, :], op=ALU.mult), reads=[o_all], writes=[sqf])
            for g, (c0, c1) in enumerate(GR):
                m.op("dve", lambda e: e.tensor_reduce(out=msb[:, g:g + 1], in_=sqf[:, c0:c1], axis=AX.X, op=ALU.add), reads=[sqf], writes=[msb])
            for g, (c0, c1) in enumerate(GR):
                m.op("act", lambda e: e.activation(out=msb[:, g:g + 1], in_=msb[:, g:g + 1], func=AF.Ln, scale=1.0 / (c1 - c0), bias=epsb[:, 0:1]),
                     reads=[msb, epsb], writes=[msb])
            m.op("act", lambda e: e.activation(out=msb[:, 0:3], in_=msb[:, 0:3], func=AF.Exp, scale=-0.5), reads=[msb], writes=[msb])
            for g, (c0, c1) in enumerate(GR):
                m.op("dve", lambda e: e.scalar_tensor_tensor(out=mixed[b][:, c0:c1], in0=o_all[:, i, c0:c1], scalar=msb[:, g:g + 1], in1=go[:, c0:c1],
                                                             op0=ALU.mult, op1=ALU.mult), reads=[o_all, msb, go], writes=[mixed[b]])
            for c in range(8):
                m.op("pe", lambda e: e.transpose(ps_T[:, c * 128:(c + 1) * 128], mixed[b][:, c * 128:(c + 1) * 128], idn[:]), reads=[mixed[b], idn], writes=[ps_T])
            cb = (i // 4) % 2
            m.op("act", lambda e: e.activation(out=mixedT[cb][:, :, (i % 4) * 128:(i % 4 + 1) * 128], in_=ps_T[:, :].rearrange("p (c t) -> p c t", t=128), func=AF.Copy),
                 reads=[ps_T], writes=[mixedT[cb]])
            if i % 4 == 3:
                ch = i // 4
                m.dma("sp", hc[cb][:], hT[:, :, ch * 512:(ch + 1) * 512], writes=[hc[cb]], dsem=dh[cb])
                for d in range(8):
                    Y = ps_y[d % 2]
                    for c in range(8):
                        m.op("pe", lambda e: e.matmul(Y[:, :], lhsT=Wo[:, c, d * 128:(d + 1) * 128], rhs=mixedT[cb][:, c, :], start=(c == 0), stop=(c == 7)),
                             reads=[Wo, mixedT[cb]], writes=[Y])
                    m.op("dve", lambda e: e.tensor_tensor(out=hc[cb][:, d, :], in0=Y[:, :], in1=hc[cb][:, d, :], op=ALU.add), reads=[Y, hc[cb]], writes=[hc[cb]])
                m.dma("sp", hmid[:, :, ch * 512:(ch + 1) * 512], hc[cb][:], reads=[hc[cb]], writes=[ob], dsem=dout)
        if DEBUG:
            m.dma("sp", dbg2, mixed[1][:], reads=[mixed[1]], writes=[obd], dsem=dout)
            m.dma("sp", dbg3, ms[1][:], reads=[ms[1]], writes=[obd], dsem=dout)
            m.dma("sp", dbg4, mixedT[1][:], reads=[mixedT[1]], writes=[obd], dsem=dout)
        m.final_wait("sp", [ob, obd] if DEBUG else [ob])
        print("P_B instrs", m.n_ins, "dmas", m.ndma)
    return nc


EPS = 1e-6


class FFNRes:
    def __init__(self, m, F, NB):
        self.m, self.F, self.NB = m, F, NB
        self.wg = [m.sb("wg%d" % i, [128, 8, 128], BF16) for i in range(3)]
        self.wu = [m.sb("wu%d" % i, [128, 8, 128], BF16) for i in range(3)]
        self.wd = [m.sb("wd%d" % i, [128, F, 128], BF16) for i in range(2)]
        self.act = m.sb("act", [128, F, NB], BF16)
        self.sg = [m.sb("sg%d" % i, [128, 512], F32) for i in range(2)]
        self.ps_g = [m.ps("ps_g%d" % i, [128, 512]) for i in range(2)]
        self.ps_u = [m.ps("ps_u%d" % i, [128, 512]) for i in range(2)]
        self.ps_y = [m.ps("ps_y%d" % i, [128, 512]) for i in range(2)]
        self.wc = 0
        self.dc = 0
        self.gc = 0
        self.yc = 0


def ffn_block(R, uT, wg_ap, wu_ap, wd_ap, sink):
    m, F, NB = R.m, R.F, R.NB
    nch = NB // 512
    for f in range(F):
        i = R.wc % 3
        R.wc += 1
        wg, wu = R.wg[i], R.wu[i]
        m.dma("pool", wg[:].rearrange("p c n -> p (c n)"), wg_ap(f), writes=[wg])
        m.dma("pool", wu[:].rearrange("p c n -> p (c n)"), wu_ap(f), writes=[wu])
        for ch in range(nch):
            j = R.gc % 2
            R.gc += 1
            G, U, sg = R.ps_g[j], R.ps_u[j], R.sg[j]
            for c in range(8):
                m.op("pe", lambda e: e.matmul(G[:, :], lhsT=wg[:, c, :], rhs=uT[:, c, ch * 512:(ch + 1) * 512], start=(c == 0), stop=(c == 7)),
                     reads=[wg, uT], writes=[G])
            for c in range(8):
                m.op("pe", lambda e: e.matmul(U[:, :], lhsT=wu[:, c, :], rhs=uT[:, c, ch * 512:(ch + 1) * 512], start=(c == 0), stop=(c == 7)),
                     reads=[wu, uT], writes=[U])
            m.op("act", lambda e: e.activation(out=sg[:, :], in_=G[:, :], func=AF.Silu), reads=[G], writes=[sg])
            m.op("dve", lambda e: e.tensor_tensor(out=R.act[:, f, ch * 512:(ch + 1) * 512], in0=U[:, :], in1=sg[:, :], op=ALU.mult),
                 reads=[U, sg], writes=[R.act])
    for d in range(8):
        i = R.dc % 2
        R.dc += 1
        wd = R.wd[i]
        m.dma("pool", wd[:].rearrange("p f n -> p (f n)"), wd_ap(d), writes=[wd])
        for ch in range(nch):
            Y = R.ps_y[R.yc % 2]
            R.yc += 1
            for f in range(F):
                m.op("pe", lambda e: e.matmul(Y[:, :], lhsT=wd[:, f, :], rhs=R.act[:, f, ch * 512:(ch + 1) * 512], start=(f == 0), stop=(f == F - 1)),
                     reads=[wd, R.act], writes=[Y])
            sink(d, ch, Y)


def build_pc_dense(F=22, NT=2048, NB=1024):
    nc = bass.Bass("TRN2", target_bir_lowering=False)
    D = lambda n, s, dt, k: nc.dram_tensor(n, s, dt, kind=k).ap()
    hT = D("hT", [128, 8, NT], F32, "ExternalInput")
    gffn = D("gffn", [128, 8], F32, "ExternalInput")
    cmat = D("cmat", [128, 3, 128], F32, "ExternalInput")
    wg = D("wg", [F, 128, 1024], F32, "ExternalInput")
    wu = D("wu", [F, 128, 1024], F32, "ExternalInput")
    wd = D("wd", [8, 128, F * 128], F32, "ExternalInput")
    out = D("outT", [128, 8, NT], F32, "ExternalOutput")
    with ExitStack() as es:
        m = MK(nc, es)
        R = FFNRes(m, F, NB)
        cm = m.sb("cm", [128, 3, 128], BF16)
        gf = m.sb("gf", [128, 8], F32)
        epsb = m.sb("epsb", [128, 1], F32)
        hb = m.sb("hb", [128, 8, NB], F32)
        uT = m.sb("uT", [128, 8, NB], BF16)
        uc = [m.sb("uc%d" % i, [128, 8, 512], BF16) for i in range(1)]
        sq = m.sb("sq", [128, 8, 512], BF16)
        rs = m.sb("rs", [128, 512], F32)
        ps_ss = m.ps("ps_ss", [128, 512])
        m.dma("pool", cm[:], cmat, writes=[cm])
        m.dma("sp", gf[:], gffn, writes=[gf])
        m.op("dve", lambda e: e.memset(epsb[:], EPS), writes=[epsb])
        ob = Buf("out")
        for hb_i in range(NT // NB):
            t0 = hb_i * NB
            for ch in range(NB // 512):
                m.dma("sp", hb[:, :, ch * 512:(ch + 1) * 512], hT[:, :, t0 + ch * 512:t0 + (ch + 1) * 512], writes=[hb])
            for ch in range(NB // 512):
                rms_view(m, hb, ch * 512, 512, gf, cm, epsb, ps_ss, sq, rs, uT)

            def sink(d, ch, Y):
                m.op("dve", lambda e: e.tensor_tensor(out=hb[:, d, ch * 512:(ch + 1) * 512], in0=Y[:, :], in1=hb[:, d, ch * 512:(ch + 1) * 512], op=ALU.add),
                     reads=[Y, hb], writes=[hb])
            ffn_block(R, uT, lambda f: wg[f], lambda f: wu[f], lambda d: wd[d], sink)
            m.dma("sp", out[:, :, t0:t0 + NB], hb[:], reads=[hb], writes=[ob])
        m.final_wait("sp", [ob])
        print("P_C instrs", m.n_ins, "dmas", m.ndma)
    return nc


def rms_view(m, src, c0, n, gvec, cm, epsb, ps_ss, sq, rs, uT, u32=None):
    m.op("act", lambda e: e.activation(out=sq[:, :, 0:n], in_=src[:, :, c0:c0 + n], func=AF.Square), reads=[src], writes=[sq])
    for c in range(8):
        m.op("pe", lambda e: e.matmul(ps_ss[:, 0:n], lhsT=cm[:, 0, :], rhs=sq[:, c, 0:n], start=(c == 0), stop=(c == 7)), reads=[sq, cm], writes=[ps_ss])
    m.op("act", lambda e: e.activation(out=rs[:, 0:n], in_=ps_ss[:, 0:n], func=AF.Ln, scale=1.0 / 1024, bias=epsb[:, 0:1]), reads=[ps_ss, epsb], writes=[rs])
    m.op("act", lambda e: e.activation(out=rs[:, 0:n], in_=rs[:, 0:n], func=AF.Exp, scale=-0.5), reads=[rs], writes=[rs])
    for c in range(8):
        m.op("dve", lambda e: e.scalar_tensor_tensor(out=uT[:, c, c0:c0 + n], in0=src[:, c, c0:c0 + n], scalar=gvec[:, c:c + 1], in1=rs[:, 0:n],
                                                     op0=ALU.mult, op1=ALU.mult), reads=[src, gvec, rs], writes=[uT])
        if u32 is not None:
            m.op("dve", lambda e: e.scalar_tensor_tensor(out=u32[:, c, 0:n], in0=src[:, c, c0:c0 + n], scalar=gvec[:, c:c + 1], in1=rs[:, 0:n],
                                                         op0=ALU.mult, op1=ALU.mult), reads=[src, gvec, rs], writes=[u32])


def build_l5(NT=2048):
    nc = bass.Bass("TRN2", target_bir_lowering=False)
    D = lambda n, s, dt, k: nc.dram_tensor(n, s, dt, kind=k).ap()
    hT = D("hT", [128, 8, NT], F32, "ExternalInput")
    gffn = D("gffn", [128, 8], F32, "ExternalInput")
    cmat = D("cmat", [128, 3, 128], F32, "ExternalInput")
    wr = D("wr", [128, 8, 8], F32, "ExternalInput")
    uT_o = D("uT", [128, 8, NT], BF16, "ExternalOutput")
    g_o = D("gates", [128, NT // 128, 8], F32, "ExternalOutput")
    with ExitStack() as es:
        m = MK(nc, es)
        cm = m.sb("cm", [128, 3, 128], BF16)
        gf = m.sb("gf", [128, 8], F32)
        wrs = m.sb("wrs", [128, 8, 8], F32)
        epsb = m.sb("epsb", [128, 1], F32)
        hb = [m.sb("hb%d" % i, [128, 8, 512], F32) for i in range(2)]
        uT = [m.sb("uT%d" % i, [128, 8, 512], BF16) for i in range(2)]
        u32 = m.sb("u32", [128, 8, 512], F32)
        sq = m.sb("sq", [128, 8, 512], BF16)
        rs = m.sb("rs", [128, 512], F32)
        gates = m.sb("gates_sb", [128, NT // 128, 8], F32)
        lg = [m.sb("lg%d" % i, [128, 8], F32) for i in range(2)]
        m8 = [m.sb("m8%d" % i, [128, 8], F32) for i in range(2)]
        tt = [m.sb("tt%d" % i, [128, 4], F32) for i in range(2)]
        ga = [m.sb("ga%d" % i, [128, 8], F32) for i in range(2)]
        ps_ss = m.ps("ps_ss", [128, 512])
        ps_l = [m.ps("ps_l%d" % i, [128, 8]) for i in range(2)]
        m.dma("pool", cm[:], cmat, writes=[cm])
        m.dma("sp", gf[:], gffn, writes=[gf])
        m.dma("sp", wrs[:], wr, writes=[wrs])
        m.op("dve", lambda e: e.memset(epsb[:], EPS), writes=[epsb])
        ob = Buf("out")
        k = 0
        for t in range(NT // 512):
            b = t % 2
            m.dma("sp", hb[b][:], hT[:, :, t * 512:(t + 1) * 512], writes=[hb[b]])
            rms_view(m, hb[b], 0, 512, gf, cm, epsb, ps_ss, sq, rs, uT[b], u32=u32)
            m.dma("sp", uT_o[:, :, t * 512:(t + 1) * 512], uT[b][:], reads=[uT[b]], writes=[ob])
            for s in range(4):
                j = k % 2
                k += 1
                L, lgb, m8b, tb, gab = ps_l[j], lg[j], m8[j], tt[j], ga[j]
                for c in range(8):
                    m.op("pe", lambda e: e.matmul(L[:, :], lhsT=u32[:, c, s * 128:(s + 1) * 128], rhs=wrs[:, c, :], start=(c == 0), stop=(c == 7)),
                         reads=[u32, wrs], writes=[L])
                m.op("dve", lambda e: e.tensor_copy(out=lgb[:, :], in_=L[:, :]), reads=[L], writes=[lgb])
                m.op("dve", lambda e: e.max(out=m8b[:, :], in_=lgb[:, :]), reads=[lgb], writes=[m8b])
                m.op("dve", lambda e: e.tensor_tensor(out=tb[:, 0:1], in0=m8b[:, 1:2], in1=m8b[:, 0:1], op=ALU.subtract), reads=[m8b], writes=[tb])
                m.op("act", lambda e: e.activation(out=tb[:, 1:2], in_=tb[:, 0:1], func=AF.Exp), reads=[tb], writes=[tb])
                m.op("dve", lambda e: e.tensor_scalar(out=tb[:, 2:3], in0=tb[:, 1:2], scalar1=1.0, scalar2=None, op0=ALU.add), reads=[tb], writes=[tb])
                m.op("dve", lambda e: e.reciprocal(out=tb[:, 2:3], in_=tb[:, 2:3]), reads=[tb], writes=[tb])
                m.op("dve", lambda e: e.tensor_tensor(out=tb[:, 3:4], in0=tb[:, 1:2], in1=tb[:, 2:3], op=ALU.mult), reads=[tb], writes=[tb])
                m.op("dve", lambda e: e.tensor_scalar(out=gab[:, :], in0=lgb[:, :], scalar1=m8b[:, 0:1], scalar2=tb[:, 2:3], op0=ALU.is_equal, op1=ALU.mult),
                     reads=[lgb, m8b, tb], writes=[gab])
                m.op("dve", lambda e: e.tensor_scalar(out=lgb[:, :], in0=lgb[:, :], scalar1=m8b[:, 1:2], scalar2=tb[:, 3:4], op0=ALU.is_equal, op1=ALU.mult),
                     reads=[lgb, m8b, tb], writes=[lgb])
                m.op("dve", lambda e: e.tensor_tensor(out=gates[:, t * 4 + s, :], in0=gab[:, :], in1=lgb[:, :], op=ALU.add), reads=[gab, lgb], writes=[gates])
        ob2 = Buf("out2")
        m.dma("sp", g_o, gates[:], reads=[gates], writes=[ob2])
        m.final_wait("sp", [ob, ob2])
        print("L5 instrs", m.n_ins, "dmas", m.ndma)
    return nc


def build_l6(F=28, NTOK=16384, NB=1024):
    nc = bass.Bass("TRN2", target_bir_lowering=False)
    D = lambda n, s, dt, k: nc.dram_tensor(n, s, dt, kind=k).ap()
    uT_i = D("uT", [128, 8, NTOK], BF16, "ExternalInput")
    gbc_i = D("gbc", [128, NTOK], F32, "ExternalInput")
    wg = D("wg", [F, 128, 1024], F32, "ExternalInput")
    wu = D("wu", [F, 128, 1024], F32, "ExternalInput")
    wd = D("wd", [8, 128, F * 128], F32, "ExternalInput")
    y_o = D("yT", [128, 8, NTOK], F32, "ExternalOutput")
    with ExitStack() as es:
        m = MK(nc, es)
        R = FFNRes(m, F, NB)
        uT = [m.sb("uT%d" % i, [128, 8, NB], BF16) for i in range(2)]
        gb = [m.sb("gb%d" % i, [128, NB], F32) for i in range(2)]
        yb = m.sb("yb", [128, 8, NB], F32)
        ob = Buf("out")
        for tb in range(NTOK // NB):
            b = tb % 2
            t0 = tb * NB
            m.dma("sp", uT[b][:], uT_i[:, :, t0:t0 + NB], writes=[uT[b]])
            m.dma("sp", gb[b][:], gbc_i[:, t0:t0 + NB], writes=[gb[b]])

            def sink(d, ch, Y):
                m.op("dve", lambda e: e.tensor_tensor(out=yb[:, d, ch * 512:(ch + 1) * 512], in0=Y[:, :], in1=gb[b][:, ch * 512:(ch + 1) * 512], op=ALU.mult),
                     reads=[Y, gb[b]], writes=[yb])
            ffn_block(R, uT[b], lambda f: wg[f], lambda f: wu[f], lambda d: wd[d], sink)
            m.dma("sp", y_o[:, :, t0:t0 + NB], yb[:], reads=[yb], writes=[ob])
        m.final_wait("sp", [ob])
        print("L6 instrs", m.n_ins, "dmas", m.ndma)
    return nc


def build_l7(NT=2048):
    nc = bass.Bass("TRN2", target_bir_lowering=False)
    D = lambda n, s, dt, k: nc.dram_tensor(n, s, dt, kind=k).ap()
    hT = D("hT", [128, 8, NT], F32, "ExternalInput")
    ys = D("ys", [8, 128, 8, NT], F32, "ExternalInput")
    out = D("outT", [128, 8, NT], F32, "ExternalOutput")
    with ExitStack() as es:
        m = MK(nc, es)
        acc = [m.sb("acc%d" % i, [128, 8, 512], F32) for i in range(2)]
        yb = [m.sb("yb%d" % i, [128, 8, 512], F32) for i in range(3)]
        ob = Buf("out")
        k = 0
        for t in range(NT // 512):
            a = acc[t % 2]
            m.dma("sp", a[:], hT[:, :, t * 512:(t + 1) * 512], writes=[a])
            for e_ in range(8):
                y = yb[k % 3]
                k += 1
                m.dma("sp", y[:], ys[e_, :, :, t * 512:(t + 1) * 512], writes=[y])
                m.op("dve", lambda e: e.tensor_tensor(out=a[:], in0=a[:], in1=y[:], op=ALU.add), reads=[a, y], writes=[a])
            m.dma("sp", out[:, :, t * 512:(t + 1) * 512], a[:], reads=[a], writes=[ob])
        m.final_wait("sp", [ob])
        print("L7 instrs", m.n_ins, "dmas", m.ndma)
    return nc

H_NA=6; HD=64
def fm(a):
    T = a.shape[0]
    return np.ascontiguousarray(a.reshape(T, 8, 128).transpose(2, 1, 0))
def wt(w):
    K, N = w.shape
    return np.ascontiguousarray(w.reshape(K // 128, 128, N).transpose(1, 0, 2))
def gvec(g):
    return np.ascontiguousarray(g.reshape(8, 128).T)
def cmat():
    ones = np.ones((128, 128), np.float32)
    blk = np.zeros((128, 128), np.float32); blk[:64, :64] = 1; blk[64:, 64:] = 1
    perm = np.zeros((128, 128), np.float32)
    for mm in range(128):
        k = (mm // 64) * 64 + ((mm % 64) + 32) % 64
        perm[k, mm] = 1
    return np.ascontiguousarray(np.stack([ones, blk, perm], axis=1))
def cs_table(pos):
    half = 32
    inv = np.power(np.float32(10000.0), -(2.0 / 64) * np.arange(half, dtype=np.float32)).astype(np.float32)
    ang = pos.astype(np.float32)[:, None] * inv[None, :]
    cos = np.cos(ang).astype(np.float32); sin = np.sin(ang).astype(np.float32)
    d = np.arange(128) % 64
    c = cos[:, d % 32].T
    s = np.where((d < 32)[:, None], -sin[:, d % 32].T, sin[:, d % 32].T)
    return np.ascontiguousarray(np.stack([c, s], axis=1).astype(np.float32))
def gqk_table(inp, l):
    t = np.zeros((128, 8), np.float32)
    d = np.arange(128) % 64
    t[:, 0] = inp["g_qk_na"][l, 0][d]; t[:, 1] = inp["g_qk_na"][l, 1][d]
    t[:, 2] = inp["g_qk_dil"][l, 0][d]; t[:, 3] = inp["g_qk_dil"][l, 1][d]
    t[:, 4] = inp["g_qk_mem"][l, 0][d]; t[:, 5] = inp["g_qk_mem"][l, 1][d]
    return t

BF = ml_dtypes.bfloat16
def cmask_table():
    k = np.arange(128)[:, None]; q = np.arange(128)[None, :]
    out = np.zeros((128, 17, 128), np.float32)
    for j in range(17):
        off = 128 * (j - 8) + k - q
        c = np.zeros((128, 128), np.float32)
        for w, d in ((128, 1), (512, 4), (2048, 16)):
            c += ((off % d == 0) & (np.abs(off) <= w // 2)).astype(np.float32)
        out[:, j, :] = c
    return np.ascontiguousarray(out.reshape(128, 17 * 128))
def na_bias(rpb_l, p, gb):
    ws = min(max(gb - 2, 0), 59)
    k = np.arange(128); q = np.arange(128)
    out = np.full((128, 2, 5, 128), -100.0, np.float32)
    qrow = 2 * gb + q // 64; qcol = q % 64
    rstart = np.clip(qrow - 4, 0, 120); cstart = np.clip(qcol - 8, 0, 48)
    for j in range(5):
        krow = 2 * (ws + j) + k // 64; kcol = k % 64
        inr = (krow[:, None] >= rstart[None, :]) & (krow[:, None] < rstart[None, :] + 8)
        inc = (kcol[:, None] >= cstart[None, :]) & (kcol[:, None] < cstart[None, :] + 16)
        ok = inr & inc
        ri = np.clip(krow[:, None] - qrow[None, :] + 7, 0, 14); ci = np.clip(kcol[:, None] - qcol[None, :] + 15, 0, 30)
        for hh in range(2):
            vals = rpb_l[2 * p + hh][ri, ci]
            out[:, hh, j, :] = np.where(ok, vals, np.float32(-100.0))
    return out.reshape(128, 1280)
def prep_pb(core, l, inp, pa_res, h_full_T):
    b = core // 4; ci = core % 4; s0 = ci * 2048; tile0 = ci * 16
    K = np.concatenate([np.asarray(pa_res[b * 4 + c]["kT"]) for c in range(4)], axis=2)
    V = np.concatenate([np.asarray(pa_res[b * 4 + c]["v"]) for c in range(4)], axis=1)
    Kp = np.zeros((128, 6, 8192 + 2048), K.dtype); Kp[:, :, 1024:1024 + 8192] = K
    Vp = np.zeros((128, 64 + 16, 780), V.dtype); Vp[:, 8:72] = V
    r = pa_res[core]
    d = {"qT": np.asarray(r["qT"])}
    d["kA"] = np.ascontiguousarray(Kp[:, 0:3, 1024 + s0 - 256:1024 + s0 + 2304])
    d["kB"] = np.ascontiguousarray(Kp[:, 3:6, s0:s0 + 4096])
    vA = Vp[:, 8 + tile0 - 2:8 + tile0 + 18, 0:390].reshape(128, 20, 3, 130).transpose(0, 2, 1, 3)
    vB = Vp[:, tile0:tile0 + 32, 390:780].reshape(128, 32, 3, 130).transpose(0, 2, 1, 3)
    d["vA"] = np.ascontiguousarray(vA); d["vB"] = np.ascontiguousarray(vB)
    kAe = np.zeros((128, 3, 4, 640), K.dtype); vAe = np.zeros((128, 3, 4, 650), V.dtype)
    for slot, i in enumerate((0, 1, 14, 15)):
        gb = tile0 + i; ws = min(max(gb - 2, 0), 59)
        kAe[:, :, slot, :] = K[:, 0:3, ws * 128:ws * 128 + 640]
        vv = V[:, ws:ws + 5, 0:390].reshape(128, 5, 3, 130).transpose(0, 2, 1, 3)
        vAe[:, :, slot, :] = vv.reshape(128, 3, 650)
    d["kAe"] = kAe; d["vAe"] = vAe
    d["kM"] = np.asarray(r["kmT"])
    d["vM"] = np.ascontiguousarray(np.asarray(r["vm"]).reshape(128, 2, 2, 130).transpose(0, 2, 1, 3))
    nab = np.zeros((128, 3, 5, 1280), np.float32)
    for p in range(3):
        for v, i in enumerate((0, 1, 2, 14, 15)):
            nab[:, p, v, :] = na_bias(inp["rpb_na"][l], p, tile0 + i)
    d["nab"] = nab
    d["cmask"] = cmask_table()
    d["ident"] = np.eye(128, dtype=np.float32)
    d["gout"] = np.ascontiguousarray(np.broadcast_to(inp["g_out"][l][None, :], (128, 1024))).astype(np.float32)
    d["w_out"] = wt(inp["w_out"][l])
    d["hT"] = h_full_T[core]
    return d
def prep_pa(core, l, inp, hT_core):
    b = core // 4; s0 = (core % 4) * 2048
    return {"hT": hT_core, "w_in": wt(inp["w_in"][l]), "gattn": gvec(inp["g_attn"][l]), "gqk": gqk_table(inp, l), "cmat": cmat(),
            "cs": cs_table(np.arange(s0, s0 + 2048)), "memT": fm(inp["mem"][b]), "gmem": gvec(inp["g_mem"][l]), "w_kv": wt(inp["w_mem_kv"][l])}

def wgu_t(w, F):
    return np.ascontiguousarray(w.reshape(8, 128, F, 128).transpose(2, 1, 0, 3).reshape(F, 128, 1024))
def wd_t(w, F):
    return np.ascontiguousarray(w.reshape(F, 128, 8, 128).transpose(2, 1, 0, 3).reshape(8, 128, F * 128))
def unfm(aT):
    return np.ascontiguousarray(aT.transpose(2, 1, 0).reshape(aT.shape[2], 1024))


from concourse.bass_utils import run_bass_kernel_spmd
_CORES = list(range(8))
_PROGS = {}


def _prog(name, fn):
    if name not in _PROGS:
        _PROGS[name] = fn()
    return _PROGS[name]


def _run(name, fn, ims):
    nc = _prog(name, fn)
    return run_bass_kernel_spmd(nc, ims, core_ids=_CORES).results


def _attn_layer(inp, l, hTs):
    ra = _run("pa", build_pa, [prep_pa(c, l, inp, hTs[c]) for c in range(8)])
    rb = _run("pb", build_pb, [prep_pb(c, l, inp, ra, hTs) for c in range(8)])
    return [np.asarray(rb[c]["hmidT"]) for c in range(8)]


def kernel(**inp):
    inp = {k: np.asarray(v) for k, v in inp.items()}
    x = inp["x"]
    hTs = [fm(x[c // 4, (c % 4) * 2048:(c % 4 + 1) * 2048]) for c in range(8)]
    hm = _attn_layer(inp, 0, hTs)
    wg = wgu_t(inp["w_gate_dense"][0], 22); wu = wgu_t(inp["w_up_dense"][0], 22); wd = wd_t(inp["w_down_dense"][0], 22)
    cm = cmat()
    rc = _run("pc", build_pc_dense, [{"hT": hm[c], "gffn": gvec(inp["g_ffn"][0]), "cmat": cm, "wg": wg, "wu": wu, "wd": wd} for c in range(8)])
    h1 = [np.asarray(rc[c]["outT"]) for c in range(8)]
    hm1 = _attn_layer(inp, 1, h1)
    wr = wt(inp["w_router"][0])
    r5 = _run("l5", build_l5, [{"hT": hm1[c], "gffn": gvec(inp["g_ffn"][1]), "cmat": cm, "wr": wr} for c in range(8)])
    uT_all = np.ascontiguousarray(np.concatenate([np.asarray(r5[c]["uT"]) for c in range(8)], axis=2))
    gates = np.concatenate([np.asarray(r5[c]["gates"]).transpose(1, 0, 2).reshape(2048, 8) for c in range(8)], axis=0)
    ims = []
    for e in range(8):
        ims.append({"uT": uT_all, "gbc": np.ascontiguousarray(np.broadcast_to(gates[:, e][None, :], (128, 16384))),
                    "wg": wgu_t(inp["w_gate_moe"][0, e], 28), "wu": wgu_t(inp["w_up_moe"][0, e], 28), "wd": wd_t(inp["w_down_moe"][0, e], 28)})
    r6 = _run("l6", build_l6, ims)
    ims = []
    for c in range(8):
        ys = np.ascontiguousarray(np.stack([np.asarray(r6[e]["yT"])[:, :, c * 2048:(c + 1) * 2048] for e in range(8)]))
        ims.append({"hT": hm1[c], "ys": ys})
    r7 = _run("l7", build_l7, ims)
    out = np.stack([unfm(np.asarray(r7[c]["outT"])) for c in range(8)]).reshape(2, 8192, 1024)
    return out.astype(np.float32)
```

```python
import numpy as np
import ml_dtypes
import numpy as np
from contextlib import ExitStack
import concourse.bass as bass
import concourse.mybir as mybir

F32 = mybir.dt.float32
BF16 = mybir.dt.bfloat16
AF = mybir.ActivationFunctionType
ALU = mybir.AluOpType
AX = mybir.AxisListType


class Buf:
    __slots__ = ("name", "w", "r", "t", "dsem")

    def __init__(self, name, t=None):
        self.name = name
        self.w = None
        self.r = []
        self.t = t
        self.dsem = None

    def __getitem__(self, idx):
        return self.t[idx]


class MK:
    SAME_ENGINE_SYNC = False

    def __init__(self, nc, es):
        self.nc = nc
        self.es = es
        self.eng = {"pe": nc.tensor, "act": nc.scalar, "dve": nc.vector, "pool": nc.gpsimd, "sp": nc.sync}
        self.esem = {k: es.enter_context(nc.semaphore("s_" + k)) for k in self.eng}
        self.ecnt = {k: 0 for k in self.eng}
        self.waited = {k: {} for k in self.eng}
        self.dsems = []
        self.dcnt = {}
        self.ndma = 0
        self.n_ins = 0

    def sb(self, name, shape, dt):
        t = self.es.enter_context(self.nc.sbuf_tensor(name, list(shape), dt))
        return Buf(name, t)

    def ps(self, name, shape, dt=F32):
        t = self.es.enter_context(self.nc.psum_tensor(name, list(shape), dt))
        return Buf(name, t)

    def new_dsem(self, name):
        s = self.es.enter_context(self.nc.semaphore(name))
        self.dcnt[id(s)] = [s, 0]
        return s

    def _wait(self, E, raw, other, strict=False):
        eng = self.eng[E]
        best = {}
        own = self.esem[E]
        for lst, is_raw in ((raw, True), (other, False)):
            for ev in lst:
                if ev is None:
                    continue
                sem, val = ev
                if sem is own and not self.SAME_ENGINE_SYNC:
                    if not (is_raw and (E != "pe" or strict)):
                        continue
                k = id(sem)
                if k not in best or best[k][1] < val:
                    best[k] = (sem, val)
        for k, (sem, val) in best.items():
            if self.waited[E].get(k, 0) >= val:
                continue
            eng.wait_ge(sem, val)
            self.waited[E][k] = val

    def _deps(self, reads, writes):
        raw = [b.w for b in reads]
        other = []
        for b in writes:
            other.append(b.w)
            other.extend(b.r)
        return raw, other

    def _commit(self, ev, reads, writes):
        for b in reads:
            b.r.append(ev)
            if len(b.r) > 64:
                best = {}
                for s, v in b.r:
                    if id(s) not in best or best[id(s)][1] < v:
                        best[id(s)] = (s, v)
                b.r = list(best.values())
        for b in writes:
            b.w = ev
            b.r = []

    def op(self, E, fn, reads=(), writes=(), strict=False):
        self._wait(E, *self._deps(reads, writes), strict=strict)
        ins = fn(self.eng[E])
        self.ecnt[E] += 1
        ins.then_inc(self.esem[E], 1)
        ev = (self.esem[E], self.ecnt[E])
        self._commit(ev, reads, writes)
        self.n_ins += 1
        return ins

    def dma(self, E, out, in_, reads=(), writes=(), dsem=None, **kw):
        self._wait(E, *self._deps(reads, writes))
        ins = self.eng[E].dma_start(out=out, in_=in_, **kw)
        wb = writes[0]
        if wb.dsem is None:
            wb.dsem = self.new_dsem("d_" + wb.name)
        dsem = wb.dsem
        rec = self.dcnt[id(dsem)]
        rec[1] += 16
        ins.then_inc(dsem, 16)
        ev = (dsem, rec[1])
        self._commit(ev, reads, writes)
        self.ndma += 1
        return ins

    def final_wait(self, E, bufs):
        self._wait(E, [b.w for b in bufs], [])


NT = 2048
CH = 512
NCH = NT // CH
EPS = 1e-6

QK_GROUPS = ([("q", j, 0 + 128 * j, 0, False) for j in range(3)]
             + [("k", j, 384 + 128 * j, 1, False) for j in range(3)]
             + [("q", 3 + j, 1152 + 128 * j, 2, True) for j in range(3)]
             + [("k", 3 + j, 1536 + 128 * j, 3, True) for j in range(3)]
             + [("q", 6 + j, 2304 + 128 * j, 4, False) for j in range(2)])


def rms_chunk(m, src, n, gvec, ones, epsb, ps_ss, sq, rs, uT, u32=None):
    m.op("act", lambda e: e.activation(out=sq[:, :, 0:n], in_=src[:, :, 0:n], func=AF.Square), reads=[src], writes=[sq])
    for c in range(8):
        m.op("pe", lambda e: e.matmul(ps_ss[:, 0:n], lhsT=ones[:, 0, :], rhs=sq[:, c, 0:n], start=(c == 0), stop=(c == 7)),
             reads=[sq, ones], writes=[ps_ss])
    m.op("act", lambda e: e.activation(out=rs[:, 0:n], in_=ps_ss[:, 0:n], func=AF.Ln, scale=1.0 / 1024, bias=epsb[:, 0:1]),
         reads=[ps_ss, epsb], writes=[rs])
    m.op("act", lambda e: e.activation(out=rs[:, 0:n], in_=rs[:, 0:n], func=AF.Exp, scale=-0.5), reads=[rs], writes=[rs])
    for c in range(8):
        m.op("dve", lambda e: e.scalar_tensor_tensor(out=uT[:, c, 0:n], in0=src[:, c, 0:n], scalar=gvec[:, c:c + 1],
                                                     in1=rs[:, 0:n], op0=ALU.mult, op1=ALU.mult),
             reads=[src, gvec, rs], writes=[uT])
        if u32 is not None:
            m.op("pool", lambda e: e.scalar_tensor_tensor(out=u32[:, c, 0:n], in0=src[:, c, 0:n], scalar=gvec[:, c:c + 1],
                                                          in1=rs[:, 0:n], op0=ALU.mult, op1=ALU.mult),
                 reads=[src, gvec, rs], writes=[u32])


def build_pa():
    nc = bass.Bass("TRN2", target_bir_lowering=False)
    D = lambda n, s, dt, k: nc.dram_tensor(n, s, dt, kind=k).ap()
    hT = D("hT", [128, 8, NT], F32, "ExternalInput")
    w_in = D("w_in", [128, 8, 2560], F32, "ExternalInput")
    gattn = D("gattn", [128, 8], F32, "ExternalInput")
    gqk = D("gqk", [128, 8], F32, "ExternalInput")
    cmat = D("cmat", [128, 3, 128], F32, "ExternalInput")
    cs = D("cs", [128, 2, NT], F32, "ExternalInput")
    memT = D("memT", [128, 8, 256], F32, "ExternalInput")
    gmem = D("gmem", [128, 8], F32, "ExternalInput")
    w_kv = D("w_kv", [128, 8, 512], F32, "ExternalInput")
    qT_o = D("qT", [128, 8, NT], BF16, "ExternalOutput")
    kT_o = D("kT", [128, 6, NT], BF16, "ExternalOutput")
    v_o = D("v", [128, 16, 12 * 65], BF16, "ExternalOutput")
    kmT_o = D("kmT", [128, 2, 256], BF16, "ExternalOutput")
    vm_o = D("vm", [128, 2, 4 * 65], BF16, "ExternalOutput")

    with ExitStack() as es:
        m = MK(nc, es)
        W = m.sb("W", [128, 8, 2560], BF16)
        Wkv = m.sb("Wkv", [128, 8, 512], BF16)
        cm = m.sb("cm", [128, 3, 128], BF16)
        ga = m.sb("ga", [128, 8], F32)
        gm = m.sb("gm", [128, 8], F32)
        gq = m.sb("gq", [128, 8], F32)
        epsb = m.sb("epsb", [128, 1], F32)
        cst = [m.sb("cst%d" % i, [128, 2, CH], F32) for i in range(2)]
        hc = [m.sb("hc%d" % i, [128, 8, CH], F32) for i in range(2)]
        sq = m.sb("sq", [128, 8, CH], BF16)
        rs = m.sb("rs", [128, CH], F32)
        uT = [m.sb("uT%d" % i, [128, 8, CH], BF16) for i in range(2)]
        sq2 = [m.sb("sq2%d" % i, [128, CH], BF16) for i in range(2)]
        r2 = [m.sb("r2%d" % i, [128, CH], F32) for i in range(2)]
        qn = [m.sb("qn%d" % i, [128, CH], BF16) for i in range(2)]
        t1 = [m.sb("t1%d" % i, [128, CH], F32) for i in range(2)]
        t2 = [m.sb("t2%d" % i, [128, CH], F32) for i in range(2)]
        qTs2 = [m.sb("qTs%d" % i, [128, 8, CH], BF16) for i in range(2)]
        kTs2 = [m.sb("kTs%d" % i, [128, 6, CH], BF16) for i in range(2)]
        vs2 = [m.sb("vs%d" % i, [128, 4, 12 * 65], BF16) for i in range(2)]
        kms = m.sb("kms", [128, 2, 256], BF16)
        vms = m.sb("vms", [128, 2, 4 * 65], BF16)
        ps_ss = m.ps("ps_ss", [128, 512])
        ps_y = [m.ps("ps_y%d" % i, [128, 512]) for i in range(2)]
        ps_s2 = [m.ps("ps_s2%d" % i, [128, 512]) for i in range(2)]
        ps_qp = m.ps("ps_qp", [128, 512])
        ps_v = [m.ps("ps_v%d" % i, [128, 512]) for i in range(2)]

        dW = [m.new_dsem("dW%d" % i) for i in range(4)]
        dsm = m.new_dsem("dsm")
        dh = [m.new_dsem("dh%d" % i) for i in range(2)]
        dc = [m.new_dsem("dc%d" % i) for i in range(2)]
        dout = m.new_dsem("dout")

        m.dma("sp", ga[:], gattn, writes=[ga], dsem=dsm)
        m.dma("sp", gm[:], gmem, writes=[gm], dsem=dsm)
        m.dma("sp", gq[:], gqk, writes=[gq], dsem=dsm)
        m.dma("pool", cm[:], cmat, writes=[cm], dsem=dW[0])
        m.op("dve", lambda e: e.memset(epsb[:], EPS), writes=[epsb])
        for i in range(2):
            m.op("dve", lambda e: e.memset(vs2[i][:], 1.0), writes=[vs2[i]])
        m.op("dve", lambda e: e.memset(vms[:], 1.0), writes=[vms])
        Wb = [Buf("Wb%d" % i) for i in range(4)]
        for i in range(4):
            m.dma("pool", W[:, :, 640 * i:640 * (i + 1)], w_in[:, :, 640 * i:640 * (i + 1)], writes=[Wb[i]], dsem=dW[i])
        m.dma("pool", Wkv[:], w_kv, writes=[Wkv], dsem=dW[0])

        def wbufs(c0, c1):
            return [Wb[i] for i in range(4) if c0 < 640 * (i + 1) and c1 > 640 * i]

        cnt = {"y": 0, "v": 0}

        def qk_group(u, n, Wt, wdeps, col0, gcolumn, rope, dst, dsti, tok0, csb):
            i = cnt["y"] % 2
            cnt["y"] += 1
            y, s2, sq2b, r2b, qnb = ps_y[i], ps_s2[i], sq2[i], r2[i], qn[i]
            for c in range(8):
                m.op("pe", lambda e: e.matmul(y[:, 0:n], lhsT=Wt[:, c, col0:col0 + 128], rhs=u[:, c, 0:n], start=(c == 0), stop=(c == 7)),
                     reads=[u] + wdeps, writes=[y])
            m.op("act", lambda e: e.activation(out=sq2b[:, 0:n], in_=y[:, 0:n], func=AF.Square), reads=[y], writes=[sq2b])
            m.op("pe", lambda e: e.matmul(s2[:, 0:n], lhsT=cm[:, 1, :], rhs=sq2b[:, 0:n], start=True, stop=True), reads=[sq2b, cm], writes=[s2])
            m.op("act", lambda e: e.activation(out=r2b[:, 0:n], in_=s2[:, 0:n], func=AF.Ln, scale=1.0 / 64, bias=epsb[:, 0:1]),
                 reads=[s2, epsb], writes=[r2b])
            m.op("act", lambda e: e.activation(out=r2b[:, 0:n], in_=r2b[:, 0:n], func=AF.Exp, scale=-0.5), reads=[r2b], writes=[r2b])
            d = dst[:, dsti, tok0:tok0 + n]
            if not rope:
                m.op("dve", lambda e: e.scalar_tensor_tensor(out=d, in0=y[:, 0:n], scalar=gq[:, gcolumn:gcolumn + 1], in1=r2b[:, 0:n],
                                                             op0=ALU.mult, op1=ALU.mult), reads=[y, gq, r2b], writes=[dst])
            else:
                t1b, t2b = t1[i], t2[i]
                m.op("dve", lambda e: e.scalar_tensor_tensor(out=qnb[:, 0:n], in0=y[:, 0:n], scalar=gq[:, gcolumn:gcolumn + 1], in1=r2b[:, 0:n],
                                                             op0=ALU.mult, op1=ALU.mult), reads=[y, gq, r2b], writes=[qnb])
                m.op("pe", lambda e: e.matmul(ps_qp[:, 0:n], lhsT=cm[:, 2, :], rhs=qnb[:, 0:n], start=True, stop=True), reads=[qnb, cm], writes=[ps_qp])
                m.op("pool", lambda e: e.tensor_tensor(out=t1b[:, 0:n], in0=qnb[:, 0:n], in1=csb[:, 0, 0:n], op=ALU.mult), reads=[qnb, csb], writes=[t1b])
                m.op("dve", lambda e: e.tensor_tensor(out=t2b[:, 0:n], in0=ps_qp[:, 0:n], in1=csb[:, 1, 0:n], op=ALU.mult), reads=[ps_qp, csb], writes=[t2b])
                m.op("pool", lambda e: e.tensor_tensor(out=d, in0=t1b[:, 0:n], in1=t2b[:, 0:n], op=ALU.add), reads=[t1b, t2b], writes=[dst])

        def v_tile(u, Wt, wdeps, col0, ncols, sub, dst, tile, head0, nheads):
            i = cnt["v"] % 2
            cnt["v"] += 1
            pv = ps_v[i]
            for c in range(8):
                m.op("pe", lambda e: e.matmul(pv[:, 0:ncols], lhsT=u[:, c, sub * 128:(sub + 1) * 128], rhs=Wt[:, c, col0:col0 + ncols],
                                              start=(c == 0), stop=(c == 7)), reads=[u] + wdeps, writes=[pv])
            dv = dst[:, tile, head0 * 65:(head0 + nheads) * 65].rearrange("p (h d) -> p h d", d=65)[:, :, 0:64]
            sv = pv[:, 0:ncols].rearrange("p (h d) -> p h d", d=64)
            m.op("act", lambda e: e.activation(out=dv, in_=sv, func=AF.Copy), reads=[pv], writes=[dst])

        m.dma("sp", hc[0][:, :, 0:256], memT, writes=[hc[0]], dsem=dh[0])
        rms_chunk(m, hc[0], 256, gm, cm, epsb, ps_ss, sq, rs, uT[0])
        for j in range(2):
            qk_group(uT[0], 256, Wkv, [Wkv], 128 * j, 5, False, kms, j, 0, None)
        for sub in range(2):
            v_tile(uT[0], Wkv, [Wkv], 256, 256, sub, vms, sub, 0, 4)
        obm = [Buf("o1"), Buf("o2")]
        m.dma("sp", kmT_o, kms[:], reads=[kms], writes=[obm[0]], dsem=dout)
        m.dma("sp", vm_o, vms[:], reads=[vms], writes=[obm[1]], dsem=dout)

        ob = [Buf("oq"), Buf("ok"), Buf("ov")]
        for t in range(NCH):
            b = (t + 1) % 2
            tok0 = t * CH
            m.dma("sp", hc[b][:], hT[:, :, tok0:tok0 + CH], writes=[hc[b]], dsem=dh[b])
            m.dma("sp", cst[b][:], cs[:, :, tok0:tok0 + CH], writes=[cst[b]], dsem=dc[b])
            rms_chunk(m, hc[b], CH, ga, cm, epsb, ps_ss, sq, rs, uT[b])
            qTs, kTs, vs = qTs2[b], kTs2[b], vs2[b]
            for (dn, di, col0, gc, rope) in QK_GROUPS:
                qk_group(uT[b], CH, W, wbufs(col0, col0 + 128), col0, gc, rope, qTs if dn == "q" else kTs, di, 0, cst[b])
            for sub in range(4):
                v_tile(uT[b], W, wbufs(768, 1152), 768, 384, sub, vs, sub, 0, 6)
                v_tile(uT[b], W, wbufs(1920, 2304), 1920, 384, sub, vs, sub, 6, 6)
            m.dma("sp", qT_o[:, :, tok0:tok0 + CH], qTs[:], reads=[qTs], writes=[ob[0]], dsem=dout)
            m.dma("sp", kT_o[:, :, tok0:tok0 + CH], kTs[:], reads=[kTs], writes=[ob[1]], dsem=dout)
            m.dma("sp", v_o[:, 4 * t:4 * t + 4, :], vs[:], reads=[vs], writes=[ob[2]], dsem=dout)
        m.final_wait("sp", ob + obm)
        print("P_A instrs", m.n_ins, "dmas", m.ndma)
    return nc


NT = 2048
EPS = 1e-6
SCALE = 0.125
DEBUG = False


def build_pb():
    nc = bass.Bass("TRN2", target_bir_lowering=False)
    D = lambda n, s, dt, k: nc.dram_tensor(n, s, dt, kind=k).ap()
    qT = D("qT", [128, 8, NT], BF16, "ExternalInput")
    kA = D("kA", [128, 3, 2560], BF16, "ExternalInput")
    vA = D("vA", [128, 3, 20, 130], BF16, "ExternalInput")
    kAe = D("kAe", [128, 3, 4, 640], BF16, "ExternalInput")
    vAe = D("vAe", [128, 3, 4, 650], BF16, "ExternalInput")
    kB = D("kB", [128, 3, 4096], BF16, "ExternalInput")
    vB = D("vB", [128, 3, 32, 130], BF16, "ExternalInput")
    kM = D("kM", [128, 2, 256], BF16, "ExternalInput")
    vM = D("vM", [128, 2, 2, 130], BF16, "ExternalInput")
    nab = D("nab", [128, 3, 5, 1280], F32, "ExternalInput")
    cmask = D("cmask", [128, 17 * 128], F32, "ExternalInput")
    ident = D("ident", [128, 128], F32, "ExternalInput")
    gout = D("gout", [128, 1024], F32, "ExternalInput")
    w_out = D("w_out", [128, 8, 1024], F32, "ExternalInput")
    hT = D("hT", [128, 8, NT], F32, "ExternalInput")
    hmid = D("hmidT", [128, 8, NT], F32, "ExternalOutput")
    o_dbg = D("o_dbg", [128, 16, 1024], BF16, "ExternalOutput") if DEBUG else None
    dbg2 = D("dbg2", [128, 1024], BF16, "ExternalOutput") if DEBUG else None
    dbg3 = D("dbg3", [128, 4], F32, "ExternalOutput") if DEBUG else None
    dbg4 = D("dbg4", [128, 8, 512], BF16, "ExternalOutput") if DEBUG else None

    with ExitStack() as es:
        m = MK(nc, es)
        qp = [m.sb("qp%d" % i, [128, NT], BF16) for i in range(2)]
        kb = [m.sb("kb%d" % i, [128, 4096], BF16) for i in range(2)]
        vb = [m.sb("vb%d" % i, [128, 32, 130], BF16) for i in range(2)]
        ke = [m.sb("ke0", [128, 4, 640], BF16)] * 2
        ve = [m.sb("ve0", [128, 4, 650], BF16)] * 2
        stage = [m.sb("stage%d" % i, [128, 1280], F32) for i in range(2)]
        Mna = [m.sb("Mna0", [128, 5, 1280], BF16)] * 2
        Cm = m.sb("Cm", [128, 17 * 128], BF16)
        idn = m.sb("idn", [128, 128], BF16)
        go = m.sb("go", [128, 1024], F32)
        Wo = m.sb("Wo", [128, 8, 1024], BF16)
        epsb = m.sb("epsb", [128, 1], F32)
        o_all = m.sb("o_all", [128, 16, 1024], BF16)
        Eb = [m.sb("E%d" % i, [128, 512], BF16) for i in range(3)]
        Pb = [m.sb("P%d" % i, [128, 512], BF16) for i in range(3)]
        rec = [m.sb("rec%d" % i, [128, 2], F32) for i in range(2)]
        sqf = m.sb("sqf", [128, 1024], F32)
        ms = [m.sb("ms%d" % i, [128, 4], F32) for i in range(2)]
        mixed = [m.sb("mixed%d" % i, [128, 1024], BF16) for i in range(2)]
        mixedT = [m.sb("mixedT%d" % i, [128, 8, 512], BF16) for i in range(2)]
        hc = [m.sb("hc0", [128, 8, 512], F32)] * 2
        ps_s = [m.ps("ps_s%d" % i, [128, 512]) for i in range(3)]
        ps_o = [m.ps("ps_o%d" % i, [128, 512]) for i in range(2)]
        ps_T = m.ps("ps_T", [128, 1024], BF16)
        ps_y = [m.ps("ps_y%d" % i, [128, 512]) for i in range(2)]

        dl = [m.new_dsem("dl%d" % i) for i in range(2)]
        dst_ = [m.new_dsem("dst%d" % i) for i in range(2)]
        dcst = m.new_dsem("dcst")
        dh = [m.new_dsem("dh%d" % i) for i in range(2)]
        dout = m.new_dsem("dout")

        m.dma("pool", Cm[:], cmask, writes=[Cm], dsem=dcst)
        m.dma("pool", idn[:], ident, writes=[idn], dsem=dcst)
        m.dma("sp", go[:], gout, writes=[go], dsem=dcst)
        m.op("dve", lambda e: e.memset(epsb[:], EPS), writes=[epsb])
        Wo_loaded = [False]

        gcnt = [0]
        stc = [0]
        SLOT = {0: 0, 1: 1, 14: 2, 15: 3}
        VAR = {0: 0, 1: 1, 14: 3, 15: 4}

        def kind_of(p):
            return "A" if p < 3 else ("B" if p < 6 else "M")

        def load_main(p):
            b = p % 2
            kind = kind_of(p)
            m.dma("sp", qp[b][:], qT[:, p, :], writes=[qp[b]])
            if kind == "A":
                m.dma("sp", kb[b][:, 0:2560], kA[:, p, :], writes=[kb[b]])
                m.dma("sp", vb[b][:, 0:20, :], vA[:, p], writes=[vb[b]])
            elif kind == "B":
                m.dma("sp", kb[b][:], kB[:, p - 3, :], writes=[kb[b]])
                m.dma("sp", vb[b][:], vB[:, p - 3], writes=[vb[b]])
            else:
                m.dma("sp", kb[b][:, 0:256], kM[:, p - 6, :], writes=[kb[b]])
                m.dma("sp", vb[b][:, 0:2, :], vM[:, p - 6], writes=[vb[b]])

        def load_edge(p):
            b = p % 2
            m.dma("sp", ke[b][:], kAe[:, p], writes=[ke[b]])
            m.dma("sp", ve[b][:], vAe[:, p], writes=[ve[b]])
            for v in range(5):
                sb_ = stc[0] % 2
                stc[0] += 1
                m.dma("sp", stage[sb_][:], nab[:, p, v, :], writes=[stage[sb_]])
                m.op("act", lambda e: e.activation(out=Mna[b][:, v, :], in_=stage[sb_][:], func=AF.Exp), reads=[stage[sb_]], writes=[Mna[b]])

        units = []
        for p in range(8):
            kind = kind_of(p)
            b = p % 2
            for i in range(16):
                O = ps_o[i % 2]
                for hh in range(2):
                    pbs = 64 * hh
                    tiles = []
                    if kind == "A":
                        slot = SLOT.get(i)
                        var = VAR.get(i, 2)
                        for j in range(5):
                            if slot is None:
                                k_ap = kb[b][pbs:pbs + 64, (i + j) * 128:(i + j + 1) * 128]
                                v_ap = vb[b][:, i + j, hh * 65:(hh + 1) * 65]
                            else:
                                k_ap = ke[b][pbs:pbs + 64, slot, j * 128:(j + 1) * 128]
                                v_ap = ve[b][:, slot, j * 130 + hh * 65:j * 130 + (hh + 1) * 65]
                            tiles.append((k_ap, v_ap, (Mna[b], var, hh * 640 + j * 128)))
                        kdeps = [kb[b], ke[b]]
                        vdeps = [vb[b], ve[b]]
                    elif kind == "B":
                        for j in range(17):
                            k_ap = kb[b][pbs:pbs + 64, (i + j) * 128:(i + j + 1) * 128]
                            v_ap = vb[b][:, i + j, hh * 65:(hh + 1) * 65]
                            tiles.append((k_ap, v_ap, (Cm, None, j * 128)))
                        kdeps = [kb[b]]
                        vdeps = [vb[b]]
                    else:
                        for j in range(2):
                            k_ap = kb[b][pbs:pbs + 64, j * 128:(j + 1) * 128]
                            v_ap = vb[b][:, j, hh * 65:(hh + 1) * 65]
                            tiles.append((k_ap, v_ap, None))
                        kdeps = [kb[b]]
                        vdeps = [vb[b]]
                    q_ap = qp[b][pbs:pbs + 64, i * 128:(i + 1) * 128]
                    nt = len(tiles)
                    for g0 in range(0, nt, 4):
                        units.append(dict(p=p, b=b, i=i, hh=hh, O=O, grp=tiles[g0:g0 + 4], g0=g0, nt=nt, q_ap=q_ap, kdeps=kdeps, vdeps=vdeps,
                                          first_of_pair=(i == 0 and hh == 0 and g0 == 0), last_of_tile=(hh == 1 and g0 + 4 >= nt)))
        for n_, u in enumerate(units):
            u["gi"] = n_ % 3

        def emit_qk(u):
            S = ps_s[u["gi"]]
            for t, (k_ap, v_ap, mk_) in enumerate(u["grp"]):
                m.op("pe", lambda e: e.matmul(S[:, t * 128:(t + 1) * 128], lhsT=k_ap, rhs=u["q_ap"], start=True, stop=True),
                     reads=u["kdeps"] + [qp[u["b"]]], writes=[S])

        def emit_softmax(u):
            S, E, P = ps_s[u["gi"]], Eb[u["gi"]], Pb[u["gi"]]
            n = len(u["grp"]) * 128
            m.op("act", lambda e: e.activation(out=E[:, 0:n], in_=S[:, 0:n], func=AF.Exp, scale=SCALE), reads=[S], writes=[E])
            mk0 = u["grp"][0][2]
            if mk0 is not None:
                mb, var, c0 = mk0
                m_ap = mb[:, c0:c0 + n] if var is None else mb[:, var, c0:c0 + n]
                m.op("dve", lambda e: e.tensor_tensor(out=P[:, 0:n], in0=E[:, 0:n], in1=m_ap, op=ALU.mult), reads=[E, mb], writes=[P])
                u["src"] = P
            else:
                u["src"] = E

        def emit_pv(u):
            O, hh, src = u["O"], u["hh"], u["src"]
            for t, (k_ap, v_ap, mk_) in enumerate(u["grp"]):
                first = (u["g0"] + t == 0)
                last = (u["g0"] + t == u["nt"] - 1)
                m.op("pe", lambda e: e.matmul(O[:, hh * 65:(hh + 1) * 65], lhsT=src[:, t * 128:(t + 1) * 128], rhs=v_ap, start=first, stop=last),
                     reads=u["vdeps"] + [src], writes=[O])
            if u["last_of_tile"]:
                i, p = u["i"], u["p"]
                rc = rec[i % 2]
                Ov = O[:, 0:130].rearrange("p (h d) -> p h d", d=65)
                m.op("dve", lambda e: e.reciprocal(out=rc[:, 0:2], in_=Ov[:, :, 64]), reads=[O], writes=[rc])
                for h2 in range(2):
                    m.op("dve", lambda e: e.tensor_scalar(out=o_all[:, i, p * 128 + h2 * 64:p * 128 + h2 * 64 + 64], in0=O[:, h2 * 65:h2 * 65 + 64],
                                                          scalar1=rc[:, h2:h2 + 1], scalar2=None, op0=ALU.mult), reads=[O, rc], writes=[o_all])

        def pre_pair(p):
            if p + 1 < 8:
                load_main(p + 1)
            if p == 2:
                for cc in range(0, 8, 2):
                    m.dma("pool", Wo[:, cc:cc + 2, :], w_out[:, cc:cc + 2, :], writes=[Wo])
            if kind_of(p) == "A":
                load_edge(p)

        load_main(0)
        pre_pair(0)
        emit_qk(units[0])
        for n_, u in enumerate(units):
            nu = units[n_ + 1] if n_ + 1 < len(units) else None
            if nu is not None and not nu["first_of_pair"]:
                emit_qk(nu)
            emit_softmax(u)
            emit_pv(u)
            if nu is not None and nu["first_of_pair"]:
                pre_pair(nu["p"])
                emit_qk(nu)

        obd = Buf("odbg_out")
        if DEBUG:
            m.dma("sp", o_dbg, o_all[:], reads=[o_all], writes=[obd], dsem=dout)
        GR = [(0, 384), (384, 768), (768, 1024)]
        ob = Buf("hmid_out")
        for i in range(16):
            b = i % 2
            msb = ms[b]
            m.op("dve", lambda e: e.tensor_tensor(out=sqf[:, :], in0=o_all[:, i, :], in1=o_all[:, i, :], op=ALU.mult), reads=[o_all], writes=[sqf])
            for g, (c0, c1) in enumerate(GR):
                m.op("dve", lambda e: e.tensor_reduce(out=msb[:, g:g + 1], in_=sqf[:, c0:c1], axis=AX.X, op=ALU.add), reads=[sqf], writes=[msb])
            for g, (c0, c1) in enumerate(GR):
                m.op("act", lambda e: e.activation(out=msb[:, g:g + 1], in_=msb[:, g:g + 1], func=AF.Ln, scale=1.0 / (c1 - c0), bias=epsb[:, 0:1]),
                     reads=[msb, epsb], writes=[msb])
            m.op("act", lambda e: e.activation(out=msb[:, 0:3], in_=msb[:, 0:3], func=AF.Exp, scale=-0.5), reads=[msb], writes=[msb])
            for g, (c0, c1) in enumerate(GR):
                m.op("dve", lambda e: e.scalar_tensor_tensor(out=mixed[b][:, c0:c1], in0=o_all[:, i, c0:c1], scalar=msb[:, g:g + 1], in1=go[:, c0:c1],
                                                             op0=ALU.mult, op1=ALU.mult), reads=[o_all, msb, go], writes=[mixed[b]])
            for c in range(8):
                m.op("pe", lambda e: e.transpose(ps_T[:, c * 128:(c + 1) * 128], mixed[b][:, c * 128:(c + 1) * 128], idn[:]), reads=[mixed[b], idn], writes=[ps_T])
            cb = (i // 4) % 2
            m.op("act", lambda e: e.activation(out=mixedT[cb][:, :, (i % 4) * 128:(i % 4 + 1) * 128], in_=ps_T[:, :].rearrange("p (c t) -> p c t", t=128), func=AF.Copy),
                 reads=[ps_T], writes=[mixedT[cb]])
            if i % 4 == 3:
                ch = i // 4
                m.dma("sp", hc[cb][:], hT[:, :, ch * 512:(ch + 1) * 512], writes=[hc[cb]], dsem=dh[cb])
                for d in range(8):
                    Y = ps_y[d % 2]
                    for c in range(8):
                        m.op("pe", lambda e: e.matmul(Y[:, :], lhsT=Wo[:, c, d * 128:(d + 1) * 128], rhs=mixedT[cb][:, c, :], start=(c == 0), stop=(c == 7)),
                             reads=[Wo, mixedT[cb]], writes=[Y])
                    m.op("dve", lambda e: e.tensor_tensor(out=hc[cb][:, d, :], in0=Y[:, :], in1=hc[cb][:, d, :], op=ALU.add), reads=[Y, hc[cb]], writes=[hc[cb]])
                m.dma("sp", hmid[:, :, ch * 512:(ch + 1) * 512], hc[cb][:], reads=[hc[cb]], writes=[ob], dsem=dout)
        if DEBUG:
            m.dma("sp", dbg2, mixed[1][:], reads=[mixed[1]], writes=[obd], dsem=dout)
            m.dma("sp", dbg3, ms[1][:], reads=[ms[1]], writes=[obd], dsem=dout)
            m.dma("sp", dbg4, mixedT[1][:], reads=[mixedT[1]], writes=[obd], dsem=dout)
        m.final_wait("sp", [ob, obd] if DEBUG else [ob])
        print("P_B instrs", m.n_ins, "dmas", m.ndma)
    return nc


EPS = 1e-6


class FFNRes:
    def __init__(self, m, F, NB):
        self.m, self.F, self.NB = m, F, NB
        self.wg = [m.sb("wg%d" % i, [128, 8, 128], BF16) for i in range(3)]
        self.wu = [m.sb("wu%d" % i, [128, 8, 128], BF16) for i in range(3)]
        self.wd = [m.sb("wd%d" % i, [128, F, 128], BF16) for i in range(2)]
        self.act = m.sb("act", [128, F, NB], BF16)
        self.sg = [m.sb("sg%d" % i, [128, 512], F32) for i in range(2)]
        self.ps_g = [m.ps("ps_g%d" % i, [128, 512]) for i in range(2)]
        self.ps_u = [m.ps("ps_u%d" % i, [128, 512]) for i in range(2)]
        self.ps_y = [m.ps("ps_y%d" % i, [128, 512]) for i in range(2)]
        self.wc = 0
        self.dc = 0
        self.gc = 0
        self.yc = 0


def ffn_block(R, uT, wg_ap, wu_ap, wd_ap, sink):
    m, F, NB = R.m, R.F, R.NB
    nch = NB // 512
    for f in range(F):
        i = R.wc % 3
        R.wc += 1
        wg, wu = R.wg[i], R.wu[i]
        m.dma("pool", wg[:].rearrange("p c n -> p (c n)"), wg_ap(f), writes=[wg])
        m.dma("pool", wu[:].rearrange("p c n -> p (c n)"), wu_ap(f), writes=[wu])
        for ch in range(nch):
            j = R.gc % 2
            R.gc += 1
            G, U, sg = R.ps_g[j], R.ps_u[j], R.sg[j]
            for c in range(8):
                m.op("pe", lambda e: e.matmul(G[:, :], lhsT=wg[:, c, :], rhs=uT[:, c, ch * 512:(ch + 1) * 512], start=(c == 0), stop=(c == 7)),
                     reads=[wg, uT], writes=[G])
            for c in range(8):
                m.op("pe", lambda e: e.matmul(U[:, :], lhsT=wu[:, c, :], rhs=uT[:, c, ch * 512:(ch + 1) * 512], start=(c == 0), stop=(c == 7)),
                     reads=[wu, uT], writes=[U])
            m.op("act", lambda e: e.activation(out=sg[:, :], in_=G[:, :], func=AF.Silu), reads=[G], writes=[sg])
            m.op("dve", lambda e: e.tensor_tensor(out=R.act[:, f, ch * 512:(ch + 1) * 512], in0=U[:, :], in1=sg[:, :], op=ALU.mult),
                 reads=[U, sg], writes=[R.act])
    for d in range(8):
        i = R.dc % 2
        R.dc += 1
        wd = R.wd[i]
        m.dma("pool", wd[:].rearrange("p f n -> p (f n)"), wd_ap(d), writes=[wd])
        for ch in range(nch):
            Y = R.ps_y[R.yc % 2]
            R.yc += 1
            for f in range(F):
                m.op("pe", lambda e: e.matmul(Y[:, :], lhsT=wd[:, f, :], rhs=R.act[:, f, ch * 512:(ch + 1) * 512], start=(f == 0), stop=(f == F - 1)),
                     reads=[wd, R.act], writes=[Y])
            sink(d, ch, Y)


def build_pc_dense(F=22, NT=2048, NB=1024):
    nc = bass.Bass("TRN2", target_bir_lowering=False)
    D = lambda n, s, dt, k: nc.dram_tensor(n, s, dt, kind=k).ap()
    hT = D("hT", [128, 8, NT], F32, "ExternalInput")
    gffn = D("gffn", [128, 8], F32, "ExternalInput")
    cmat = D("cmat", [128, 3, 128], F32, "ExternalInput")
    wg = D("wg", [F, 128, 1024], F32, "ExternalInput")
    wu = D("wu", [F, 128, 1024], F32, "ExternalInput")
    wd = D("wd", [8, 128, F * 128], F32, "ExternalInput")
    out = D("outT", [128, 8, NT], F32, "ExternalOutput")
    with ExitStack() as es:
        m = MK(nc, es)
        R = FFNRes(m, F, NB)
        cm = m.sb("cm", [128, 3, 128], BF16)
        gf = m.sb("gf", [128, 8], F32)
        epsb = m.sb("epsb", [128, 1], F32)
        hb = m.sb("hb", [128, 8, NB], F32)
        uT = m.sb("uT", [128, 8, NB], BF16)
        uc = [m.sb("uc%d" % i, [128, 8, 512], BF16) for i in range(1)]
        sq = m.sb("sq", [128, 8, 512], BF16)
        rs = m.sb("rs", [128, 512], F32)
        ps_ss = m.ps("ps_ss", [128, 512])
        m.dma("pool", cm[:], cmat, writes=[cm])
        m.dma("sp", gf[:], gffn, writes=[gf])
        m.op("dve", lambda e: e.memset(epsb[:], EPS), writes=[epsb])
        ob = Buf("out")
        for hb_i in range(NT // NB):
            t0 = hb_i * NB
            for ch in range(NB // 512):
                m.dma("sp", hb[:, :, ch * 512:(ch + 1) * 512], hT[:, :, t0 + ch * 512:t0 + (ch + 1) * 512], writes=[hb])
            for ch in range(NB // 512):
                rms_view(m, hb, ch * 512, 512, gf, cm, epsb, ps_ss, sq, rs, uT)

            def sink(d, ch, Y):
                m.op("dve", lambda e: e.tensor_tensor(out=hb[:, d, ch * 512:(ch + 1) * 512], in0=Y[:, :], in1=hb[:, d, ch * 512:(ch + 1) * 512], op=ALU.add),
                     reads=[Y, hb], writes=[hb])
            ffn_block(R, uT, lambda f: wg[f], lambda f: wu[f], lambda d: wd[d], sink)
            m.dma("sp", out[:, :, t0:t0 + NB], hb[:], reads=[hb], writes=[ob])
        m.final_wait("sp", [ob])
        print("P_C instrs", m.n_ins, "dmas", m.ndma)
    return nc


def rms_view(m, src, c0, n, gvec, cm, epsb, ps_ss, sq, rs, uT, u32=None):
    m.op("act", lambda e: e.activation(out=sq[:, :, 0:n], in_=src[:, :, c0:c0 + n], func=AF.Square), reads=[src], writes=[sq])
    for c in range(8):
        m.op("pe", lambda e: e.matmul(ps_ss[:, 0:n], lhsT=cm[:, 0, :], rhs=sq[:, c, 0:n], start=(c == 0), stop=(c == 7)), reads=[sq, cm], writes=[ps_ss])
    m.op("act", lambda e: e.activation(out=rs[:, 0:n], in_=ps_ss[:, 0:n], func=AF.Ln, scale=1.0 / 1024, bias=epsb[:, 0:1]), reads=[ps_ss, epsb], writes=[rs])
    m.op("act", lambda e: e.activation(out=rs[:, 0:n], in_=rs[:, 0:n], func=AF.Exp, scale=-0.5), reads=[rs], writes=[rs])
    for c in range(8):
        m.op("dve", lambda e: e.scalar_tensor_tensor(out=uT[:, c, c0:c0 + n], in0=src[:, c, c0:c0 + n], scalar=gvec[:, c:c + 1], in1=rs[:, 0:n],
                                                     op0=ALU.mult, op1=ALU.mult), reads=[src, gvec, rs], writes=[uT])
        if u32 is not None:
            m.op("dve", lambda e: e.scalar_tensor_tensor(out=u32[:, c, 0:n], in0=src[:, c, c0:c0 + n], scalar=gvec[:, c:c + 1], in1=rs[:, 0:n],
                                                         op0=ALU.mult, op1=ALU.mult), reads=[src, gvec, rs], writes=[u32])


def build_l5(NT=2048):
    nc = bass.Bass("TRN2", target_bir_lowering=False)
    D = lambda n, s, dt, k: nc.dram_tensor(n, s, dt, kind=k).ap()
    hT = D("hT", [128, 8, NT], F32, "ExternalInput")
    gffn = D("gffn", [128, 8], F32, "ExternalInput")
    cmat = D("cmat", [128, 3, 128], F32, "ExternalInput")
    wr = D("wr", [128, 8, 8], F32, "ExternalInput")
    uT_o = D("uT", [128, 8, NT], BF16, "ExternalOutput")
    g_o = D("gates", [128, NT // 128, 8], F32, "ExternalOutput")
    with ExitStack() as es:
        m = MK(nc, es)
        cm = m.sb("cm", [128, 3, 128], BF16)
        gf = m.sb("gf", [128, 8], F32)
        wrs = m.sb("wrs", [128, 8, 8], F32)
        epsb = m.sb("epsb", [128, 1], F32)
        hb = [m.sb("hb%d" % i, [128, 8, 512], F32) for i in range(2)]
        uT = [m.sb("uT%d" % i, [128, 8, 512], BF16) for i in range(2)]
        u32 = m.sb("u32", [128, 8, 512], F32)
        sq = m.sb("sq", [128, 8, 512], BF16)
        rs = m.sb("rs", [128, 512], F32)
        gates = m.sb("gates_sb", [128, NT // 128, 8], F32)
        lg = [m.sb("lg%d" % i, [128, 8], F32) for i in range(2)]
        m8 = [m.sb("m8%d" % i, [128, 8], F32) for i in range(2)]
        tt = [m.sb("tt%d" % i, [128, 4], F32) for i in range(2)]
        ga = [m.sb("ga%d" % i, [128, 8], F32) for i in range(2)]
        ps_ss = m.ps("ps_ss", [128, 512])
        ps_l = [m.ps("ps_l%d" % i, [128, 8]) for i in range(2)]
        m.dma("pool", cm[:], cmat, writes=[cm])
        m.dma("sp", gf[:], gffn, writes=[gf])
        m.dma("sp", wrs[:], wr, writes=[wrs])
        m.op("dve", lambda e: e.memset(epsb[:], EPS), writes=[epsb])
        ob = Buf("out")
        k = 0
        for t in range(NT // 512):
            b = t % 2
            m.dma("sp", hb[b][:], hT[:, :, t * 512:(t + 1) * 512], writes=[hb[b]])
            rms_view(m, hb[b], 0, 512, gf, cm, epsb, ps_ss, sq, rs, uT[b], u32=u32)
            m.dma("sp", uT_o[:, :, t * 512:(t + 1) * 512], uT[b][:], reads=[uT[b]], writes=[ob])
            for s in range(4):
                j = k % 2
                k += 1
                L, lgb, m8b, tb, gab = ps_l[j], lg[j], m8[j], tt[j], ga[j]
                for c in range(8):
                    m.op("pe", lambda e: e.matmul(L[:, :], lhsT=u32[:, c, s * 128:(s + 1) * 128], rhs=wrs[:, c, :], start=(c == 0), stop=(c == 7)),
                         reads=[u32, wrs], writes=[L])
                m.op("dve", lambda e: e.tensor_copy(out=lgb[:, :], in_=L[:, :]), reads=[L], writes=[lgb])
                m.op("dve", lambda e: e.max(out=m8b[:, :], in_=lgb[:, :]), reads=[lgb], writes=[m8b])
                m.op("dve", lambda e: e.tensor_tensor(out=tb[:, 0:1], in0=m8b[:, 1:2], in1=m8b[:, 0:1], op=ALU.subtract), reads=[m8b], writes=[tb])
                m.op("act", lambda e: e.activation(out=tb[:, 1:2], in_=tb[:, 0:1], func=AF.Exp), reads=[tb], writes=[tb])
                m.op("dve", lambda e: e.tensor_scalar(out=tb[:, 2:3], in0=tb[:, 1:2], scalar1=1.0, scalar2=None, op0=ALU.add), reads=[tb], writes=[tb])
                m.op("dve", lambda e: e.reciprocal(out=tb[:, 2:3], in_=tb[:, 2:3]), reads=[tb], writes=[tb])
                m.op("dve", lambda e: e.tensor_tensor(out=tb[:, 3:4], in0=tb[:, 1:2], in1=tb[:, 2:3], op=ALU.mult), reads=[tb], writes=[tb])
                m.op("dve", lambda e: e.tensor_scalar(out=gab[:, :], in0=lgb[:, :], scalar1=m8b[:, 0:1], scalar2=tb[:, 2:3], op0=ALU.is_equal, op1=ALU.mult),
                     reads=[lgb, m8b, tb], writes=[gab])
                m.op("dve", lambda e: e.tensor_scalar(out=lgb[:, :], in0=lgb[:, :], scalar1=m8b[:, 1:2], scalar2=tb[:, 3:4], op0=ALU.is_equal, op1=ALU.mult),
                     reads=[lgb, m8b, tb], writes=[lgb])
                m.op("dve", lambda e: e.tensor_tensor(out=gates[:, t * 4 + s, :], in0=gab[:, :], in1=lgb[:, :], op=ALU.add), reads=[gab, lgb], writes=[gates])
        ob2 = Buf("out2")
        m.dma("sp", g_o, gates[:], reads=[gates], writes=[ob2])
        m.final_wait("sp", [ob, ob2])
        print("L5 instrs", m.n_ins, "dmas", m.ndma)
    return nc


def build_l6(F=28, NTOK=16384, NB=1024):
    nc = bass.Bass("TRN2", target_bir_lowering=False)
    D = lambda n, s, dt, k: nc.dram_tensor(n, s, dt, kind=k).ap()
    uT_i = D("uT", [128, 8, NTOK], BF16, "ExternalInput")
    gbc_i = D("gbc", [128, NTOK], F32, "ExternalInput")
    wg = D("wg", [F, 128, 1024], F32, "ExternalInput")
    wu = D("wu", [F, 128, 1024], F32, "ExternalInput")
    wd = D("wd", [8, 128, F * 128], F32, "ExternalInput")
    y_o = D("yT", [128, 8, NTOK], F32, "ExternalOutput")
    with ExitStack() as es:
        m = MK(nc, es)
        R = FFNRes(m, F, NB)
        uT = [m.sb("uT%d" % i, [128, 8, NB], BF16) for i in range(2)]
        gb = [m.sb("gb%d" % i, [128, NB], F32) for i in range(2)]
        yb = m.sb("yb", [128, 8, NB], F32)
        ob = Buf("out")
        for tb in range(NTOK // NB):
            b = tb % 2
            t0 = tb * NB
            m.dma("sp", uT[b][:], uT_i[:, :, t0:t0 + NB], writes=[uT[b]])
            m.dma("sp", gb[b][:], gbc_i[:, t0:t0 + NB], writes=[gb[b]])

            def sink(d, ch, Y):
                m.op("dve", lambda e: e.tensor_tensor(out=yb[:, d, ch * 512:(ch + 1) * 512], in0=Y[:, :], in1=gb[b][:, ch * 512:(ch + 1) * 512], op=ALU.mult),
                     reads=[Y, gb[b]], writes=[yb])
            ffn_block(R, uT[b], lambda f: wg[f], lambda f: wu[f], lambda d: wd[d], sink)
            m.dma("sp", y_o[:, :, t0:t0 + NB], yb[:], reads=[yb], writes=[ob])
        m.final_wait("sp", [ob])
        print("L6 instrs", m.n_ins, "dmas", m.ndma)
    return nc


def build_l7(NT=2048):
    nc = bass.Bass("TRN2", target_bir_lowering=False)
    D = lambda n, s, dt, k: nc.dram_tensor(n, s, dt, kind=k).ap()
    hT = D("hT", [128, 8, NT], F32, "ExternalInput")
    ys = D("ys", [8, 128, 8, NT], F32, "ExternalInput")
    out = D("outT", [128, 8, NT], F32, "ExternalOutput")
    with ExitStack() as es:
        m = MK(nc, es)
        acc = [m.sb("acc%d" % i, [128, 8, 512], F32) for i in range(2)]
        yb = [m.sb("yb%d" % i, [128, 8, 512], F32) for i in range(3)]
        ob = Buf("out")
        k = 0
        for t in range(NT // 512):
            a = acc[t % 2]
            m.dma("sp", a[:], hT[:, :, t * 512:(t + 1) * 512], writes=[a])
            for e_ in range(8):
                y = yb[k % 3]
                k += 1
                m.dma("sp", y[:], ys[e_, :, :, t * 512:(t + 1) * 512], writes=[y])
                m.op("dve", lambda e: e.tensor_tensor(out=a[:], in0=a[:], in1=y[:], op=ALU.add), reads=[a, y], writes=[a])
            m.dma("sp", out[:, :, t * 512:(t + 1) * 512], a[:], reads=[a], writes=[ob])
        m.final_wait("sp", [ob])
        print("L7 instrs", m.n_ins, "dmas", m.ndma)
    return nc

H_NA=6; HD=64
def fm(a):
    T = a.shape[0]
    return np.ascontiguousarray(a.reshape(T, 8, 128).transpose(2, 1, 0))
def wt(w):
    K, N = w.shape
    return np.ascontiguousarray(w.reshape(K // 128, 128, N).transpose(1, 0, 2))
def gvec(g):
    return np.ascontiguousarray(g.reshape(8, 128).T)
def cmat():
    ones = np.ones((128, 128), np.float32)
    blk = np.zeros((128, 128), np.float32); blk[:64, :64] = 1; blk[64:, 64:] = 1
    perm = np.zeros((128, 128), np.float32)
    for mm in range(128):
        k = (mm // 64) * 64 + ((mm % 64) + 32) % 64
        perm[k, mm] = 1
    return np.ascontiguousarray(np.stack([ones, blk, perm], axis=1))
def cs_table(pos):
    half = 32
    inv = np.power(np.float32(10000.0), -(2.0 / 64) * np.arange(half, dtype=np.float32)).astype(np.float32)
    ang = pos.astype(np.float32)[:, None] * inv[None, :]
    cos = np.cos(ang).astype(np.float32); sin = np.sin(ang).astype(np.float32)
    d = np.arange(128) % 64
    c = cos[:, d % 32].T
    s = np.where((d < 32)[:, None], -sin[:, d % 32].T, sin[:, d % 32].T)
    return np.ascontiguousarray(np.stack([c, s], axis=1).astype(np.float32))
def gqk_table(inp, l):
    t = np.zeros((128, 8), np.float32)
    d = np.arange(128) % 64
    t[:, 0] = inp["g_qk_na"][l, 0][d]; t[:, 1] = inp["g_qk_na"][l, 1][d]
    t[:, 2] = inp["g_qk_dil"][l, 0][d]; t[:, 3] = inp["g_qk_dil"][l, 1][d]
    t[:, 4] = inp["g_qk_mem"][l, 0][d]; t[:, 5] = inp["g_qk_mem"][l, 1][d]
    return t

BF = ml_dtypes.bfloat16
def cmask_table():
    k = np.arange(128)[:, None]; q = np.arange(128)[None, :]
    out = np.zeros((128, 17, 128), np.float32)
    for j in range(17):
        off = 128 * (j - 8) + k - q
        c = np.zeros((128, 128), np.float32)
        for w, d in ((128, 1), (512, 4), (2048, 16)):
            c += ((off % d == 0) & (np.abs(off) <= w // 2)).astype(np.float32)
        out[:, j, :] = c
    return np.ascontiguousarray(out.reshape(128, 17 * 128))
def na_bias(rpb_l, p, gb):
    ws = min(max(gb - 2, 0), 59)
    k = np.arange(128); q = np.arange(128)
    out = np.full((128, 2, 5, 128), -100.0, np.float32)
    qrow = 2 * gb + q // 64; qcol = q % 64
    rstart = np.clip(qrow - 4, 0, 120); cstart = np.clip(qcol - 8, 0, 48)
    for j in range(5):
        krow = 2 * (ws + j) + k // 64; kcol = k % 64
        inr = (krow[:, None] >= rstart[None, :]) & (krow[:, None] < rstart[None, :] + 8)
        inc = (kcol[:, None] >= cstart[None, :]) & (kcol[:, None] < cstart[None, :] + 16)
        ok = inr & inc
        ri = np.clip(krow[:, None] - qrow[None, :] + 7, 0, 14); ci = np.clip(kcol[:, None] - qcol[None, :] + 15, 0, 30)
        for hh in range(2):
            vals = rpb_l[2 * p + hh][ri, ci]
            out[:, hh, j, :] = np.where(ok, vals, np.float32(-100.0))
    return out.reshape(128, 1280)
def prep_pb(core, l, inp, pa_res, h_full_T):
    b = core // 4; ci = core % 4; s0 = ci * 2048; tile0 = ci * 16
    K = np.concatenate([np.asarray(pa_res[b * 4 + c]["kT"]) for c in range(4)], axis=2)
    V = np.concatenate([np.asarray(pa_res[b * 4 + c]["v"]) for c in range(4)], axis=1)
    Kp = np.zeros((128, 6, 8192 + 2048), K.dtype); Kp[:, :, 1024:1024 + 8192] = K
    Vp = np.zeros((128, 64 + 16, 780), V.dtype); Vp[:, 8:72] = V
    r = pa_res[core]
    d = {"qT": np.asarray(r["qT"])}
    d["kA"] = np.ascontiguousarray(Kp[:, 0:3, 1024 + s0 - 256:1024 + s0 + 2304])
    d["kB"] = np.ascontiguousarray(Kp[:, 3:6, s0:s0 + 4096])
    vA = Vp[:, 8 + tile0 - 2:8 + tile0 + 18, 0:390].reshape(128, 20, 3, 130).transpose(0, 2, 1, 3)
    vB = Vp[:, tile0:tile0 + 32, 390:780].reshape(128, 32, 3, 130).transpose(0, 2, 1, 3)
    d["vA"] = np.ascontiguousarray(vA); d["vB"] = np.ascontiguousarray(vB)
    kAe = np.zeros((128, 3, 4, 640), K.dtype); vAe = np.zeros((128, 3, 4, 650), V.dtype)
    for slot, i in enumerate((0, 1, 14, 15)):
        gb = tile0 + i; ws = min(max(gb - 2, 0), 59)
        kAe[:, :, slot, :] = K[:, 0:3, ws * 128:ws * 128 + 640]
        vv = V[:, ws:ws + 5, 0:390].reshape(128, 5, 3, 130).transpose(0, 2, 1, 3)
        vAe[:, :, slot, :] = vv.reshape(128, 3, 650)
    d["kAe"] = kAe; d["vAe"] = vAe
    d["kM"] = np.asarray(r["kmT"])
    d["vM"] = np.ascontiguousarray(np.asarray(r["vm"]).reshape(128, 2, 2, 130).transpose(0, 2, 1, 3))
    nab = np.zeros((128, 3, 5, 1280), np.float32)
    for p in range(3):
        for v, i in enumerate((0, 1, 2, 14, 15)):
            nab[:, p, v, :] = na_bias(inp["rpb_na"][l], p, tile0 + i)
    d["nab"] = nab
    d["cmask"] = cmask_table()
    d["ident"] = np.eye(128, dtype=np.float32)
    d["gout"] = np.ascontiguousarray(np.broadcast_to(inp["g_out"][l][None, :], (128, 1024))).astype(np.float32)
    d["w_out"] = wt(inp["w_out"][l])
    d["hT"] = h_full_T[core]
    return d
def prep_pa(core, l, inp, hT_core):
    b = core // 4; s0 = (core % 4) * 2048
    return {"hT": hT_core, "w_in": wt(inp["w_in"][l]), "gattn": gvec(inp["g_attn"][l]), "gqk": gqk_table(inp, l), "cmat": cmat(),
            "cs": cs_table(np.arange(s0, s0 + 2048)), "memT": fm(inp["mem"][b]), "gmem": gvec(inp["g_mem"][l]), "w_kv": wt(inp["w_mem_kv"][l])}

def wgu_t(w, F):
    return np.ascontiguousarray(w.reshape(8, 128, F, 128).transpose(2, 1, 0, 3).reshape(F, 128, 1024))
def wd_t(w, F):
    return np.ascontiguousarray(w.reshape(F, 128, 8, 128).transpose(2, 1, 0, 3).reshape(8, 128, F * 128))
def unfm(aT):
    return np.ascontiguousarray(aT.transpose(2, 1, 0).reshape(aT.shape[2], 1024))

from concourse.bass_utils import run_bass_kernel_spmd
_CORES = list(range(8))
_PROGS = {}


def _prog(name, fn):
    if name not in _PROGS:
        _PROGS[name] = fn()
    return _PROGS[name]


def _run(name, fn, ims):
    nc = _prog(name, fn)
    return run_bass_kernel_spmd(nc, ims, core_ids=_CORES).results


def _attn_layer(inp, l, hTs):
    ra = _run("pa", build_pa, [prep_pa(c, l, inp, hTs[c]) for c in range(8)])
    rb = _run("pb", build_pb, [prep_pb(c, l, inp, ra, hTs) for c in range(8)])
    return [np.asarray(rb[c]["hmidT"]) for c in range(8)]


def kernel(**inp):
    inp = {k: np.asarray(v) for k, v in inp.items()}
    x = inp["x"]
    hTs = [fm(x[c // 4, (c % 4) * 2048:(c % 4 + 1) * 2048]) for c in range(8)]
    hm = _attn_layer(inp, 0, hTs)
    wg = wgu_t(inp["w_gate_dense"][0], 22); wu = wgu_t(inp["w_up_dense"][0], 22); wd = wd_t(inp["w_down_dense"][0], 22)
    cm = cmat()
    rc = _run("pc", build_pc_dense, [{"hT": hm[c], "gffn": gvec(inp["g_ffn"][0]), "cmat": cm, "wg": wg, "wu": wu, "wd": wd} for c in range(8)])
    h1 = [np.asarray(rc[c]["outT"]) for c in range(8)]
    hm1 = _attn_layer(inp, 1, h1)
    wr = wt(inp["w_router"][0])
    r5 = _run("l5", build_l5, [{"hT": hm1[c], "gffn": gvec(inp["g_ffn"][1]), "cmat": cm, "wr": wr} for c in range(8)])
    uT_all = np.ascontiguousarray(np.concatenate([np.asarray(r5[c]["uT"]) for c in range(8)], axis=2))
    gates = np.concatenate([np.asarray(r5[c]["gates"]).transpose(1, 0, 2).reshape(2048, 8) for c in range(8)], axis=0)
    ims = []
    for e in range(8):
        ims.append({"uT": uT_all, "gbc": np.ascontiguousarray(np.broadcast_to(gates[:, e][None, :], (128, 16384))),
                    "wg": wgu_t(inp["w_gate_moe"][0, e], 28), "wu": wgu_t(inp["w_up_moe"][0, e], 28), "wd": wd_t(inp["w_down_moe"][0, e], 28)})
    r6 = _run("l6", build_l6, ims)
    ims = []
    for c in range(8):
        ys = np.ascontiguousarray(np.stack([np.asarray(r6[e]["yT"])[:, :, c * 2048:(c + 1) * 2048] for e in range(8)]))
        ims.append({"hT": hm1[c], "ys": ys})
    r7 = _run("l7", build_l7, ims)
    out = np.stack([unfm(np.asarray(r7[c]["outT"])) for c in range(8)]).reshape(2, 8192, 1024)
    return out.astype(np.float32)
```

```python
import numpy as np
import ml_dtypes
import numpy as np
from contextlib import ExitStack
import concourse.bass as bass
import concourse.mybir as mybir

F32 = mybir.dt.float32
BF16 = mybir.dt.bfloat16
AF = mybir.ActivationFunctionType
ALU = mybir.AluOpType
AX = mybir.AxisListType


class Buf:
    __slots__ = ("name", "w", "r", "t", "dsem")

    def __init__(self, name, t=None):
        self.name = name
        self.w = None
        self.r = []
        self.t = t
        self.dsem = None

    def __getitem__(self, idx):
        return self.t[idx]


class MK:
    SAME_ENGINE_SYNC = False

    def __init__(self, nc, es, tag=""):
        self.nc = nc
        self.es = es
        self.tag = tag
        self.eng = {"pe": nc.tensor, "act": nc.scalar, "dve": nc.vector, "pool": nc.gpsimd, "sp": nc.sync}
        self.esem = {k: es.enter_context(nc.semaphore("s_" + tag + k)) for k in self.eng}
        self.ecnt = {k: 0 for k in self.eng}
        self.waited = {k: {} for k in self.eng}
        self.dsems = []
        self.dcnt = {}
        self.ndma = 0
        self.n_ins = 0

    def sb(self, name, shape, dt):
        t = self.es.enter_context(self.nc.sbuf_tensor(name, list(shape), dt))
        return Buf(name, t)

    def ps(self, name, shape, dt=F32):
        t = self.es.enter_context(self.nc.psum_tensor(name, list(shape), dt))
        return Buf(name, t)

    def new_dsem(self, name):
        s = self.es.enter_context(self.nc.semaphore(self.tag + name))
        self.dcnt[id(s)] = [s, 0]
        return s

    def _wait(self, E, raw, other, strict=False):
        eng = self.eng[E]
        best = {}
        own = self.esem[E]
        for lst, is_raw in ((raw, True), (other, False)):
            for ev in lst:
                if ev is None:
                    continue
                sem, val = ev
                if sem is own and not self.SAME_ENGINE_SYNC:
                    if not (is_raw and (E != "pe" or strict)):
                        continue
                k = id(sem)
                if k not in best or best[k][1] < val:
                    best[k] = (sem, val)
        for k, (sem, val) in best.items():
            if self.waited[E].get(k, 0) >= val:
                continue
            eng.wait_ge(sem, val)
            self.waited[E][k] = val

    def _deps(self, reads, writes):
        raw = [b.w for b in reads]
        other = []
        for b in writes:
            other.append(b.w)
            other.extend(b.r)
        return raw, other

    def _commit(self, ev, reads, writes):
        for b in reads:
            b.r.append(ev)
            if len(b.r) > 64:
                best = {}
                for s, v in b.r:
                    if id(s) not in best or best[id(s)][1] < v:
                        best[id(s)] = (s, v)
                b.r = list(best.values())
        for b in writes:
            b.w = ev
            b.r = []

    def op(self, E, fn, reads=(), writes=(), strict=False):
        self._wait(E, *self._deps(reads, writes), strict=strict)
        ins = fn(self.eng[E])
        self.ecnt[E] += 1
        ins.then_inc(self.esem[E], 1)
        ev = (self.esem[E], self.ecnt[E])
        self._commit(ev, reads, writes)
        self.n_ins += 1
        return ins

    def dma(self, E, out, in_, reads=(), writes=(), dsem=None, **kw):
        self._wait(E, *self._deps(reads, writes))
        ins = self.eng[E].dma_start(out=out, in_=in_, **kw)
        wb = writes[0]
        if wb.dsem is None:
            wb.dsem = self.new_dsem("d_" + wb.name)
        dsem = wb.dsem
        rec = self.dcnt[id(dsem)]
        rec[1] += 16
        ins.then_inc(dsem, 16)
        ev = (dsem, rec[1])
        self._commit(ev, reads, writes)
        self.ndma += 1
        return ins

    def idma(self, out, in_, idx_ap, scatter, bound, reads=(), writes=()):
        self._wait("pool", *self._deps(reads, writes))
        if not hasattr(self, "_bregs"):
            self._bregs = {}
        if bound not in self._bregs:
            reg = self.nc.gpsimd.alloc_register("bnd%d" % len(self._bregs))
            self.nc.gpsimd.reg_mov(reg, bound)
            self._bregs[bound] = reg
        bound = self._bregs[bound]
        off = bass.IndirectOffsetOnAxis(ap=idx_ap, axis=0)
        if scatter:
            ins = self.nc.gpsimd.indirect_dma_start(out=out, out_offset=off, in_=in_, in_offset=None, bounds_check=bound, oob_is_err=False)
        else:
            ins = self.nc.gpsimd.indirect_dma_start(out=out, out_offset=None, in_=in_, in_offset=off, bounds_check=bound, oob_is_err=False)
        wb = writes[0]
        if wb.dsem is None:
            wb.dsem = self.new_dsem("d_" + wb.name)
        rec = self.dcnt[id(wb.dsem)]
        rec[1] += 16
        ins.then_inc(wb.dsem, 16)
        ev = (wb.dsem, rec[1])
        self._commit(ev, reads, writes)
        self.ndma += 1
        return ins

    def final_wait(self, E, bufs):
        self._wait(E, [b.w for b in bufs], [])


NT = 2048
CH = 512
NCH = NT // CH
EPS = 1e-6

QK_GROUPS = ([("q", j, 0 + 128 * j, 0, False) for j in range(3)]
             + [("k", j, 384 + 128 * j, 1, False) for j in range(3)]
             + [("q", 3 + j, 1152 + 128 * j, 2, True) for j in range(3)]
             + [("k", 3 + j, 1536 + 128 * j, 3, True) for j in range(3)]
             + [("q", 6 + j, 2304 + 128 * j, 4, False) for j in range(2)])


def rms_chunk(m, src, n, gvec, ones, epsb, ps_ss, sq, rs, uT, u32=None):
    m.op("act", lambda e: e.activation(out=sq[:, :, 0:n], in_=src[:, :, 0:n], func=AF.Square), reads=[src], writes=[sq])
    for c in range(8):
        m.op("pe", lambda e: e.matmul(ps_ss[:, 0:n], lhsT=ones[:, 0, :], rhs=sq[:, c, 0:n], start=(c == 0), stop=(c == 7)),
             reads=[sq, ones], writes=[ps_ss])
    m.op("act", lambda e: e.activation(out=rs[:, 0:n], in_=ps_ss[:, 0:n], func=AF.Ln, scale=1.0 / 1024, bias=epsb[:, 0:1]),
         reads=[ps_ss, epsb], writes=[rs])
    m.op("act", lambda e: e.activation(out=rs[:, 0:n], in_=rs[:, 0:n], func=AF.Exp, scale=-0.5), reads=[rs], writes=[rs])
    for c in range(8):
        m.op("dve", lambda e: e.scalar_tensor_tensor(out=uT[:, c, 0:n], in0=src[:, c, 0:n], scalar=gvec[:, c:c + 1],
                                                     in1=rs[:, 0:n], op0=ALU.mult, op1=ALU.mult),
             reads=[src, gvec, rs], writes=[uT])
        if u32 is not None:
            m.op("pool", lambda e: e.scalar_tensor_tensor(out=u32[:, c, 0:n], in0=src[:, c, 0:n], scalar=gvec[:, c:c + 1],
                                                          in1=rs[:, 0:n], op0=ALU.mult, op1=ALU.mult),
                 reads=[src, gvec, rs], writes=[u32])


def build_pa():
    nc = bass.Bass("TRN2", target_bir_lowering=False)
    D = lambda n, s, dt, k: nc.dram_tensor(n, s, dt, kind=k).ap()
    hT = D("hT", [128, 8, NT], F32, "ExternalInput")
    w_in = D("w_in", [128, 8, 2560], F32, "ExternalInput")
    gattn = D("gattn", [128, 8], F32, "ExternalInput")
    gqk = D("gqk", [128, 8], F32, "ExternalInput")
    cmat = D("cmat", [128, 3, 128], F32, "ExternalInput")
    cs = D("cs", [128, 2, NT], F32, "ExternalInput")
    memT = D("memT", [128, 8, 256], F32, "ExternalInput")
    gmem = D("gmem", [128, 8], F32, "ExternalInput")
    w_kv = D("w_kv", [128, 8, 512], F32, "ExternalInput")
    qT_o = D("qT", [128, 8, NT], BF16, "ExternalOutput")
    kT_o = D("kT", [128, 6, NT], BF16, "ExternalOutput")
    v_o = D("v", [128, 16, 12 * 65], BF16, "ExternalOutput")
    kmT_o = D("kmT", [128, 2, 256], BF16, "ExternalOutput")
    vm_o = D("vm", [128, 2, 4 * 65], BF16, "ExternalOutput")

    with ExitStack() as es:
        m = MK(nc, es)
        W = m.sb("W", [128, 8, 2560], BF16)
        Wkv = m.sb("Wkv", [128, 8, 512], BF16)
        cm = m.sb("cm", [128, 3, 128], BF16)
        ga = m.sb("ga", [128, 8], F32)
        gm = m.sb("gm", [128, 8], F32)
        gq = m.sb("gq", [128, 8], F32)
        epsb = m.sb("epsb", [128, 1], F32)
        cst = [m.sb("cst%d" % i, [128, 2, CH], F32) for i in range(2)]
        hc = [m.sb("hc%d" % i, [128, 8, CH], F32) for i in range(2)]
        sq = m.sb("sq", [128, 8, CH], BF16)
        rs = m.sb("rs", [128, CH], F32)
        uT = [m.sb("uT%d" % i, [128, 8, CH], BF16) for i in range(2)]
        sq2 = [m.sb("sq2%d" % i, [128, CH], BF16) for i in range(2)]
        r2 = [m.sb("r2%d" % i, [128, CH], F32) for i in range(2)]
        qn = [m.sb("qn%d" % i, [128, CH], BF16) for i in range(2)]
        t1 = [m.sb("t1%d" % i, [128, CH], F32) for i in range(2)]
        t2 = [m.sb("t2%d" % i, [128, CH], F32) for i in range(2)]
        qTs2 = [m.sb("qTs%d" % i, [128, 8, CH], BF16) for i in range(2)]
        kTs2 = [m.sb("kTs%d" % i, [128, 6, CH], BF16) for i in range(2)]
        vs2 = [m.sb("vs%d" % i, [128, 4, 12 * 65], BF16) for i in range(2)]
        kms = m.sb("kms", [128, 2, 256], BF16)
        vms = m.sb("vms", [128, 2, 4 * 65], BF16)
        ps_ss = m.ps("ps_ss", [128, 512])
        ps_y = [m.ps("ps_y%d" % i, [128, 512]) for i in range(2)]
        ps_s2 = [m.ps("ps_s2%d" % i, [128, 512]) for i in range(2)]
        ps_qp = m.ps("ps_qp", [128, 512])
        ps_v = [m.ps("ps_v%d" % i, [128, 512]) for i in range(2)]

        dW = [m.new_dsem("dW%d" % i) for i in range(4)]
        dsm = m.new_dsem("dsm")
        dh = [m.new_dsem("dh%d" % i) for i in range(2)]
        dc = [m.new_dsem("dc%d" % i) for i in range(2)]
        dout = m.new_dsem("dout")

        m.dma("sp", ga[:], gattn, writes=[ga], dsem=dsm)
        m.dma("sp", gm[:], gmem, writes=[gm], dsem=dsm)
        m.dma("sp", gq[:], gqk, writes=[gq], dsem=dsm)
        m.dma("pool", cm[:], cmat, writes=[cm], dsem=dW[0])
        m.op("dve", lambda e: e.memset(epsb[:], EPS), writes=[epsb])
        for i in range(2):
            m.op("dve", lambda e: e.memset(vs2[i][:], 1.0), writes=[vs2[i]])
        m.op("dve", lambda e: e.memset(vms[:], 1.0), writes=[vms])
        Wb = [Buf("Wb%d" % i) for i in range(4)]
        for i in range(4):
            m.dma("pool", W[:, :, 640 * i:640 * (i + 1)], w_in[:, :, 640 * i:640 * (i + 1)], writes=[Wb[i]], dsem=dW[i])
        m.dma("pool", Wkv[:], w_kv, writes=[Wkv], dsem=dW[0])

        def wbufs(c0, c1):
            return [Wb[i] for i in range(4) if c0 < 640 * (i + 1) and c1 > 640 * i]

        cnt = {"y": 0, "v": 0}

        def qk_group(u, n, Wt, wdeps, col0, gcolumn, rope, dst, dsti, tok0, csb):
            i = cnt["y"] % 2
            cnt["y"] += 1
            y, s2, sq2b, r2b, qnb = ps_y[i], ps_s2[i], sq2[i], r2[i], qn[i]
            for c in range(8):
                m.op("pe", lambda e: e.matmul(y[:, 0:n], lhsT=Wt[:, c, col0:col0 + 128], rhs=u[:, c, 0:n], start=(c == 0), stop=(c == 7)),
                     reads=[u] + wdeps, writes=[y])
            m.op("act", lambda e: e.activation(out=sq2b[:, 0:n], in_=y[:, 0:n], func=AF.Square), reads=[y], writes=[sq2b])
            m.op("pe", lambda e: e.matmul(s2[:, 0:n], lhsT=cm[:, 1, :], rhs=sq2b[:, 0:n], start=True, stop=True), reads=[sq2b, cm], writes=[s2])
            m.op("act", lambda e: e.activation(out=r2b[:, 0:n], in_=s2[:, 0:n], func=AF.Ln, scale=1.0 / 64, bias=epsb[:, 0:1]),
                 reads=[s2, epsb], writes=[r2b])
            m.op("act", lambda e: e.activation(out=r2b[:, 0:n], in_=r2b[:, 0:n], func=AF.Exp, scale=-0.5), reads=[r2b], writes=[r2b])
            d = dst[:, dsti, tok0:tok0 + n]
            if not rope:
                m.op("dve", lambda e: e.scalar_tensor_tensor(out=d, in0=y[:, 0:n], scalar=gq[:, gcolumn:gcolumn + 1], in1=r2b[:, 0:n],
                                                             op0=ALU.mult, op1=ALU.mult), reads=[y, gq, r2b], writes=[dst])
            else:
                t1b, t2b = t1[i], t2[i]
                m.op("dve", lambda e: e.scalar_tensor_tensor(out=qnb[:, 0:n], in0=y[:, 0:n], scalar=gq[:, gcolumn:gcolumn + 1], in1=r2b[:, 0:n],
                                                             op0=ALU.mult, op1=ALU.mult), reads=[y, gq, r2b], writes=[qnb])
                m.op("pe", lambda e: e.matmul(ps_qp[:, 0:n], lhsT=cm[:, 2, :], rhs=qnb[:, 0:n], start=True, stop=True), reads=[qnb, cm], writes=[ps_qp])
                m.op("pool", lambda e: e.tensor_tensor(out=t1b[:, 0:n], in0=qnb[:, 0:n], in1=csb[:, 0, 0:n], op=ALU.mult), reads=[qnb, csb], writes=[t1b])
                m.op("dve", lambda e: e.tensor_tensor(out=t2b[:, 0:n], in0=ps_qp[:, 0:n], in1=csb[:, 1, 0:n], op=ALU.mult), reads=[ps_qp, csb], writes=[t2b])
                m.op("pool", lambda e: e.tensor_tensor(out=d, in0=t1b[:, 0:n], in1=t2b[:, 0:n], op=ALU.add), reads=[t1b, t2b], writes=[dst])

        def v_tile(u, Wt, wdeps, col0, ncols, sub, dst, tile, head0, nheads):
            i = cnt["v"] % 2
            cnt["v"] += 1
            pv = ps_v[i]
            for c in range(8):
                m.op("pe", lambda e: e.matmul(pv[:, 0:ncols], lhsT=u[:, c, sub * 128:(sub + 1) * 128], rhs=Wt[:, c, col0:col0 + ncols],
                                              start=(c == 0), stop=(c == 7)), reads=[u] + wdeps, writes=[pv])
            dv = dst[:, tile, head0 * 65:(head0 + nheads) * 65].rearrange("p (h d) -> p h d", d=65)[:, :, 0:64]
            sv = pv[:, 0:ncols].rearrange("p (h d) -> p h d", d=64)
            m.op("act", lambda e: e.activation(out=dv, in_=sv, func=AF.Copy), reads=[pv], writes=[dst])

        m.dma("sp", hc[0][:, :, 0:256], memT, writes=[hc[0]], dsem=dh[0])
        rms_chunk(m, hc[0], 256, gm, cm, epsb, ps_ss, sq, rs, uT[0])
        for j in range(2):
            qk_group(uT[0], 256, Wkv, [Wkv], 128 * j, 5, False, kms, j, 0, None)
        for sub in range(2):
            v_tile(uT[0], Wkv, [Wkv], 256, 256, sub, vms, sub, 0, 4)
        obm = [Buf("o1"), Buf("o2")]
        m.dma("sp", kmT_o, kms[:], reads=[kms], writes=[obm[0]], dsem=dout)
        m.dma("sp", vm_o, vms[:], reads=[vms], writes=[obm[1]], dsem=dout)

        ob = [Buf("oq"), Buf("ok"), Buf("ov")]
        for t in range(NCH):
            b = (t + 1) % 2
            tok0 = t * CH
            m.dma("sp", hc[b][:], hT[:, :, tok0:tok0 + CH], writes=[hc[b]], dsem=dh[b])
            m.dma("sp", cst[b][:], cs[:, :, tok0:tok0 + CH], writes=[cst[b]], dsem=dc[b])
            rms_chunk(m, hc[b], CH, ga, cm, epsb, ps_ss, sq, rs, uT[b])
            qTs, kTs, vs = qTs2[b], kTs2[b], vs2[b]
            for (dn, di, col0, gc, rope) in QK_GROUPS:
                qk_group(uT[b], CH, W, wbufs(col0, col0 + 128), col0, gc, rope, qTs if dn == "q" else kTs, di, 0, cst[b])
            for sub in range(4):
                v_tile(uT[b], W, wbufs(768, 1152), 768, 384, sub, vs, sub, 0, 6)
                v_tile(uT[b], W, wbufs(1920, 2304), 1920, 384, sub, vs, sub, 6, 6)
            m.dma("sp", qT_o[:, :, tok0:tok0 + CH], qTs[:], reads=[qTs], writes=[ob[0]], dsem=dout)
            m.dma("sp", kT_o[:, :, tok0:tok0 + CH], kTs[:], reads=[kTs], writes=[ob[1]], dsem=dout)
            m.dma("sp", v_o[:, 4 * t:4 * t + 4, :], vs[:], reads=[vs], writes=[ob[2]], dsem=dout)
        m.final_wait("sp", ob + obm)
        print("P_A instrs", m.n_ins, "dmas", m.ndma)
    return nc


NT = 2048
EPS = 1e-6
SCALE = 0.125
DEBUG = False


def build_pb():
    nc = bass.Bass("TRN2", target_bir_lowering=False)
    D = lambda n, s, dt, k: nc.dram_tensor(n, s, dt, kind=k).ap()
    qT = D("qT", [128, 8, NT], BF16, "ExternalInput")
    kA = D("kA", [128, 3, 2560], BF16, "ExternalInput")
    vA = D("vA", [128, 3, 20, 130], BF16, "ExternalInput")
    kAe = D("kAe", [128, 3, 4, 640], BF16, "ExternalInput")
    vAe = D("vAe", [128, 3, 4, 650], BF16, "ExternalInput")
    kB = D("kB", [128, 3, 4096], BF16, "ExternalInput")
    vB = D("vB", [128, 3, 32, 130], BF16, "ExternalInput")
    kM = D("kM", [128, 2, 256], BF16, "ExternalInput")
    vM = D("vM", [128, 2, 2, 130], BF16, "ExternalInput")
    nab = D("nab", [128, 3, 5, 1280], F32, "ExternalInput")
    cmask = D("cmask", [128, 17 * 128], F32, "ExternalInput")
    ident = D("ident", [128, 128], F32, "ExternalInput")
    gout = D("gout", [128, 1024], F32, "ExternalInput")
    w_out = D("w_out", [128, 8, 1024], F32, "ExternalInput")
    hT = D("hT", [128, 8, NT], F32, "ExternalInput")
    hmid = D("hmidT", [128, 8, NT], F32, "ExternalOutput")
    o_dbg = D("o_dbg", [128, 16, 1024], BF16, "ExternalOutput") if DEBUG else None
    dbg2 = D("dbg2", [128, 1024], BF16, "ExternalOutput") if DEBUG else None
    dbg3 = D("dbg3", [128, 4], F32, "ExternalOutput") if DEBUG else None
    dbg4 = D("dbg4", [128, 8, 512], BF16, "ExternalOutput") if DEBUG else None

    with ExitStack() as es:
        m = MK(nc, es)
        qp = [m.sb("qp%d" % i, [128, NT], BF16) for i in range(2)]
        kb = [m.sb("kb%d" % i, [128, 4096], BF16) for i in range(2)]
        vb = [m.sb("vb%d" % i, [128, 32, 130], BF16) for i in range(2)]
        ke = [m.sb("ke0", [128, 4, 640], BF16)] * 2
        ve = [m.sb("ve0", [128, 4, 650], BF16)] * 2
        stage = [m.sb("stage%d" % i, [128, 1280], F32) for i in range(2)]
        Mna = [m.sb("Mna0", [128, 5, 1280], BF16)] * 2
        Cm = m.sb("Cm", [128, 17 * 128], BF16)
        idn = m.sb("idn", [128, 128], BF16)
        go = m.sb("go", [128, 1024], F32)
        Wo = m.sb("Wo", [128, 8, 1024], BF16)
        epsb = m.sb("epsb", [128, 1], F32)
        o_all = m.sb("o_all", [128, 16, 1024], BF16)
        Eb = [m.sb("E%d" % i, [128, 512], BF16) for i in range(3)]
        Pb = [m.sb("P%d" % i, [128, 512], BF16) for i in range(3)]
        rec = [m.sb("rec%d" % i, [128, 2], F32) for i in range(2)]
        sqf = m.sb("sqf", [128, 1024], F32)
        ms = [m.sb("ms%d" % i, [128, 4], F32) for i in range(2)]
        mixed = [m.sb("mixed%d" % i, [128, 1024], BF16) for i in range(2)]
        mixedT = [m.sb("mixedT%d" % i, [128, 8, 512], BF16) for i in range(2)]
        hc = [m.sb("hc0", [128, 8, 512], F32)] * 2
        ps_s = [m.ps("ps_s%d" % i, [128, 512]) for i in range(3)]
        ps_o = [m.ps("ps_o%d" % i, [128, 512]) for i in range(2)]
        ps_T = m.ps("ps_T", [128, 1024], BF16)
        ps_y = [m.ps("ps_y%d" % i, [128, 512]) for i in range(2)]

        dl = [m.new_dsem("dl%d" % i) for i in range(2)]
        dst_ = [m.new_dsem("dst%d" % i) for i in range(2)]
        dcst = m.new_dsem("dcst")
        dh = [m.new_dsem("dh%d" % i) for i in range(2)]
        dout = m.new_dsem("dout")

        m.dma("pool", Cm[:], cmask, writes=[Cm], dsem=dcst)
        m.dma("pool", idn[:], ident, writes=[idn], dsem=dcst)
        m.dma("sp", go[:], gout, writes=[go], dsem=dcst)
        m.op("dve", lambda e: e.memset(epsb[:], EPS), writes=[epsb])
        Wo_loaded = [False]

        gcnt = [0]
        stc = [0]
        SLOT = {0: 0, 1: 1, 14: 2, 15: 3}
        VAR = {0: 0, 1: 1, 14: 3, 15: 4}

        def kind_of(p):
            return "A" if p < 3 else ("B" if p < 6 else "M")

        def load_main(p):
            b = p % 2
            kind = kind_of(p)
            m.dma("sp", qp[b][:], qT[:, p, :], writes=[qp[b]])
            if kind == "A":
                m.dma("sp", kb[b][:, 0:2560], kA[:, p, :], writes=[kb[b]])
                m.dma("sp", vb[b][:, 0:20, :], vA[:, p], writes=[vb[b]])
            elif kind == "B":
                m.dma("sp", kb[b][:], kB[:, p - 3, :], writes=[kb[b]])
                m.dma("sp", vb[b][:], vB[:, p - 3], writes=[vb[b]])
            else:
                m.dma("sp", kb[b][:, 0:256], kM[:, p - 6, :], writes=[kb[b]])
                m.dma("sp", vb[b][:, 0:2, :], vM[:, p - 6], writes=[vb[b]])

        def load_edge(p):
            b = p % 2
            m.dma("sp", ke[b][:], kAe[:, p], writes=[ke[b]])
            m.dma("sp", ve[b][:], vAe[:, p], writes=[ve[b]])
            for v in range(5):
                sb_ = stc[0] % 2
                stc[0] += 1
                m.dma("sp", stage[sb_][:], nab[:, p, v, :], writes=[stage[sb_]])
                m.op("act", lambda e: e.activation(out=Mna[b][:, v, :], in_=stage[sb_][:], func=AF.Exp), reads=[stage[sb_]], writes=[Mna[b]])

        units = []
        for p in range(8):
            kind = kind_of(p)
            b = p % 2
            for i in range(16):
                O = ps_o[i % 2]
                for hh in range(2):
                    pbs = 64 * hh
                    tiles = []
                    if kind == "A":
                        slot = SLOT.get(i)
                        var = VAR.get(i, 2)
                        for j in range(5):
                            if slot is None:
                                k_ap = kb[b][pbs:pbs + 64, (i + j) * 128:(i + j + 1) * 128]
                                v_ap = vb[b][:, i + j, hh * 65:(hh + 1) * 65]
                            else:
                                k_ap = ke[b][pbs:pbs + 64, slot, j * 128:(j + 1) * 128]
                                v_ap = ve[b][:, slot, j * 130 + hh * 65:j * 130 + (hh + 1) * 65]
                            tiles.append((k_ap, v_ap, (Mna[b], var, hh * 640 + j * 128)))
                        kdeps = [kb[b], ke[b]]
                        vdeps = [vb[b], ve[b]]
                    elif kind == "B":
                        for j in range(17):
                            k_ap = kb[b][pbs:pbs + 64, (i + j) * 128:(i + j + 1) * 128]
                            v_ap = vb[b][:, i + j, hh * 65:(hh + 1) * 65]
                            tiles.append((k_ap, v_ap, (Cm, None, j * 128)))
                        kdeps = [kb[b]]
                        vdeps = [vb[b]]
                    else:
                        for j in range(2):
                            k_ap = kb[b][pbs:pbs + 64, j * 128:(j + 1) * 128]
                            v_ap = vb[b][:, j, hh * 65:(hh + 1) * 65]
                            tiles.append((k_ap, v_ap, None))
                        kdeps = [kb[b]]
                        vdeps = [vb[b]]
                    q_ap = qp[b][pbs:pbs + 64, i * 128:(i + 1) * 128]
                    nt = len(tiles)
                    for g0 in range(0, nt, 4):
                        units.append(dict(p=p, b=b, i=i, hh=hh, O=O, grp=tiles[g0:g0 + 4], g0=g0, nt=nt, q_ap=q_ap, kdeps=kdeps, vdeps=vdeps,
                                          first_of_pair=(i == 0 and hh == 0 and g0 == 0), last_of_tile=(hh == 1 and g0 + 4 >= nt)))
        for n_, u in enumerate(units):
            u["gi"] = n_ % 3

        def emit_qk(u):
            S = ps_s[u["gi"]]
            for t, (k_ap, v_ap, mk_) in enumerate(u["grp"]):
                m.op("pe", lambda e: e.matmul(S[:, t * 128:(t + 1) * 128], lhsT=k_ap, rhs=u["q_ap"], start=True, stop=True),
                     reads=u["kdeps"] + [qp[u["b"]]], writes=[S])

        def emit_softmax(u):
            S, E, P = ps_s[u["gi"]], Eb[u["gi"]], Pb[u["gi"]]
            n = len(u["grp"]) * 128
            m.op("act", lambda e: e.activation(out=E[:, 0:n], in_=S[:, 0:n], func=AF.Exp, scale=SCALE), reads=[S], writes=[E])
            mk0 = u["grp"][0][2]
            if mk0 is not None:
                mb, var, c0 = mk0
                m_ap = mb[:, c0:c0 + n] if var is None else mb[:, var, c0:c0 + n]
                m.op("dve", lambda e: e.tensor_tensor(out=P[:, 0:n], in0=E[:, 0:n], in1=m_ap, op=ALU.mult), reads=[E, mb], writes=[P])
                u["src"] = P
            else:
                u["src"] = E

        def emit_pv(u):
            O, hh, src = u["O"], u["hh"], u["src"]
            for t, (k_ap, v_ap, mk_) in enumerate(u["grp"]):
                first = (u["g0"] + t == 0)
                last = (u["g0"] + t == u["nt"] - 1)
                m.op("pe", lambda e: e.matmul(O[:, hh * 65:(hh + 1) * 65], lhsT=src[:, t * 128:(t + 1) * 128], rhs=v_ap, start=first, stop=last),
                     reads=u["vdeps"] + [src], writes=[O])
            if u["last_of_tile"]:
                i, p = u["i"], u["p"]
                rc = rec[i % 2]
                Ov = O[:, 0:130].rearrange("p (h d) -> p h d", d=65)
                m.op("dve", lambda e: e.reciprocal(out=rc[:, 0:2], in_=Ov[:, :, 64]), reads=[O], writes=[rc])
                for h2 in range(2):
                    m.op("dve", lambda e: e.tensor_scalar(out=o_all[:, i, p * 128 + h2 * 64:p * 128 + h2 * 64 + 64], in0=O[:, h2 * 65:h2 * 65 + 64],
                                                          scalar1=rc[:, h2:h2 + 1], scalar2=None, op0=ALU.mult), reads=[O, rc], writes=[o_all])

        def pre_pair(p):
            if p + 1 < 8:
                load_main(p + 1)
            if p == 2:
                for cc in range(0, 8, 2):
                    m.dma("pool", Wo[:, cc:cc + 2, :], w_out[:, cc:cc + 2, :], writes=[Wo])
            if kind_of(p) == "A":
                load_edge(p)

        load_main(0)
        pre_pair(0)
        emit_qk(units[0])
        for n_, u in enumerate(units):
            nu = units[n_ + 1] if n_ + 1 < len(units) else None
            if nu is not None and not nu["first_of_pair"]:
                emit_qk(nu)
            emit_softmax(u)
            emit_pv(u)
            if nu is not None and nu["first_of_pair"]:
                pre_pair(nu["p"])
                emit_qk(nu)

        obd = Buf("odbg_out")
        if DEBUG:
            m.dma("sp", o_dbg, o_all[:], reads=[o_all], writes=[obd], dsem=dout)
        GR = [(0, 384), (384, 768), (768, 1024)]
        ob = Buf("hmid_out")
        for i in range(16):
            b = i % 2
            msb = ms[b]
            m.op("dve", lambda e: e.tensor_tensor(out=sqf[:, :], in0=o_all[:, i, :], in1=o_all[:, i, :], op=ALU.mult), reads=[o_all], writes=[sqf])
            for g, (c0, c1) in enumerate(GR):
                m.op("dve", lambda e: e.tensor_reduce(out=msb[:, g:g + 1], in_=sqf[:, c0:c1], axis=AX.X, op=ALU.add), reads=[sqf], writes=[msb])
            for g, (c0, c1) in enumerate(GR):
                m.op("act", lambda e: e.activation(out=msb[:, g:g + 1], in_=msb[:, g:g + 1], func=AF.Ln, scale=1.0 / (c1 - c0), bias=epsb[:, 0:1]),
                     reads=[msb, epsb], writes=[msb])
            m.op("act", lambda e: e.activation(out=msb[:, 0:3], in_=msb[:, 0:3], func=AF.Exp, scale=-0.5), reads=[msb], writes=[msb])
            for g, (c0, c1) in enumerate(GR):
                m.op("dve", lambda e: e.scalar_tensor_tensor(out=mixed[b][:, c0:c1], in0=o_all[:, i, c0:c1], scalar=msb[:, g:g + 1], in1=go[:, c0:c1],
                                                             op0=ALU.mult, op1=ALU.mult), reads=[o_all, msb, go], writes=[mixed[b]])
            for c in range(8):
                m.op("pe", lambda e: e.transpose(ps_T[:, c * 128:(c + 1) * 128], mixed[b][:, c * 128:(c + 1) * 128], idn[:]), reads=[mixed[b], idn], writes=[ps_T])
            cb = (i // 4) % 2
            m.op("act", lambda e: e.activation(out=mixedT[cb][:, :, (i % 4) * 128:(i % 4 + 1) * 128], in_=ps_T[:, :].rearrange("p (c t) -> p c t", t=128), func=AF.Copy),
                 reads=[ps_T], writes=[mixedT[cb]])
            if i % 4 == 3:
                ch = i // 4
                m.dma("sp", hc[cb][:], hT[:, :, ch * 512:(ch + 1) * 512], writes=[hc[cb]], dsem=dh[cb])
                for d in range(8):
                    Y = ps_y[d % 2]
                    for c in range(8):
                        m.op("pe", lambda e: e.matmul(Y[:, :], lhsT=Wo[:, c, d * 128:(d + 1) * 128], rhs=mixedT[cb][:, c, :], start=(c == 0), stop=(c == 7)),
                             reads=[Wo, mixedT[cb]], writes=[Y])
                    m.op("dve", lambda e: e.tensor_tensor(out=hc[cb][:, d, :], in0=Y[:, :], in1=hc[cb][:, d, :], op=ALU.add), reads=[Y, hc[cb]], writes=[hc[cb]])
                m.dma("sp", hmid[:, :, ch * 512:(ch + 1) * 512], hc[cb][:], reads=[hc[cb]], writes=[ob], dsem=dout)
        if DEBUG:
            m.dma("sp", dbg2, mixed[1][:], reads=[mixed[1]], writes=[obd], dsem=dout)
            m.dma("sp", dbg3, ms[1][:], reads=[ms[1]], writes=[obd], dsem=dout)
            m.dma("sp", dbg4, mixedT[1][:], reads=[mixedT[1]], writes=[obd], dsem=dout)
        m.final_wait("sp", [ob, obd] if DEBUG else [ob])
        print("P_B instrs", m.n_ins, "dmas", m.ndma)
    return nc


EPS = 1e-6


class FFNRes:
    def __init__(self, m, F, NB):
        self.m, self.F, self.NB = m, F, NB
        self.wg = [m.sb("wg%d" % i, [128, 8, 128], BF16) for i in range(3)]
        self.wu = [m.sb("wu%d" % i, [128, 8, 128], BF16) for i in range(3)]
        self.wd = [m.sb("wd%d" % i, [128, F, 128], BF16) for i in range(2)]
        self.act = m.sb("act", [128, F, NB], BF16)
        self.sg = [m.sb("sg%d" % i, [128, 512], F32) for i in range(2)]
        self.ps_g = [m.ps("ps_g%d" % i, [128, 512]) for i in range(2)]
        self.ps_u = [m.ps("ps_u%d" % i, [128, 512]) for i in range(2)]
        self.ps_y = [m.ps("ps_y%d" % i, [128, 512]) for i in range(2)]
        self.wc = 0
        self.dc = 0
        self.gc = 0
        self.yc = 0


def ffn_block(R, uT, wg_ap, wu_ap, wd_ap, sink):
    m, F, NB = R.m, R.F, R.NB
    nch = NB // 512
    for f in range(F):
        i = R.wc % 3
        R.wc += 1
        wg, wu = R.wg[i], R.wu[i]
        m.dma("pool", wg[:].rearrange("p c n -> p (c n)"), wg_ap(f), writes=[wg])
        m.dma("pool", wu[:].rearrange("p c n -> p (c n)"), wu_ap(f), writes=[wu])
        for ch in range(nch):
            j = R.gc % 2
            R.gc += 1
            G, U, sg = R.ps_g[j], R.ps_u[j], R.sg[j]
            for c in range(8):
                m.op("pe", lambda e: e.matmul(G[:, :], lhsT=wg[:, c, :], rhs=uT[:, c, ch * 512:(ch + 1) * 512], start=(c == 0), stop=(c == 7)),
                     reads=[wg, uT], writes=[G])
            for c in range(8):
                m.op("pe", lambda e: e.matmul(U[:, :], lhsT=wu[:, c, :], rhs=uT[:, c, ch * 512:(ch + 1) * 512], start=(c == 0), stop=(c == 7)),
                     reads=[wu, uT], writes=[U])
            m.op("act", lambda e: e.activation(out=sg[:, :], in_=G[:, :], func=AF.Silu), reads=[G], writes=[sg])
            m.op("dve", lambda e: e.tensor_tensor(out=R.act[:, f, ch * 512:(ch + 1) * 512], in0=U[:, :], in1=sg[:, :], op=ALU.mult),
                 reads=[U, sg], writes=[R.act])
    for d in range(8):
        i = R.dc % 2
        R.dc += 1
        wd = R.wd[i]
        m.dma("pool", wd[:].rearrange("p f n -> p (f n)"), wd_ap(d), writes=[wd])
        for ch in range(nch):
            Y = R.ps_y[R.yc % 2]
            R.yc += 1
            for f in range(F):
                m.op("pe", lambda e: e.matmul(Y[:, :], lhsT=wd[:, f, :], rhs=R.act[:, f, ch * 512:(ch + 1) * 512], start=(f == 0), stop=(f == F - 1)),
                     reads=[wd, R.act], writes=[Y])
            sink(d, ch, Y)


def build_pc_dense(F=22, NT=2048, NB=1024):
    nc = bass.Bass("TRN2", target_bir_lowering=False)
    D = lambda n, s, dt, k: nc.dram_tensor(n, s, dt, kind=k).ap()
    hT = D("hT", [128, 8, NT], F32, "ExternalInput")
    gffn = D("gffn", [128, 8], F32, "ExternalInput")
    cmat = D("cmat", [128, 3, 128], F32, "ExternalInput")
    wg = D("wg", [F, 128, 1024], F32, "ExternalInput")
    wu = D("wu", [F, 128, 1024], F32, "ExternalInput")
    wd = D("wd", [8, 128, F * 128], F32, "ExternalInput")
    out = D("outT", [128, 8, NT], F32, "ExternalOutput")
    with ExitStack() as es:
        m = MK(nc, es)
        R = FFNRes(m, F, NB)
        cm = m.sb("cm", [128, 3, 128], BF16)
        gf = m.sb("gf", [128, 8], F32)
        epsb = m.sb("epsb", [128, 1], F32)
        hb = m.sb("hb", [128, 8, NB], F32)
        uT = m.sb("uT", [128, 8, NB], BF16)
        uc = [m.sb("uc%d" % i, [128, 8, 512], BF16) for i in range(1)]
        sq = m.sb("sq", [128, 8, 512], BF16)
        rs = m.sb("rs", [128, 512], F32)
        ps_ss = m.ps("ps_ss", [128, 512])
        m.dma("pool", cm[:], cmat, writes=[cm])
        m.dma("sp", gf[:], gffn, writes=[gf])
        m.op("dve", lambda e: e.memset(epsb[:], EPS), writes=[epsb])
        ob = Buf("out")
        for hb_i in range(NT // NB):
            t0 = hb_i * NB
            for ch in range(NB // 512):
                m.dma("sp", hb[:, :, ch * 512:(ch + 1) * 512], hT[:, :, t0 + ch * 512:t0 + (ch + 1) * 512], writes=[hb])
            for ch in range(NB // 512):
                rms_view(m, hb, ch * 512, 512, gf, cm, epsb, ps_ss, sq, rs, uT)

            def sink(d, ch, Y):
                m.op("dve", lambda e: e.tensor_tensor(out=hb[:, d, ch * 512:(ch + 1) * 512], in0=Y[:, :], in1=hb[:, d, ch * 512:(ch + 1) * 512], op=ALU.add),
                     reads=[Y, hb], writes=[hb])
            ffn_block(R, uT, lambda f: wg[f], lambda f: wu[f], lambda d: wd[d], sink)
            m.dma("sp", out[:, :, t0:t0 + NB], hb[:], reads=[hb], writes=[ob])
        m.final_wait("sp", [ob])
        print("P_C instrs", m.n_ins, "dmas", m.ndma)
    return nc


def rms_view(m, src, c0, n, gvec, cm, epsb, ps_ss, sq, rs, uT, u32=None):
    m.op("act", lambda e: e.activation(out=sq[:, :, 0:n], in_=src[:, :, c0:c0 + n], func=AF.Square), reads=[src], writes=[sq])
    for c in range(8):
        m.op("pe", lambda e: e.matmul(ps_ss[:, 0:n], lhsT=cm[:, 0, :], rhs=sq[:, c, 0:n], start=(c == 0), stop=(c == 7)), reads=[sq, cm], writes=[ps_ss])
    m.op("act", lambda e: e.activation(out=rs[:, 0:n], in_=ps_ss[:, 0:n], func=AF.Ln, scale=1.0 / 1024, bias=epsb[:, 0:1]), reads=[ps_ss, epsb], writes=[rs])
    m.op("act", lambda e: e.activation(out=rs[:, 0:n], in_=rs[:, 0:n], func=AF.Exp, scale=-0.5), reads=[rs], writes=[rs])
    for c in range(8):
        m.op("dve", lambda e: e.scalar_tensor_tensor(out=uT[:, c, c0:c0 + n], in0=src[:, c, c0:c0 + n], scalar=gvec[:, c:c + 1], in1=rs[:, 0:n],
                                                     op0=ALU.mult, op1=ALU.mult), reads=[src, gvec, rs], writes=[uT])
        if u32 is not None:
            m.op("dve", lambda e: e.scalar_tensor_tensor(out=u32[:, c, 0:n], in0=src[:, c, c0:c0 + n], scalar=gvec[:, c:c + 1], in1=rs[:, 0:n],
                                                         op0=ALU.mult, op1=ALU.mult), reads=[src, gvec, rs], writes=[u32])


def build_l5(NT=2048):
    nc = bass.Bass("TRN2", target_bir_lowering=False)
    D = lambda n, s, dt, k: nc.dram_tensor(n, s, dt, kind=k).ap()
    hT = D("hT", [128, 8, NT], F32, "ExternalInput")
    gffn = D("gffn", [128, 8], F32, "ExternalInput")
    cmat = D("cmat", [128, 3, 128], F32, "ExternalInput")
    wr = D("wr", [128, 8, 8], F32, "ExternalInput")
    uT_o = D("uT", [128, 8, NT], BF16, "ExternalOutput")
    g_o = D("gates", [128, NT // 128, 8], F32, "ExternalOutput")
    with ExitStack() as es:
        m = MK(nc, es)
        cm = m.sb("cm", [128, 3, 128], BF16)
        gf = m.sb("gf", [128, 8], F32)
        wrs = m.sb("wrs", [128, 8, 8], F32)
        epsb = m.sb("epsb", [128, 1], F32)
        hb = [m.sb("hb%d" % i, [128, 8, 512], F32) for i in range(2)]
        uT = [m.sb("uT%d" % i, [128, 8, 512], BF16) for i in range(2)]
        u32 = m.sb("u32", [128, 8, 512], F32)
        sq = m.sb("sq", [128, 8, 512], BF16)
        rs = m.sb("rs", [128, 512], F32)
        gates = m.sb("gates_sb", [128, NT // 128, 8], F32)
        lg = [m.sb("lg%d" % i, [128, 8], F32) for i in range(2)]
        m8 = [m.sb("m8%d" % i, [128, 8], F32) for i in range(2)]
        tt = [m.sb("tt%d" % i, [128, 4], F32) for i in range(2)]
        ga = [m.sb("ga%d" % i, [128, 8], F32) for i in range(2)]
        ps_ss = m.ps("ps_ss", [128, 512])
        ps_l = [m.ps("ps_l%d" % i, [128, 8]) for i in range(2)]
        m.dma("pool", cm[:], cmat, writes=[cm])
        m.dma("sp", gf[:], gffn, writes=[gf])
        m.dma("sp", wrs[:], wr, writes=[wrs])
        m.op("dve", lambda e: e.memset(epsb[:], EPS), writes=[epsb])
        ob = Buf("out")
        k = 0
        for t in range(NT // 512):
            b = t % 2
            m.dma("sp", hb[b][:], hT[:, :, t * 512:(t + 1) * 512], writes=[hb[b]])
            rms_view(m, hb[b], 0, 512, gf, cm, epsb, ps_ss, sq, rs, uT[b], u32=u32)
            m.dma("sp", uT_o[:, :, t * 512:(t + 1) * 512], uT[b][:], reads=[uT[b]], writes=[ob])
            for s in range(4):
                j = k % 2
                k += 1
                L, lgb, m8b, tb, gab = ps_l[j], lg[j], m8[j], tt[j], ga[j]
                for c in range(8):
                    m.op("pe", lambda e: e.matmul(L[:, :], lhsT=u32[:, c, s * 128:(s + 1) * 128], rhs=wrs[:, c, :], start=(c == 0), stop=(c == 7)),
                         reads=[u32, wrs], writes=[L])
                m.op("dve", lambda e: e.tensor_copy(out=lgb[:, :], in_=L[:, :]), reads=[L], writes=[lgb])
                m.op("dve", lambda e: e.max(out=m8b[:, :], in_=lgb[:, :]), reads=[lgb], writes=[m8b])
                m.op("dve", lambda e: e.tensor_tensor(out=tb[:, 0:1], in0=m8b[:, 1:2], in1=m8b[:, 0:1], op=ALU.subtract), reads=[m8b], writes=[tb])
                m.op("act", lambda e: e.activation(out=tb[:, 1:2], in_=tb[:, 0:1], func=AF.Exp), reads=[tb], writes=[tb])
                m.op("dve", lambda e: e.tensor_scalar(out=tb[:, 2:3], in0=tb[:, 1:2], scalar1=1.0, scalar2=None, op0=ALU.add), reads=[tb], writes=[tb])
                m.op("dve", lambda e: e.reciprocal(out=tb[:, 2:3], in_=tb[:, 2:3]), reads=[tb], writes=[tb])
                m.op("dve", lambda e: e.tensor_tensor(out=tb[:, 3:4], in0=tb[:, 1:2], in1=tb[:, 2:3], op=ALU.mult), reads=[tb], writes=[tb])
                m.op("dve", lambda e: e.tensor_scalar(out=gab[:, :], in0=lgb[:, :], scalar1=m8b[:, 0:1], scalar2=tb[:, 2:3], op0=ALU.is_equal, op1=ALU.mult),
                     reads=[lgb, m8b, tb], writes=[gab])
                m.op("dve", lambda e: e.tensor_scalar(out=lgb[:, :], in0=lgb[:, :], scalar1=m8b[:, 1:2], scalar2=tb[:, 3:4], op0=ALU.is_equal, op1=ALU.mult),
                     reads=[lgb, m8b, tb], writes=[lgb])
                m.op("dve", lambda e: e.tensor_tensor(out=gates[:, t * 4 + s, :], in0=gab[:, :], in1=lgb[:, :], op=ALU.add), reads=[gab, lgb], writes=[gates])
        ob2 = Buf("out2")
        m.dma("sp", g_o, gates[:], reads=[gates], writes=[ob2])
        m.final_wait("sp", [ob, ob2])
        print("L5 instrs", m.n_ins, "dmas", m.ndma)
    return nc


def build_l6(F=28, NTOK=16384, NB=1024):
    nc = bass.Bass("TRN2", target_bir_lowering=False)
    D = lambda n, s, dt, k: nc.dram_tensor(n, s, dt, kind=k).ap()
    uT_i = D("uT", [128, 8, NTOK], BF16, "ExternalInput")
    gbc_i = D("gbc", [128, NTOK], F32, "ExternalInput")
    wg = D("wg", [F, 128, 1024], F32, "ExternalInput")
    wu = D("wu", [F, 128, 1024], F32, "ExternalInput")
    wd = D("wd", [8, 128, F * 128], F32, "ExternalInput")
    y_o = D("yT", [128, 8, NTOK], F32, "ExternalOutput")
    with ExitStack() as es:
        m = MK(nc, es)
        R = FFNRes(m, F, NB)
        uT = [m.sb("uT%d" % i, [128, 8, NB], BF16) for i in range(2)]
        gb = [m.sb("gb%d" % i, [128, NB], F32) for i in range(2)]
        yb = m.sb("yb", [128, 8, NB], F32)
        ob = Buf("out")
        for tb in range(NTOK // NB):
            b = tb % 2
            t0 = tb * NB
            m.dma("sp", uT[b][:], uT_i[:, :, t0:t0 + NB], writes=[uT[b]])
            m.dma("sp", gb[b][:], gbc_i[:, t0:t0 + NB], writes=[gb[b]])

            def sink(d, ch, Y):
                m.op("dve", lambda e: e.tensor_tensor(out=yb[:, d, ch * 512:(ch + 1) * 512], in0=Y[:, :], in1=gb[b][:, ch * 512:(ch + 1) * 512], op=ALU.mult),
                     reads=[Y, gb[b]], writes=[yb])
            ffn_block(R, uT[b], lambda f: wg[f], lambda f: wu[f], lambda d: wd[d], sink)
            m.dma("sp", y_o[:, :, t0:t0 + NB], yb[:], reads=[yb], writes=[ob])
        m.final_wait("sp", [ob])
        print("L6 instrs", m.n_ins, "dmas", m.ndma)
    return nc


def build_l7(NT=2048):
    nc = bass.Bass("TRN2", target_bir_lowering=False)
    D = lambda n, s, dt, k: nc.dram_tensor(n, s, dt, kind=k).ap()
    hT = D("hT", [128, 8, NT], F32, "ExternalInput")
    ys = D("ys", [8, 128, 8, NT], F32, "ExternalInput")
    out = D("outT", [128, 8, NT], F32, "ExternalOutput")
    with ExitStack() as es:
        m = MK(nc, es)
        acc = [m.sb("acc%d" % i, [128, 8, 512], F32) for i in range(2)]
        yb = [m.sb("yb%d" % i, [128, 8, 512], F32) for i in range(3)]
        ob = Buf("out")
        k = 0
        for t in range(NT // 512):
            a = acc[t % 2]
            m.dma("sp", a[:], hT[:, :, t * 512:(t + 1) * 512], writes=[a])
            for e_ in range(8):
                y = yb[k % 3]
                k += 1
                m.dma("sp", y[:], ys[e_, :, :, t * 512:(t + 1) * 512], writes=[y])
                m.op("dve", lambda e: e.tensor_tensor(out=a[:], in0=a[:], in1=y[:], op=ALU.add), reads=[a, y], writes=[a])
            m.dma("sp", out[:, :, t * 512:(t + 1) * 512], a[:], reads=[a], writes=[ob])
        m.final_wait("sp", [ob])
        print("L7 instrs", m.n_ins, "dmas", m.ndma)
    return nc


U32 = mybir.dt.uint32
I32 = mybir.dt.int32
BIGPOS = 1.0e6
CB = 1024


def build_l5b():
    nc = bass.Bass("TRN2", target_bir_lowering=False)
    D = lambda n, s, dt, k: nc.dram_tensor(n, s, dt, kind=k).ap()
    gates = D("gates_all", [128, 128 * 8], F32, "ExternalInput")
    cst = D("cst", [128, 2, 128], F32, "ExternalInput")
    thr = D("thr", [128, 16], F32, "ExternalInput")
    nb_o = D("nblk", [1, 1], I32, "ExternalOutput")
    with ExitStack() as es:
        m = MK(nc, es)
        g = m.sb("g", [128, 1024], F32)
        mk_ = m.sb("mk", [128, 1024], BF16)
        cm = m.sb("cm", [128, 2, 128], BF16)
        th = m.sb("th", [128, 16], F32)
        tot = m.sb("tot", [128, 1024], F32)
        cnt = m.sb("cnt", [128, 8], F32)
        nbf = m.sb("nbf", [128, 16], F32)
        nbe = m.sb("nbe", [128, 8], F32)
        nbm = m.sb("nbm", [128, 1], F32)
        nbi = m.sb("nbi", [128, 1], I32)
        ps = [m.ps("ps%d" % i, [128, 512]) for i in range(2)]
        m.dma("sp", g[:], gates, writes=[g])
        m.dma("pool", cm[:], cst, writes=[cm])
        m.dma("sp", th[:], thr, writes=[th])
        m.op("dve", lambda e: e.tensor_scalar(out=mk_[:], in0=g[:], scalar1=0.0, scalar2=None, op0=ALU.is_gt), reads=[g], writes=[mk_])
        for h in range(2):
            m.op("pe", lambda e: e.matmul(ps[h][:, :], lhsT=cm[:, 1, :], rhs=mk_[:, h * 512:(h + 1) * 512], start=True, stop=True), reads=[cm, mk_], writes=[ps[h]])
            m.op("dve", lambda e: e.tensor_copy(out=tot[:, h * 512:(h + 1) * 512], in_=ps[h][:, :]), reads=[ps[h]], writes=[tot])
        m.op("dve", lambda e: e.tensor_reduce(out=cnt[:, :], in_=tot[:, :].rearrange("p (t e) -> p e t", e=8), axis=AX.X, op=ALU.add), reads=[tot], writes=[cnt])
        for e_ in range(8):
            m.op("dve", lambda e: e.tensor_scalar(out=nbf[:], in0=th[:], scalar1=cnt[:, e_:e_ + 1], scalar2=None, op0=ALU.is_lt), reads=[th, cnt], writes=[nbf])
            m.op("dve", lambda e: e.tensor_reduce(out=nbe[:, e_:e_ + 1], in_=nbf[:], axis=AX.X, op=ALU.add), reads=[nbf], writes=[nbe])
        m.op("dve", lambda e: e.tensor_reduce(out=nbm[:, :], in_=nbe[:, :], axis=AX.X, op=ALU.max), reads=[nbe], writes=[nbm])
        m.op("dve", lambda e: e.tensor_copy(out=nbi[:], in_=nbm[:]), reads=[nbm], writes=[nbi])
        ob = Buf("o")
        m.dma("sp", nb_o, nbi[0:1, 0:1], reads=[nbi], writes=[ob])
        m.final_wait("sp", [ob])
    return nc


def build_l6s(NBLK, F=28, NTOK=16384):
    nc = bass.Bass("TRN2", target_bir_lowering=False)
    D = lambda n, s, dt, k: nc.dram_tensor(n, s, dt, kind=k).ap()
    NT = NTOK // 128
    CAP = NBLK * CB
    U = D("U", [NTOK, 1024], BF16, "ExternalInput")
    gate = D("gate", [128, NT], F32, "ExternalInput")
    cst = D("cst", [128, 3, 128], F32, "ExternalInput")
    wg = D("wg", [F, 128, 1024], F32, "ExternalInput")
    wu = D("wu", [F, 128, 1024], F32, "ExternalInput")
    wd = D("wd", [F, 128, 1024], F32, "ExternalInput")
    y = D("y", [NTOK, 1024], F32, "ExternalOutput")
    Uc = D("Uc_i", [CAP, 1024], BF16, "Internal")
    Yc = D("Yc_i", [CAP, 1024], F32, "Internal")
    with ExitStack() as es:
        m = MK(nc, es)
        g = m.sb("g", [128, NT], F32)
        cm = m.sb("cm", [128, 3, 128], BF16)
        mk_ = m.sb("mk", [128, NT], F32)
        mkb = m.sb("mkb", [128, NT], BF16)
        onesr = m.sb("onesr", [128, NT], F32)
        tot = m.sb("tot", [128, NT], F32)
        cum = m.sb("cum", [128, NT], F32)
        pos = m.sb("pos", [128, NT], F32)
        posu = m.sb("posu", [128, NT], U32)
        Wd = m.sb("Wd", [128, F, 1024], BF16)
        ut = [m.sb("ut%d" % i, [128, 1024], BF16) for i in range(3)]
        uT = m.sb("uT", [128, 8, CB], BF16)
        act = m.sb("act", [128, F, CB], BF16)
        wgb = [m.sb("wg%d" % i, [128, 8, 128], BF16) for i in range(3)]
        wub = [m.sb("wu%d" % i, [128, 8, 128], BF16) for i in range(3)]
        sg = [m.sb("sg%d" % i, [128, 512], F32) for i in range(2)]
        yb = [m.sb("yb%d" % i, [128, 1024], F32) for i in range(2)]
        gt = [m.sb("gt%d" % i, [128, 1024], F32) for i in range(3)]
        ps_a = m.ps("ps_a", [128, 512])
        ps_T = m.ps("ps_T", [128, 1024], BF16)
        ps_g = [m.ps("ps_g%d" % i, [128, 512]) for i in range(2)]
        ps_u = [m.ps("ps_u%d" % i, [128, 512]) for i in range(2)]
        ps_y = [m.ps("ps_y%d" % i, [128, 512]) for i in range(2)]

        m.dma("sp", g[:], gate, writes=[g])
        m.dma("pool", cm[:], cst, writes=[cm])
        m.op("dve", lambda e: e.tensor_scalar(out=mk_[:], in0=g[:], scalar1=0.0, scalar2=None, op0=ALU.is_gt), reads=[g], writes=[mk_])
        m.op("dve", lambda e: e.tensor_copy(out=mkb[:], in_=mk_[:]), reads=[mk_], writes=[mkb])
        m.op("dve", lambda e: e.memset(onesr[:], 1.0), writes=[onesr])
        m.op("pe", lambda e: e.matmul(ps_a[:, 0:NT], lhsT=cm[:, 1, :], rhs=mkb[:, :], start=True, stop=True), reads=[cm, mkb], writes=[ps_a])
        m.op("dve", lambda e: e.tensor_copy(out=tot[:], in_=ps_a[:, 0:NT]), reads=[ps_a], writes=[tot])
        m.op("pe", lambda e: e.matmul(ps_a[:, 0:NT], lhsT=cm[:, 0, :], rhs=mkb[:, :], start=True, stop=True), reads=[cm, mkb], writes=[ps_a])
        m.op("dve", lambda e: e.tensor_tensor_scan(out=cum[:], data0=onesr[:], data1=tot[:], initial=0.0, op0=ALU.mult, op1=ALU.add), reads=[onesr, tot], writes=[cum])
        m.op("dve", lambda e: e.tensor_tensor(out=pos[:], in0=cum[:], in1=tot[:], op=ALU.subtract), reads=[cum, tot], writes=[pos])
        m.op("dve", lambda e: e.tensor_tensor(out=pos[:], in0=pos[:], in1=ps_a[:, 0:NT], op=ALU.add), reads=[pos, ps_a], writes=[pos])
        m.op("dve", lambda e: e.tensor_scalar(out=pos[:], in0=pos[:], scalar1=-1.0 - BIGPOS, scalar2=None, op0=ALU.add), reads=[pos], writes=[pos])
        m.op("dve", lambda e: e.tensor_tensor(out=pos[:], in0=pos[:], in1=mk_[:], op=ALU.mult), reads=[pos, mk_], writes=[pos])
        m.op("dve", lambda e: e.tensor_scalar(out=pos[:], in0=pos[:], scalar1=BIGPOS, scalar2=None, op0=ALU.add), reads=[pos], writes=[pos])
        m.op("dve", lambda e: e.tensor_copy(out=posu[:], in_=pos[:]), reads=[pos], writes=[posu])
        for f0 in range(0, F, 4):
            m.dma("pool", Wd[:, f0:f0 + 4, :], wd[f0:f0 + 4].rearrange("f p n -> p f n"), writes=[Wd])
        bUc = Buf("Uc")
        for t in range(NT):
            u = ut[t % 3]
            m.dma("sp", u[:], U[t * 128:(t + 1) * 128, :], writes=[u])
            m.idma(Uc, u[:], posu[:, t:t + 1], True, CAP - 1, reads=[u, posu], writes=[bUc])
        bYc = Buf("Yc")
        wc = 0
        gc = 0
        yc = 0
        for blk in range(NBLK):
            for tt in range(CB // 128):
                u = ut[tt % 3]
                r0 = blk * CB + tt * 128
                m.dma("sp", u[:], Uc[r0:r0 + 128, :], reads=[bUc], writes=[u])
                for c in range(8):
                    m.op("pe", lambda e: e.transpose(ps_T[:, c * 128:(c + 1) * 128], u[:, c * 128:(c + 1) * 128], cm[:, 2, :]), reads=[u, cm], writes=[ps_T])
                m.op("act", lambda e: e.activation(out=uT[:, :, tt * 128:(tt + 1) * 128], in_=ps_T[:, :].rearrange("p (c t) -> p c t", t=128), func=AF.Copy),
                     reads=[ps_T], writes=[uT])
            for f in range(F):
                i = wc % 3
                wc += 1
                wgt, wut = wgb[i], wub[i]
                m.dma("pool", wgt[:].rearrange("p c n -> p (c n)"), wg[f], writes=[wgt])
                m.dma("pool", wut[:].rearrange("p c n -> p (c n)"), wu[f], writes=[wut])
                for ch in range(CB // 512):
                    j = gc % 2
                    gc += 1
                    G, Uu, sgb = ps_g[j], ps_u[j], sg[j]
                    for c in range(8):
                        m.op("pe", lambda e: e.matmul(G[:, :], lhsT=wgt[:, c, :], rhs=uT[:, c, ch * 512:(ch + 1) * 512], start=(c == 0), stop=(c == 7)), reads=[wgt, uT], writes=[G])
                    for c in range(8):
                        m.op("pe", lambda e: e.matmul(Uu[:, :], lhsT=wut[:, c, :], rhs=uT[:, c, ch * 512:(ch + 1) * 512], start=(c == 0), stop=(c == 7)), reads=[wut, uT], writes=[Uu])
                    m.op("act", lambda e: e.activation(out=sgb[:, :], in_=G[:, :], func=AF.Silu), reads=[G], writes=[sgb])
                    m.op("dve", lambda e: e.tensor_tensor(out=act[:, f, ch * 512:(ch + 1) * 512], in0=Uu[:, :], in1=sgb[:, :], op=ALU.mult), reads=[Uu, sgb], writes=[act])
            for tt in range(CB // 128):
                ybb = yb[tt % 2]
                for hf in range(2):
                    Y = ps_y[yc % 2]
                    yc += 1
                    for f in range(F):
                        m.op("pe", lambda e: e.matmul(Y[:, :], lhsT=act[:, f, tt * 128:(tt + 1) * 128], rhs=Wd[:, f, hf * 512:(hf + 1) * 512], start=(f == 0), stop=(f == F - 1)),
                             reads=[act, Wd], writes=[Y])
                    m.op("act", lambda e: e.activation(out=ybb[:, hf * 512:(hf + 1) * 512], in_=Y[:, :], func=AF.Copy), reads=[Y], writes=[ybb])
                r0 = blk * CB + tt * 128
                m.dma("sp", Yc[r0:r0 + 128, :], ybb[:], reads=[ybb], writes=[bYc])
        by = Buf("y")
        for t in range(NT):
            gb = gt[t % 3]
            m.op("dve", lambda e: e.memset(gb[:], 0.0), writes=[gb])
            m.idma(gb[:], Yc, posu[:, t:t + 1], False, CAP - 1, reads=[bYc, posu], writes=[gb])
            m.op("dve", lambda e: e.tensor_scalar(out=gb[:], in0=gb[:], scalar1=g[:, t:t + 1], scalar2=None, op0=ALU.mult), reads=[gb, g], writes=[gb])
            m.dma("sp", y[t * 128:(t + 1) * 128, :], gb[:], reads=[gb], writes=[by])
        m.final_wait("sp", [by])
        print("L6s instrs", m.n_ins, "dmas", m.ndma, "NBLK", NBLK)
    return nc


def build_l7t(NT=2048):
    nc = bass.Bass("TRN2", target_bir_lowering=False)
    D = lambda n, s, dt, k: nc.dram_tensor(n, s, dt, kind=k).ap()
    h = D("h", [NT, 1024], F32, "ExternalInput")
    ys = D("ys", [8, NT, 1024], F32, "ExternalInput")
    out = D("out", [NT, 1024], F32, "ExternalOutput")
    with ExitStack() as es:
        m = MK(nc, es)
        acc = [m.sb("acc%d" % i, [128, 4, 1024], F32) for i in range(2)]
        yb = [m.sb("yb%d" % i, [128, 4, 1024], F32) for i in range(3)]
        ob = Buf("out")
        k = 0
        for t in range(NT // 512):
            a = acc[t % 2]
            m.dma("sp", a[:], h[t * 512:(t + 1) * 512, :].rearrange("(j p) n -> p j n", p=128), writes=[a])
            for e_ in range(8):
                yy = yb[k % 3]
                k += 1
                m.dma("sp", yy[:], ys[e_, t * 512:(t + 1) * 512, :].rearrange("(j p) n -> p j n", p=128), writes=[yy])
                m.op("dve", lambda e: e.tensor_tensor(out=a[:], in0=a[:], in1=yy[:], op=ALU.add), reads=[a, yy], writes=[a])
            m.dma("sp", out[t * 512:(t + 1) * 512, :].rearrange("(j p) n -> p j n", p=128), a[:], reads=[a], writes=[ob])
        m.final_wait("sp", [ob])
    return nc

H_NA=6; HD=64
def fm(a):
    T = a.shape[0]
    return np.ascontiguousarray(a.reshape(T, 8, 128).transpose(2, 1, 0))
def wt(w):
    K, N = w.shape
    return np.ascontiguousarray(w.reshape(K // 128, 128, N).transpose(1, 0, 2))
def gvec(g):
    return np.ascontiguousarray(g.reshape(8, 128).T)
def cmat():
    ones = np.ones((128, 128), np.float32)
    blk = np.zeros((128, 128), np.float32); blk[:64, :64] = 1; blk[64:, 64:] = 1
    perm = np.zeros((128, 128), np.float32)
    for mm in range(128):
        k = (mm // 64) * 64 + ((mm % 64) + 32) % 64
        perm[k, mm] = 1
    return np.ascontiguousarray(np.stack([ones, blk, perm], axis=1))
def cs_table(pos):
    half = 32
    inv = np.power(np.float32(10000.0), -(2.0 / 64) * np.arange(half, dtype=np.float32)).astype(np.float32)
    ang = pos.astype(np.float32)[:, None] * inv[None, :]
    cos = np.cos(ang).astype(np.float32); sin = np.sin(ang).astype(np.float32)
    d = np.arange(128) % 64
    c = cos[:, d % 32].T
    s = np.where((d < 32)[:, None], -sin[:, d % 32].T, sin[:, d % 32].T)
    return np.ascontiguousarray(np.stack([c, s], axis=1).astype(np.float32))
def gqk_table(inp, l):
    t = np.zeros((128, 8), np.float32)
    d = np.arange(128) % 64
    t[:, 0] = inp["g_qk_na"][l, 0][d]; t[:, 1] = inp["g_qk_na"][l, 1][d]
    t[:, 2] = inp["g_qk_dil"][l, 0][d]; t[:, 3] = inp["g_qk_dil"][l, 1][d]
    t[:, 4] = inp["g_qk_mem"][l, 0][d]; t[:, 5] = inp["g_qk_mem"][l, 1][d]
    return t

BF = ml_dtypes.bfloat16
def cmask_table():
    k = np.arange(128)[:, None]; q = np.arange(128)[None, :]
    out = np.zeros((128, 17, 128), np.float32)
    for j in range(17):
        off = 128 * (j - 8) + k - q
        c = np.zeros((128, 128), np.float32)
        for w, d in ((128, 1), (512, 4), (2048, 16)):
            c += ((off % d == 0) & (np.abs(off) <= w // 2)).astype(np.float32)
        out[:, j, :] = c
    return np.ascontiguousarray(out.reshape(128, 17 * 128))
def na_bias(rpb_l, p, gb):
    ws = min(max(gb - 2, 0), 59)
    k = np.arange(128); q = np.arange(128)
    out = np.full((128, 2, 5, 128), -100.0, np.float32)
    qrow = 2 * gb + q // 64; qcol = q % 64
    rstart = np.clip(qrow - 4, 0, 120); cstart = np.clip(qcol - 8, 0, 48)
    for j in range(5):
        krow = 2 * (ws + j) + k // 64; kcol = k % 64
        inr = (krow[:, None] >= rstart[None, :]) & (krow[:, None] < rstart[None, :] + 8)
        inc = (kcol[:, None] >= cstart[None, :]) & (kcol[:, None] < cstart[None, :] + 16)
        ok = inr & inc
        ri = np.clip(krow[:, None] - qrow[None, :] + 7, 0, 14); ci = np.clip(kcol[:, None] - qcol[None, :] + 15, 0, 30)
        for hh in range(2):
            vals = rpb_l[2 * p + hh][ri, ci]
            out[:, hh, j, :] = np.where(ok, vals, np.float32(-100.0))
    return out.reshape(128, 1280)
def prep_pb(core, l, inp, pa_res, h_full_T):
    b = core // 4; ci = core % 4; s0 = ci * 2048; tile0 = ci * 16
    K = np.concatenate([np.asarray(pa_res[b * 4 + c]["kT"]) for c in range(4)], axis=2)
    V = np.concatenate([np.asarray(pa_res[b * 4 + c]["v"]) for c in range(4)], axis=1)
    Kp = np.zeros((128, 6, 8192 + 2048), K.dtype); Kp[:, :, 1024:1024 + 8192] = K
    Vp = np.zeros((128, 64 + 16, 780), V.dtype); Vp[:, 8:72] = V
    r = pa_res[core]
    d = {"qT": np.asarray(r["qT"])}
    d["kA"] = np.ascontiguousarray(Kp[:, 0:3, 1024 + s0 - 256:1024 + s0 + 2304])
    d["kB"] = np.ascontiguousarray(Kp[:, 3:6, s0:s0 + 4096])
    vA = Vp[:, 8 + tile0 - 2:8 + tile0 + 18, 0:390].reshape(128, 20, 3, 130).transpose(0, 2, 1, 3)
    vB = Vp[:, tile0:tile0 + 32, 390:780].reshape(128, 32, 3, 130).transpose(0, 2, 1, 3)
    d["vA"] = np.ascontiguousarray(vA); d["vB"] = np.ascontiguousarray(vB)
    kAe = np.zeros((128, 3, 4, 640), K.dtype); vAe = np.zeros((128, 3, 4, 650), V.dtype)
    for slot, i in enumerate((0, 1, 14, 15)):
        gb = tile0 + i; ws = min(max(gb - 2, 0), 59)
        kAe[:, :, slot, :] = K[:, 0:3, ws * 128:ws * 128 + 640]
        vv = V[:, ws:ws + 5, 0:390].reshape(128, 5, 3, 130).transpose(0, 2, 1, 3)
        vAe[:, :, slot, :] = vv.reshape(128, 3, 650)
    d["kAe"] = kAe; d["vAe"] = vAe
    d["kM"] = np.asarray(r["kmT"])
    d["vM"] = np.ascontiguousarray(np.asarray(r["vm"]).reshape(128, 2, 2, 130).transpose(0, 2, 1, 3))
    nab = np.zeros((128, 3, 5, 1280), np.float32)
    for p in range(3):
        for v, i in enumerate((0, 1, 2, 14, 15)):
            nab[:, p, v, :] = na_bias(inp["rpb_na"][l], p, tile0 + i)
    d["nab"] = nab
    d["cmask"] = cmask_table()
    d["ident"] = np.eye(128, dtype=np.float32)
    d["gout"] = np.ascontiguousarray(np.broadcast_to(inp["g_out"][l][None, :], (128, 1024))).astype(np.float32)
    d["w_out"] = wt(inp["w_out"][l])
    d["hT"] = h_full_T[core]
    return d
def prep_pa(core, l, inp, hT_core):
    b = core // 4; s0 = (core % 4) * 2048
    return {"hT": hT_core, "w_in": wt(inp["w_in"][l]), "gattn": gvec(inp["g_attn"][l]), "gqk": gqk_table(inp, l), "cmat": cmat(),
            "cs": cs_table(np.arange(s0, s0 + 2048)), "memT": fm(inp["mem"][b]), "gmem": gvec(inp["g_mem"][l]), "w_kv": wt(inp["w_mem_kv"][l])}

def wgu_t(w, F):
    return np.ascontiguousarray(w.reshape(8, 128, F, 128).transpose(2, 1, 0, 3).reshape(F, 128, 1024))
def wd_t(w, F):
    return np.ascontiguousarray(w.reshape(F, 128, 8, 128).transpose(2, 1, 0, 3).reshape(8, 128, F * 128))
def unfm(aT):
    return np.ascontiguousarray(aT.transpose(2, 1, 0).reshape(aT.shape[2], 1024))

def cst3():
    tri = (np.arange(128)[:, None] <= np.arange(128)[None, :]).astype(np.float32)
    return np.ascontiguousarray(np.stack([tri, np.ones((128, 128), np.float32), np.eye(128, dtype=np.float32)], 1))

from concourse.bass_utils import run_bass_kernel_spmd
_CORES = list(range(8))
_PROGS = {}


def _prog(name, fn):
    if name not in _PROGS:
        _PROGS[name] = fn()
    return _PROGS[name]


def _run(name, fn, ims):
    nc = _prog(name, fn)
    return run_bass_kernel_spmd(nc, ims, core_ids=_CORES).results


def _attn_layer(inp, l, hTs):
    ra = _run("pa", build_pa, [prep_pa(c, l, inp, hTs[c]) for c in range(8)])
    rb = _run("pb", build_pb, [prep_pb(c, l, inp, ra, hTs) for c in range(8)])
    return [np.asarray(rb[c]["hmidT"]) for c in range(8)]


def kernel(**inp):
    inp = {k: np.asarray(v) for k, v in inp.items()}
    x = inp["x"]
    hTs = [fm(x[c // 4, (c % 4) * 2048:(c % 4 + 1) * 2048]) for c in range(8)]
    hm = _attn_layer(inp, 0, hTs)
    wg = wgu_t(inp["w_gate_dense"][0], 22); wu = wgu_t(inp["w_up_dense"][0], 22); wd = wd_t(inp["w_down_dense"][0], 22)
    cm = cmat()
    rc = _run("pc", build_pc_dense, [{"hT": hm[c], "gffn": gvec(inp["g_ffn"][0]), "cmat": cm, "wg": wg, "wu": wu, "wd": wd} for c in range(8)])
    h1 = [np.asarray(rc[c]["outT"]) for c in range(8)]
    hm1 = _attn_layer(inp, 1, h1)
    wr = wt(inp["w_router"][0])
    r5 = _run("l5", build_l5, [{"hT": hm1[c], "gffn": gvec(inp["g_ffn"][1]), "cmat": cm, "wr": wr} for c in range(8)])
    U_all = np.ascontiguousarray(np.concatenate([np.asarray(r5[c]["uT"]).transpose(2, 1, 0).reshape(2048, 1024) for c in range(8)], axis=0))
    gates = np.concatenate([np.asarray(r5[c]["gates"]).transpose(1, 0, 2).reshape(2048, 8) for c in range(8)], axis=0)
    g_lay = np.ascontiguousarray(gates.reshape(128, 128, 8).transpose(1, 0, 2).reshape(128, 1024))
    thr = np.ascontiguousarray(np.broadcast_to((1024.0 * np.arange(16, dtype=np.float32))[None, :], (128, 16)))
    c3 = cst3()
    nb = run_bass_kernel_spmd(_prog("l5b", build_l5b), [{"gates_all": g_lay, "cst": np.ascontiguousarray(c3[:, 0:2, :]), "thr": thr}], core_ids=[0]).results[0]["nblk"]
    NBLK = max(1, int(np.asarray(nb).reshape(-1)[0]))
    ims = []
    for e in range(8):
        ims.append({"U": U_all, "gate": np.ascontiguousarray(gates[:, e].reshape(128, 128).T), "cst": c3,
                    "wg": wgu_t(inp["w_gate_moe"][0, e], 28), "wu": wgu_t(inp["w_up_moe"][0, e], 28),
                    "wd": np.ascontiguousarray(inp["w_down_moe"][0, e].reshape(28, 128, 1024))})
    r6 = _run("l6s_%d" % NBLK, lambda: build_l6s(NBLK), ims)
    ims = []
    for c in range(8):
        ys = np.ascontiguousarray(np.stack([np.asarray(r6[e]["y"])[c * 2048:(c + 1) * 2048] for e in range(8)]))
        ims.append({"h": unfm(hm1[c]), "ys": ys})
    r7 = _run("l7t", build_l7t, ims)
    out = np.stack([np.asarray(r7[c]["out"]) for c in range(8)]).reshape(2, 8192, 1024)
    return out.astype(np.float32)
```

```python
import numpy as np
import ml_dtypes
import numpy as np
from contextlib import ExitStack
import concourse.bass as bass
import concourse.mybir as mybir

F32 = mybir.dt.float32
BF16 = mybir.dt.bfloat16
AF = mybir.ActivationFunctionType
ALU = mybir.AluOpType
AX = mybir.AxisListType


class Buf:
    __slots__ = ("name", "w", "r", "t", "dsem")

    def __init__(self, name, t=None):
        self.name = name
        self.w = None
        self.r = []
        self.t = t
        self.dsem = None

    def __getitem__(self, idx):
        return self.t[idx]


class MK:
    SAME_ENGINE_SYNC = False

    def __init__(self, nc, es, tag=""):
        self.nc = nc
        self.es = es
        self.tag = tag
        self.eng = {"pe": nc.tensor, "act": nc.scalar, "dve": nc.vector, "pool": nc.gpsimd, "sp": nc.sync}
        self.esem = {k: es.enter_context(nc.semaphore("s_" + tag + k)) for k in self.eng}
        self.ecnt = {k: 0 for k in self.eng}
        self.waited = {k: {} for k in self.eng}
        self.dsems = []
        self.dcnt = {}
        self.ndma = 0
        self.n_ins = 0

    def sb(self, name, shape, dt):
        t = self.es.enter_context(self.nc.sbuf_tensor(name, list(shape), dt))
        return Buf(name, t)

    def ps(self, name, shape, dt=F32):
        t = self.es.enter_context(self.nc.psum_tensor(name, list(shape), dt))
        return Buf(name, t)

    def new_dsem(self, name):
        s = self.es.enter_context(self.nc.semaphore(self.tag + name))
        self.dcnt[id(s)] = [s, 0]
        return s

    def _wait(self, E, raw, other, strict=False):
        eng = self.eng[E]
        best = {}
        own = self.esem[E]
        for lst, is_raw in ((raw, True), (other, False)):
            for ev in lst:
                if ev is None:
                    continue
                sem, val = ev
                if sem is own and not self.SAME_ENGINE_SYNC:
                    if not (is_raw and (E != "pe" or strict)):
                        continue
                k = id(sem)
                if k not in best or best[k][1] < val:
                    best[k] = (sem, val)
        for k, (sem, val) in best.items():
            if self.waited[E].get(k, 0) >= val:
                continue
            eng.wait_ge(sem, val)
            self.waited[E][k] = val

    def _deps(self, reads, writes):
        raw = [b.w for b in reads]
        other = []
        for b in writes:
            other.append(b.w)
            other.extend(b.r)
        return raw, other

    def _commit(self, ev, reads, writes):
        for b in reads:
            b.r.append(ev)
            if len(b.r) > 64:
                best = {}
                for s, v in b.r:
                    if id(s) not in best or best[id(s)][1] < v:
                        best[id(s)] = (s, v)
                b.r = list(best.values())
        for b in writes:
            b.w = ev
            b.r = []

    def op(self, E, fn, reads=(), writes=(), strict=False):
        self._wait(E, *self._deps(reads, writes), strict=strict)
        ins = fn(self.eng[E])
        self.ecnt[E] += 1
        ins.then_inc(self.esem[E], 1)
        ev = (self.esem[E], self.ecnt[E])
        self._commit(ev, reads, writes)
        self.n_ins += 1
        return ins

    def dma(self, E, out, in_, reads=(), writes=(), dsem=None, **kw):
        self._wait(E, *self._deps(reads, writes))
        ins = self.eng[E].dma_start(out=out, in_=in_, **kw)
        wb = writes[0]
        if wb.dsem is None:
            wb.dsem = self.new_dsem("d_" + wb.name)
        dsem = wb.dsem
        rec = self.dcnt[id(dsem)]
        rec[1] += 16
        ins.then_inc(dsem, 16)
        ev = (dsem, rec[1])
        self._commit(ev, reads, writes)
        self.ndma += 1
        return ins

    def idma(self, out, in_, idx_ap, scatter, bound, reads=(), writes=()):
        self._wait("pool", *self._deps(reads, writes))
        if not hasattr(self, "_bregs"):
            self._bregs = {}
        if bound not in self._bregs:
            reg = self.nc.gpsimd.alloc_register("bnd%d" % len(self._bregs))
            self.nc.gpsimd.reg_mov(reg, bound)
            self._bregs[bound] = reg
        bound = self._bregs[bound]
        off = bass.IndirectOffsetOnAxis(ap=idx_ap, axis=0)
        if scatter:
            ins = self.nc.gpsimd.indirect_dma_start(out=out, out_offset=off, in_=in_, in_offset=None, bounds_check=bound, oob_is_err=False)
        else:
            ins = self.nc.gpsimd.indirect_dma_start(out=out, out_offset=None, in_=in_, in_offset=off, bounds_check=bound, oob_is_err=False)
        wb = writes[0]
        if wb.dsem is None:
            wb.dsem = self.new_dsem("d_" + wb.name)
        rec = self.dcnt[id(wb.dsem)]
        rec[1] += 16
        ins.then_inc(wb.dsem, 16)
        ev = (wb.dsem, rec[1])
        self._commit(ev, reads, writes)
        self.ndma += 1
        return ins

    def final_wait(self, E, bufs):
        self._wait(E, [b.w for b in bufs], [])


NT = 2048
CH = 512
NCH = NT // CH
EPS = 1e-6

QK_GROUPS = ([("q", j, 0 + 128 * j, 0, False) for j in range(3)]
             + [("k", j, 384 + 128 * j, 1, False) for j in range(3)]
             + [("q", 3 + j, 1152 + 128 * j, 2, True) for j in range(3)]
             + [("k", 3 + j, 1536 + 128 * j, 3, True) for j in range(3)]
             + [("q", 6 + j, 2304 + 128 * j, 4, False) for j in range(2)])


def rms_chunk(m, src, n, gvec, ones, epsb, ps_ss, sq, rs, uT, u32=None):
    m.op("act", lambda e: e.activation(out=sq[:, :, 0:n], in_=src[:, :, 0:n], func=AF.Square), reads=[src], writes=[sq])
    for c in range(8):
        m.op("pe", lambda e: e.matmul(ps_ss[:, 0:n], lhsT=ones[:, 0, :], rhs=sq[:, c, 0:n], start=(c == 0), stop=(c == 7)),
             reads=[sq, ones], writes=[ps_ss])
    m.op("act", lambda e: e.activation(out=rs[:, 0:n], in_=ps_ss[:, 0:n], func=AF.Ln, scale=1.0 / 1024, bias=epsb[:, 0:1]),
         reads=[ps_ss, epsb], writes=[rs])
    m.op("act", lambda e: e.activation(out=rs[:, 0:n], in_=rs[:, 0:n], func=AF.Exp, scale=-0.5), reads=[rs], writes=[rs])
    for c in range(8):
        m.op("dve", lambda e: e.scalar_tensor_tensor(out=uT[:, c, 0:n], in0=src[:, c, 0:n], scalar=gvec[:, c:c + 1],
                                                     in1=rs[:, 0:n], op0=ALU.mult, op1=ALU.mult),
             reads=[src, gvec, rs], writes=[uT])
        if u32 is not None:
            m.op("pool", lambda e: e.scalar_tensor_tensor(out=u32[:, c, 0:n], in0=src[:, c, 0:n], scalar=gvec[:, c:c + 1],
                                                          in1=rs[:, 0:n], op0=ALU.mult, op1=ALU.mult),
                 reads=[src, gvec, rs], writes=[u32])


def build_pa():
    nc = bass.Bass("TRN2", target_bir_lowering=False)
    D = lambda n, s, dt, k: nc.dram_tensor(n, s, dt, kind=k).ap()
    hT = D("hT", [128, 8, NT], F32, "ExternalInput")
    w_in = D("w_in", [128, 8, 2560], F32, "ExternalInput")
    gattn = D("gattn", [128, 8], F32, "ExternalInput")
    gqk = D("gqk", [128, 8], F32, "ExternalInput")
    cmat = D("cmat", [128, 3, 128], F32, "ExternalInput")
    cs = D("cs", [128, 2, NT], F32, "ExternalInput")
    memT = D("memT", [128, 8, 256], F32, "ExternalInput")
    gmem = D("gmem", [128, 8], F32, "ExternalInput")
    w_kv = D("w_kv", [128, 8, 512], F32, "ExternalInput")
    qT_o = D("qT", [128, 8, NT], BF16, "ExternalOutput")
    kT_o = D("kT", [128, 6, NT], BF16, "ExternalOutput")
    v_o = D("v", [128, 16, 12 * 65], BF16, "ExternalOutput")
    kmT_o = D("kmT", [128, 2, 256], BF16, "ExternalOutput")
    vm_o = D("vm", [128, 2, 4 * 65], BF16, "ExternalOutput")

    with ExitStack() as es:
        m = MK(nc, es)
        W = m.sb("W", [128, 8, 2560], BF16)
        Wkv = m.sb("Wkv", [128, 8, 512], BF16)
        cm = m.sb("cm", [128, 3, 128], BF16)
        ga = m.sb("ga", [128, 8], F32)
        gm = m.sb("gm", [128, 8], F32)
        gq = m.sb("gq", [128, 8], F32)
        epsb = m.sb("epsb", [128, 1], F32)
        cst = [m.sb("cst%d" % i, [128, 2, CH], F32) for i in range(2)]
        hc = [m.sb("hc%d" % i, [128, 8, CH], F32) for i in range(2)]
        sq = m.sb("sq", [128, 8, CH], BF16)
        rs = m.sb("rs", [128, CH], F32)
        uT = [m.sb("uT%d" % i, [128, 8, CH], BF16) for i in range(2)]
        sq2 = [m.sb("sq2%d" % i, [128, CH], BF16) for i in range(4)]
        r2 = [m.sb("r2%d" % i, [128, CH], F32) for i in range(4)]
        qn = [m.sb("qn%d" % i, [128, CH], BF16) for i in range(4)]
        ysb = [m.sb("ysb%d" % i, [128, CH], F32) for i in range(4)]
        t1 = [m.sb("t1%d" % i, [128, CH], F32) for i in range(4)]
        t2 = [m.sb("t2%d" % i, [128, CH], F32) for i in range(4)]
        qTs2 = [m.sb("qTs%d" % i, [128, 8, CH], BF16) for i in range(2)]
        kTs2 = [m.sb("kTs%d" % i, [128, 6, CH], BF16) for i in range(2)]
        vs2 = [m.sb("vs%d" % i, [128, 4, 12 * 65], BF16) for i in range(2)]
        kms = m.sb("kms", [128, 2, 256], BF16)
        vms = m.sb("vms", [128, 2, 4 * 65], BF16)
        ps_ss = m.ps("ps_ss", [128, 512])
        ps_y = [m.ps("ps_y%d" % i, [128, 512]) for i in range(3)]
        ps_s2 = [m.ps("ps_s2%d" % i, [128, 512]) for i in range(2)]
        ps_qp = m.ps("ps_qp", [128, 512])
        ps_v = [m.ps("ps_v%d" % i, [128, 512]) for i in range(1)]

        dW = [m.new_dsem("dW%d" % i) for i in range(4)]
        dsm = m.new_dsem("dsm")
        dh = [m.new_dsem("dh%d" % i) for i in range(2)]
        dc = [m.new_dsem("dc%d" % i) for i in range(2)]
        dout = m.new_dsem("dout")

        m.dma("sp", ga[:], gattn, writes=[ga], dsem=dsm)
        m.dma("sp", gm[:], gmem, writes=[gm], dsem=dsm)
        m.dma("sp", gq[:], gqk, writes=[gq], dsem=dsm)
        m.dma("pool", cm[:], cmat, writes=[cm], dsem=dW[0])
        m.op("dve", lambda e: e.memset(epsb[:], EPS), writes=[epsb])
        for i in range(2):
            m.op("dve", lambda e: e.memset(vs2[i][:], 1.0), writes=[vs2[i]])
        m.op("dve", lambda e: e.memset(vms[:], 1.0), writes=[vms])
        Wb = [Buf("Wb%d" % i) for i in range(4)]
        for i in range(4):
            m.dma("pool", W[:, :, 640 * i:640 * (i + 1)], w_in[:, :, 640 * i:640 * (i + 1)], writes=[Wb[i]], dsem=dW[i])
        m.dma("pool", Wkv[:], w_kv, writes=[Wkv], dsem=dW[0])

        def wbufs(c0, c1):
            return [Wb[i] for i in range(4) if c0 < 640 * (i + 1) and c1 > 640 * i]

        cnt = {"y": 0, "v": 0}

        def qk_stages(u, n, Wt, wdeps, col0, gcolumn, rope, dst, dsti, tok0, csb):
            k_ = cnt["y"]
            cnt["y"] += 1
            i = k_ % 4
            yp, s2, sq2b, r2b, qnb = ps_y[k_ % 3], ps_s2[k_ % 2], sq2[i], r2[i], qn[i]
            y = ysb[i]
            d = dst[:, dsti, tok0:tok0 + n]

            def s1():
                for c in range(8):
                    m.op("pe", lambda e: e.matmul(yp[:, 0:n], lhsT=Wt[:, c, col0:col0 + 128], rhs=u[:, c, 0:n], start=(c == 0), stop=(c == 7)),
                         reads=[u] + wdeps, writes=[yp])
                m.op("dve", lambda e: e.tensor_copy(out=y[:, 0:n], in_=yp[:, 0:n]), reads=[yp], writes=[y])
                m.op("act", lambda e: e.activation(out=sq2b[:, 0:n], in_=y[:, 0:n], func=AF.Square), reads=[y], writes=[sq2b])

            def s2f():
                m.op("pe", lambda e: e.matmul(s2[:, 0:n], lhsT=cm[:, 1, :], rhs=sq2b[:, 0:n], start=True, stop=True), reads=[sq2b, cm], writes=[s2])
                m.op("act", lambda e: e.activation(out=r2b[:, 0:n], in_=s2[:, 0:n], func=AF.Ln, scale=1.0 / 64, bias=epsb[:, 0:1]),
                     reads=[s2, epsb], writes=[r2b])
                m.op("act", lambda e: e.activation(out=r2b[:, 0:n], in_=r2b[:, 0:n], func=AF.Exp, scale=-0.5), reads=[r2b], writes=[r2b])
                if not rope:
                    m.op("dve", lambda e: e.scalar_tensor_tensor(out=d, in0=y[:, 0:n], scalar=gq[:, gcolumn:gcolumn + 1], in1=r2b[:, 0:n],
                                                                 op0=ALU.mult, op1=ALU.mult), reads=[y, gq, r2b], writes=[dst])
                else:
                    m.op("dve", lambda e: e.scalar_tensor_tensor(out=qnb[:, 0:n], in0=y[:, 0:n], scalar=gq[:, gcolumn:gcolumn + 1], in1=r2b[:, 0:n],
                                                                 op0=ALU.mult, op1=ALU.mult), reads=[y, gq, r2b], writes=[qnb])

            def s3():
                if not rope:
                    return
                t1b, t2b = t1[i], t2[i]
                m.op("pe", lambda e: e.matmul(ps_qp[:, 0:n], lhsT=cm[:, 2, :], rhs=qnb[:, 0:n], start=True, stop=True), reads=[qnb, cm], writes=[ps_qp])
                m.op("pool", lambda e: e.tensor_tensor(out=t1b[:, 0:n], in0=qnb[:, 0:n], in1=csb[:, 0, 0:n], op=ALU.mult), reads=[qnb, csb], writes=[t1b])
                m.op("dve", lambda e: e.tensor_tensor(out=t2b[:, 0:n], in0=ps_qp[:, 0:n], in1=csb[:, 1, 0:n], op=ALU.mult), reads=[ps_qp, csb], writes=[t2b])
                m.op("pool", lambda e: e.tensor_tensor(out=d, in0=t1b[:, 0:n], in1=t2b[:, 0:n], op=ALU.add), reads=[t1b, t2b], writes=[dst])
            return s1, s2f, s3

        def run_pipelined(groups, fillers=()):
            fillers = list(fillers)
            n_g = len(groups)
            groups[0][0]()
            if n_g > 1:
                groups[1][0]()
            for j in range(n_g + 1):
                if j + 2 < n_g:
                    groups[j + 2][0]()
                if j < n_g:
                    groups[j][1]()
                if fillers:
                    fillers.pop(0)()
                if j >= 1:
                    groups[j - 1][2]()
            for f in fillers:
                f()

        def qk_group(*args):
            s1, s2f, s3 = qk_stages(*args)
            s1(); s2f(); s3()

        def v_tile(u, Wt, wdeps, col0, ncols, sub, dst, tile, head0, nheads):
            cnt["v"] += 1
            pv = ps_v[0]
            for c in range(8):
                m.op("pe", lambda e: e.matmul(pv[:, 0:ncols], lhsT=u[:, c, sub * 128:(sub + 1) * 128], rhs=Wt[:, c, col0:col0 + ncols],
                                              start=(c == 0), stop=(c == 7)), reads=[u] + wdeps, writes=[pv])
            dv = dst[:, tile, head0 * 65:(head0 + nheads) * 65].rearrange("p (h d) -> p h d", d=65)[:, :, 0:64]
            sv = pv[:, 0:ncols].rearrange("p (h d) -> p h d", d=64)
            m.op("act", lambda e: e.activation(out=dv, in_=sv, func=AF.Copy), reads=[pv], writes=[dst])

        m.dma("sp", hc[0][:, :, 0:256], memT, writes=[hc[0]], dsem=dh[0])
        rms_chunk(m, hc[0], 256, gm, cm, epsb, ps_ss, sq, rs, uT[0])
        for j in range(2):
            qk_group(uT[0], 256, Wkv, [Wkv], 128 * j, 5, False, kms, j, 0, None)
        for sub in range(2):
            v_tile(uT[0], Wkv, [Wkv], 256, 256, sub, vms, sub, 0, 4)
        obm = [Buf("o1"), Buf("o2")]
        m.dma("sp", kmT_o, kms[:], reads=[kms], writes=[obm[0]], dsem=dout)
        m.dma("sp", vm_o, vms[:], reads=[vms], writes=[obm[1]], dsem=dout)

        ob = [Buf("oq"), Buf("ok"), Buf("ov")]
        def load_chunk(t):
            b = (t + 1) % 2
            m.dma("sp", hc[b][:], hT[:, :, t * CH:(t + 1) * CH], writes=[hc[b]])
            m.dma("sp", cst[b][:], cs[:, :, t * CH:(t + 1) * CH], writes=[cst[b]])

        load_chunk(0)
        for t in range(NCH):
            b = (t + 1) % 2
            tok0 = t * CH
            if t + 1 < NCH:
                load_chunk(t + 1)
            rms_chunk(m, hc[b], CH, ga, cm, epsb, ps_ss, sq, rs, uT[b])
            qTs, kTs, vs = qTs2[b], kTs2[b], vs2[b]
            groups = [qk_stages(uT[b], CH, W, wbufs(col0, col0 + 128), col0, gc, rope, qTs if dn == "q" else kTs, di, 0, cst[b])
                      for (dn, di, col0, gc, rope) in QK_GROUPS]
            fillers = []
            for sub in range(4):
                fillers.append(lambda sub=sub, b=b, vs=vs: v_tile(uT[b], W, wbufs(768, 1152), 768, 384, sub, vs, sub, 0, 6))
                fillers.append(lambda sub=sub, b=b, vs=vs: v_tile(uT[b], W, wbufs(1920, 2304), 1920, 384, sub, vs, sub, 6, 6))
            run_pipelined(groups, fillers)
            m.dma("sp", qT_o[:, :, tok0:tok0 + CH], qTs[:], reads=[qTs], writes=[ob[0]], dsem=dout)
            m.dma("sp", kT_o[:, :, tok0:tok0 + CH], kTs[:], reads=[kTs], writes=[ob[1]], dsem=dout)
            m.dma("sp", v_o[:, 4 * t:4 * t + 4, :], vs[:], reads=[vs], writes=[ob[2]], dsem=dout)
        m.final_wait("sp", ob + obm)
        print("P_A instrs", m.n_ins, "dmas", m.ndma)
    return nc


NT = 2048
EPS = 1e-6
SCALE = 0.125
NS = 6
PIPE = 4
DEBUG = False


def build_pb():
    nc = bass.Bass("TRN2", target_bir_lowering=False)
    D = lambda n, s, dt, k: nc.dram_tensor(n, s, dt, kind=k).ap()
    qT = D("qT", [128, 8, NT], BF16, "ExternalInput")
    kA = D("kA", [128, 3, 2560], BF16, "ExternalInput")
    vA = D("vA", [128, 3, 20, 130], BF16, "ExternalInput")
    kAe = D("kAe", [128, 3, 4, 640], BF16, "ExternalInput")
    vAe = D("vAe", [128, 3, 4, 650], BF16, "ExternalInput")
    kB = D("kB", [128, 3, 4096], BF16, "ExternalInput")
    vB = D("vB", [128, 3, 32, 130], BF16, "ExternalInput")
    kM = D("kM", [128, 2, 256], BF16, "ExternalInput")
    vM = D("vM", [128, 2, 2, 130], BF16, "ExternalInput")
    nab = D("nab", [128, 3, 5, 1280], F32, "ExternalInput")
    cmask = D("cmask", [128, 17 * 128], F32, "ExternalInput")
    ident = D("ident", [128, 128], F32, "ExternalInput")
    gout = D("gout", [128, 1024], F32, "ExternalInput")
    w_out = D("w_out", [128, 8, 1024], F32, "ExternalInput")
    hT = D("hT", [128, 8, NT], F32, "ExternalInput")
    hmid = D("hmidT", [128, 8, NT], F32, "ExternalOutput")
    o_dbg = D("o_dbg", [128, 16, 1024], BF16, "ExternalOutput") if DEBUG else None
    dbg2 = D("dbg2", [128, 1024], BF16, "ExternalOutput") if DEBUG else None
    dbg3 = D("dbg3", [128, 4], F32, "ExternalOutput") if DEBUG else None
    dbg4 = D("dbg4", [128, 8, 512], BF16, "ExternalOutput") if DEBUG else None

    with ExitStack() as es:
        m = MK(nc, es)
        qp = [m.sb("qp%d" % i, [128, NT], BF16) for i in range(2)]
        kb = [m.sb("kb%d" % i, [128, 4096], BF16) for i in range(2)]
        vb = [m.sb("vb%d" % i, [128, 32, 130], BF16) for i in range(2)]
        ke = [m.sb("ke0", [128, 4, 640], BF16)] * 2
        ve = [m.sb("ve0", [128, 4, 650], BF16)] * 2
        stage = [m.sb("stage%d" % i, [128, 1280], F32) for i in range(2)]
        Mna = [m.sb("Mna0", [128, 5, 1280], BF16)] * 2
        Cm = m.sb("Cm", [128, 17 * 128], BF16)
        idn = m.sb("idn", [128, 128], BF16)
        go = m.sb("go", [128, 1024], F32)
        Wo = m.sb("Wo", [128, 8, 1024], BF16)
        epsb = m.sb("epsb", [128, 1], F32)
        o_all = m.sb("o_all", [128, 16, 1024], BF16)
        Eb = [m.sb("E%d" % i, [128, 512], BF16) for i in range(NS)]
        Pb = [m.sb("P%d" % i, [128, 512], BF16) for i in range(NS)]
        rec = [m.sb("rec%d" % i, [128, 2], F32) for i in range(2)]
        sqf = m.sb("sqf", [128, 1024], F32)
        ms = [m.sb("ms%d" % i, [128, 4], F32) for i in range(2)]
        mixed = [m.sb("mixed%d" % i, [128, 1024], BF16) for i in range(2)]
        mixedT = [m.sb("mixedT%d" % i, [128, 8, 512], BF16) for i in range(2)]
        hc = [m.sb("hc0", [128, 8, 512], F32)] * 2
        es_att = ExitStack()
        es_main, m.es = m.es, es_att
        ps_s = [m.ps("ps_s%d" % i, [128, 512]) for i in range(NS)]
        ps_o = [m.ps("ps_o%d" % i, [128, 512]) for i in range(2)]
        m.es = es_main

        dl = [m.new_dsem("dl%d" % i) for i in range(2)]
        dst_ = [m.new_dsem("dst%d" % i) for i in range(2)]
        dcst = m.new_dsem("dcst")
        dh = [m.new_dsem("dh%d" % i) for i in range(2)]
        dout = m.new_dsem("dout")

        m.dma("pool", Cm[:], cmask, writes=[Cm], dsem=dcst)
        m.dma("pool", idn[:], ident, writes=[idn], dsem=dcst)
        m.dma("sp", go[:], gout, writes=[go], dsem=dcst)
        m.op("dve", lambda e: e.memset(epsb[:], EPS), writes=[epsb])
        Wo_loaded = [False]

        gcnt = [0]
        stc = [0]
        SLOT = {0: 0, 1: 1, 14: 2, 15: 3}
        VAR = {0: 0, 1: 1, 14: 3, 15: 4}

        def kind_of(p):
            return "A" if p < 3 else ("B" if p < 6 else "M")

        def load_main(p):
            b = p % 2
            kind = kind_of(p)
            m.dma("sp", qp[b][:], qT[:, p, :], writes=[qp[b]])
            if kind == "A":
                m.dma("sp", kb[b][:, 0:2560], kA[:, p, :], writes=[kb[b]])
                m.dma("sp", vb[b][:, 0:20, :], vA[:, p], writes=[vb[b]])
            elif kind == "B":
                m.dma("sp", kb[b][:], kB[:, p - 3, :], writes=[kb[b]])
                m.dma("sp", vb[b][:], vB[:, p - 3], writes=[vb[b]])
            else:
                m.dma("sp", kb[b][:, 0:256], kM[:, p - 6, :], writes=[kb[b]])
                m.dma("sp", vb[b][:, 0:2, :], vM[:, p - 6], writes=[vb[b]])

        def load_edge(p):
            b = p % 2
            m.dma("sp", ke[b][:], kAe[:, p], writes=[ke[b]])
            m.dma("sp", ve[b][:], vAe[:, p], writes=[ve[b]])
            for v in range(5):
                sb_ = stc[0] % 2
                stc[0] += 1
                m.dma("sp", stage[sb_][:], nab[:, p, v, :], writes=[stage[sb_]])
                m.op("act", lambda e: e.activation(out=Mna[b][:, v, :], in_=stage[sb_][:], func=AF.Exp), reads=[stage[sb_]], writes=[Mna[b]])

        units = []
        for p in range(8):
            kind = kind_of(p)
            b = p % 2
            for i in range(16):
                O = ps_o[i % 2]
                for hh in range(2):
                    pbs = 64 * hh
                    tiles = []
                    if kind == "A":
                        slot = SLOT.get(i)
                        var = VAR.get(i, 2)
                        for j in range(5):
                            if slot is None:
                                k_ap = kb[b][pbs:pbs + 64, (i + j) * 128:(i + j + 1) * 128]
                                v_ap = vb[b][:, i + j, hh * 65:(hh + 1) * 65]
                            else:
                                k_ap = ke[b][pbs:pbs + 64, slot, j * 128:(j + 1) * 128]
                                v_ap = ve[b][:, slot, j * 130 + hh * 65:j * 130 + (hh + 1) * 65]
                            tiles.append((k_ap, v_ap, (Mna[b], var, hh * 640 + j * 128)))
                        kdeps = [kb[b], ke[b]]
                        vdeps = [vb[b], ve[b]]
                    elif kind == "B":
                        for j in range(17):
                            k_ap = kb[b][pbs:pbs + 64, (i + j) * 128:(i + j + 1) * 128]
                            v_ap = vb[b][:, i + j, hh * 65:(hh + 1) * 65]
                            tiles.append((k_ap, v_ap, (Cm, None, j * 128)))
                        kdeps = [kb[b]]
                        vdeps = [vb[b]]
                    else:
                        for j in range(2):
                            k_ap = kb[b][pbs:pbs + 64, j * 128:(j + 1) * 128]
                            v_ap = vb[b][:, j, hh * 65:(hh + 1) * 65]
                            tiles.append((k_ap, v_ap, None))
                        kdeps = [kb[b]]
                        vdeps = [vb[b]]
                    q_ap = qp[b][pbs:pbs + 64, i * 128:(i + 1) * 128]
                    nt = len(tiles)
                    for g0 in range(0, nt, 4):
                        units.append(dict(p=p, b=b, i=i, hh=hh, O=O, grp=tiles[g0:g0 + 4], g0=g0, nt=nt, q_ap=q_ap, kdeps=kdeps, vdeps=vdeps,
                                          first_of_pair=(i == 0 and hh == 0 and g0 == 0), last_of_tile=(hh == 1 and g0 + 4 >= nt)))
        for n_, u in enumerate(units):
            u["gi"] = n_ % NS

        def emit_qk(u):
            S = ps_s[u["gi"]]
            for t, (k_ap, v_ap, mk_) in enumerate(u["grp"]):
                m.op("pe", lambda e: e.matmul(S[:, t * 128:(t + 1) * 128], lhsT=k_ap, rhs=u["q_ap"], start=True, stop=True),
                     reads=u["kdeps"] + [qp[u["b"]]], writes=[S])

        def emit_softmax(u):
            S, E, P = ps_s[u["gi"]], Eb[u["gi"]], Pb[u["gi"]]
            n = len(u["grp"]) * 128
            m.op("act", lambda e: e.activation(out=E[:, 0:n], in_=S[:, 0:n], func=AF.Exp, scale=SCALE), reads=[S], writes=[E])
            mk0 = u["grp"][0][2]
            if mk0 is not None:
                mb, var, c0 = mk0
                m_ap = mb[:, c0:c0 + n] if var is None else mb[:, var, c0:c0 + n]
                m.op("dve", lambda e: e.tensor_tensor(out=P[:, 0:n], in0=E[:, 0:n], in1=m_ap, op=ALU.mult), reads=[E, mb], writes=[P])
                u["src"] = P
            else:
                u["src"] = E

        def emit_pv(u):
            O, hh, src = u["O"], u["hh"], u["src"]
            for t, (k_ap, v_ap, mk_) in enumerate(u["grp"]):
                first = (u["g0"] + t == 0)
                last = (u["g0"] + t == u["nt"] - 1)
                m.op("pe", lambda e: e.matmul(O[:, hh * 65:(hh + 1) * 65], lhsT=src[:, t * 128:(t + 1) * 128], rhs=v_ap, start=first, stop=last),
                     reads=u["vdeps"] + [src], writes=[O])
            if u["last_of_tile"]:
                i, p = u["i"], u["p"]
                rc = rec[i % 2]
                Ov = O[:, 0:130].rearrange("p (h d) -> p h d", d=65)
                m.op("dve", lambda e: e.reciprocal(out=rc[:, 0:2], in_=Ov[:, :, 64]), reads=[O], writes=[rc])
                for h2 in range(2):
                    m.op("dve", lambda e: e.tensor_scalar(out=o_all[:, i, p * 128 + h2 * 64:p * 128 + h2 * 64 + 64], in0=O[:, h2 * 65:h2 * 65 + 64],
                                                          scalar1=rc[:, h2:h2 + 1], scalar2=None, op0=ALU.mult), reads=[O, rc], writes=[o_all])

        def pre_pair(p):
            if p + 1 < 8:
                load_main(p + 1)
            if p == 2:
                for cc in range(0, 8, 2):
                    m.dma("pool", Wo[:, cc:cc + 2, :], w_out[:, cc:cc + 2, :], writes=[Wo])
            if kind_of(p) == "A":
                load_edge(p)

        load_main(0)
        pair_units = [[u for u in units if u["p"] == p] for p in range(8)]
        for p in range(8):
            pre_pair(p)
            pu = pair_units[p]
            for u in pu[:PIPE]:
                emit_qk(u)
            for n_, u in enumerate(pu):
                if n_ + PIPE < len(pu):
                    emit_qk(pu[n_ + PIPE])
                emit_softmax(u)
                emit_pv(u)
        es_att.close()
        ps_T = m.ps("ps_T", [128, 1024], BF16)
        ps_y = [m.ps("ps_y%d" % i, [128, 512]) for i in range(2)]

        obd = Buf("odbg_out")
        if DEBUG:
            m.dma("sp", o_dbg, o_all[:], reads=[o_all], writes=[obd], dsem=dout)
        GR = [(0, 384), (384, 768), (768, 1024)]
        ob = Buf("hmid_out")
        for i in range(16):
            b = i % 2
            msb = ms[b]
            m.op("dve", lambda e: e.tensor_tensor(out=sqf[:, :], in0=o_all[:, i, :], in1=o_all[:, i, :], op=ALU.mult), reads=[o_all], writes=[sqf])
            for g, (c0, c1) in enumerate(GR):
                m.op("dve", lambda e: e.tensor_reduce(out=msb[:, g:g + 1], in_=sqf[:, c0:c1], axis=AX.X, op=ALU.add), reads=[sqf], writes=[msb])
            for g, (c0, c1) in enumerate(GR):
                m.op("act", lambda e: e.activation(out=msb[:, g:g + 1], in_=msb[:, g:g + 1], func=AF.Ln, scale=1.0 / (c1 - c0), bias=epsb[:, 0:1]),
                     reads=[msb, epsb], writes=[msb])
            m.op("act", lambda e: e.activation(out=msb[:, 0:3], in_=msb[:, 0:3], func=AF.Exp, scale=-0.5), reads=[msb], writes=[msb])
            for g, (c0, c1) in enumerate(GR):
                m.op("dve", lambda e: e.scalar_tensor_tensor(out=mixed[b][:, c0:c1], in0=o_all[:, i, c0:c1], scalar=msb[:, g:g + 1], in1=go[:, c0:c1],
                                                             op0=ALU.mult, op1=ALU.mult), reads=[o_all, msb, go], writes=[mixed[b]])
            for c in range(8):
                m.op("pe", lambda e: e.transpose(ps_T[:, c * 128:(c + 1) * 128], mixed[b][:, c * 128:(c + 1) * 128], idn[:]), reads=[mixed[b], idn], writes=[ps_T])
            cb = (i // 4) % 2
            m.op("act", lambda e: e.activation(out=mixedT[cb][:, :, (i % 4) * 128:(i % 4 + 1) * 128], in_=ps_T[:, :].rearrange("p (c t) -> p c t", t=128), func=AF.Copy),
                 reads=[ps_T], writes=[mixedT[cb]])
            if i % 4 == 3:
                ch = i // 4
                m.dma("sp", hc[cb][:], hT[:, :, ch * 512:(ch + 1) * 512], writes=[hc[cb]], dsem=dh[cb])
                for d in range(8):
                    Y = ps_y[d % 2]
                    for c in range(8):
                        m.op("pe", lambda e: e.matmul(Y[:, :], lhsT=Wo[:, c, d * 128:(d + 1) * 128], rhs=mixedT[cb][:, c, :], start=(c == 0), stop=(c == 7)),
                             reads=[Wo, mixedT[cb]], writes=[Y])
                    m.op("dve", lambda e: e.tensor_tensor(out=hc[cb][:, d, :], in0=Y[:, :], in1=hc[cb][:, d, :], op=ALU.add), reads=[Y, hc[cb]], writes=[hc[cb]])
                m.dma("sp", hmid[:, :, ch * 512:(ch + 1) * 512], hc[cb][:], reads=[hc[cb]], writes=[ob], dsem=dout)
        if DEBUG:
            m.dma("sp", dbg2, mixed[1][:], reads=[mixed[1]], writes=[obd], dsem=dout)
            m.dma("sp", dbg3, ms[1][:], reads=[ms[1]], writes=[obd], dsem=dout)
            m.dma("sp", dbg4, mixedT[1][:], reads=[mixedT[1]], writes=[obd], dsem=dout)
        m.final_wait("sp", [ob, obd] if DEBUG else [ob])
        print("P_B instrs", m.n_ins, "dmas", m.ndma)
    return nc


EPS = 1e-6


class FFNRes:
    def __init__(self, m, F, NB):
        self.m, self.F, self.NB = m, F, NB
        self.wg = [m.sb("wg%d" % i, [128, 8, 128], BF16) for i in range(3)]
        self.wu = [m.sb("wu%d" % i, [128, 8, 128], BF16) for i in range(3)]
        self.wd = [m.sb("wd%d" % i, [128, F, 128], BF16) for i in range(2)]
        self.act = m.sb("act", [128, F, NB], BF16)
        self.sg = [m.sb("sg%d" % i, [128, 512], F32) for i in range(2)]
        self.ps_g = [m.ps("ps_g%d" % i, [128, 512]) for i in range(2)]
        self.ps_u = [m.ps("ps_u%d" % i, [128, 512]) for i in range(2)]
        self.ps_y = [m.ps("ps_y%d" % i, [128, 512]) for i in range(2)]
        self.wc = 0
        self.dc = 0
        self.gc = 0
        self.yc = 0


def ffn_block(R, uT, wg_ap, wu_ap, wd_ap, sink):
    m, F, NB = R.m, R.F, R.NB
    nch = NB // 512
    for f in range(F):
        i = R.wc % 3
        R.wc += 1
        wg, wu = R.wg[i], R.wu[i]
        m.dma("pool", wg[:].rearrange("p c n -> p (c n)"), wg_ap(f), writes=[wg])
        m.dma("pool", wu[:].rearrange("p c n -> p (c n)"), wu_ap(f), writes=[wu])
        for ch in range(nch):
            j = R.gc % 2
            R.gc += 1
            G, U, sg = R.ps_g[j], R.ps_u[j], R.sg[j]
            for c in range(8):
                m.op("pe", lambda e: e.matmul(G[:, :], lhsT=wg[:, c, :], rhs=uT[:, c, ch * 512:(ch + 1) * 512], start=(c == 0), stop=(c == 7)),
                     reads=[wg, uT], writes=[G])
            for c in range(8):
                m.op("pe", lambda e: e.matmul(U[:, :], lhsT=wu[:, c, :], rhs=uT[:, c, ch * 512:(ch + 1) * 512], start=(c == 0), stop=(c == 7)),
                     reads=[wu, uT], writes=[U])
            m.op("act", lambda e: e.activation(out=sg[:, :], in_=G[:, :], func=AF.Silu), reads=[G], writes=[sg])
            m.op("dve", lambda e: e.tensor_tensor(out=R.act[:, f, ch * 512:(ch + 1) * 512], in0=U[:, :], in1=sg[:, :], op=ALU.mult),
                 reads=[U, sg], writes=[R.act])
    for d in range(8):
        i = R.dc % 2
        R.dc += 1
        wd = R.wd[i]
        m.dma("pool", wd[:].rearrange("p f n -> p (f n)"), wd_ap(d), writes=[wd])
        for ch in range(nch):
            Y = R.ps_y[R.yc % 2]
            R.yc += 1
            for f in range(F):
                m.op("pe", lambda e: e.matmul(Y[:, :], lhsT=wd[:, f, :], rhs=R.act[:, f, ch * 512:(ch + 1) * 512], start=(f == 0), stop=(f == F - 1)),
                     reads=[wd, R.act], writes=[Y])
            sink(d, ch, Y)


def build_pc_dense(F=22, NT=2048, NB=1024):
    nc = bass.Bass("TRN2", target_bir_lowering=False)
    D = lambda n, s, dt, k: nc.dram_tensor(n, s, dt, kind=k).ap()
    hT = D("hT", [128, 8, NT], F32, "ExternalInput")
    gffn = D("gffn", [128, 8], F32, "ExternalInput")
    cmat = D("cmat", [128, 3, 128], F32, "ExternalInput")
    wg = D("wg", [F, 128, 1024], F32, "ExternalInput")
    wu = D("wu", [F, 128, 1024], F32, "ExternalInput")
    wd = D("wd", [8, 128, F * 128], F32, "ExternalInput")
    out = D("outT", [128, 8, NT], F32, "ExternalOutput")
    with ExitStack() as es:
        m = MK(nc, es)
        R = FFNRes(m, F, NB)
        cm = m.sb("cm", [128, 3, 128], BF16)
        gf = m.sb("gf", [128, 8], F32)
        epsb = m.sb("epsb", [128, 1], F32)
        hbs = [m.sb("hb%d" % i, [128, 8, NB], F32) for i in range(2)]
        uT = m.sb("uT", [128, 8, NB], BF16)
        sq = m.sb("sq", [128, 8, 512], BF16)
        rs = m.sb("rs", [128, 512], F32)
        ps_ss = m.ps("ps_ss", [128, 512])
        m.dma("pool", cm[:], cmat, writes=[cm])
        m.dma("sp", gf[:], gffn, writes=[gf])
        m.op("dve", lambda e: e.memset(epsb[:], EPS), writes=[epsb])
        ob = Buf("out")

        def load_half(i):
            for ch in range(NB // 512):
                m.dma("sp", hbs[i % 2][:, :, ch * 512:(ch + 1) * 512], hT[:, :, i * NB + ch * 512:i * NB + (ch + 1) * 512], writes=[hbs[i % 2]])

        load_half(0)
        for hb_i in range(NT // NB):
            t0 = hb_i * NB
            hb = hbs[hb_i % 2]
            if hb_i + 1 < NT // NB:
                load_half(hb_i + 1)
            for ch in range(NB // 512):
                rms_view(m, hb, ch * 512, 512, gf, cm, epsb, ps_ss, sq, rs, uT)

            def sink(d, ch, Y, hb=hb):
                m.op("dve", lambda e: e.tensor_tensor(out=hb[:, d, ch * 512:(ch + 1) * 512], in0=Y[:, :], in1=hb[:, d, ch * 512:(ch + 1) * 512], op=ALU.add),
                     reads=[Y, hb], writes=[hb])
            ffn_block(R, uT, lambda f: wg[f], lambda f: wu[f], lambda d: wd[d], sink)
            m.dma("sp", out[:, :, t0:t0 + NB], hb[:], reads=[hb], writes=[ob])
        m.final_wait("sp", [ob])
        print("P_C instrs", m.n_ins, "dmas", m.ndma)
    return nc


def rms_view(m, src, c0, n, gvec, cm, epsb, ps_ss, sq, rs, uT, u32=None):
    m.op("act", lambda e: e.activation(out=sq[:, :, 0:n], in_=src[:, :, c0:c0 + n], func=AF.Square), reads=[src], writes=[sq])
    for c in range(8):
        m.op("pe", lambda e: e.matmul(ps_ss[:, 0:n], lhsT=cm[:, 0, :], rhs=sq[:, c, 0:n], start=(c == 0), stop=(c == 7)), reads=[sq, cm], writes=[ps_ss])
    m.op("act", lambda e: e.activation(out=rs[:, 0:n], in_=ps_ss[:, 0:n], func=AF.Ln, scale=1.0 / 1024, bias=epsb[:, 0:1]), reads=[ps_ss, epsb], writes=[rs])
    m.op("act", lambda e: e.activation(out=rs[:, 0:n], in_=rs[:, 0:n], func=AF.Exp, scale=-0.5), reads=[rs], writes=[rs])
    for c in range(8):
        m.op("dve", lambda e: e.scalar_tensor_tensor(out=uT[:, c, c0:c0 + n], in0=src[:, c, c0:c0 + n], scalar=gvec[:, c:c + 1], in1=rs[:, 0:n],
                                                     op0=ALU.mult, op1=ALU.mult), reads=[src, gvec, rs], writes=[uT])
        if u32 is not None:
            m.op("dve", lambda e: e.scalar_tensor_tensor(out=u32[:, c, 0:n], in0=src[:, c, c0:c0 + n], scalar=gvec[:, c:c + 1], in1=rs[:, 0:n],
                                                         op0=ALU.mult, op1=ALU.mult), reads=[src, gvec, rs], writes=[u32])


def build_l5(NT=2048):
    nc = bass.Bass("TRN2", target_bir_lowering=False)
    D = lambda n, s, dt, k: nc.dram_tensor(n, s, dt, kind=k).ap()
    hT = D("hT", [128, 8, NT], F32, "ExternalInput")
    gffn = D("gffn", [128, 8], F32, "ExternalInput")
    cmat = D("cmat", [128, 3, 128], F32, "ExternalInput")
    wr = D("wr", [128, 8, 8], F32, "ExternalInput")
    uT_o = D("uT", [128, 8, NT], BF16, "ExternalOutput")
    g_o = D("gates", [128, NT // 128, 8], F32, "ExternalOutput")
    with ExitStack() as es:
        m = MK(nc, es)
        cm = m.sb("cm", [128, 3, 128], BF16)
        gf = m.sb("gf", [128, 8], F32)
        wrs = m.sb("wrs", [128, 8, 8], F32)
        epsb = m.sb("epsb", [128, 1], F32)
        hb = [m.sb("hb%d" % i, [128, 8, 512], F32) for i in range(2)]
        uT = [m.sb("uT%d" % i, [128, 8, 512], BF16) for i in range(2)]
        u32 = m.sb("u32", [128, 8, 512], F32)
        sq = m.sb("sq", [128, 8, 512], BF16)
        rs = m.sb("rs", [128, 512], F32)
        gates = m.sb("gates_sb", [128, NT // 128, 8], F32)
        lg = [m.sb("lg%d" % i, [128, 8], F32) for i in range(2)]
        m8 = [m.sb("m8%d" % i, [128, 8], F32) for i in range(2)]
        tt = [m.sb("tt%d" % i, [128, 4], F32) for i in range(2)]
        ga = [m.sb("ga%d" % i, [128, 8], F32) for i in range(2)]
        ps_ss = m.ps("ps_ss", [128, 512])
        ps_l = [m.ps("ps_l%d" % i, [128, 8]) for i in range(2)]
        m.dma("pool", cm[:], cmat, writes=[cm])
        m.dma("sp", gf[:], gffn, writes=[gf])
        m.dma("sp", wrs[:], wr, writes=[wrs])
        m.op("dve", lambda e: e.memset(epsb[:], EPS), writes=[epsb])
        ob = Buf("out")
        k = 0
        for t in range(NT // 512):
            b = t % 2
            m.dma("sp", hb[b][:], hT[:, :, t * 512:(t + 1) * 512], writes=[hb[b]])
            rms_view(m, hb[b], 0, 512, gf, cm, epsb, ps_ss, sq, rs, uT[b], u32=u32)
            m.dma("sp", uT_o[:, :, t * 512:(t + 1) * 512], uT[b][:], reads=[uT[b]], writes=[ob])
            for s in range(4):
                j = k % 2
                k += 1
                L, lgb, m8b, tb, gab = ps_l[j], lg[j], m8[j], tt[j], ga[j]
                for c in range(8):
                    m.op("pe", lambda e: e.matmul(L[:, :], lhsT=u32[:, c, s * 128:(s + 1) * 128], rhs=wrs[:, c, :], start=(c == 0), stop=(c == 7)),
                         reads=[u32, wrs], writes=[L])
                m.op("dve", lambda e: e.tensor_copy(out=lgb[:, :], in_=L[:, :]), reads=[L], writes=[lgb])
                m.op("dve", lambda e: e.max(out=m8b[:, :], in_=lgb[:, :]), reads=[lgb], writes=[m8b])
                m.op("dve", lambda e: e.tensor_tensor(out=tb[:, 0:1], in0=m8b[:, 1:2], in1=m8b[:, 0:1], op=ALU.subtract), reads=[m8b], writes=[tb])
                m.op("act", lambda e: e.activation(out=tb[:, 1:2], in_=tb[:, 0:1], func=AF.Exp), reads=[tb], writes=[tb])
                m.op("dve", lambda e: e.tensor_scalar(out=tb[:, 2:3], in0=tb[:, 1:2], scalar1=1.0, scalar2=None, op0=ALU.add), reads=[tb], writes=[tb])
                m.op("dve", lambda e: e.reciprocal(out=tb[:, 2:3], in_=tb[:, 2:3]), reads=[tb], writes=[tb])
                m.op("dve", lambda e: e.tensor_tensor(out=tb[:, 3:4], in0=tb[:, 1:2], in1=tb[:, 2:3], op=ALU.mult), reads=[tb], writes=[tb])
                m.op("dve", lambda e: e.tensor_scalar(out=gab[:, :], in0=lgb[:, :], scalar1=m8b[:, 0:1], scalar2=tb[:, 2:3], op0=ALU.is_equal, op1=ALU.mult),
                     reads=[lgb, m8b, tb], writes=[gab])
                m.op("dve", lambda e: e.tensor_scalar(out=lgb[:, :], in0=lgb[:, :], scalar1=m8b[:, 1:2], scalar2=tb[:, 3:4], op0=ALU.is_equal, op1=ALU.mult),
                     reads=[lgb, m8b, tb], writes=[lgb])
                m.op("dve", lambda e: e.tensor_tensor(out=gates[:, t * 4 + s, :], in0=gab[:, :], in1=lgb[:, :], op=ALU.add), reads=[gab, lgb], writes=[gates])
        ob2 = Buf("out2")
        m.dma("sp", g_o, gates[:], reads=[gates], writes=[ob2])
        m.final_wait("sp", [ob, ob2])
        print("L5 instrs", m.n_ins, "dmas", m.ndma)
    return nc


def build_l6(F=28, NTOK=16384, NB=1024):
    nc = bass.Bass("TRN2", target_bir_lowering=False)
    D = lambda n, s, dt, k: nc.dram_tensor(n, s, dt, kind=k).ap()
    uT_i = D("uT", [128, 8, NTOK], BF16, "ExternalInput")
    gbc_i = D("gbc", [128, NTOK], F32, "ExternalInput")
    wg = D("wg", [F, 128, 1024], F32, "ExternalInput")
    wu = D("wu", [F, 128, 1024], F32, "ExternalInput")
    wd = D("wd", [8, 128, F * 128], F32, "ExternalInput")
    y_o = D("yT", [128, 8, NTOK], F32, "ExternalOutput")
    with ExitStack() as es:
        m = MK(nc, es)
        R = FFNRes(m, F, NB)
        uT = [m.sb("uT%d" % i, [128, 8, NB], BF16) for i in range(2)]
        gb = [m.sb("gb%d" % i, [128, NB], F32) for i in range(2)]
        yb = m.sb("yb", [128, 8, NB], F32)
        ob = Buf("out")
        for tb in range(NTOK // NB):
            b = tb % 2
            t0 = tb * NB
            m.dma("sp", uT[b][:], uT_i[:, :, t0:t0 + NB], writes=[uT[b]])
            m.dma("sp", gb[b][:], gbc_i[:, t0:t0 + NB], writes=[gb[b]])

            def sink(d, ch, Y):
                m.op("dve", lambda e: e.tensor_tensor(out=yb[:, d, ch * 512:(ch + 1) * 512], in0=Y[:, :], in1=gb[b][:, ch * 512:(ch + 1) * 512], op=ALU.mult),
                     reads=[Y, gb[b]], writes=[yb])
            ffn_block(R, uT[b], lambda f: wg[f], lambda f: wu[f], lambda d: wd[d], sink)
            m.dma("sp", y_o[:, :, t0:t0 + NB], yb[:], reads=[yb], writes=[ob])
        m.final_wait("sp", [ob])
        print("L6 instrs", m.n_ins, "dmas", m.ndma)
    return nc


def build_l7(NT=2048):
    nc = bass.Bass("TRN2", target_bir_lowering=False)
    D = lambda n, s, dt, k: nc.dram_tensor(n, s, dt, kind=k).ap()
    hT = D("hT", [128, 8, NT], F32, "ExternalInput")
    ys = D("ys", [8, 128, 8, NT], F32, "ExternalInput")
    out = D("outT", [128, 8, NT], F32, "ExternalOutput")
    with ExitStack() as es:
        m = MK(nc, es)
        acc = [m.sb("acc%d" % i, [128, 8, 512], F32) for i in range(2)]
        yb = [m.sb("yb%d" % i, [128, 8, 512], F32) for i in range(3)]
        ob = Buf("out")
        k = 0
        for t in range(NT // 512):
            a = acc[t % 2]
            m.dma("sp", a[:], hT[:, :, t * 512:(t + 1) * 512], writes=[a])
            for e_ in range(8):
                y = yb[k % 3]
                k += 1
                m.dma("sp", y[:], ys[e_, :, :, t * 512:(t + 1) * 512], writes=[y])
                m.op("dve", lambda e: e.tensor_tensor(out=a[:], in0=a[:], in1=y[:], op=ALU.add), reads=[a, y], writes=[a])
            m.dma("sp", out[:, :, t * 512:(t + 1) * 512], a[:], reads=[a], writes=[ob])
        m.final_wait("sp", [ob])
        print("L7 instrs", m.n_ins, "dmas", m.ndma)
    return nc


U32 = mybir.dt.uint32
I32 = mybir.dt.int32
BIGPOS = 1.0e6
CB = 1024


def build_l5b():
    nc = bass.Bass("TRN2", target_bir_lowering=False)
    D = lambda n, s, dt, k: nc.dram_tensor(n, s, dt, kind=k).ap()
    gates = D("gates_all", [128, 128 * 8], F32, "ExternalInput")
    cst = D("cst", [128, 2, 128], F32, "ExternalInput")
    thr = D("thr", [128, 16], F32, "ExternalInput")
    nb_o = D("nblk", [1, 1], I32, "ExternalOutput")
    with ExitStack() as es:
        m = MK(nc, es)
        g = m.sb("g", [128, 1024], F32)
        mk_ = m.sb("mk", [128, 1024], BF16)
        cm = m.sb("cm", [128, 2, 128], BF16)
        th = m.sb("th", [128, 16], F32)
        tot = m.sb("tot", [128, 1024], F32)
        cnt = m.sb("cnt", [128, 8], F32)
        nbf = m.sb("nbf", [128, 16], F32)
        nbe = m.sb("nbe", [128, 8], F32)
        nbm = m.sb("nbm", [128, 1], F32)
        nbi = m.sb("nbi", [128, 1], I32)
        ps = [m.ps("ps%d" % i, [128, 512]) for i in range(2)]
        m.dma("sp", g[:], gates, writes=[g])
        m.dma("pool", cm[:], cst, writes=[cm])
        m.dma("sp", th[:], thr, writes=[th])
        m.op("dve", lambda e: e.tensor_scalar(out=mk_[:], in0=g[:], scalar1=0.0, scalar2=None, op0=ALU.is_gt), reads=[g], writes=[mk_])
        for h in range(2):
            m.op("pe", lambda e: e.matmul(ps[h][:, :], lhsT=cm[:, 1, :], rhs=mk_[:, h * 512:(h + 1) * 512], start=True, stop=True), reads=[cm, mk_], writes=[ps[h]])
            m.op("dve", lambda e: e.tensor_copy(out=tot[:, h * 512:(h + 1) * 512], in_=ps[h][:, :]), reads=[ps[h]], writes=[tot])
        m.op("dve", lambda e: e.tensor_reduce(out=cnt[:, :], in_=tot[:, :].rearrange("p (t e) -> p e t", e=8), axis=AX.X, op=ALU.add), reads=[tot], writes=[cnt])
        for e_ in range(8):
            m.op("dve", lambda e: e.tensor_scalar(out=nbf[:], in0=th[:], scalar1=cnt[:, e_:e_ + 1], scalar2=None, op0=ALU.is_lt), reads=[th, cnt], writes=[nbf])
            m.op("dve", lambda e: e.tensor_reduce(out=nbe[:, e_:e_ + 1], in_=nbf[:], axis=AX.X, op=ALU.add), reads=[nbf], writes=[nbe])
        m.op("dve", lambda e: e.tensor_reduce(out=nbm[:, :], in_=nbe[:, :], axis=AX.X, op=ALU.max), reads=[nbe], writes=[nbm])
        m.op("dve", lambda e: e.tensor_copy(out=nbi[:], in_=nbm[:]), reads=[nbm], writes=[nbi])
        ob = Buf("o")
        m.dma("sp", nb_o, nbi[0:1, 0:1], reads=[nbi], writes=[ob])
        m.final_wait("sp", [ob])
    return nc


def build_l6s(NBLK, F=28, NTOK=16384, skip=""):
    nc = bass.Bass("TRN2", target_bir_lowering=False)
    D = lambda n, s, dt, k: nc.dram_tensor(n, s, dt, kind=k).ap()
    NT = NTOK // 128
    CAP = NBLK * CB
    U = D("U", [NTOK, 1024], BF16, "ExternalInput")
    gate = D("gate", [128, NT], F32, "ExternalInput")
    tokid = D("tokid", [128, NT], F32, "ExternalInput")
    cst = D("cst", [128, 3, 128], F32, "ExternalInput")
    wg = D("wg", [F, 128, 1024], F32, "ExternalInput")
    wu = D("wu", [F, 128, 1024], F32, "ExternalInput")
    wd = D("wd", [F, 128, 1024], F32, "ExternalInput")
    y = D("y", [NTOK, 1024], F32, "ExternalOutput")
    Uc = D("Uc_i", [CAP, 1028], BF16, "Internal")
    with ExitStack() as es:
        m = MK(nc, es)
        g = m.sb("g", [128, NT], F32)
        cm = m.sb("cm", [128, 3, 128], BF16)
        mk_ = m.sb("mk", [128, NT], F32)
        mkb = m.sb("mkb", [128, NT], BF16)
        onesr = m.sb("onesr", [128, NT], F32)
        tot = m.sb("tot", [128, NT], F32)
        cum = m.sb("cum", [128, NT], F32)
        pos = m.sb("pos", [128, NT], F32)
        posu = m.sb("posu", [128, NT], U32)
        Wd = m.sb("Wd", [128, F, 1024], BF16)
        ps_a = m.ps("ps_a", [128, 512])
        ps_T = m.ps("ps_T", [128, 1024], BF16)
        ps_g = [m.ps("ps_g%d" % i, [128, 512]) for i in range(2)]
        ps_u = [m.ps("ps_u%d" % i, [128, 512]) for i in range(2)]
        ps_y = [m.ps("ps_y%d" % i, [128, 512]) for i in range(2)]

        aux = m.sb("aux", [128, NT, 2], F32)
        m.dma("sp", g[:], gate, writes=[g])
        m.dma("sp", aux[:, :, 0], tokid, writes=[aux], allow_slow_non_contiguous=True) if False else None
        tk = m.sb("tk", [128, NT], F32)
        m.dma("sp", tk[:], tokid, writes=[tk])
        m.op("dve", lambda e: e.tensor_copy(out=aux[:, :, 0], in_=tk[:]), reads=[tk], writes=[aux])
        m.op("dve", lambda e: e.tensor_copy(out=aux[:, :, 1], in_=g[:]), reads=[g, aux], writes=[aux])
        m.dma("pool", cm[:], cst, writes=[cm])
        m.op("dve", lambda e: e.tensor_scalar(out=mk_[:], in0=g[:], scalar1=0.0, scalar2=None, op0=ALU.is_gt), reads=[g], writes=[mk_])
        m.op("dve", lambda e: e.tensor_copy(out=mkb[:], in_=mk_[:]), reads=[mk_], writes=[mkb])
        m.op("dve", lambda e: e.memset(onesr[:], 1.0), writes=[onesr])
        m.op("pe", lambda e: e.matmul(ps_a[:, 0:NT], lhsT=cm[:, 1, :], rhs=mkb[:, :], start=True, stop=True), reads=[cm, mkb], writes=[ps_a])
        m.op("dve", lambda e: e.tensor_copy(out=tot[:], in_=ps_a[:, 0:NT]), reads=[ps_a], writes=[tot])
        m.op("pe", lambda e: e.matmul(ps_a[:, 0:NT], lhsT=cm[:, 0, :], rhs=mkb[:, :], start=True, stop=True), reads=[cm, mkb], writes=[ps_a])
        m.op("dve", lambda e: e.tensor_tensor_scan(out=cum[:], data0=onesr[:], data1=tot[:], initial=0.0, op0=ALU.mult, op1=ALU.add), reads=[onesr, tot], writes=[cum])
        m.op("dve", lambda e: e.tensor_tensor(out=pos[:], in0=cum[:], in1=tot[:], op=ALU.subtract), reads=[cum, tot], writes=[pos])
        m.op("dve", lambda e: e.tensor_tensor(out=pos[:], in0=pos[:], in1=ps_a[:, 0:NT], op=ALU.add), reads=[pos, ps_a], writes=[pos])
        m.op("dve", lambda e: e.tensor_scalar(out=pos[:], in0=pos[:], scalar1=-1.0 - BIGPOS, scalar2=None, op0=ALU.add), reads=[pos], writes=[pos])
        m.op("dve", lambda e: e.tensor_tensor(out=pos[:], in0=pos[:], in1=mk_[:], op=ALU.mult), reads=[pos, mk_], writes=[pos])
        m.op("dve", lambda e: e.tensor_scalar(out=pos[:], in0=pos[:], scalar1=BIGPOS, scalar2=None, op0=ALU.add), reads=[pos], writes=[pos])
        m.op("dve", lambda e: e.tensor_copy(out=posu[:], in_=pos[:]), reads=[pos], writes=[posu])
        for f0 in range(0, F, 4):
            m.dma("pool", Wd[:, f0:f0 + 4, :], wd[f0:f0 + 4].rearrange("f p n -> p f n"), writes=[Wd])
        bUc = Buf("Uc")
        es_main = m.es
        es_b = ExitStack()
        m.es = es_b
        NUB = 4
        utb = [m.sb("utb%d" % i, [128, 4, 1028], BF16) for i in range(NUB)]
        pre = m.sb("pre", [128, 1028], BF16)
        m.es = es_main
        m.op("dve", lambda e: e.memset(pre[:], 0.0), writes=[pre])
        m.op("dve", lambda e: e.memset(pre[:, 1024:1028].bitcast(F32)[:, 0:1], BIGPOS), reads=[pre], writes=[pre])
        for r0 in range(0, CAP, 128):
            m.dma("sp", Uc[r0:r0 + 128, :], pre[:], reads=[pre], writes=[bUc])
        for gq_ in range(1 if "B" in skip else NT // 4):
            u = utb[gq_ % NUB]
            m.dma("sp", u[:, :, 0:1024], U[gq_ * 512:(gq_ + 1) * 512, :].rearrange("(p j) n -> p j n", j=4), writes=[u])
            for j in range(4):
                m.op("dve", lambda e: e.tensor_copy(out=u[:, j, 1024:1028].bitcast(F32), in_=aux[:, 4 * gq_ + j, :]), reads=[aux, u], writes=[u])
            for j in range(4):
                m.idma(Uc, u[:, j, :], posu[:, 4 * gq_ + j:4 * gq_ + j + 1], True, CAP - 1, reads=[u, posu], writes=[bUc])
        es_b.close()
        es_c = ExitStack()
        m.es = es_c
        ut = [m.sb("ut%d" % i, [128, 1028], BF16) for i in range(3)]
        auxc = m.sb("auxc", [128, CB // 128, 2], F32)
        tokc = m.sb("tokc", [128, CB // 128], U32)
        zt = m.sb("zt", [128, 4, 1024], F32)
        uT = m.sb("uT", [128, 8, CB], BF16)
        act = m.sb("act", [128, F, CB], BF16)
        wgb = [m.sb("wg%d" % i, [128, 8, 128], BF16) for i in range(3)]
        wub = [m.sb("wu%d" % i, [128, 8, 128], BF16) for i in range(3)]
        sg = [m.sb("sg%d" % i, [128, 512], F32) for i in range(2)]
        yb = [m.sb("yb%d" % i, [128, 1024], F32) for i in range(2)]
        m.es = es_main
        by = Buf("y")
        bZ = Buf("yzero")
        m.op("dve", lambda e: e.memset(zt[:], 0.0), reads=[bUc], writes=[zt])
        for gq_ in range(NT // 4):
            m.dma("sp", y[gq_ * 512:(gq_ + 1) * 512, :].rearrange("(p j) n -> p j n", j=4), zt[:], reads=[zt], writes=[bZ])
        wc = 0
        gc = 0
        yc = 0
        for blk in range(0 if "C" in skip else NBLK):
            for tt in range(CB // 128):
                u = ut[tt % 3]
                r0 = blk * CB + tt * 128
                m.dma("sp", u[:], Uc[r0:r0 + 128, :], reads=[bUc], writes=[u])
                for c in range(8):
                    m.op("pe", lambda e: e.transpose(ps_T[:, c * 128:(c + 1) * 128], u[:, c * 128:(c + 1) * 128], cm[:, 2, :]), reads=[u, cm], writes=[ps_T])
                m.op("act", lambda e: e.activation(out=uT[:, :, tt * 128:(tt + 1) * 128], in_=ps_T[:, :].rearrange("p (c t) -> p c t", t=128), func=AF.Copy),
                     reads=[ps_T], writes=[uT])
                m.op("dve", lambda e: e.tensor_copy(out=auxc[:, tt, :], in_=u[:, 1024:1028].bitcast(F32)), reads=[u, auxc], writes=[auxc])
            m.op("dve", lambda e: e.tensor_copy(out=tokc[:, :], in_=auxc[:, :, 0]), reads=[auxc], writes=[tokc])
            for f in range(F):
                i = wc % 3
                wc += 1
                wgt, wut = wgb[i], wub[i]
                m.dma("pool", wgt[:].rearrange("p c n -> p (c n)"), wg[f], reads=[bUc], writes=[wgt])
                m.dma("pool", wut[:].rearrange("p c n -> p (c n)"), wu[f], reads=[bUc], writes=[wut])
                for ch in range(CB // 512):
                    j = gc % 2
                    gc += 1
                    G, Uu, sgb = ps_g[j], ps_u[j], sg[j]
                    for c in range(8):
                        m.op("pe", lambda e: e.matmul(G[:, :], lhsT=wgt[:, c, :], rhs=uT[:, c, ch * 512:(ch + 1) * 512], start=(c == 0), stop=(c == 7)), reads=[wgt, uT], writes=[G])
                    for c in range(8):
                        m.op("pe", lambda e: e.matmul(Uu[:, :], lhsT=wut[:, c, :], rhs=uT[:, c, ch * 512:(ch + 1) * 512], start=(c == 0), stop=(c == 7)), reads=[wut, uT], writes=[Uu])
                    m.op("act", lambda e: e.activation(out=sgb[:, :], in_=G[:, :], func=AF.Silu), reads=[G], writes=[sgb])
                    m.op("dve", lambda e: e.tensor_tensor(out=act[:, f, ch * 512:(ch + 1) * 512], in0=Uu[:, :], in1=sgb[:, :], op=ALU.mult), reads=[Uu, sgb], writes=[act])
            for tt in range(CB // 128):
                ybb = yb[tt % 2]
                for hf in range(2):
                    Y = ps_y[yc % 2]
                    yc += 1
                    for f in range(F):
                        m.op("pe", lambda e: e.matmul(Y[:, :], lhsT=act[:, f, tt * 128:(tt + 1) * 128], rhs=Wd[:, f, hf * 512:(hf + 1) * 512], start=(f == 0), stop=(f == F - 1)),
                             reads=[act, Wd], writes=[Y])
                    m.op("act", lambda e: e.activation(out=ybb[:, hf * 512:(hf + 1) * 512], in_=Y[:, :], func=AF.Copy, scale=auxc[:, tt, 1:2]),
                         reads=[Y, auxc], writes=[ybb])
                m.idma(y, ybb[:], tokc[:, tt:tt + 1], True, NTOK - 1, reads=[ybb, tokc, bZ], writes=[by])
        es_c.close()
        m.final_wait("sp", [by, bZ])
        print("L6s instrs", m.n_ins, "dmas", m.ndma, "NBLK", NBLK)
    return nc


def build_l7t(NT=2048):
    nc = bass.Bass("TRN2", target_bir_lowering=False)
    D = lambda n, s, dt, k: nc.dram_tensor(n, s, dt, kind=k).ap()
    h = D("h", [NT, 1024], F32, "ExternalInput")
    ys = D("ys", [8, NT, 1024], F32, "ExternalInput")
    out = D("out", [NT, 1024], F32, "ExternalOutput")
    with ExitStack() as es:
        m = MK(nc, es)
        acc = [m.sb("acc%d" % i, [128, 4, 1024], F32) for i in range(2)]
        yb = [m.sb("yb%d" % i, [128, 4, 1024], F32) for i in range(3)]
        ob = Buf("out")
        k = 0
        for t in range(NT // 512):
            a = acc[t % 2]
            m.dma("sp", a[:], h[t * 512:(t + 1) * 512, :].rearrange("(j p) n -> p j n", p=128), writes=[a])
            for e_ in range(8):
                yy = yb[k % 3]
                k += 1
                m.dma("sp", yy[:], ys[e_, t * 512:(t + 1) * 512, :].rearrange("(j p) n -> p j n", p=128), writes=[yy])
                m.op("dve", lambda e: e.tensor_tensor(out=a[:], in0=a[:], in1=yy[:], op=ALU.add), reads=[a, yy], writes=[a])
            m.dma("sp", out[t * 512:(t + 1) * 512, :].rearrange("(j p) n -> p j n", p=128), a[:], reads=[a], writes=[ob])
        m.final_wait("sp", [ob])
    return nc

H_NA=6; HD=64
def fm(a):
    T = a.shape[0]
    return np.ascontiguousarray(a.reshape(T, 8, 128).transpose(2, 1, 0))
def wt(w):
    K, N = w.shape
    return np.ascontiguousarray(w.reshape(K // 128, 128, N).transpose(1, 0, 2))
def gvec(g):
    return np.ascontiguousarray(g.reshape(8, 128).T)
def cmat():
    ones = np.ones((128, 128), np.float32)
    blk = np.zeros((128, 128), np.float32); blk[:64, :64] = 1; blk[64:, 64:] = 1
    perm = np.zeros((128, 128), np.float32)
    for mm in range(128):
        k = (mm // 64) * 64 + ((mm % 64) + 32) % 64
        perm[k, mm] = 1
    return np.ascontiguousarray(np.stack([ones, blk, perm], axis=1))
def cs_table(pos):
    half = 32
    inv = np.power(np.float32(10000.0), -(2.0 / 64) * np.arange(half, dtype=np.float32)).astype(np.float32)
    ang = pos.astype(np.float32)[:, None] * inv[None, :]
    cos = np.cos(ang).astype(np.float32); sin = np.sin(ang).astype(np.float32)
    d = np.arange(128) % 64
    c = cos[:, d % 32].T
    s = np.where((d < 32)[:, None], -sin[:, d % 32].T, sin[:, d % 32].T)
    return np.ascontiguousarray(np.stack([c, s], axis=1).astype(np.float32))
def gqk_table(inp, l):
    t = np.zeros((128, 8), np.float32)
    d = np.arange(128) % 64
    t[:, 0] = inp["g_qk_na"][l, 0][d]; t[:, 1] = inp["g_qk_na"][l, 1][d]
    t[:, 2] = inp["g_qk_dil"][l, 0][d]; t[:, 3] = inp["g_qk_dil"][l, 1][d]
    t[:, 4] = inp["g_qk_mem"][l, 0][d]; t[:, 5] = inp["g_qk_mem"][l, 1][d]
    return t

BF = ml_dtypes.bfloat16
def cmask_table():
    k = np.arange(128)[:, None]; q = np.arange(128)[None, :]
    out = np.zeros((128, 17, 128), np.float32)
    for j in range(17):
        off = 128 * (j - 8) + k - q
        c = np.zeros((128, 128), np.float32)
        for w, d in ((128, 1), (512, 4), (2048, 16)):
            c += ((off % d == 0) & (np.abs(off) <= w // 2)).astype(np.float32)
        out[:, j, :] = c
    return np.ascontiguousarray(out.reshape(128, 17 * 128))
def na_bias(rpb_l, p, gb):
    ws = min(max(gb - 2, 0), 59)
    k = np.arange(128); q = np.arange(128)
    out = np.full((128, 2, 5, 128), -100.0, np.float32)
    qrow = 2 * gb + q // 64; qcol = q % 64
    rstart = np.clip(qrow - 4, 0, 120); cstart = np.clip(qcol - 8, 0, 48)
    for j in range(5):
        krow = 2 * (ws + j) + k // 64; kcol = k % 64
        inr = (krow[:, None] >= rstart[None, :]) & (krow[:, None] < rstart[None, :] + 8)
        inc = (kcol[:, None] >= cstart[None, :]) & (kcol[:, None] < cstart[None, :] + 16)
        ok = inr & inc
        ri = np.clip(krow[:, None] - qrow[None, :] + 7, 0, 14); ci = np.clip(kcol[:, None] - qcol[None, :] + 15, 0, 30)
        for hh in range(2):
            vals = rpb_l[2 * p + hh][ri, ci]
            out[:, hh, j, :] = np.where(ok, vals, np.float32(-100.0))
    return out.reshape(128, 1280)
def prep_pb(core, l, inp, pa_res, h_full_T):
    b = core // 4; ci = core % 4; s0 = ci * 2048; tile0 = ci * 16
    K = np.concatenate([np.asarray(pa_res[b * 4 + c]["kT"]) for c in range(4)], axis=2)
    V = np.concatenate([np.asarray(pa_res[b * 4 + c]["v"]) for c in range(4)], axis=1)
    Kp = np.zeros((128, 6, 8192 + 2048), K.dtype); Kp[:, :, 1024:1024 + 8192] = K
    Vp = np.zeros((128, 64 + 16, 780), V.dtype); Vp[:, 8:72] = V
    r = pa_res[core]
    d = {"qT": np.asarray(r["qT"])}
    d["kA"] = np.ascontiguousarray(Kp[:, 0:3, 1024 + s0 - 256:1024 + s0 + 2304])
    d["kB"] = np.ascontiguousarray(Kp[:, 3:6, s0:s0 + 4096])
    vA = Vp[:, 8 + tile0 - 2:8 + tile0 + 18, 0:390].reshape(128, 20, 3, 130).transpose(0, 2, 1, 3)
    vB = Vp[:, tile0:tile0 + 32, 390:780].reshape(128, 32, 3, 130).transpose(0, 2, 1, 3)
    d["vA"] = np.ascontiguousarray(vA); d["vB"] = np.ascontiguousarray(vB)
    kAe = np.zeros((128, 3, 4, 640), K.dtype); vAe = np.zeros((128, 3, 4, 650), V.dtype)
    for slot, i in enumerate((0, 1, 14, 15)):
        gb = tile0 + i; ws = min(max(gb - 2, 0), 59)
        kAe[:, :, slot, :] = K[:, 0:3, ws * 128:ws * 128 + 640]
        vv = V[:, ws:ws + 5, 0:390].reshape(128, 5, 3, 130).transpose(0, 2, 1, 3)
        vAe[:, :, slot, :] = vv.reshape(128, 3, 650)
    d["kAe"] = kAe; d["vAe"] = vAe
    d["kM"] = np.asarray(r["kmT"])
    d["vM"] = np.ascontiguousarray(np.asarray(r["vm"]).reshape(128, 2, 2, 130).transpose(0, 2, 1, 3))
    nab = np.zeros((128, 3, 5, 1280), np.float32)
    for p in range(3):
        for v, i in enumerate((0, 1, 2, 14, 15)):
            nab[:, p, v, :] = na_bias(inp["rpb_na"][l], p, tile0 + i)
    d["nab"] = nab
    d["cmask"] = cmask_table()
    d["ident"] = np.eye(128, dtype=np.float32)
    d["gout"] = np.ascontiguousarray(np.broadcast_to(inp["g_out"][l][None, :], (128, 1024))).astype(np.float32)
    d["w_out"] = wt(inp["w_out"][l])
    d["hT"] = h_full_T[core]
    return d
def prep_pa(core, l, inp, hT_core):
    b = core // 4; s0 = (core % 4) * 2048
    return {"hT": hT_core, "w_in": wt(inp["w_in"][l]), "gattn": gvec(inp["g_attn"][l]), "gqk": gqk_table(inp, l), "cmat": cmat(),
            "cs": cs_table(np.arange(s0, s0 + 2048)), "memT": fm(inp["mem"][b]), "gmem": gvec(inp["g_mem"][l]), "w_kv": wt(inp["w_mem_kv"][l])}

def wgu_t(w, F):
    return np.ascontiguousarray(w.reshape(8, 128, F, 128).transpose(2, 1, 0, 3).reshape(F, 128, 1024))
def wd_t(w, F):
    return np.ascontiguousarray(w.reshape(F, 128, 8, 128).transpose(2, 1, 0, 3).reshape(8, 128, F * 128))
def unfm(aT):
    return np.ascontiguousarray(aT.transpose(2, 1, 0).reshape(aT.shape[2], 1024))

def cst3():
    tri = (np.arange(128)[:, None] <= np.arange(128)[None, :]).astype(np.float32)
    return np.ascontiguousarray(np.stack([tri, np.ones((128, 128), np.float32), np.eye(128, dtype=np.float32)], 1))

from concourse.bass_utils import run_bass_kernel_spmd
_CORES = list(range(8))
_PROGS = {}


def _prog(name, fn):
    if name not in _PROGS:
        _PROGS[name] = fn()
    return _PROGS[name]


def _run(name, fn, ims):
    nc = _prog(name, fn)
    return run_bass_kernel_spmd(nc, ims, core_ids=_CORES).results


def _attn_layer(inp, l, hTs):
    ra = _run("pa", build_pa, [prep_pa(c, l, inp, hTs[c]) for c in range(8)])
    rb = _run("pb", build_pb, [prep_pb(c, l, inp, ra, hTs) for c in range(8)])
    return [np.asarray(rb[c]["hmidT"]) for c in range(8)]


def kernel(**inp):
    inp = {k: np.asarray(v) for k, v in inp.items()}
    x = inp["x"]
    hTs = [fm(x[c // 4, (c % 4) * 2048:(c % 4 + 1) * 2048]) for c in range(8)]
    hm = _attn_layer(inp, 0, hTs)
    wg = wgu_t(inp["w_gate_dense"][0], 22); wu = wgu_t(inp["w_up_dense"][0], 22); wd = wd_t(inp["w_down_dense"][0], 22)
    cm = cmat()
    rc = _run("pc", build_pc_dense, [{"hT": hm[c], "gffn": gvec(inp["g_ffn"][0]), "cmat": cm, "wg": wg, "wu": wu, "wd": wd} for c in range(8)])
    h1 = [np.asarray(rc[c]["outT"]) for c in range(8)]
    hm1 = _attn_layer(inp, 1, h1)
    wr = wt(inp["w_router"][0])
    r5 = _run("l5", build_l5, [{"hT": hm1[c], "gffn": gvec(inp["g_ffn"][1]), "cmat": cm, "wr": wr} for c in range(8)])
    U_all = np.ascontiguousarray(np.concatenate([np.asarray(r5[c]["uT"]).transpose(2, 1, 0).reshape(2048, 1024) for c in range(8)], axis=0))
    gates = np.concatenate([np.asarray(r5[c]["gates"]).transpose(1, 0, 2).reshape(2048, 8) for c in range(8)], axis=0)
    g_lay = np.ascontiguousarray(gates.reshape(128, 128, 8).transpose(1, 0, 2).reshape(128, 1024))
    thr = np.ascontiguousarray(np.broadcast_to((1024.0 * np.arange(16, dtype=np.float32))[None, :], (128, 16)))
    c3 = cst3()
    nb = run_bass_kernel_spmd(_prog("l5b", build_l5b), [{"gates_all": g_lay, "cst": np.ascontiguousarray(c3[:, 0:2, :]), "thr": thr}], core_ids=[0]).results[0]["nblk"]
    NBLK = max(1, int(np.asarray(nb).reshape(-1)[0]))
    tokid = np.ascontiguousarray(np.arange(16384, dtype=np.float32).reshape(32, 128, 4).transpose(1, 0, 2).reshape(128, 128))
    ims = []
    for e in range(8):
        ims.append({"U": U_all, "tokid": tokid, "gate": np.ascontiguousarray(gates[:, e].reshape(32, 128, 4).transpose(1, 0, 2).reshape(128, 128)), "cst": c3,
                    "wg": wgu_t(inp["w_gate_moe"][0, e], 28), "wu": wgu_t(inp["w_up_moe"][0, e], 28),
                    "wd": np.ascontiguousarray(inp["w_down_moe"][0, e].reshape(28, 128, 1024))})
    r6 = _run("l6s_%d" % NBLK, lambda: build_l6s(NBLK), ims)
    ims = []
    for c in range(8):
        ys = np.ascontiguousarray(np.stack([np.asarray(r6[e]["y"])[c * 2048:(c + 1) * 2048] for e in range(8)]))
        ims.append({"h": unfm(hm1[c]), "ys": ys})
    r7 = _run("l7t", build_l7t, ims)
    out = np.stack([np.asarray(r7[c]["out"]) for c in range(8)]).reshape(2, 8192, 1024)
    return out.astype(np.float32)
```

```python
import numpy as np
import ml_dtypes
import numpy as np
from contextlib import ExitStack
import concourse.bass as bass
import concourse.mybir as mybir

F32 = mybir.dt.float32
BF16 = mybir.dt.bfloat16
AF = mybir.ActivationFunctionType
ALU = mybir.AluOpType
AX = mybir.AxisListType


class Buf:
    __slots__ = ("name", "w", "r", "t", "dsem")

    def __init__(self, name, t=None):
        self.name = name
        self.w = None
        self.r = []
        self.t = t
        self.dsem = None

    def __getitem__(self, idx):
        return self.t[idx]


class MK:
    SAME_ENGINE_SYNC = False

    def __init__(self, nc, es, tag=""):
        self.nc = nc
        self.es = es
        self.tag = tag
        self.eng = {"pe": nc.tensor, "act": nc.scalar, "dve": nc.vector, "pool": nc.gpsimd, "sp": nc.sync}
        self.esem = {k: es.enter_context(nc.semaphore("s_" + tag + k)) for k in self.eng}
        self.ecnt = {k: 0 for k in self.eng}
        self.waited = {k: {} for k in self.eng}
        self.dsems = []
        self.dcnt = {}
        self.ndma = 0
        self.n_ins = 0

    def sb(self, name, shape, dt):
        t = self.es.enter_context(self.nc.sbuf_tensor(name, list(shape), dt))
        return Buf(name, t)

    def ps(self, name, shape, dt=F32):
        t = self.es.enter_context(self.nc.psum_tensor(name, list(shape), dt))
        return Buf(name, t)

    def new_dsem(self, name):
        s = self.es.enter_context(self.nc.semaphore(self.tag + name))
        self.dcnt[id(s)] = [s, 0]
        return s

    def _wait(self, E, raw, other, strict=False):
        eng = self.eng[E]
        best = {}
        own = self.esem[E]
        for lst, is_raw in ((raw, True), (other, False)):
            for ev in lst:
                if ev is None:
                    continue
                sem, val = ev
                if sem is own and not self.SAME_ENGINE_SYNC:
                    if not (is_raw and (E != "pe" or strict)):
                        continue
                k = id(sem)
                if k not in best or best[k][1] < val:
                    best[k] = (sem, val)
        for k, (sem, val) in best.items():
            if self.waited[E].get(k, 0) >= val:
                continue
            eng.wait_ge(sem, val)
            self.waited[E][k] = val

    def _deps(self, reads, writes):
        raw = [b.w for b in reads]
        other = []
        for b in writes:
            other.append(b.w)
            other.extend(b.r)
        return raw, other

    def _commit(self, ev, reads, writes):
        for b in reads:
            b.r.append(ev)
            if len(b.r) > 64:
                best = {}
                for s, v in b.r:
                    if id(s) not in best or best[id(s)][1] < v:
                        best[id(s)] = (s, v)
                b.r = list(best.values())
        for b in writes:
            b.w = ev
            b.r = []

    def op(self, E, fn, reads=(), writes=(), strict=False):
        self._wait(E, *self._deps(reads, writes), strict=strict)
        ins = fn(self.eng[E])
        self.ecnt[E] += 1
        ins.then_inc(self.esem[E], 1)
        ev = (self.esem[E], self.ecnt[E])
        self._commit(ev, reads, writes)
        self.n_ins += 1
        return ins

    def dma(self, E, out, in_, reads=(), writes=(), dsem=None, **kw):
        self._wait(E, *self._deps(reads, writes))
        ins = self.eng[E].dma_start(out=out, in_=in_, **kw)
        wb = writes[0]
        if wb.dsem is None:
            wb.dsem = self.new_dsem("d_" + wb.name)
        dsem = wb.dsem
        rec = self.dcnt[id(dsem)]
        rec[1] += 16
        ins.then_inc(dsem, 16)
        ev = (dsem, rec[1])
        self._commit(ev, reads, writes)
        self.ndma += 1
        return ins

    def idma(self, out, in_, idx_ap, scatter, bound, reads=(), writes=()):
        self._wait("pool", *self._deps(reads, writes))
        if not hasattr(self, "_bregs"):
            self._bregs = {}
        if bound not in self._bregs:
            reg = self.nc.gpsimd.alloc_register("bnd%d" % len(self._bregs))
            self.nc.gpsimd.reg_mov(reg, bound)
            self._bregs[bound] = reg
        bound = self._bregs[bound]
        off = bass.IndirectOffsetOnAxis(ap=idx_ap, axis=0)
        if scatter:
            ins = self.nc.gpsimd.indirect_dma_start(out=out, out_offset=off, in_=in_, in_offset=None, bounds_check=bound, oob_is_err=False)
        else:
            ins = self.nc.gpsimd.indirect_dma_start(out=out, out_offset=None, in_=in_, in_offset=off, bounds_check=bound, oob_is_err=False)
        wb = writes[0]
        if wb.dsem is None:
            wb.dsem = self.new_dsem("d_" + wb.name)
        rec = self.dcnt[id(wb.dsem)]
        rec[1] += 16
        ins.then_inc(wb.dsem, 16)
        ev = (wb.dsem, rec[1])
        self._commit(ev, reads, writes)
        self.ndma += 1
        return ins

    def final_wait(self, E, bufs):
        self._wait(E, [b.w for b in bufs], [])


NT = 2048
CH = 512
NCH = NT // CH
EPS = 1e-6

QK_GROUPS = ([("q", j, 0 + 128 * j, 0, False) for j in range(3)]
             + [("k", j, 384 + 128 * j, 1, False) for j in range(3)]
             + [("q", 3 + j, 1152 + 128 * j, 2, True) for j in range(3)]
             + [("k", 3 + j, 1536 + 128 * j, 3, True) for j in range(3)]
             + [("q", 6 + j, 2304 + 128 * j, 4, False) for j in range(2)])


def rms_chunk(m, src, n, gvec, ones, epsb, ps_ss, sq, rs, uT, u32=None):
    m.op("act", lambda e: e.activation(out=sq[:, :, 0:n], in_=src[:, :, 0:n], func=AF.Square), reads=[src], writes=[sq])
    for c in range(8):
        m.op("pe", lambda e: e.matmul(ps_ss[:, 0:n], lhsT=ones[:, 0, :], rhs=sq[:, c, 0:n], start=(c == 0), stop=(c == 7)),
             reads=[sq, ones], writes=[ps_ss])
    m.op("act", lambda e: e.activation(out=rs[:, 0:n], in_=ps_ss[:, 0:n], func=AF.Ln, scale=1.0 / 1024, bias=epsb[:, 0:1]),
         reads=[ps_ss, epsb], writes=[rs])
    m.op("act", lambda e: e.activation(out=rs[:, 0:n], in_=rs[:, 0:n], func=AF.Exp, scale=-0.5), reads=[rs], writes=[rs])
    for c in range(8):
        m.op("dve", lambda e: e.scalar_tensor_tensor(out=uT[:, c, 0:n], in0=src[:, c, 0:n], scalar=gvec[:, c:c + 1],
                                                     in1=rs[:, 0:n], op0=ALU.mult, op1=ALU.mult),
             reads=[src, gvec, rs], writes=[uT])
        if u32 is not None:
            m.op("pool", lambda e: e.scalar_tensor_tensor(out=u32[:, c, 0:n], in0=src[:, c, 0:n], scalar=gvec[:, c:c + 1],
                                                          in1=rs[:, 0:n], op0=ALU.mult, op1=ALU.mult),
                 reads=[src, gvec, rs], writes=[u32])


def build_pa():
    nc = bass.Bass("TRN2", target_bir_lowering=False)
    D = lambda n, s, dt, k: nc.dram_tensor(n, s, dt, kind=k).ap()
    hT = D("hT", [128, 8, NT], F32, "ExternalInput")
    w_in = D("w_in", [128, 8, 2560], F32, "ExternalInput")
    gattn = D("gattn", [128, 8], F32, "ExternalInput")
    gqk = D("gqk", [128, 8], F32, "ExternalInput")
    cmat = D("cmat", [128, 3, 128], F32, "ExternalInput")
    cs = D("cs", [128, 2, NT], F32, "ExternalInput")
    memT = D("memT", [128, 8, 256], F32, "ExternalInput")
    gmem = D("gmem", [128, 8], F32, "ExternalInput")
    w_kv = D("w_kv", [128, 8, 512], F32, "ExternalInput")
    qT_o = D("qT", [128, 8, NT], BF16, "ExternalOutput")
    kT_o = D("kT", [128, 6, NT], BF16, "ExternalOutput")
    v_o = D("v", [128, 16, 12 * 65], BF16, "ExternalOutput")
    kmT_o = D("kmT", [128, 2, 256], BF16, "ExternalOutput")
    vm_o = D("vm", [128, 2, 4 * 65], BF16, "ExternalOutput")

    with ExitStack() as es:
        m = MK(nc, es)
        W = m.sb("W", [128, 8, 2560], BF16)
        Wkv = m.sb("Wkv", [128, 8, 512], BF16)
        cm = m.sb("cm", [128, 3, 128], BF16)
        ga = m.sb("ga", [128, 8], F32)
        gm = m.sb("gm", [128, 8], F32)
        gq = m.sb("gq", [128, 8], F32)
        epsb = m.sb("epsb", [128, 1], F32)
        cst = [m.sb("cst%d" % i, [128, 2, CH], F32) for i in range(2)]
        hc = [m.sb("hc%d" % i, [128, 8, CH], F32) for i in range(2)]
        sq = m.sb("sq", [128, 8, CH], BF16)
        rs = m.sb("rs", [128, CH], F32)
        uT = [m.sb("uT%d" % i, [128, 8, CH], BF16) for i in range(2)]
        sq2 = [m.sb("sq2%d" % i, [128, CH], BF16) for i in range(4)]
        r2 = [m.sb("r2%d" % i, [128, CH], F32) for i in range(4)]
        qn = [m.sb("qn%d" % i, [128, CH], BF16) for i in range(4)]
        ysb = [m.sb("ysb%d" % i, [128, CH], F32) for i in range(4)]
        t1 = [m.sb("t1%d" % i, [128, CH], F32) for i in range(4)]
        t2 = [m.sb("t2%d" % i, [128, CH], F32) for i in range(4)]
        qTs2 = [m.sb("qTs%d" % i, [128, 8, CH], BF16) for i in range(2)]
        kTs2 = [m.sb("kTs%d" % i, [128, 6, CH], BF16) for i in range(2)]
        vs2 = [m.sb("vs%d" % i, [128, 4, 12 * 65], BF16) for i in range(2)]
        kms = m.sb("kms", [128, 2, 256], BF16)
        vms = m.sb("vms", [128, 2, 4 * 65], BF16)
        ps_ss = m.ps("ps_ss", [128, 512])
        ps_y = [m.ps("ps_y%d" % i, [128, 512]) for i in range(3)]
        ps_s2 = [m.ps("ps_s2%d" % i, [128, 512]) for i in range(2)]
        ps_qp = m.ps("ps_qp", [128, 512])
        ps_v = [m.ps("ps_v%d" % i, [128, 512]) for i in range(1)]

        dW = [m.new_dsem("dW%d" % i) for i in range(4)]
        dsm = m.new_dsem("dsm")
        dh = [m.new_dsem("dh%d" % i) for i in range(2)]
        dc = [m.new_dsem("dc%d" % i) for i in range(2)]
        dout = m.new_dsem("dout")

        m.dma("sp", ga[:], gattn, writes=[ga], dsem=dsm)
        m.dma("sp", gm[:], gmem, writes=[gm], dsem=dsm)
        m.dma("sp", gq[:], gqk, writes=[gq], dsem=dsm)
        m.dma("pool", cm[:], cmat, writes=[cm], dsem=dW[0])
        m.op("dve", lambda e: e.memset(epsb[:], EPS), writes=[epsb])
        for i in range(2):
            m.op("dve", lambda e: e.memset(vs2[i][:], 1.0), writes=[vs2[i]])
        m.op("dve", lambda e: e.memset(vms[:], 1.0), writes=[vms])
        Wb = [Buf("Wb%d" % i) for i in range(4)]
        for i in range(4):
            m.dma("pool", W[:, :, 640 * i:640 * (i + 1)], w_in[:, :, 640 * i:640 * (i + 1)], writes=[Wb[i]], dsem=dW[i])
        m.dma("pool", Wkv[:], w_kv, writes=[Wkv], dsem=dW[0])

        def wbufs(c0, c1):
            return [Wb[i] for i in range(4) if c0 < 640 * (i + 1) and c1 > 640 * i]

        cnt = {"y": 0, "v": 0}

        def qk_stages(u, n, Wt, wdeps, col0, gcolumn, rope, dst, dsti, tok0, csb):
            k_ = cnt["y"]
            cnt["y"] += 1
            i = k_ % 4
            yp, s2, sq2b, r2b, qnb = ps_y[k_ % 3], ps_s2[k_ % 2], sq2[i], r2[i], qn[i]
            y = ysb[i]
            d = dst[:, dsti, tok0:tok0 + n]

            def s1():
                for c in range(8):
                    m.op("pe", lambda e: e.matmul(yp[:, 0:n], lhsT=Wt[:, c, col0:col0 + 128], rhs=u[:, c, 0:n], start=(c == 0), stop=(c == 7)),
                         reads=[u] + wdeps, writes=[yp])
                m.op("dve", lambda e: e.tensor_copy(out=y[:, 0:n], in_=yp[:, 0:n]), reads=[yp], writes=[y])
                m.op("act", lambda e: e.activation(out=sq2b[:, 0:n], in_=y[:, 0:n], func=AF.Square), reads=[y], writes=[sq2b])

            def s2f():
                m.op("pe", lambda e: e.matmul(s2[:, 0:n], lhsT=cm[:, 1, :], rhs=sq2b[:, 0:n], start=True, stop=True), reads=[sq2b, cm], writes=[s2])
                m.op("act", lambda e: e.activation(out=r2b[:, 0:n], in_=s2[:, 0:n], func=AF.Ln, scale=1.0 / 64, bias=epsb[:, 0:1]),
                     reads=[s2, epsb], writes=[r2b])
                m.op("act", lambda e: e.activation(out=r2b[:, 0:n], in_=r2b[:, 0:n], func=AF.Exp, scale=-0.5), reads=[r2b], writes=[r2b])
                if not rope:
                    m.op("dve", lambda e: e.scalar_tensor_tensor(out=d, in0=y[:, 0:n], scalar=gq[:, gcolumn:gcolumn + 1], in1=r2b[:, 0:n],
                                                                 op0=ALU.mult, op1=ALU.mult), reads=[y, gq, r2b], writes=[dst])
                else:
                    m.op("dve", lambda e: e.scalar_tensor_tensor(out=qnb[:, 0:n], in0=y[:, 0:n], scalar=gq[:, gcolumn:gcolumn + 1], in1=r2b[:, 0:n],
                                                                 op0=ALU.mult, op1=ALU.mult), reads=[y, gq, r2b], writes=[qnb])

            def s3():
                if not rope:
                    return
                t1b, t2b = t1[i], t2[i]
                m.op("pe", lambda e: e.matmul(ps_qp[:, 0:n], lhsT=cm[:, 2, :], rhs=qnb[:, 0:n], start=True, stop=True), reads=[qnb, cm], writes=[ps_qp])
                m.op("pool", lambda e: e.tensor_tensor(out=t1b[:, 0:n], in0=qnb[:, 0:n], in1=csb[:, 0, 0:n], op=ALU.mult), reads=[qnb, csb], writes=[t1b])
                m.op("dve", lambda e: e.tensor_tensor(out=t2b[:, 0:n], in0=ps_qp[:, 0:n], in1=csb[:, 1, 0:n], op=ALU.mult), reads=[ps_qp, csb], writes=[t2b])
                m.op("pool", lambda e: e.tensor_tensor(out=d, in0=t1b[:, 0:n], in1=t2b[:, 0:n], op=ALU.add), reads=[t1b, t2b], writes=[dst])
            return s1, s2f, s3

        def run_pipelined(groups, fillers=()):
            fillers = list(fillers)
            n_g = len(groups)
            groups[0][0]()
            if n_g > 1:
                groups[1][0]()
            for j in range(n_g + 1):
                if j + 2 < n_g:
                    groups[j + 2][0]()
                if j < n_g:
                    groups[j][1]()
                if fillers:
                    fillers.pop(0)()
                if j >= 1:
                    groups[j - 1][2]()
            for f in fillers:
                f()

        def qk_group(*args):
            s1, s2f, s3 = qk_stages(*args)
            s1(); s2f(); s3()

        def v_tile(u, Wt, wdeps, col0, ncols, sub, dst, tile, head0, nheads):
            cnt["v"] += 1
            pv = ps_v[0]
            for c in range(8):
                m.op("pe", lambda e: e.matmul(pv[:, 0:ncols], lhsT=u[:, c, sub * 128:(sub + 1) * 128], rhs=Wt[:, c, col0:col0 + ncols],
                                              start=(c == 0), stop=(c == 7)), reads=[u] + wdeps, writes=[pv])
            dv = dst[:, tile, head0 * 65:(head0 + nheads) * 65].rearrange("p (h d) -> p h d", d=65)[:, :, 0:64]
            sv = pv[:, 0:ncols].rearrange("p (h d) -> p h d", d=64)
            m.op("act", lambda e: e.activation(out=dv, in_=sv, func=AF.Copy), reads=[pv], writes=[dst])

        m.dma("sp", hc[0][:, :, 0:256], memT, writes=[hc[0]], dsem=dh[0])
        rms_chunk(m, hc[0], 256, gm, cm, epsb, ps_ss, sq, rs, uT[0])
        for j in range(2):
            qk_group(uT[0], 256, Wkv, [Wkv], 128 * j, 5, False, kms, j, 0, None)
        for sub in range(2):
            v_tile(uT[0], Wkv, [Wkv], 256, 256, sub, vms, sub, 0, 4)
        obm = [Buf("o1"), Buf("o2")]
        m.dma("sp", kmT_o, kms[:], reads=[kms], writes=[obm[0]], dsem=dout)
        m.dma("sp", vm_o, vms[:], reads=[vms], writes=[obm[1]], dsem=dout)

        ob = [Buf("oq"), Buf("ok"), Buf("ov")]
        def load_chunk(t):
            b = (t + 1) % 2
            m.dma("sp", hc[b][:], hT[:, :, t * CH:(t + 1) * CH], writes=[hc[b]])
            m.dma("sp", cst[b][:], cs[:, :, t * CH:(t + 1) * CH], writes=[cst[b]])

        load_chunk(0)
        for t in range(NCH):
            b = (t + 1) % 2
            tok0 = t * CH
            if t + 1 < NCH:
                load_chunk(t + 1)
            rms_chunk(m, hc[b], CH, ga, cm, epsb, ps_ss, sq, rs, uT[b])
            qTs, kTs, vs = qTs2[b], kTs2[b], vs2[b]
            groups = [qk_stages(uT[b], CH, W, wbufs(col0, col0 + 128), col0, gc, rope, qTs if dn == "q" else kTs, di, 0, cst[b])
                      for (dn, di, col0, gc, rope) in QK_GROUPS]
            fillers = []
            for sub in range(4):
                fillers.append(lambda sub=sub, b=b, vs=vs: v_tile(uT[b], W, wbufs(768, 1152), 768, 384, sub, vs, sub, 0, 6))
                fillers.append(lambda sub=sub, b=b, vs=vs: v_tile(uT[b], W, wbufs(1920, 2304), 1920, 384, sub, vs, sub, 6, 6))
            run_pipelined(groups, fillers)
            m.dma("sp", qT_o[:, :, tok0:tok0 + CH], qTs[:], reads=[qTs], writes=[ob[0]], dsem=dout)
            m.dma("sp", kT_o[:, :, tok0:tok0 + CH], kTs[:], reads=[kTs], writes=[ob[1]], dsem=dout)
            m.dma("sp", v_o[:, 4 * t:4 * t + 4, :], vs[:], reads=[vs], writes=[ob[2]], dsem=dout)
        m.final_wait("sp", ob + obm)
        print("P_A instrs", m.n_ins, "dmas", m.ndma)
    return nc


NT = 2048
EPS = 1e-6
SCALE = 0.125
NS = 6
PIPE = 4
DEBUG = False


def build_pb():
    nc = bass.Bass("TRN2", target_bir_lowering=False)
    D = lambda n, s, dt, k: nc.dram_tensor(n, s, dt, kind=k).ap()
    qT = D("qT", [128, 8, NT], BF16, "ExternalInput")
    kA = D("kA", [128, 3, 2560], BF16, "ExternalInput")
    vA = D("vA", [128, 3, 20, 130], BF16, "ExternalInput")
    kAe = D("kAe", [128, 3, 4, 640], BF16, "ExternalInput")
    vAe = D("vAe", [128, 3, 4, 650], BF16, "ExternalInput")
    kB = D("kB", [128, 3, 4096], BF16, "ExternalInput")
    vB = D("vB", [128, 3, 32, 130], BF16, "ExternalInput")
    kM = D("kM", [128, 2, 256], BF16, "ExternalInput")
    vM = D("vM", [128, 2, 2, 130], BF16, "ExternalInput")
    nab = D("nab", [128, 3, 5, 1280], F32, "ExternalInput")
    cmask = D("cmask", [128, 17 * 128], F32, "ExternalInput")
    ident = D("ident", [128, 128], F32, "ExternalInput")
    gout = D("gout", [128, 1024], F32, "ExternalInput")
    w_out = D("w_out", [128, 8, 1024], F32, "ExternalInput")
    hT = D("hT", [128, 8, NT], F32, "ExternalInput")
    hmid = D("hmidT", [128, 8, NT], F32, "ExternalOutput")
    o_dbg = D("o_dbg", [128, 16, 1024], BF16, "ExternalOutput") if DEBUG else None
    dbg2 = D("dbg2", [128, 1024], BF16, "ExternalOutput") if DEBUG else None
    dbg3 = D("dbg3", [128, 4], F32, "ExternalOutput") if DEBUG else None
    dbg4 = D("dbg4", [128, 8, 512], BF16, "ExternalOutput") if DEBUG else None

    with ExitStack() as es:
        m = MK(nc, es)
        qp = [m.sb("qp%d" % i, [128, NT], BF16) for i in range(2)]
        kb = [m.sb("kb%d" % i, [128, 4096], BF16) for i in range(2)]
        vb = [m.sb("vb%d" % i, [128, 32, 130], BF16) for i in range(2)]
        ke = [m.sb("ke0", [128, 4, 640], BF16)] * 2
        ve = [m.sb("ve0", [128, 4, 650], BF16)] * 2
        stage = [m.sb("stage%d" % i, [128, 1280], F32) for i in range(2)]
        Mna = [m.sb("Mna0", [128, 5, 1280], BF16)] * 2
        Cm = m.sb("Cm", [128, 17 * 128], BF16)
        idn = m.sb("idn", [128, 128], BF16)
        go = m.sb("go", [128, 1024], F32)
        Wo = m.sb("Wo", [128, 8, 1024], BF16)
        epsb = m.sb("epsb", [128, 1], F32)
        o_all = m.sb("o_all", [128, 16, 1024], BF16)
        Eb = [m.sb("E%d" % i, [128, 512], BF16) for i in range(NS)]
        Pb = [m.sb("P%d" % i, [128, 512], BF16) for i in range(NS)]
        rec = [m.sb("rec%d" % i, [128, 2], F32) for i in range(2)]
        sqf = m.sb("sqf", [128, 1024], F32)
        ms = [m.sb("ms%d" % i, [128, 4], F32) for i in range(2)]
        mixed = [m.sb("mixed%d" % i, [128, 1024], BF16) for i in range(2)]
        mixedT = [m.sb("mixedT%d" % i, [128, 8, 512], BF16) for i in range(2)]
        hc = [m.sb("hc0", [128, 8, 512], F32)] * 2
        es_att = ExitStack()
        es_main, m.es = m.es, es_att
        ps_s = [m.ps("ps_s%d" % i, [128, 512]) for i in range(NS)]
        ps_o = [m.ps("ps_o%d" % i, [128, 512]) for i in range(2)]
        m.es = es_main

        dl = [m.new_dsem("dl%d" % i) for i in range(2)]
        dst_ = [m.new_dsem("dst%d" % i) for i in range(2)]
        dcst = m.new_dsem("dcst")
        dh = [m.new_dsem("dh%d" % i) for i in range(2)]
        dout = m.new_dsem("dout")

        m.dma("pool", Cm[:], cmask, writes=[Cm], dsem=dcst)
        m.dma("pool", idn[:], ident, writes=[idn], dsem=dcst)
        m.dma("sp", go[:], gout, writes=[go], dsem=dcst)
        m.op("dve", lambda e: e.memset(epsb[:], EPS), writes=[epsb])
        Wo_loaded = [False]

        gcnt = [0]
        stc = [0]
        SLOT = {0: 0, 1: 1, 14: 2, 15: 3}
        VAR = {0: 0, 1: 1, 14: 3, 15: 4}

        def kind_of(p):
            return "A" if p < 3 else ("B" if p < 6 else "M")

        def load_main(p):
            b = p % 2
            kind = kind_of(p)
            m.dma("sp", qp[b][:], qT[:, p, :], writes=[qp[b]])
            if kind == "A":
                m.dma("sp", kb[b][:, 0:2560], kA[:, p, :], writes=[kb[b]])
                m.dma("sp", vb[b][:, 0:20, :], vA[:, p], writes=[vb[b]])
            elif kind == "B":
                m.dma("sp", kb[b][:], kB[:, p - 3, :], writes=[kb[b]])
                m.dma("sp", vb[b][:], vB[:, p - 3], writes=[vb[b]])
            else:
                m.dma("sp", kb[b][:, 0:256], kM[:, p - 6, :], writes=[kb[b]])
                m.dma("sp", vb[b][:, 0:2, :], vM[:, p - 6], writes=[vb[b]])

        def load_edge(p):
            b = p % 2
            m.dma("sp", ke[b][:], kAe[:, p], writes=[ke[b]])
            m.dma("sp", ve[b][:], vAe[:, p], writes=[ve[b]])
            for v in range(5):
                sb_ = stc[0] % 2
                stc[0] += 1
                m.dma("sp", stage[sb_][:], nab[:, p, v, :], writes=[stage[sb_]])
                m.op("act", lambda e: e.activation(out=Mna[b][:, v, :], in_=stage[sb_][:], func=AF.Exp), reads=[stage[sb_]], writes=[Mna[b]])

        units = []
        for p in range(8):
            kind = kind_of(p)
            b = p % 2
            for i in range(16):
                O = ps_o[i % 2]
                for hh in range(2):
                    pbs = 64 * hh
                    tiles = []
                    if kind == "A":
                        slot = SLOT.get(i)
                        var = VAR.get(i, 2)
                        for j in range(5):
                            if slot is None:
                                k_ap = kb[b][pbs:pbs + 64, (i + j) * 128:(i + j + 1) * 128]
                                v_ap = vb[b][:, i + j, hh * 65:(hh + 1) * 65]
                            else:
                                k_ap = ke[b][pbs:pbs + 64, slot, j * 128:(j + 1) * 128]
                                v_ap = ve[b][:, slot, j * 130 + hh * 65:j * 130 + (hh + 1) * 65]
                            tiles.append((k_ap, v_ap, (Mna[b], var, hh * 640 + j * 128)))
                        kdeps = [kb[b], ke[b]]
                        vdeps = [vb[b], ve[b]]
                    elif kind == "B":
                        for j in range(17):
                            k_ap = kb[b][pbs:pbs + 64, (i + j) * 128:(i + j + 1) * 128]
                            v_ap = vb[b][:, i + j, hh * 65:(hh + 1) * 65]
                            tiles.append((k_ap, v_ap, (Cm, None, j * 128)))
                        kdeps = [kb[b]]
                        vdeps = [vb[b]]
                    else:
                        for j in range(2):
                            k_ap = kb[b][pbs:pbs + 64, j * 128:(j + 1) * 128]
                            v_ap = vb[b][:, j, hh * 65:(hh + 1) * 65]
                            tiles.append((k_ap, v_ap, None))
                        kdeps = [kb[b]]
                        vdeps = [vb[b]]
                    q_ap = qp[b][pbs:pbs + 64, i * 128:(i + 1) * 128]
                    nt = len(tiles)
                    for g0 in range(0, nt, 4):
                        units.append(dict(p=p, b=b, i=i, hh=hh, O=O, grp=tiles[g0:g0 + 4], g0=g0, nt=nt, q_ap=q_ap, kdeps=kdeps, vdeps=vdeps,
                                          first_of_pair=(i == 0 and hh == 0 and g0 == 0), last_of_tile=(hh == 1 and g0 + 4 >= nt)))
        for n_, u in enumerate(units):
            u["gi"] = n_ % NS

        def emit_qk(u):
            S = ps_s[u["gi"]]
            for t, (k_ap, v_ap, mk_) in enumerate(u["grp"]):
                m.op("pe", lambda e: e.matmul(S[:, t * 128:(t + 1) * 128], lhsT=k_ap, rhs=u["q_ap"], start=True, stop=True),
                     reads=u["kdeps"] + [qp[u["b"]]], writes=[S])

        def emit_softmax(u):
            S, E, P = ps_s[u["gi"]], Eb[u["gi"]], Pb[u["gi"]]
            n = len(u["grp"]) * 128
            m.op("act", lambda e: e.activation(out=E[:, 0:n], in_=S[:, 0:n], func=AF.Exp, scale=SCALE), reads=[S], writes=[E])
            mk0 = u["grp"][0][2]
            if mk0 is not None:
                mb, var, c0 = mk0
                m_ap = mb[:, c0:c0 + n] if var is None else mb[:, var, c0:c0 + n]
                m.op("dve", lambda e: e.tensor_tensor(out=P[:, 0:n], in0=E[:, 0:n], in1=m_ap, op=ALU.mult), reads=[E, mb], writes=[P])
                u["src"] = P
            else:
                u["src"] = E

        def emit_pv(u):
            O, hh, src = u["O"], u["hh"], u["src"]
            for t, (k_ap, v_ap, mk_) in enumerate(u["grp"]):
                first = (u["g0"] + t == 0)
                last = (u["g0"] + t == u["nt"] - 1)
                m.op("pe", lambda e: e.matmul(O[:, hh * 65:(hh + 1) * 65], lhsT=src[:, t * 128:(t + 1) * 128], rhs=v_ap, start=first, stop=last),
                     reads=u["vdeps"] + [src], writes=[O])
            if u["last_of_tile"]:
                i, p = u["i"], u["p"]
                rc = rec[i % 2]
                Ov = O[:, 0:130].rearrange("p (h d) -> p h d", d=65)
                m.op("dve", lambda e: e.reciprocal(out=rc[:, 0:2], in_=Ov[:, :, 64]), reads=[O], writes=[rc])
                for h2 in range(2):
                    m.op("dve", lambda e: e.tensor_scalar(out=o_all[:, i, p * 128 + h2 * 64:p * 128 + h2 * 64 + 64], in0=O[:, h2 * 65:h2 * 65 + 64],
                                                          scalar1=rc[:, h2:h2 + 1], scalar2=None, op0=ALU.mult), reads=[O, rc], writes=[o_all])

        def pre_pair(p):
            if p + 1 < 8:
                load_main(p + 1)
            if p == 2:
                for cc in range(0, 8, 2):
                    m.dma("pool", Wo[:, cc:cc + 2, :], w_out[:, cc:cc + 2, :], writes=[Wo])
            if kind_of(p) == "A":
                load_edge(p)

        load_main(0)
        pair_units = [[u for u in units if u["p"] == p] for p in range(8)]
        for p in range(8):
            pre_pair(p)
            pu = pair_units[p]
            for u in pu[:PIPE]:
                emit_qk(u)
            for n_, u in enumerate(pu):
                if n_ + PIPE < len(pu):
                    emit_qk(pu[n_ + PIPE])
                emit_softmax(u)
                emit_pv(u)
        es_att.close()
        ps_T = m.ps("ps_T", [128, 1024], BF16)
        ps_y = [m.ps("ps_y%d" % i, [128, 512]) for i in range(2)]

        obd = Buf("odbg_out")
        if DEBUG:
            m.dma("sp", o_dbg, o_all[:], reads=[o_all], writes=[obd], dsem=dout)
        GR = [(0, 384), (384, 768), (768, 1024)]
        ob = Buf("hmid_out")
        for i in range(16):
            b = i % 2
            msb = ms[b]
            m.op("dve", lambda e: e.tensor_tensor(out=sqf[:, :], in0=o_all[:, i, :], in1=o_all[:, i, :], op=ALU.mult), reads=[o_all], writes=[sqf])
            for g, (c0, c1) in enumerate(GR):
                m.op("dve", lambda e: e.tensor_reduce(out=msb[:, g:g + 1], in_=sqf[:, c0:c1], axis=AX.X, op=ALU.add), reads=[sqf], writes=[msb])
            for g, (c0, c1) in enumerate(GR):
                m.op("act", lambda e: e.activation(out=msb[:, g:g + 1], in_=msb[:, g:g + 1], func=AF.Ln, scale=1.0 / (c1 - c0), bias=epsb[:, 0:1]),
                     reads=[msb, epsb], writes=[msb])
            m.op("act", lambda e: e.activation(out=msb[:, 0:3], in_=msb[:, 0:3], func=AF.Exp, scale=-0.5), reads=[msb], writes=[msb])
            for g, (c0, c1) in enumerate(GR):
                m.op("dve", lambda e: e.scalar_tensor_tensor(out=mixed[b][:, c0:c1], in0=o_all[:, i, c0:c1], scalar=msb[:, g:g + 1], in1=go[:, c0:c1],
                                                             op0=ALU.mult, op1=ALU.mult), reads=[o_all, msb, go], writes=[mixed[b]])
            for c in range(8):
                m.op("pe", lambda e: e.transpose(ps_T[:, c * 128:(c + 1) * 128], mixed[b][:, c * 128:(c + 1) * 128], idn[:]), reads=[mixed[b], idn], writes=[ps_T])
            cb = (i // 4) % 2
            m.op("act", lambda e: e.activation(out=mixedT[cb][:, :, (i % 4) * 128:(i % 4 + 1) * 128], in_=ps_T[:, :].rearrange("p (c t) -> p c t", t=128), func=AF.Copy),
                 reads=[ps_T], writes=[mixedT[cb]])
            if i % 4 == 3:
                ch = i // 4
                m.dma("sp", hc[cb][:], hT[:, :, ch * 512:(ch + 1) * 512], writes=[hc[cb]], dsem=dh[cb])
                for d in range(8):
                    Y = ps_y[d % 2]
                    for c in range(8):
                        m.op("pe", lambda e: e.matmul(Y[:, :], lhsT=Wo[:, c, d * 128:(d + 1) * 128], rhs=mixedT[cb][:, c, :], start=(c == 0), stop=(c == 7)),
                             reads=[Wo, mixedT[cb]], writes=[Y])
                    m.op("dve", lambda e: e.tensor_tensor(out=hc[cb][:, d, :], in0=Y[:, :], in1=hc[cb][:, d, :], op=ALU.add), reads=[Y, hc[cb]], writes=[hc[cb]])
                m.dma("sp", hmid[:, :, ch * 512:(ch + 1) * 512], hc[cb][:], reads=[hc[cb]], writes=[ob], dsem=dout)
        if DEBUG:
            m.dma("sp", dbg2, mixed[1][:], reads=[mixed[1]], writes=[obd], dsem=dout)
            m.dma("sp", dbg3, ms[1][:], reads=[ms[1]], writes=[obd], dsem=dout)
            m.dma("sp", dbg4, mixedT[1][:], reads=[mixedT[1]], writes=[obd], dsem=dout)
        m.final_wait("sp", [ob, obd] if DEBUG else [ob])
        print("P_B instrs", m.n_ins, "dmas", m.ndma)
    return nc


EPS = 1e-6


class FFNRes:
    def __init__(self, m, F, NB):
        self.m, self.F, self.NB = m, F, NB
        self.wg = [m.sb("wg%d" % i, [128, 8, 128], BF16) for i in range(3)]
        self.wu = [m.sb("wu%d" % i, [128, 8, 128], BF16) for i in range(3)]
        self.wd = [m.sb("wd%d" % i, [128, F, 128], BF16) for i in range(2)]
        self.act = m.sb("act", [128, F, NB], BF16)
        self.sg = [m.sb("sg%d" % i, [128, 512], F32) for i in range(2)]
        self.ps_g = [m.ps("ps_g%d" % i, [128, 512]) for i in range(2)]
        self.ps_u = [m.ps("ps_u%d" % i, [128, 512]) for i in range(2)]
        self.ps_y = [m.ps("ps_y%d" % i, [128, 512]) for i in range(2)]
        self.wc = 0
        self.dc = 0
        self.gc = 0
        self.yc = 0


def ffn_block(R, uT, wg_ap, wu_ap, wd_ap, sink):
    m, F, NB = R.m, R.F, R.NB
    nch = NB // 512
    for f in range(F):
        i = R.wc % 3
        R.wc += 1
        wg, wu = R.wg[i], R.wu[i]
        m.dma("pool", wg[:].rearrange("p c n -> p (c n)"), wg_ap(f), writes=[wg])
        m.dma("pool", wu[:].rearrange("p c n -> p (c n)"), wu_ap(f), writes=[wu])
        for ch in range(nch):
            j = R.gc % 2
            R.gc += 1
            G, U, sg = R.ps_g[j], R.ps_u[j], R.sg[j]
            for c in range(8):
                m.op("pe", lambda e: e.matmul(G[:, :], lhsT=wg[:, c, :], rhs=uT[:, c, ch * 512:(ch + 1) * 512], start=(c == 0), stop=(c == 7)),
                     reads=[wg, uT], writes=[G])
            for c in range(8):
                m.op("pe", lambda e: e.matmul(U[:, :], lhsT=wu[:, c, :], rhs=uT[:, c, ch * 512:(ch + 1) * 512], start=(c == 0), stop=(c == 7)),
                     reads=[wu, uT], writes=[U])
            m.op("act", lambda e: e.activation(out=sg[:, :], in_=G[:, :], func=AF.Silu), reads=[G], writes=[sg])
            m.op("dve", lambda e: e.tensor_tensor(out=R.act[:, f, ch * 512:(ch + 1) * 512], in0=U[:, :], in1=sg[:, :], op=ALU.mult),
                 reads=[U, sg], writes=[R.act])
    for d in range(8):
        i = R.dc % 2
        R.dc += 1
        wd = R.wd[i]
        m.dma("pool", wd[:].rearrange("p f n -> p (f n)"), wd_ap(d), writes=[wd])
        for ch in range(nch):
            Y = R.ps_y[R.yc % 2]
            R.yc += 1
            for f in range(F):
                m.op("pe", lambda e: e.matmul(Y[:, :], lhsT=wd[:, f, :], rhs=R.act[:, f, ch * 512:(ch + 1) * 512], start=(f == 0), stop=(f == F - 1)),
                     reads=[wd, R.act], writes=[Y])
            sink(d, ch, Y)


def build_pc_dense(F=22, NT=2048, NB=1024):
    nc = bass.Bass("TRN2", target_bir_lowering=False)
    D = lambda n, s, dt, k: nc.dram_tensor(n, s, dt, kind=k).ap()
    hT = D("hT", [128, 8, NT], F32, "ExternalInput")
    gffn = D("gffn", [128, 8], F32, "ExternalInput")
    cmat = D("cmat", [128, 3, 128], F32, "ExternalInput")
    wg = D("wg", [F, 128, 1024], F32, "ExternalInput")
    wu = D("wu", [F, 128, 1024], F32, "ExternalInput")
    wd = D("wd", [8, 128, F * 128], F32, "ExternalInput")
    out = D("outT", [128, 8, NT], F32, "ExternalOutput")
    with ExitStack() as es:
        m = MK(nc, es)
        R = FFNRes(m, F, NB)
        cm = m.sb("cm", [128, 3, 128], BF16)
        gf = m.sb("gf", [128, 8], F32)
        epsb = m.sb("epsb", [128, 1], F32)
        hbs = [m.sb("hb%d" % i, [128, 8, NB], F32) for i in range(2)]
        uT = m.sb("uT", [128, 8, NB], BF16)
        sq = m.sb("sq", [128, 8, 512], BF16)
        rs = m.sb("rs", [128, 512], F32)
        ps_ss = m.ps("ps_ss", [128, 512])
        m.dma("pool", cm[:], cmat, writes=[cm])
        m.dma("sp", gf[:], gffn, writes=[gf])
        m.op("dve", lambda e: e.memset(epsb[:], EPS), writes=[epsb])
        ob = Buf("out")

        def load_half(i):
            for ch in range(NB // 512):
                m.dma("sp", hbs[i % 2][:, :, ch * 512:(ch + 1) * 512], hT[:, :, i * NB + ch * 512:i * NB + (ch + 1) * 512], writes=[hbs[i % 2]])

        load_half(0)
        for hb_i in range(NT // NB):
            t0 = hb_i * NB
            hb = hbs[hb_i % 2]
            if hb_i + 1 < NT // NB:
                load_half(hb_i + 1)
            for ch in range(NB // 512):
                rms_view(m, hb, ch * 512, 512, gf, cm, epsb, ps_ss, sq, rs, uT)

            def sink(d, ch, Y, hb=hb):
                m.op("dve", lambda e: e.tensor_tensor(out=hb[:, d, ch * 512:(ch + 1) * 512], in0=Y[:, :], in1=hb[:, d, ch * 512:(ch + 1) * 512], op=ALU.add),
                     reads=[Y, hb], writes=[hb])
            ffn_block(R, uT, lambda f: wg[f], lambda f: wu[f], lambda d: wd[d], sink)
            m.dma("sp", out[:, :, t0:t0 + NB], hb[:], reads=[hb], writes=[ob])
        m.final_wait("sp", [ob])
        print("P_C instrs", m.n_ins, "dmas", m.ndma)
    return nc


def rms_view(m, src, c0, n, gvec, cm, epsb, ps_ss, sq, rs, uT, u32=None):
    m.op("act", lambda e: e.activation(out=sq[:, :, 0:n], in_=src[:, :, c0:c0 + n], func=AF.Square), reads=[src], writes=[sq])
    for c in range(8):
        m.op("pe", lambda e: e.matmul(ps_ss[:, 0:n], lhsT=cm[:, 0, :], rhs=sq[:, c, 0:n], start=(c == 0), stop=(c == 7)), reads=[sq, cm], writes=[ps_ss])
    m.op("act", lambda e: e.activation(out=rs[:, 0:n], in_=ps_ss[:, 0:n], func=AF.Ln, scale=1.0 / 1024, bias=epsb[:, 0:1]), reads=[ps_ss, epsb], writes=[rs])
    m.op("act", lambda e: e.activation(out=rs[:, 0:n], in_=rs[:, 0:n], func=AF.Exp, scale=-0.5), reads=[rs], writes=[rs])
    for c in range(8):
        m.op("dve", lambda e: e.scalar_tensor_tensor(out=uT[:, c, c0:c0 + n], in0=src[:, c, c0:c0 + n], scalar=gvec[:, c:c + 1], in1=rs[:, 0:n],
                                                     op0=ALU.mult, op1=ALU.mult), reads=[src, gvec, rs], writes=[uT])
        if u32 is not None:
            m.op("dve", lambda e: e.scalar_tensor_tensor(out=u32[:, c, 0:n], in0=src[:, c, c0:c0 + n], scalar=gvec[:, c:c + 1], in1=rs[:, 0:n],
                                                         op0=ALU.mult, op1=ALU.mult), reads=[src, gvec, rs], writes=[u32])


def build_l5(NT=2048):
    nc = bass.Bass("TRN2", target_bir_lowering=False)
    D = lambda n, s, dt, k: nc.dram_tensor(n, s, dt, kind=k).ap()
    hT = D("hT", [128, 8, NT], F32, "ExternalInput")
    gffn = D("gffn", [128, 8], F32, "ExternalInput")
    cmat = D("cmat", [128, 3, 128], F32, "ExternalInput")
    wr = D("wr", [128, 8, 8], F32, "ExternalInput")
    uT_o = D("uT", [128, 8, NT], BF16, "ExternalOutput")
    g_o = D("gates", [128, NT // 128, 8], F32, "ExternalOutput")
    with ExitStack() as es:
        m = MK(nc, es)
        cm = m.sb("cm", [128, 3, 128], BF16)
        gf = m.sb("gf", [128, 8], F32)
        wrs = m.sb("wrs", [128, 8, 8], F32)
        epsb = m.sb("epsb", [128, 1], F32)
        hb = [m.sb("hb%d" % i, [128, 8, 512], F32) for i in range(2)]
        uT = [m.sb("uT%d" % i, [128, 8, 512], BF16) for i in range(2)]
        u32 = m.sb("u32", [128, 8, 512], F32)
        sq = m.sb("sq", [128, 8, 512], BF16)
        rs = m.sb("rs", [128, 512], F32)
        gates = m.sb("gates_sb", [128, NT // 128, 8], F32)
        lg = [m.sb("lg%d" % i, [128, 8], F32) for i in range(2)]
        m8 = [m.sb("m8%d" % i, [128, 8], F32) for i in range(2)]
        tt = [m.sb("tt%d" % i, [128, 4], F32) for i in range(2)]
        ga = [m.sb("ga%d" % i, [128, 8], F32) for i in range(2)]
        ps_ss = m.ps("ps_ss", [128, 512])
        ps_l = [m.ps("ps_l%d" % i, [128, 8]) for i in range(2)]
        m.dma("pool", cm[:], cmat, writes=[cm])
        m.dma("sp", gf[:], gffn, writes=[gf])
        m.dma("sp", wrs[:], wr, writes=[wrs])
        m.op("dve", lambda e: e.memset(epsb[:], EPS), writes=[epsb])
        ob = Buf("out")
        k = 0
        for t in range(NT // 512):
            b = t % 2
            m.dma("sp", hb[b][:], hT[:, :, t * 512:(t + 1) * 512], writes=[hb[b]])
            rms_view(m, hb[b], 0, 512, gf, cm, epsb, ps_ss, sq, rs, uT[b], u32=u32)
            m.dma("sp", uT_o[:, :, t * 512:(t + 1) * 512], uT[b][:], reads=[uT[b]], writes=[ob])
            for s in range(4):
                j = k % 2
                k += 1
                L, lgb, m8b, tb, gab = ps_l[j], lg[j], m8[j], tt[j], ga[j]
                for c in range(8):
                    m.op("pe", lambda e: e.matmul(L[:, :], lhsT=u32[:, c, s * 128:(s + 1) * 128], rhs=wrs[:, c, :], start=(c == 0), stop=(c == 7)),
                         reads=[u32, wrs], writes=[L])
                m.op("dve", lambda e: e.tensor_copy(out=lgb[:, :], in_=L[:, :]), reads=[L], writes=[lgb])
                m.op("dve", lambda e: e.max(out=m8b[:, :], in_=lgb[:, :]), reads=[lgb], writes=[m8b])
                m.op("dve", lambda e: e.tensor_tensor(out=tb[:, 0:1], in0=m8b[:, 1:2], in1=m8b[:, 0:1], op=ALU.subtract), reads=[m8b], writes=[tb])
                m.op("act", lambda e: e.activation(out=tb[:, 1:2], in_=tb[:, 0:1], func=AF.Exp), reads=[tb], writes=[tb])
                m.op("dve", lambda e: e.tensor_scalar(out=tb[:, 2:3], in0=tb[:, 1:2], scalar1=1.0, scalar2=None, op0=ALU.add), reads=[tb], writes=[tb])
                m.op("dve", lambda e: e.reciprocal(out=tb[:, 2:3], in_=tb[:, 2:3]), reads=[tb], writes=[tb])
                m.op("dve", lambda e: e.tensor_tensor(out=tb[:, 3:4], in0=tb[:, 1:2], in1=tb[:, 2:3], op=ALU.mult), reads=[tb], writes=[tb])
                m.op("dve", lambda e: e.tensor_scalar(out=gab[:, :], in0=lgb[:, :], scalar1=m8b[:, 0:1], scalar2=tb[:, 2:3], op0=ALU.is_equal, op1=ALU.mult),
                     reads=[lgb, m8b, tb], writes=[gab])
                m.op("dve", lambda e: e.tensor_scalar(out=lgb[:, :], in0=lgb[:, :], scalar1=m8b[:, 1:2], scalar2=tb[:, 3:4], op0=ALU.is_equal, op1=ALU.mult),
                     reads=[lgb, m8b, tb], writes=[lgb])
                m.op("dve", lambda e: e.tensor_tensor(out=gates[:, t * 4 + s, :], in0=gab[:, :], in1=lgb[:, :], op=ALU.add), reads=[gab, lgb], writes=[gates])
        ob2 = Buf("out2")
        m.dma("sp", g_o, gates[:], reads=[gates], writes=[ob2])
        m.final_wait("sp", [ob, ob2])
        print("L5 instrs", m.n_ins, "dmas", m.ndma)
    return nc


def build_l6(F=28, NTOK=16384, NB=1024):
    nc = bass.Bass("TRN2", target_bir_lowering=False)
    D = lambda n, s, dt, k: nc.dram_tensor(n, s, dt, kind=k).ap()
    uT_i = D("uT", [128, 8, NTOK], BF16, "ExternalInput")
    gbc_i = D("gbc", [128, NTOK], F32, "ExternalInput")
    wg = D("wg", [F, 128, 1024], F32, "ExternalInput")
    wu = D("wu", [F, 128, 1024], F32, "ExternalInput")
    wd = D("wd", [8, 128, F * 128], F32, "ExternalInput")
    y_o = D("yT", [128, 8, NTOK], F32, "ExternalOutput")
    with ExitStack() as es:
        m = MK(nc, es)
        R = FFNRes(m, F, NB)
        uT = [m.sb("uT%d" % i, [128, 8, NB], BF16) for i in range(2)]
        gb = [m.sb("gb%d" % i, [128, NB], F32) for i in range(2)]
        yb = m.sb("yb", [128, 8, NB], F32)
        ob = Buf("out")
        for tb in range(NTOK // NB):
            b = tb % 2
            t0 = tb * NB
            m.dma("sp", uT[b][:], uT_i[:, :, t0:t0 + NB], writes=[uT[b]])
            m.dma("sp", gb[b][:], gbc_i[:, t0:t0 + NB], writes=[gb[b]])

            def sink(d, ch, Y):
                m.op("dve", lambda e: e.tensor_tensor(out=yb[:, d, ch * 512:(ch + 1) * 512], in0=Y[:, :], in1=gb[b][:, ch * 512:(ch + 1) * 512], op=ALU.mult),
                     reads=[Y, gb[b]], writes=[yb])
            ffn_block(R, uT[b], lambda f: wg[f], lambda f: wu[f], lambda d: wd[d], sink)
            m.dma("sp", y_o[:, :, t0:t0 + NB], yb[:], reads=[yb], writes=[ob])
        m.final_wait("sp", [ob])
        print("L6 instrs", m.n_ins, "dmas", m.ndma)
    return nc


def build_l7(NT=2048):
    nc = bass.Bass("TRN2", target_bir_lowering=False)
    D = lambda n, s, dt, k: nc.dram_tensor(n, s, dt, kind=k).ap()
    hT = D("hT", [128, 8, NT], F32, "ExternalInput")
    ys = D("ys", [8, 128, 8, NT], F32, "ExternalInput")
    out = D("outT", [128, 8, NT], F32, "ExternalOutput")
    with ExitStack() as es:
        m = MK(nc, es)
        acc = [m.sb("acc%d" % i, [128, 8, 512], F32) for i in range(2)]
        yb = [m.sb("yb%d" % i, [128, 8, 512], F32) for i in range(3)]
        ob = Buf("out")
        k = 0
        for t in range(NT // 512):
            a = acc[t % 2]
            m.dma("sp", a[:], hT[:, :, t * 512:(t + 1) * 512], writes=[a])
            for e_ in range(8):
                y = yb[k % 3]
                k += 1
                m.dma("sp", y[:], ys[e_, :, :, t * 512:(t + 1) * 512], writes=[y])
                m.op("dve", lambda e: e.tensor_tensor(out=a[:], in0=a[:], in1=y[:], op=ALU.add), reads=[a, y], writes=[a])
            m.dma("sp", out[:, :, t * 512:(t + 1) * 512], a[:], reads=[a], writes=[ob])
        m.final_wait("sp", [ob])
        print("L7 instrs", m.n_ins, "dmas", m.ndma)
    return nc


U32 = mybir.dt.uint32
I32 = mybir.dt.int32
BIGPOS = 1.0e6
CB = 1024


def build_l5b():
    nc = bass.Bass("TRN2", target_bir_lowering=False)
    D = lambda n, s, dt, k: nc.dram_tensor(n, s, dt, kind=k).ap()
    gates = D("gates_all", [128, 128 * 8], F32, "ExternalInput")
    cst = D("cst", [128, 2, 128], F32, "ExternalInput")
    thr = D("thr", [128, 16], F32, "ExternalInput")
    nb_o = D("nblk", [1, 1], I32, "ExternalOutput")
    with ExitStack() as es:
        m = MK(nc, es)
        g = m.sb("g", [128, 1024], F32)
        mk_ = m.sb("mk", [128, 1024], BF16)
        cm = m.sb("cm", [128, 2, 128], BF16)
        th = m.sb("th", [128, 16], F32)
        tot = m.sb("tot", [128, 1024], F32)
        cnt = m.sb("cnt", [128, 8], F32)
        nbf = m.sb("nbf", [128, 16], F32)
        nbe = m.sb("nbe", [128, 8], F32)
        nbm = m.sb("nbm", [128, 1], F32)
        nbi = m.sb("nbi", [128, 1], I32)
        ps = [m.ps("ps%d" % i, [128, 512]) for i in range(2)]
        m.dma("sp", g[:], gates, writes=[g])
        m.dma("pool", cm[:], cst, writes=[cm])
        m.dma("sp", th[:], thr, writes=[th])
        m.op("dve", lambda e: e.tensor_scalar(out=mk_[:], in0=g[:], scalar1=0.0, scalar2=None, op0=ALU.is_gt), reads=[g], writes=[mk_])
        for h in range(2):
            m.op("pe", lambda e: e.matmul(ps[h][:, :], lhsT=cm[:, 1, :], rhs=mk_[:, h * 512:(h + 1) * 512], start=True, stop=True), reads=[cm, mk_], writes=[ps[h]])
            m.op("dve", lambda e: e.tensor_copy(out=tot[:, h * 512:(h + 1) * 512], in_=ps[h][:, :]), reads=[ps[h]], writes=[tot])
        m.op("dve", lambda e: e.tensor_reduce(out=cnt[:, :], in_=tot[:, :].rearrange("p (t e) -> p e t", e=8), axis=AX.X, op=ALU.add), reads=[tot], writes=[cnt])
        for e_ in range(8):
            m.op("dve", lambda e: e.tensor_scalar(out=nbf[:], in0=th[:], scalar1=cnt[:, e_:e_ + 1], scalar2=None, op0=ALU.is_lt), reads=[th, cnt], writes=[nbf])
            m.op("dve", lambda e: e.tensor_reduce(out=nbe[:, e_:e_ + 1], in_=nbf[:], axis=AX.X, op=ALU.add), reads=[nbf], writes=[nbe])
        m.op("dve", lambda e: e.tensor_reduce(out=nbm[:, :], in_=nbe[:, :], axis=AX.X, op=ALU.max), reads=[nbe], writes=[nbm])
        m.op("dve", lambda e: e.tensor_copy(out=nbi[:], in_=nbm[:]), reads=[nbm], writes=[nbi])
        ob = Buf("o")
        m.dma("sp", nb_o, nbi[0:1, 0:1], reads=[nbi], writes=[ob])
        m.final_wait("sp", [ob])
    return nc


def build_l6s(NBLK, F=28, NTOK=16384, skip=""):
    nc = bass.Bass("TRN2", target_bir_lowering=False)
    D = lambda n, s, dt, k: nc.dram_tensor(n, s, dt, kind=k).ap()
    NT = NTOK // 128
    CAP = NBLK * CB
    U = D("U", [NTOK, 1024], BF16, "ExternalInput")
    gate = D("gate", [128, NT], F32, "ExternalInput")
    tokid = D("tokid", [128, NT], F32, "ExternalInput")
    rowid = D("rowid", [128, 128], F32, "ExternalInput")
    cst = D("cst", [128, 3, 128], F32, "ExternalInput")
    wg = D("wg", [F, 128, 1024], F32, "ExternalInput")
    wu = D("wu", [F, 128, 1024], F32, "ExternalInput")
    wd = D("wd", [F, 128, 1024], F32, "ExternalInput")
    y = D("y", [NTOK, 1024], F32, "ExternalOutput")
    Uc = D("Uc_i", [CAP, 1028], BF16, "Internal")
    with ExitStack() as es:
        m = MK(nc, es)
        g = m.sb("g", [128, NT], F32)
        cm = m.sb("cm", [128, 3, 128], BF16)
        mk_ = m.sb("mk", [128, NT], F32)
        mkb = m.sb("mkb", [128, NT], BF16)
        onesr = m.sb("onesr", [128, NT], F32)
        tot = m.sb("tot", [128, NT], F32)
        cum = m.sb("cum", [128, NT], F32)
        pos = m.sb("pos", [128, NT], F32)
        posu = m.sb("posu", [128, NT], U32)
        Wd = m.sb("Wd", [128, F, 1024], BF16)
        ps_a = m.ps("ps_a", [128, 512])
        ps_T = m.ps("ps_T", [128, 1024], BF16)
        ps_g = [m.ps("ps_g%d" % i, [128, 512]) for i in range(2)]
        ps_u = [m.ps("ps_u%d" % i, [128, 512]) for i in range(2)]
        ps_y = [m.ps("ps_y%d" % i, [128, 512]) for i in range(2)]

        aux = m.sb("aux", [128, NT, 2], F32)
        zt = m.sb("zt", [128, 4, 1024], F32)
        by = Buf("y")
        bZ = Buf("yzero")
        m.op("dve", lambda e: e.memset(zt[:], 0.0), writes=[zt])
        m.dma("sp", g[:], gate, writes=[g])
        rid = m.sb("rid", [128, 128], F32)
        m.dma("sp", rid[:], rowid, writes=[rid])
        m.dma("sp", aux[:, :, 0], tokid, writes=[aux], allow_slow_non_contiguous=True) if False else None
        tk = m.sb("tk", [128, NT], F32)
        m.dma("sp", tk[:], tokid, writes=[tk])
        m.op("dve", lambda e: e.tensor_copy(out=aux[:, :, 0], in_=tk[:]), reads=[tk], writes=[aux])
        m.op("dve", lambda e: e.tensor_copy(out=aux[:, :, 1], in_=g[:]), reads=[g, aux], writes=[aux])
        m.dma("pool", cm[:], cst, writes=[cm])
        m.op("dve", lambda e: e.tensor_scalar(out=mk_[:], in0=g[:], scalar1=0.0, scalar2=None, op0=ALU.is_gt), reads=[g], writes=[mk_])
        m.op("dve", lambda e: e.tensor_copy(out=mkb[:], in_=mk_[:]), reads=[mk_], writes=[mkb])
        m.op("dve", lambda e: e.memset(onesr[:], 1.0), writes=[onesr])
        m.op("pe", lambda e: e.matmul(ps_a[:, 0:NT], lhsT=cm[:, 1, :], rhs=mkb[:, :], start=True, stop=True), reads=[cm, mkb], writes=[ps_a])
        m.op("dve", lambda e: e.tensor_copy(out=tot[:], in_=ps_a[:, 0:NT]), reads=[ps_a], writes=[tot])
        m.op("pe", lambda e: e.matmul(ps_a[:, 0:NT], lhsT=cm[:, 0, :], rhs=mkb[:, :], start=True, stop=True), reads=[cm, mkb], writes=[ps_a])
        m.op("dve", lambda e: e.tensor_tensor_scan(out=cum[:], data0=onesr[:], data1=tot[:], initial=0.0, op0=ALU.mult, op1=ALU.add), reads=[onesr, tot], writes=[cum])
        m.op("dve", lambda e: e.tensor_tensor(out=pos[:], in0=cum[:], in1=tot[:], op=ALU.subtract), reads=[cum, tot], writes=[pos])
        m.op("dve", lambda e: e.tensor_tensor(out=pos[:], in0=pos[:], in1=ps_a[:, 0:NT], op=ALU.add), reads=[pos, ps_a], writes=[pos])
        m.op("dve", lambda e: e.tensor_scalar(out=pos[:], in0=pos[:], scalar1=-1.0 - BIGPOS, scalar2=None, op0=ALU.add), reads=[pos], writes=[pos])
        m.op("dve", lambda e: e.tensor_tensor(out=pos[:], in0=pos[:], in1=mk_[:], op=ALU.mult), reads=[pos, mk_], writes=[pos])
        m.op("dve", lambda e: e.tensor_scalar(out=pos[:], in0=pos[:], scalar1=BIGPOS, scalar2=None, op0=ALU.add), reads=[pos], writes=[pos])
        m.op("dve", lambda e: e.tensor_copy(out=posu[:], in_=pos[:]), reads=[pos], writes=[posu])
        bUc = Buf("Uc")
        es_main = m.es
        es_b = ExitStack()
        m.es = es_b
        NUB = 4
        utb = [m.sb("utb%d" % i, [128, 4, 1028], BF16) for i in range(NUB)]
        m.es = es_main
        for gq_ in range(1 if "B" in skip else NT // 4):
            u = utb[gq_ % NUB]
            m.dma("sp", u[:, :, 0:1024], U[gq_ * 512:(gq_ + 1) * 512, :].rearrange("(p j) n -> p j n", j=4), writes=[u])
            for j in range(4):
                m.op("dve", lambda e: e.tensor_copy(out=u[:, j, 1024:1028].bitcast(F32), in_=aux[:, 4 * gq_ + j, :]), reads=[aux, u], writes=[u])
            for j in range(4):
                m.idma(Uc, u[:, j, :], posu[:, 4 * gq_ + j:4 * gq_ + j + 1], True, CAP - 1, reads=[u, posu], writes=[bUc])
        es_b.close()
        for gq_ in range(NT // 4):
            m.dma("act", y[gq_ * 512:(gq_ + 1) * 512, :].rearrange("(p j) n -> p j n", j=4), zt[:], reads=[zt, bUc], writes=[bZ])
        for f0 in range(0, F, 4):
            m.dma("pool", Wd[:, f0:f0 + 4, :], wd[f0:f0 + 4].rearrange("f p n -> p f n"), writes=[Wd])
        es_c = ExitStack()
        m.es = es_c
        ut = [m.sb("ut%d" % i, [128, 1028], BF16) for i in range(3)]
        auxc = m.sb("auxc", [128, CB // 128, 2], F32)
        tokc = m.sb("tokc", [128, CB // 128], U32)
        tokf = m.sb("tokf", [128, CB // 128], F32)
        vmf = m.sb("vmf", [128, CB // 128], F32)
        vmu = m.sb("vmu", [128, CB // 128], U32)
        uT = m.sb("uT", [128, 8, CB], BF16)
        act = m.sb("act", [128, F, CB], BF16)
        wgb = [m.sb("wg%d" % i, [128, 8, 128], BF16) for i in range(3)]
        wub = [m.sb("wu%d" % i, [128, 8, 128], BF16) for i in range(3)]
        sg = [m.sb("sg%d" % i, [128, 512], F32) for i in range(2)]
        yb = [m.sb("yb%d" % i, [128, 1024], F32) for i in range(2)]
        m.es = es_main
        wc = 0
        gc = 0
        yc = 0
        for blk in range(0 if "C" in skip else NBLK):
            for tt in range(CB // 128):
                u = ut[tt % 3]
                r0 = blk * CB + tt * 128
                m.dma("sp", u[:], Uc[r0:r0 + 128, :], reads=[bUc], writes=[u])
                for c in range(8):
                    m.op("pe", lambda e: e.transpose(ps_T[:, c * 128:(c + 1) * 128], u[:, c * 128:(c + 1) * 128], cm[:, 2, :]), reads=[u, cm], writes=[ps_T])
                m.op("act", lambda e: e.activation(out=uT[:, :, tt * 128:(tt + 1) * 128], in_=ps_T[:, :].rearrange("p (c t) -> p c t", t=128), func=AF.Copy),
                     reads=[ps_T], writes=[uT])
                m.op("dve", lambda e: e.tensor_copy(out=auxc[:, tt, :], in_=u[:, 1024:1028].bitcast(F32)), reads=[u, auxc], writes=[auxc])
            nt_ = CB // 128
            m.op("dve", lambda e: e.tensor_scalar(out=vmf[:, :], in0=rid[:, blk * nt_:(blk + 1) * nt_], scalar1=cum[:, NT - 1:NT], scalar2=None, op0=ALU.is_lt),
                 reads=[rid, cum], writes=[vmf])
            m.op("dve", lambda e: e.tensor_copy(out=vmu[:, :], in_=vmf[:, :]), reads=[vmf], writes=[vmu])
            m.op("dve", lambda e: e.memset(tokf[:, :], BIGPOS), writes=[tokf])
            m.op("dve", lambda e: e.copy_predicated(out=tokf[:, :], mask=vmu[:, :], data=auxc[:, :, 0]), reads=[vmu, auxc, tokf], writes=[tokf])
            m.op("dve", lambda e: e.tensor_copy(out=tokc[:, :], in_=tokf[:, :]), reads=[tokf], writes=[tokc])
            for f in range(F):
                i = wc % 3
                wc += 1
                wgt, wut = wgb[i], wub[i]
                m.dma("pool", wgt[:].rearrange("p c n -> p (c n)"), wg[f], reads=[bUc], writes=[wgt])
                m.dma("pool", wut[:].rearrange("p c n -> p (c n)"), wu[f], reads=[bUc], writes=[wut])
                for ch in range(CB // 512):
                    j = gc % 2
                    gc += 1
                    G, Uu, sgb = ps_g[j], ps_u[j], sg[j]
                    for c in range(8):
                        m.op("pe", lambda e: e.matmul(G[:, :], lhsT=wgt[:, c, :], rhs=uT[:, c, ch * 512:(ch + 1) * 512], start=(c == 0), stop=(c == 7)), reads=[wgt, uT], writes=[G])
                    for c in range(8):
                        m.op("pe", lambda e: e.matmul(Uu[:, :], lhsT=wut[:, c, :], rhs=uT[:, c, ch * 512:(ch + 1) * 512], start=(c == 0), stop=(c == 7)), reads=[wut, uT], writes=[Uu])
                    m.op("act", lambda e: e.activation(out=sgb[:, :], in_=G[:, :], func=AF.Silu), reads=[G], writes=[sgb])
                    m.op("dve", lambda e: e.tensor_tensor(out=act[:, f, ch * 512:(ch + 1) * 512], in0=Uu[:, :], in1=sgb[:, :], op=ALU.mult), reads=[Uu, sgb], writes=[act])
            for tt in range(CB // 128):
                ybb = yb[tt % 2]
                for hf in range(2):
                    Y = ps_y[yc % 2]
                    yc += 1
                    for f in range(F):
                        m.op("pe", lambda e: e.matmul(Y[:, :], lhsT=act[:, f, tt * 128:(tt + 1) * 128], rhs=Wd[:, f, hf * 512:(hf + 1) * 512], start=(f == 0), stop=(f == F - 1)),
                             reads=[act, Wd], writes=[Y])
                    m.op("act", lambda e: e.activation(out=ybb[:, hf * 512:(hf + 1) * 512], in_=Y[:, :], func=AF.Copy, scale=auxc[:, tt, 1:2]),
                         reads=[Y, auxc], writes=[ybb])
                m.idma(y, ybb[:], tokc[:, tt:tt + 1], True, NTOK - 1, reads=[ybb, tokc, bZ], writes=[by])
        es_c.close()
        m.final_wait("sp", [by, bZ])
        print("L6s instrs", m.n_ins, "dmas", m.ndma, "NBLK", NBLK)
    return nc


def build_l7t(NT=2048):
    nc = bass.Bass("TRN2", target_bir_lowering=False)
    D = lambda n, s, dt, k: nc.dram_tensor(n, s, dt, kind=k).ap()
    h = D("h", [NT, 1024], F32, "ExternalInput")
    ys = D("ys", [8, NT, 1024], F32, "ExternalInput")
    out = D("out", [NT, 1024], F32, "ExternalOutput")
    with ExitStack() as es:
        m = MK(nc, es)
        acc = [m.sb("acc%d" % i, [128, 4, 1024], F32) for i in range(2)]
        yb = [m.sb("yb%d" % i, [128, 4, 1024], F32) for i in range(3)]
        ob = Buf("out")
        k = 0
        for t in range(NT // 512):
            a = acc[t % 2]
            m.dma("sp", a[:], h[t * 512:(t + 1) * 512, :].rearrange("(j p) n -> p j n", p=128), writes=[a])
            for e_ in range(8):
                yy = yb[k % 3]
                k += 1
                m.dma("sp", yy[:], ys[e_, t * 512:(t + 1) * 512, :].rearrange("(j p) n -> p j n", p=128), writes=[yy])
                m.op("dve", lambda e: e.tensor_tensor(out=a[:], in0=a[:], in1=yy[:], op=ALU.add), reads=[a, yy], writes=[a])
            m.dma("sp", out[t * 512:(t + 1) * 512, :].rearrange("(j p) n -> p j n", p=128), a[:], reads=[a], writes=[ob])
        m.final_wait("sp", [ob])
    return nc

H_NA=6; HD=64
def fm(a):
    T = a.shape[0]
    return np.ascontiguousarray(a.reshape(T, 8, 128).transpose(2, 1, 0))
def wt(w):
    K, N = w.shape
    return np.ascontiguousarray(w.reshape(K // 128, 128, N).transpose(1, 0, 2))
def gvec(g):
    return np.ascontiguousarray(g.reshape(8, 128).T)
def cmat():
    ones = np.ones((128, 128), np.float32)
    blk = np.zeros((128, 128), np.float32); blk[:64, :64] = 1; blk[64:, 64:] = 1
    perm = np.zeros((128, 128), np.float32)
    for mm in range(128):
        k = (mm // 64) * 64 + ((mm % 64) + 32) % 64
        perm[k, mm] = 1
    return np.ascontiguousarray(np.stack([ones, blk, perm], axis=1))
def cs_table(pos):
    half = 32
    inv = np.power(np.float32(10000.0), -(2.0 / 64) * np.arange(half, dtype=np.float32)).astype(np.float32)
    ang = pos.astype(np.float32)[:, None] * inv[None, :]
    cos = np.cos(ang).astype(np.float32); sin = np.sin(ang).astype(np.float32)
    d = np.arange(128) % 64
    c = cos[:, d % 32].T
    s = np.where((d < 32)[:, None], -sin[:, d % 32].T, sin[:, d % 32].T)
    return np.ascontiguousarray(np.stack([c, s], axis=1).astype(np.float32))
def gqk_table(inp, l):
    t = np.zeros((128, 8), np.float32)
    d = np.arange(128) % 64
    t[:, 0] = inp["g_qk_na"][l, 0][d]; t[:, 1] = inp["g_qk_na"][l, 1][d]
    t[:, 2] = inp["g_qk_dil"][l, 0][d]; t[:, 3] = inp["g_qk_dil"][l, 1][d]
    t[:, 4] = inp["g_qk_mem"][l, 0][d]; t[:, 5] = inp["g_qk_mem"][l, 1][d]
    return t

BF = ml_dtypes.bfloat16
def cmask_table():
    k = np.arange(128)[:, None]; q = np.arange(128)[None, :]
    out = np.zeros((128, 17, 128), np.float32)
    for j in range(17):
        off = 128 * (j - 8) + k - q
        c = np.zeros((128, 128), np.float32)
        for w, d in ((128, 1), (512, 4), (2048, 16)):
            c += ((off % d == 0) & (np.abs(off) <= w // 2)).astype(np.float32)
        out[:, j, :] = c
    return np.ascontiguousarray(out.reshape(128, 17 * 128))
def na_bias(rpb_l, p, gb):
    ws = min(max(gb - 2, 0), 59)
    k = np.arange(128); q = np.arange(128)
    out = np.full((128, 2, 5, 128), -100.0, np.float32)
    qrow = 2 * gb + q // 64; qcol = q % 64
    rstart = np.clip(qrow - 4, 0, 120); cstart = np.clip(qcol - 8, 0, 48)
    for j in range(5):
        krow = 2 * (ws + j) + k // 64; kcol = k % 64
        inr = (krow[:, None] >= rstart[None, :]) & (krow[:, None] < rstart[None, :] + 8)
        inc = (kcol[:, None] >= cstart[None, :]) & (kcol[:, None] < cstart[None, :] + 16)
        ok = inr & inc
        ri = np.clip(krow[:, None] - qrow[None, :] + 7, 0, 14); ci = np.clip(kcol[:, None] - qcol[None, :] + 15, 0, 30)
        for hh in range(2):
            vals = rpb_l[2 * p + hh][ri, ci]
            out[:, hh, j, :] = np.where(ok, vals, np.float32(-100.0))
    return out.reshape(128, 1280)
def prep_pb(core, l, inp, pa_res, h_full_T):
    b = core // 4; ci = core % 4; s0 = ci * 2048; tile0 = ci * 16
    K = np.concatenate([np.asarray(pa_res[b * 4 + c]["kT"]) for c in range(4)], axis=2)
    V = np.concatenate([np.asarray(pa_res[b * 4 + c]["v"]) for c in range(4)], axis=1)
    Kp = np.zeros((128, 6, 8192 + 2048), K.dtype); Kp[:, :, 1024:1024 + 8192] = K
    Vp = np.zeros((128, 64 + 16, 780), V.dtype); Vp[:, 8:72] = V
    r = pa_res[core]
    d = {"qT": np.asarray(r["qT"])}
    d["kA"] = np.ascontiguousarray(Kp[:, 0:3, 1024 + s0 - 256:1024 + s0 + 2304])
    d["kB"] = np.ascontiguousarray(Kp[:, 3:6, s0:s0 + 4096])
    vA = Vp[:, 8 + tile0 - 2:8 + tile0 + 18, 0:390].reshape(128, 20, 3, 130).transpose(0, 2, 1, 3)
    vB = Vp[:, tile0:tile0 + 32, 390:780].reshape(128, 32, 3, 130).transpose(0, 2, 1, 3)
    d["vA"] = np.ascontiguousarray(vA); d["vB"] = np.ascontiguousarray(vB)
    kAe = np.zeros((128, 3, 4, 640), K.dtype); vAe = np.zeros((128, 3, 4, 650), V.dtype)
    for slot, i in enumerate((0, 1, 14, 15)):
        gb = tile0 + i; ws = min(max(gb - 2, 0), 59)
        kAe[:, :, slot, :] = K[:, 0:3, ws * 128:ws * 128 + 640]
        vv = V[:, ws:ws + 5, 0:390].reshape(128, 5, 3, 130).transpose(0, 2, 1, 3)
        vAe[:, :, slot, :] = vv.reshape(128, 3, 650)
    d["kAe"] = kAe; d["vAe"] = vAe
    d["kM"] = np.asarray(r["kmT"])
    d["vM"] = np.ascontiguousarray(np.asarray(r["vm"]).reshape(128, 2, 2, 130).transpose(0, 2, 1, 3))
    nab = np.zeros((128, 3, 5, 1280), np.float32)
    for p in range(3):
        for v, i in enumerate((0, 1, 2, 14, 15)):
            nab[:, p, v, :] = na_bias(inp["rpb_na"][l], p, tile0 + i)
    d["nab"] = nab
    d["cmask"] = cmask_table()
    d["ident"] = np.eye(128, dtype=np.float32)
    d["gout"] = np.ascontiguousarray(np.broadcast_to(inp["g_out"][l][None, :], (128, 1024))).astype(np.float32)
    d["w_out"] = wt(inp["w_out"][l])
    d["hT"] = h_full_T[core]
    return d
def prep_pa(core, l, inp, hT_core):
    b = core // 4; s0 = (core % 4) * 2048
    return {"hT": hT_core, "w_in": wt(inp["w_in"][l]), "gattn": gvec(inp["g_attn"][l]), "gqk": gqk_table(inp, l), "cmat": cmat(),
            "cs": cs_table(np.arange(s0, s0 + 2048)), "memT": fm(inp["mem"][b]), "gmem": gvec(inp["g_mem"][l]), "w_kv": wt(inp["w_mem_kv"][l])}

def wgu_t(w, F):
    return np.ascontiguousarray(w.reshape(8, 128, F, 128).transpose(2, 1, 0, 3).reshape(F, 128, 1024))
def wd_t(w, F):
    return np.ascontiguousarray(w.reshape(F, 128, 8, 128).transpose(2, 1, 0, 3).reshape(8, 128, F * 128))
def unfm(aT):
    return np.ascontiguousarray(aT.transpose(2, 1, 0).reshape(aT.shape[2], 1024))

def cst3():
    tri = (np.arange(128)[:, None] <= np.arange(128)[None, :]).astype(np.float32)
    return np.ascontiguousarray(np.stack([tri, np.ones((128, 128), np.float32), np.eye(128, dtype=np.float32)], 1))

from concourse.bass_utils import run_bass_kernel_spmd
_CORES = list(range(8))
_PROGS = {}


def _prog(name, fn):
    if name not in _PROGS:
        _PROGS[name] = fn()
    return _PROGS[name]


def _run(name, fn, ims):
    nc = _prog(name, fn)
    return run_bass_kernel_spmd(nc, ims, core_ids=_CORES).results


def _attn_layer(inp, l, hTs):
    ra = _run("pa", build_pa, [prep_pa(c, l, inp, hTs[c]) for c in range(8)])
    rb = _run("pb", build_pb, [prep_pb(c, l, inp, ra, hTs) for c in range(8)])
    return [np.asarray(rb[c]["hmidT"]) for c in range(8)]


def kernel(**inp):
    inp = {k: np.asarray(v) for k, v in inp.items()}
    x = inp["x"]
    hTs = [fm(x[c // 4, (c % 4) * 2048:(c % 4 + 1) * 2048]) for c in range(8)]
    hm = _attn_layer(inp, 0, hTs)
    wg = wgu_t(inp["w_gate_dense"][0], 22); wu = wgu_t(inp["w_up_dense"][0], 22); wd = wd_t(inp["w_down_dense"][0], 22)
    cm = cmat()
    rc = _run("pc", build_pc_dense, [{"hT": hm[c], "gffn": gvec(inp["g_ffn"][0]), "cmat": cm, "wg": wg, "wu": wu, "wd": wd} for c in range(8)])
    h1 = [np.asarray(rc[c]["outT"]) for c in range(8)]
    hm1 = _attn_layer(inp, 1, h1)
    wr = wt(inp["w_router"][0])
    r5 = _run("l5", build_l5, [{"hT": hm1[c], "gffn": gvec(inp["g_ffn"][1]), "cmat": cm, "wr": wr} for c in range(8)])
    U_all = np.ascontiguousarray(np.concatenate([np.asarray(r5[c]["uT"]).transpose(2, 1, 0).reshape(2048, 1024) for c in range(8)], axis=0))
    gates = np.concatenate([np.asarray(r5[c]["gates"]).transpose(1, 0, 2).reshape(2048, 8) for c in range(8)], axis=0)
    g_lay = np.ascontiguousarray(gates.reshape(128, 128, 8).transpose(1, 0, 2).reshape(128, 1024))
    thr = np.ascontiguousarray(np.broadcast_to((1024.0 * np.arange(16, dtype=np.float32))[None, :], (128, 16)))
    c3 = cst3()
    nb = run_bass_kernel_spmd(_prog("l5b", build_l5b), [{"gates_all": g_lay, "cst": np.ascontiguousarray(c3[:, 0:2, :]), "thr": thr}], core_ids=[0]).results[0]["nblk"]
    NBLK = max(1, int(np.asarray(nb).reshape(-1)[0]))
    rowid = np.ascontiguousarray((np.arange(128)[None, :] * 128 + np.arange(128)[:, None]).astype(np.float32))
    tokid = np.ascontiguousarray(np.arange(16384, dtype=np.float32).reshape(32, 128, 4).transpose(1, 0, 2).reshape(128, 128))
    ims = []
    for e in range(8):
        ims.append({"U": U_all, "tokid": tokid, "rowid": rowid, "gate": np.ascontiguousarray(gates[:, e].reshape(32, 128, 4).transpose(1, 0, 2).reshape(128, 128)), "cst": c3,
                    "wg": wgu_t(inp["w_gate_moe"][0, e], 28), "wu": wgu_t(inp["w_up_moe"][0, e], 28),
                    "wd": np.ascontiguousarray(inp["w_down_moe"][0, e].reshape(28, 128, 1024))})
    r6 = _run("l6s_%d" % NBLK, lambda: build_l6s(NBLK), ims)
    ims = []
    for c in range(8):
        ys = np.ascontiguousarray(np.stack([np.asarray(r6[e]["y"])[c * 2048:(c + 1) * 2048] for e in range(8)]))
        ims.append({"h": unfm(hm1[c]), "ys": ys})
    r7 = _run("l7t", build_l7t, ims)
    out = np.stack([np.asarray(r7[c]["out"]) for c in range(8)]).reshape(2, 8192, 1024)
    return out.astype(np.float32)
```

```python
import numpy as np
import ml_dtypes
import numpy as np
from contextlib import ExitStack
import concourse.bass as bass
import concourse.mybir as mybir

F32 = mybir.dt.float32
BF16 = mybir.dt.bfloat16
AF = mybir.ActivationFunctionType
ALU = mybir.AluOpType
AX = mybir.AxisListType


class Buf:
    __slots__ = ("name", "w", "r", "t", "dsem")

    def __init__(self, name, t=None):
        self.name = name
        self.w = None
        self.r = []
        self.t = t
        self.dsem = None

    def __getitem__(self, idx):
        return self.t[idx]


class MK:
    SAME_ENGINE_SYNC = False

    def __init__(self, nc, es, tag=""):
        self.nc = nc
        self.es = es
        self.tag = tag
        self.eng = {"pe": nc.tensor, "act": nc.scalar, "dve": nc.vector, "pool": nc.gpsimd, "sp": nc.sync}
        self.esem = {k: es.enter_context(nc.semaphore("s_" + tag + k)) for k in self.eng}
        self.ecnt = {k: 0 for k in self.eng}
        self.waited = {k: {} for k in self.eng}
        self.dsems = []
        self.dcnt = {}
        self.ndma = 0
        self.n_ins = 0

    def sb(self, name, shape, dt):
        t = self.es.enter_context(self.nc.sbuf_tensor(name, list(shape), dt))
        return Buf(name, t)

    def ps(self, name, shape, dt=F32):
        t = self.es.enter_context(self.nc.psum_tensor(name, list(shape), dt))
        return Buf(name, t)

    def new_dsem(self, name):
        s = self.es.enter_context(self.nc.semaphore(self.tag + name))
        self.dcnt[id(s)] = [s, 0]
        return s

    def _wait(self, E, raw, other, strict=False):
        eng = self.eng[E]
        best = {}
        own = self.esem[E]
        for lst, is_raw in ((raw, True), (other, False)):
            for ev in lst:
                if ev is None:
                    continue
                sem, val = ev
                if sem is own and not self.SAME_ENGINE_SYNC:
                    if not (is_raw and (E != "pe" or strict)):
                        continue
                k = id(sem)
                if k not in best or best[k][1] < val:
                    best[k] = (sem, val)
        for k, (sem, val) in best.items():
            if self.waited[E].get(k, 0) >= val:
                continue
            eng.wait_ge(sem, val)
            self.waited[E][k] = val

    def _deps(self, reads, writes):
        raw = [b.w for b in reads]
        other = []
        for b in writes:
            other.append(b.w)
            other.extend(b.r)
        return raw, other

    def _commit(self, ev, reads, writes):
        for b in reads:
            b.r.append(ev)
            if len(b.r) > 64:
                best = {}
                for s, v in b.r:
                    if id(s) not in best or best[id(s)][1] < v:
                        best[id(s)] = (s, v)
                b.r = list(best.values())
        for b in writes:
            b.w = ev
            b.r = []

    def op(self, E, fn, reads=(), writes=(), strict=False):
        self._wait(E, *self._deps(reads, writes), strict=strict)
        ins = fn(self.eng[E])
        self.ecnt[E] += 1
        ins.then_inc(self.esem[E], 1)
        ev = (self.esem[E], self.ecnt[E])
        self._commit(ev, reads, writes)
        self.n_ins += 1
        return ins

    def dma(self, E, out, in_, reads=(), writes=(), dsem=None, **kw):
        self._wait(E, *self._deps(reads, writes))
        ins = self.eng[E].dma_start(out=out, in_=in_, **kw)
        wb = writes[0]
        if wb.dsem is None:
            wb.dsem = self.new_dsem("d_" + wb.name)
        dsem = wb.dsem
        rec = self.dcnt[id(dsem)]
        rec[1] += 16
        ins.then_inc(dsem, 16)
        ev = (dsem, rec[1])
        self._commit(ev, reads, writes)
        self.ndma += 1
        return ins

    def idma(self, out, in_, idx_ap, scatter, bound, reads=(), writes=()):
        self._wait("pool", *self._deps(reads, writes))
        if not hasattr(self, "_bregs"):
            self._bregs = {}
        if bound not in self._bregs:
            reg = self.nc.gpsimd.alloc_register("bnd%d" % len(self._bregs))
            self.nc.gpsimd.reg_mov(reg, bound)
            self._bregs[bound] = reg
        bound = self._bregs[bound]
        off = bass.IndirectOffsetOnAxis(ap=idx_ap, axis=0)
        if scatter:
            ins = self.nc.gpsimd.indirect_dma_start(out=out, out_offset=off, in_=in_, in_offset=None, bounds_check=bound, oob_is_err=False)
        else:
            ins = self.nc.gpsimd.indirect_dma_start(out=out, out_offset=None, in_=in_, in_offset=off, bounds_check=bound, oob_is_err=False)
        wb = writes[0]
        if wb.dsem is None:
            wb.dsem = self.new_dsem("d_" + wb.name)
        rec = self.dcnt[id(wb.dsem)]
        rec[1] += 16
        ins.then_inc(wb.dsem, 16)
        ev = (wb.dsem, rec[1])
        self._commit(ev, reads, writes)
        self.ndma += 1
        return ins

    def final_wait(self, E, bufs):
        self._wait(E, [b.w for b in bufs], [])


NT = 2048
CH = 512
NCH = NT // CH
EPS = 1e-6

QK_GROUPS = ([("q", j, 0 + 128 * j, 0, False) for j in range(3)]
             + [("k", j, 384 + 128 * j, 1, False) for j in range(3)]
             + [("q", 3 + j, 1152 + 128 * j, 2, True) for j in range(3)]
             + [("k", 3 + j, 1536 + 128 * j, 3, True) for j in range(3)]
             + [("q", 6 + j, 2304 + 128 * j, 4, False) for j in range(2)])


def rms_chunk(m, src, n, gvec, ones, epsb, ps_ss, sq, rs, uT, u32=None):
    m.op("act", lambda e: e.activation(out=sq[:, :, 0:n], in_=src[:, :, 0:n], func=AF.Square), reads=[src], writes=[sq])
    for c in range(8):
        m.op("pe", lambda e: e.matmul(ps_ss[:, 0:n], lhsT=ones[:, 0, :], rhs=sq[:, c, 0:n], start=(c == 0), stop=(c == 7)),
             reads=[sq, ones], writes=[ps_ss])
    m.op("act", lambda e: e.activation(out=rs[:, 0:n], in_=ps_ss[:, 0:n], func=AF.Ln, scale=1.0 / 1024, bias=epsb[:, 0:1]),
         reads=[ps_ss, epsb], writes=[rs])
    m.op("act", lambda e: e.activation(out=rs[:, 0:n], in_=rs[:, 0:n], func=AF.Exp, scale=-0.5), reads=[rs], writes=[rs])
    for c in range(8):
        m.op("dve", lambda e: e.scalar_tensor_tensor(out=uT[:, c, 0:n], in0=src[:, c, 0:n], scalar=gvec[:, c:c + 1],
                                                     in1=rs[:, 0:n], op0=ALU.mult, op1=ALU.mult),
             reads=[src, gvec, rs], writes=[uT])
        if u32 is not None:
            m.op("pool", lambda e: e.scalar_tensor_tensor(out=u32[:, c, 0:n], in0=src[:, c, 0:n], scalar=gvec[:, c:c + 1],
                                                          in1=rs[:, 0:n], op0=ALU.mult, op1=ALU.mult),
                 reads=[src, gvec, rs], writes=[u32])


def build_pa():
    nc = bass.Bass("TRN2", target_bir_lowering=False)
    D = lambda n, s, dt, k: nc.dram_tensor(n, s, dt, kind=k).ap()
    hT = D("hT", [128, 8, NT], F32, "ExternalInput")
    w_in = D("w_in", [128, 8, 2560], F32, "ExternalInput")
    gattn = D("gattn", [128, 8], F32, "ExternalInput")
    gqk = D("gqk", [128, 8], F32, "ExternalInput")
    cmat = D("cmat", [128, 3, 128], F32, "ExternalInput")
    cs = D("cs", [128, 2, NT], F32, "ExternalInput")
    memT = D("memT", [128, 8, 256], F32, "ExternalInput")
    gmem = D("gmem", [128, 8], F32, "ExternalInput")
    w_kv = D("w_kv", [128, 8, 512], F32, "ExternalInput")
    qT_o = D("qT", [128, 8, NT], BF16, "ExternalOutput")
    kT_o = D("kT", [128, 6, NT], BF16, "ExternalOutput")
    v_o = D("v", [128, 16, 12 * 65], BF16, "ExternalOutput")
    kmT_o = D("kmT", [128, 2, 256], BF16, "ExternalOutput")
    vm_o = D("vm", [128, 2, 4 * 65], BF16, "ExternalOutput")

    with ExitStack() as es:
        m = MK(nc, es)
        W = m.sb("W", [128, 8, 2560], BF16)
        Wkv = m.sb("Wkv", [128, 8, 512], BF16)
        cm = m.sb("cm", [128, 3, 128], BF16)
        ga = m.sb("ga", [128, 8], F32)
        gm = m.sb("gm", [128, 8], F32)
        gq = m.sb("gq", [128, 8], F32)
        epsb = m.sb("epsb", [128, 1], F32)
        cst = [m.sb("cst%d" % i, [128, 2, CH], F32) for i in range(2)]
        hc = [m.sb("hc%d" % i, [128, 8, CH], F32) for i in range(2)]
        sq = m.sb("sq", [128, 8, CH], BF16)
        rs = m.sb("rs", [128, CH], F32)
        uT = [m.sb("uT%d" % i, [128, 8, CH], BF16) for i in range(2)]
        sq2 = [m.sb("sq2%d" % i, [128, CH], BF16) for i in range(4)]
        r2 = [m.sb("r2%d" % i, [128, CH], F32) for i in range(4)]
        qn = [m.sb("qn%d" % i, [128, CH], BF16) for i in range(4)]
        ysb = [m.sb("ysb%d" % i, [128, CH], F32) for i in range(4)]
        t1 = [m.sb("t1%d" % i, [128, CH], F32) for i in range(4)]
        t2 = [m.sb("t2%d" % i, [128, CH], F32) for i in range(4)]
        qTs2 = [m.sb("qTs%d" % i, [128, 8, CH], BF16) for i in range(2)]
        kTs2 = [m.sb("kTs%d" % i, [128, 6, CH], BF16) for i in range(2)]
        vs2 = [m.sb("vs%d" % i, [128, 4, 12 * 65], BF16) for i in range(2)]
        kms = m.sb("kms", [128, 2, 256], BF16)
        vms = m.sb("vms", [128, 2, 4 * 65], BF16)
        ps_ss = m.ps("ps_ss", [128, 512])
        ps_y = [m.ps("ps_y%d" % i, [128, 512]) for i in range(3)]
        ps_s2 = [m.ps("ps_s2%d" % i, [128, 512]) for i in range(2)]
        ps_qp = m.ps("ps_qp", [128, 512])
        ps_v = [m.ps("ps_v%d" % i, [128, 512]) for i in range(1)]

        dW = [m.new_dsem("dW%d" % i) for i in range(4)]
        dsm = m.new_dsem("dsm")
        dh = [m.new_dsem("dh%d" % i) for i in range(2)]
        dc = [m.new_dsem("dc%d" % i) for i in range(2)]
        dout = m.new_dsem("dout")

        m.dma("sp", ga[:], gattn, writes=[ga], dsem=dsm)
        m.dma("sp", gm[:], gmem, writes=[gm], dsem=dsm)
        m.dma("sp", gq[:], gqk, writes=[gq], dsem=dsm)
        m.dma("pool", cm[:], cmat, writes=[cm], dsem=dW[0])
        m.op("dve", lambda e: e.memset(epsb[:], EPS), writes=[epsb])
        for i in range(2):
            m.op("dve", lambda e: e.memset(vs2[i][:], 1.0), writes=[vs2[i]])
        m.op("dve", lambda e: e.memset(vms[:], 1.0), writes=[vms])
        Wb = [Buf("Wb%d" % i) for i in range(4)]
        for i in range(4):
            m.dma("pool", W[:, :, 640 * i:640 * (i + 1)], w_in[:, :, 640 * i:640 * (i + 1)], writes=[Wb[i]], dsem=dW[i])
        m.dma("pool", Wkv[:], w_kv, writes=[Wkv], dsem=dW[0])

        def wbufs(c0, c1):
            return [Wb[i] for i in range(4) if c0 < 640 * (i + 1) and c1 > 640 * i]

        cnt = {"y": 0, "v": 0}

        def qk_stages(u, n, Wt, wdeps, col0, gcolumn, rope, dst, dsti, tok0, csb):
            k_ = cnt["y"]
            cnt["y"] += 1
            i = k_ % 4
            yp, s2, sq2b, r2b, qnb = ps_y[k_ % 3], ps_s2[k_ % 2], sq2[i], r2[i], qn[i]
            y = ysb[i]
            d = dst[:, dsti, tok0:tok0 + n]

            def s1():
                for c in range(8):
                    m.op("pe", lambda e: e.matmul(yp[:, 0:n], lhsT=Wt[:, c, col0:col0 + 128], rhs=u[:, c, 0:n], start=(c == 0), stop=(c == 7)),
                         reads=[u] + wdeps, writes=[yp])
                m.op("dve", lambda e: e.tensor_copy(out=y[:, 0:n], in_=yp[:, 0:n]), reads=[yp], writes=[y])
                m.op("act", lambda e: e.activation(out=sq2b[:, 0:n], in_=y[:, 0:n], func=AF.Square), reads=[y], writes=[sq2b])

            def s2f():
                m.op("pe", lambda e: e.matmul(s2[:, 0:n], lhsT=cm[:, 1, :], rhs=sq2b[:, 0:n], start=True, stop=True), reads=[sq2b, cm], writes=[s2])
                m.op("act", lambda e: e.activation(out=r2b[:, 0:n], in_=s2[:, 0:n], func=AF.Ln, scale=1.0 / 64, bias=epsb[:, 0:1]),
                     reads=[s2, epsb], writes=[r2b])
                m.op("act", lambda e: e.activation(out=r2b[:, 0:n], in_=r2b[:, 0:n], func=AF.Exp, scale=-0.5), reads=[r2b], writes=[r2b])
                if not rope:
                    m.op("dve", lambda e: e.scalar_tensor_tensor(out=d, in0=y[:, 0:n], scalar=gq[:, gcolumn:gcolumn + 1], in1=r2b[:, 0:n],
                                                                 op0=ALU.mult, op1=ALU.mult), reads=[y, gq, r2b], writes=[dst])
                else:
                    m.op("dve", lambda e: e.scalar_tensor_tensor(out=qnb[:, 0:n], in0=y[:, 0:n], scalar=gq[:, gcolumn:gcolumn + 1], in1=r2b[:, 0:n],
                                                                 op0=ALU.mult, op1=ALU.mult), reads=[y, gq, r2b], writes=[qnb])

            def s3():
                if not rope:
                    return
                t1b, t2b = t1[i], t2[i]
                m.op("pe", lambda e: e.matmul(ps_qp[:, 0:n], lhsT=cm[:, 2, :], rhs=qnb[:, 0:n], start=True, stop=True), reads=[qnb, cm], writes=[ps_qp])
                m.op("pool", lambda e: e.tensor_tensor(out=t1b[:, 0:n], in0=qnb[:, 0:n], in1=csb[:, 0, 0:n], op=ALU.mult), reads=[qnb, csb], writes=[t1b])
                m.op("dve", lambda e: e.tensor_tensor(out=t2b[:, 0:n], in0=ps_qp[:, 0:n], in1=csb[:, 1, 0:n], op=ALU.mult), reads=[ps_qp, csb], writes=[t2b])
                m.op("pool", lambda e: e.tensor_tensor(out=d, in0=t1b[:, 0:n], in1=t2b[:, 0:n], op=ALU.add), reads=[t1b, t2b], writes=[dst])
            return s1, s2f, s3

        def run_pipelined(groups, fillers=()):
            fillers = list(fillers)
            n_g = len(groups)
            groups[0][0]()
            if n_g > 1:
                groups[1][0]()
            for j in range(n_g + 1):
                if j + 2 < n_g:
                    groups[j + 2][0]()
                if j < n_g:
                    groups[j][1]()
                if fillers:
                    fillers.pop(0)()
                if j >= 1:
                    groups[j - 1][2]()
            for f in fillers:
                f()

        def qk_group(*args):
            s1, s2f, s3 = qk_stages(*args)
            s1(); s2f(); s3()

        def v_tile(u, Wt, wdeps, col0, ncols, sub, dst, tile, head0, nheads):
            cnt["v"] += 1
            pv = ps_v[0]
            for c in range(8):
                m.op("pe", lambda e: e.matmul(pv[:, 0:ncols], lhsT=u[:, c, sub * 128:(sub + 1) * 128], rhs=Wt[:, c, col0:col0 + ncols],
                                              start=(c == 0), stop=(c == 7)), reads=[u] + wdeps, writes=[pv])
            dv = dst[:, tile, head0 * 65:(head0 + nheads) * 65].rearrange("p (h d) -> p h d", d=65)[:, :, 0:64]
            sv = pv[:, 0:ncols].rearrange("p (h d) -> p h d", d=64)
            m.op("act", lambda e: e.activation(out=dv, in_=sv, func=AF.Copy), reads=[pv], writes=[dst])

        m.dma("sp", hc[0][:, :, 0:256], memT, writes=[hc[0]], dsem=dh[0])
        rms_chunk(m, hc[0], 256, gm, cm, epsb, ps_ss, sq, rs, uT[0])
        for j in range(2):
            qk_group(uT[0], 256, Wkv, [Wkv], 128 * j, 5, False, kms, j, 0, None)
        for sub in range(2):
            v_tile(uT[0], Wkv, [Wkv], 256, 256, sub, vms, sub, 0, 4)
        obm = [Buf("o1"), Buf("o2")]
        m.dma("sp", kmT_o, kms[:], reads=[kms], writes=[obm[0]], dsem=dout)
        m.dma("sp", vm_o, vms[:], reads=[vms], writes=[obm[1]], dsem=dout)

        ob = [Buf("oq"), Buf("ok"), Buf("ov")]
        def load_chunk(t):
            b = (t + 1) % 2
            m.dma("sp", hc[b][:], hT[:, :, t * CH:(t + 1) * CH], writes=[hc[b]])
            m.dma("sp", cst[b][:], cs[:, :, t * CH:(t + 1) * CH], writes=[cst[b]])

        load_chunk(0)
        for t in range(NCH):
            b = (t + 1) % 2
            tok0 = t * CH
            if t + 1 < NCH:
                load_chunk(t + 1)
            rms_chunk(m, hc[b], CH, ga, cm, epsb, ps_ss, sq, rs, uT[b])
            qTs, kTs, vs = qTs2[b], kTs2[b], vs2[b]
            groups = [qk_stages(uT[b], CH, W, wbufs(col0, col0 + 128), col0, gc, rope, qTs if dn == "q" else kTs, di, 0, cst[b])
                      for (dn, di, col0, gc, rope) in QK_GROUPS]
            fillers = []
            for sub in range(4):
                fillers.append(lambda sub=sub, b=b, vs=vs: v_tile(uT[b], W, wbufs(768, 1152), 768, 384, sub, vs, sub, 0, 6))
                fillers.append(lambda sub=sub, b=b, vs=vs: v_tile(uT[b], W, wbufs(1920, 2304), 1920, 384, sub, vs, sub, 6, 6))
            run_pipelined(groups, fillers)
            m.dma("sp", qT_o[:, :, tok0:tok0 + CH], qTs[:], reads=[qTs], writes=[ob[0]], dsem=dout)
            m.dma("sp", kT_o[:, :, tok0:tok0 + CH], kTs[:], reads=[kTs], writes=[ob[1]], dsem=dout)
            m.dma("sp", v_o[:, 4 * t:4 * t + 4, :], vs[:], reads=[vs], writes=[ob[2]], dsem=dout)
        m.final_wait("sp", ob + obm)
        print("P_A instrs", m.n_ins, "dmas", m.ndma)
    return nc


NT = 2048
EPS = 1e-6
SCALE = 0.125
NS = 6
PIPE = 4
DEBUG = False


def build_pb():
    nc = bass.Bass("TRN2", target_bir_lowering=False)
    D = lambda n, s, dt, k: nc.dram_tensor(n, s, dt, kind=k).ap()
    qT = D("qT", [128, 8, NT], BF16, "ExternalInput")
    kA = D("kA", [128, 3, 2560], BF16, "ExternalInput")
    vA = D("vA", [128, 3, 20, 130], BF16, "ExternalInput")
    kAe = D("kAe", [128, 3, 4, 640], BF16, "ExternalInput")
    vAe = D("vAe", [128, 3, 4, 650], BF16, "ExternalInput")
    kB = D("kB", [128, 3, 4096], BF16, "ExternalInput")
    vB = D("vB", [128, 3, 32, 130], BF16, "ExternalInput")
    kM = D("kM", [128, 2, 256], BF16, "ExternalInput")
    vM = D("vM", [128, 2, 2, 130], BF16, "ExternalInput")
    nab = D("nab", [128, 3, 5, 1280], F32, "ExternalInput")
    cmask = D("cmask", [128, 17 * 128], F32, "ExternalInput")
    ident = D("ident", [128, 128], F32, "ExternalInput")
    gout = D("gout", [128, 1024], F32, "ExternalInput")
    w_out = D("w_out", [128, 8, 1024], F32, "ExternalInput")
    hT = D("hT", [128, 8, NT], F32, "ExternalInput")
    hmid = D("hmidT", [128, 8, NT], F32, "ExternalOutput")
    o_dbg = D("o_dbg", [128, 16, 1024], BF16, "ExternalOutput") if DEBUG else None
    dbg2 = D("dbg2", [128, 1024], BF16, "ExternalOutput") if DEBUG else None
    dbg3 = D("dbg3", [128, 4], F32, "ExternalOutput") if DEBUG else None
    dbg4 = D("dbg4", [128, 8, 512], BF16, "ExternalOutput") if DEBUG else None

    with ExitStack() as es:
        m = MK(nc, es)
        qp = [m.sb("qp%d" % i, [128, NT], BF16) for i in range(2)]
        kb = [m.sb("kb%d" % i, [128, 4096], BF16) for i in range(2)]
        vb = [m.sb("vb%d" % i, [128, 32, 130], BF16) for i in range(2)]
        ke = [m.sb("ke0", [128, 4, 640], BF16)] * 2
        ve = [m.sb("ve0", [128, 4, 650], BF16)] * 2
        stage = [m.sb("stage%d" % i, [128, 1280], F32) for i in range(2)]
        Mna = [m.sb("Mna0", [128, 5, 1280], BF16)] * 2
        Cm = m.sb("Cm", [128, 17 * 128], BF16)
        idn = m.sb("idn", [128, 128], BF16)
        go = m.sb("go", [128, 1024], F32)
        Wo = m.sb("Wo", [128, 8, 1024], BF16)
        epsb = m.sb("epsb", [128, 1], F32)
        o_all = m.sb("o_all", [128, 16, 1024], BF16)
        Eb = [m.sb("E%d" % i, [128, 512], BF16) for i in range(NS)]
        Pb = [m.sb("P%d" % i, [128, 512], BF16) for i in range(NS)]
        rec = [m.sb("rec%d" % i, [128, 2], F32) for i in range(2)]
        sqf = m.sb("sqf", [128, 1024], F32)
        ms = [m.sb("ms%d" % i, [128, 4], F32) for i in range(2)]
        mixed = [m.sb("mixed%d" % i, [128, 1024], BF16) for i in range(2)]
        mixedT = [m.sb("mixedT%d" % i, [128, 8, 512], BF16) for i in range(2)]
        hc = [m.sb("hc0", [128, 8, 512], F32)] * 2
        es_att = ExitStack()
        es_main, m.es = m.es, es_att
        ps_s = [m.ps("ps_s%d" % i, [128, 512]) for i in range(NS)]
        ps_o = [m.ps("ps_o%d" % i, [128, 512]) for i in range(2)]
        m.es = es_main

        dl = [m.new_dsem("dl%d" % i) for i in range(2)]
        dst_ = [m.new_dsem("dst%d" % i) for i in range(2)]
        dcst = m.new_dsem("dcst")
        dh = [m.new_dsem("dh%d" % i) for i in range(2)]
        dout = m.new_dsem("dout")

        m.dma("pool", Cm[:], cmask, writes=[Cm], dsem=dcst)
        m.dma("pool", idn[:], ident, writes=[idn], dsem=dcst)
        m.dma("sp", go[:], gout, writes=[go], dsem=dcst)
        m.op("dve", lambda e: e.memset(epsb[:], EPS), writes=[epsb])
        Wo_loaded = [False]

        gcnt = [0]
        stc = [0]
        SLOT = {0: 0, 1: 1, 14: 2, 15: 3}
        VAR = {0: 0, 1: 1, 14: 3, 15: 4}

        def kind_of(p):
            return "A" if p < 3 else ("B" if p < 6 else "M")

        def load_main(p):
            b = p % 2
            kind = kind_of(p)
            m.dma("sp", qp[b][:], qT[:, p, :], writes=[qp[b]])
            if kind == "A":
                m.dma("sp", kb[b][:, 0:2560], kA[:, p, :], writes=[kb[b]])
                m.dma("sp", vb[b][:, 0:20, :], vA[:, p], writes=[vb[b]])
            elif kind == "B":
                m.dma("sp", kb[b][:], kB[:, p - 3, :], writes=[kb[b]])
                m.dma("sp", vb[b][:], vB[:, p - 3], writes=[vb[b]])
            else:
                m.dma("sp", kb[b][:, 0:256], kM[:, p - 6, :], writes=[kb[b]])
                m.dma("sp", vb[b][:, 0:2, :], vM[:, p - 6], writes=[vb[b]])

        def load_edge(p):
            b = p % 2
            m.dma("sp", ke[b][:], kAe[:, p], writes=[ke[b]])
            m.dma("sp", ve[b][:], vAe[:, p], writes=[ve[b]])
            for v in range(5):
                sb_ = stc[0] % 2
                stc[0] += 1
                m.dma("sp", stage[sb_][:], nab[:, p, v, :], writes=[stage[sb_]])
                m.op("act", lambda e: e.activation(out=Mna[b][:, v, :], in_=stage[sb_][:], func=AF.Exp), reads=[stage[sb_]], writes=[Mna[b]])

        units = []
        for p in range(8):
            kind = kind_of(p)
            b = p % 2
            for i in range(16):
                O = ps_o[i % 2]
                for hh in range(2):
                    pbs = 64 * hh
                    tiles = []
                    if kind == "A":
                        slot = SLOT.get(i)
                        var = VAR.get(i, 2)
                        for j in range(5):
                            if slot is None:
                                k_ap = kb[b][pbs:pbs + 64, (i + j) * 128:(i + j + 1) * 128]
                                v_ap = vb[b][:, i + j, hh * 65:(hh + 1) * 65]
                            else:
                                k_ap = ke[b][pbs:pbs + 64, slot, j * 128:(j + 1) * 128]
                                v_ap = ve[b][:, slot, j * 130 + hh * 65:j * 130 + (hh + 1) * 65]
                            tiles.append((k_ap, v_ap, (Mna[b], var, hh * 640 + j * 128)))
                        kdeps = [kb[b], ke[b]]
                        vdeps = [vb[b], ve[b]]
                    elif kind == "B":
                        for j in range(17):
                            k_ap = kb[b][pbs:pbs + 64, (i + j) * 128:(i + j + 1) * 128]
                            v_ap = vb[b][:, i + j, hh * 65:(hh + 1) * 65]
                            tiles.append((k_ap, v_ap, (Cm, None, j * 128)))
                        kdeps = [kb[b]]
                        vdeps = [vb[b]]
                    else:
                        for j in range(2):
                            k_ap = kb[b][pbs:pbs + 64, j * 128:(j + 1) * 128]
                            v_ap = vb[b][:, j, hh * 65:(hh + 1) * 65]
                            tiles.append((k_ap, v_ap, None))
                        kdeps = [kb[b]]
                        vdeps = [vb[b]]
                    q_ap = qp[b][pbs:pbs + 64, i * 128:(i + 1) * 128]
                    nt = len(tiles)
                    for g0 in range(0, nt, 4):
                        units.append(dict(p=p, b=b, i=i, hh=hh, O=O, grp=tiles[g0:g0 + 4], g0=g0, nt=nt, q_ap=q_ap, kdeps=kdeps, vdeps=vdeps,
                                          first_of_pair=(i == 0 and hh == 0 and g0 == 0), last_of_tile=(hh == 1 and g0 + 4 >= nt)))
        for n_, u in enumerate(units):
            u["gi"] = n_ % NS

        def emit_qk(u):
            S = ps_s[u["gi"]]
            for t, (k_ap, v_ap, mk_) in enumerate(u["grp"]):
                m.op("pe", lambda e: e.matmul(S[:, t * 128:(t + 1) * 128], lhsT=k_ap, rhs=u["q_ap"], start=True, stop=True),
                     reads=u["kdeps"] + [qp[u["b"]]], writes=[S])

        def emit_softmax(u):
            S, E, P = ps_s[u["gi"]], Eb[u["gi"]], Pb[u["gi"]]
            n = len(u["grp"]) * 128
            m.op("act", lambda e: e.activation(out=E[:, 0:n], in_=S[:, 0:n], func=AF.Exp, scale=SCALE), reads=[S], writes=[E])
            mk0 = u["grp"][0][2]
            if mk0 is not None:
                mb, var, c0 = mk0
                m_ap = mb[:, c0:c0 + n] if var is None else mb[:, var, c0:c0 + n]
                m.op("dve", lambda e: e.tensor_tensor(out=P[:, 0:n], in0=E[:, 0:n], in1=m_ap, op=ALU.mult), reads=[E, mb], writes=[P])
                u["src"] = P
            else:
                u["src"] = E

        def emit_pv(u):
            O, hh, src = u["O"], u["hh"], u["src"]
            for t, (k_ap, v_ap, mk_) in enumerate(u["grp"]):
                first = (u["g0"] + t == 0)
                last = (u["g0"] + t == u["nt"] - 1)
                m.op("pe", lambda e: e.matmul(O[:, hh * 65:(hh + 1) * 65], lhsT=src[:, t * 128:(t + 1) * 128], rhs=v_ap, start=first, stop=last),
                     reads=u["vdeps"] + [src], writes=[O])
            if u["last_of_tile"]:
                i, p = u["i"], u["p"]
                rc = rec[i % 2]
                Ov = O[:, 0:130].rearrange("p (h d) -> p h d", d=65)
                m.op("dve", lambda e: e.reciprocal(out=rc[:, 0:2], in_=Ov[:, :, 64]), reads=[O], writes=[rc])
                for h2 in range(2):
                    m.op("dve", lambda e: e.tensor_scalar(out=o_all[:, i, p * 128 + h2 * 64:p * 128 + h2 * 64 + 64], in0=O[:, h2 * 65:h2 * 65 + 64],
                                                          scalar1=rc[:, h2:h2 + 1], scalar2=None, op0=ALU.mult), reads=[O, rc], writes=[o_all])

        def pre_pair(p):
            if p + 1 < 8:
                load_main(p + 1)
            if p == 2:
                for cc in range(0, 8, 2):
                    m.dma("pool", Wo[:, cc:cc + 2, :], w_out[:, cc:cc + 2, :], writes=[Wo])
            if kind_of(p) == "A":
                load_edge(p)

        load_main(0)
        pair_units = [[u for u in units if u["p"] == p] for p in range(8)]
        for p in range(8):
            pre_pair(p)
            pu = pair_units[p]
            for u in pu[:PIPE]:
                emit_qk(u)
            for n_, u in enumerate(pu):
                if n_ + PIPE < len(pu):
                    emit_qk(pu[n_ + PIPE])
                emit_softmax(u)
                emit_pv(u)
        es_att.close()
        ps_T = m.ps("ps_T", [128, 1024], BF16)
        ps_y = [m.ps("ps_y%d" % i, [128, 512]) for i in range(2)]

        obd = Buf("odbg_out")
        if DEBUG:
            m.dma("sp", o_dbg, o_all[:], reads=[o_all], writes=[obd], dsem=dout)
        GR = [(0, 384), (384, 768), (768, 1024)]
        ob = Buf("hmid_out")
        for i in range(16):
            b = i % 2
            msb = ms[b]
            m.op("dve", lambda e: e.tensor_tensor(out=sqf[:, :], in0=o_all[:, i, :], in1=o_all[:, i, :], op=ALU.mult), reads=[o_all], writes=[sqf])
            for g, (c0, c1) in enumerate(GR):
                m.op("dve", lambda e: e.tensor_reduce(out=msb[:, g:g + 1], in_=sqf[:, c0:c1], axis=AX.X, op=ALU.add), reads=[sqf], writes=[msb])
            for g, (c0, c1) in enumerate(GR):
                m.op("act", lambda e: e.activation(out=msb[:, g:g + 1], in_=msb[:, g:g + 1], func=AF.Ln, scale=1.0 / (c1 - c0), bias=epsb[:, 0:1]),
                     reads=[msb, epsb], writes=[msb])
            m.op("act", lambda e: e.activation(out=msb[:, 0:3], in_=msb[:, 0:3], func=AF.Exp, scale=-0.5), reads=[msb], writes=[msb])
            for g, (c0, c1) in enumerate(GR):
                m.op("dve", lambda e: e.scalar_tensor_tensor(out=mixed[b][:, c0:c1], in0=o_all[:, i, c0:c1], scalar=msb[:, g:g + 1], in1=go[:, c0:c1],
                                                             op0=ALU.mult, op1=ALU.mult), reads=[o_all, msb, go], writes=[mixed[b]])
            for c in range(8):
                m.op("pe", lambda e: e.transpose(ps_T[:, c * 128:(c + 1) * 128], mixed[b][:, c * 128:(c + 1) * 128], idn[:]), reads=[mixed[b], idn], writes=[ps_T])
            cb = (i // 4) % 2
            m.op("act", lambda e: e.activation(out=mixedT[cb][:, :, (i % 4) * 128:(i % 4 + 1) * 128], in_=ps_T[:, :].rearrange("p (c t) -> p c t", t=128), func=AF.Copy),
                 reads=[ps_T], writes=[mixedT[cb]])
            if i % 4 == 3:
                ch = i // 4
                m.dma("sp", hc[cb][:], hT[:, :, ch * 512:(ch + 1) * 512], writes=[hc[cb]], dsem=dh[cb])
                for d in range(8):
                    Y = ps_y[d % 2]
                    for c in range(8):
                        m.op("pe", lambda e: e.matmul(Y[:, :], lhsT=Wo[:, c, d * 128:(d + 1) * 128], rhs=mixedT[cb][:, c, :], start=(c == 0), stop=(c == 7)),
                             reads=[Wo, mixedT[cb]], writes=[Y])
                    m.op("dve", lambda e: e.tensor_tensor(out=hc[cb][:, d, :], in0=Y[:, :], in1=hc[cb][:, d, :], op=ALU.add), reads=[Y, hc[cb]], writes=[hc[cb]])
                m.dma("sp", hmid[:, :, ch * 512:(ch + 1) * 512], hc[cb][:], reads=[hc[cb]], writes=[ob], dsem=dout)
        if DEBUG:
            m.dma("sp", dbg2, mixed[1][:], reads=[mixed[1]], writes=[obd], dsem=dout)
            m.dma("sp", dbg3, ms[1][:], reads=[ms[1]], writes=[obd], dsem=dout)
            m.dma("sp", dbg4, mixedT[1][:], reads=[mixedT[1]], writes=[obd], dsem=dout)
        m.final_wait("sp", [ob, obd] if DEBUG else [ob])
        print("P_B instrs", m.n_ins, "dmas", m.ndma)
    return nc


EPS = 1e-6


class FFNRes:
    def __init__(self, m, F, NB):
        self.m, self.F, self.NB = m, F, NB
        self.wg = [m.sb("wg%d" % i, [128, 8, 128], BF16) for i in range(3)]
        self.wu = [m.sb("wu%d" % i, [128, 8, 128], BF16) for i in range(3)]
        self.wd = [m.sb("wd%d" % i, [128, F, 128], BF16) for i in range(2)]
        self.act = m.sb("act", [128, F, NB], BF16)
        self.sg = [m.sb("sg%d" % i, [128, 512], F32) for i in range(2)]
        self.ps_g = [m.ps("ps_g%d" % i, [128, 512]) for i in range(2)]
        self.ps_u = [m.ps("ps_u%d" % i, [128, 512]) for i in range(2)]
        self.ps_y = [m.ps("ps_y%d" % i, [128, 512]) for i in range(2)]
        self.wc = 0
        self.dc = 0
        self.gc = 0
        self.yc = 0


def ffn_block(R, uT, wg_ap, wu_ap, wd_ap, sink):
    m, F, NB = R.m, R.F, R.NB
    nch = NB // 512
    for f in range(F):
        i = R.wc % 3
        R.wc += 1
        wg, wu = R.wg[i], R.wu[i]
        m.dma("pool", wg[:].rearrange("p c n -> p (c n)"), wg_ap(f), writes=[wg])
        m.dma("pool", wu[:].rearrange("p c n -> p (c n)"), wu_ap(f), writes=[wu])
        for ch in range(nch):
            j = R.gc % 2
            R.gc += 1
            G, U, sg = R.ps_g[j], R.ps_u[j], R.sg[j]
            for c in range(8):
                m.op("pe", lambda e: e.matmul(G[:, :], lhsT=wg[:, c, :], rhs=uT[:, c, ch * 512:(ch + 1) * 512], start=(c == 0), stop=(c == 7)),
                     reads=[wg, uT], writes=[G])
            for c in range(8):
                m.op("pe", lambda e: e.matmul(U[:, :], lhsT=wu[:, c, :], rhs=uT[:, c, ch * 512:(ch + 1) * 512], start=(c == 0), stop=(c == 7)),
                     reads=[wu, uT], writes=[U])
            m.op("act", lambda e: e.activation(out=sg[:, :], in_=G[:, :], func=AF.Silu), reads=[G], writes=[sg])
            m.op("dve", lambda e: e.tensor_tensor(out=R.act[:, f, ch * 512:(ch + 1) * 512], in0=U[:, :], in1=sg[:, :], op=ALU.mult),
                 reads=[U, sg], writes=[R.act])
    for d in range(8):
        i = R.dc % 2
        R.dc += 1
        wd = R.wd[i]
        m.dma("pool", wd[:].rearrange("p f n -> p (f n)"), wd_ap(d), writes=[wd])
        for ch in range(nch):
            Y = R.ps_y[R.yc % 2]
            R.yc += 1
            for f in range(F):
                m.op("pe", lambda e: e.matmul(Y[:, :], lhsT=wd[:, f, :], rhs=R.act[:, f, ch * 512:(ch + 1) * 512], start=(f == 0), stop=(f == F - 1)),
                     reads=[wd, R.act], writes=[Y])
            sink(d, ch, Y)


def build_pc_dense(F=22, NT=2048, NB=1024):
    nc = bass.Bass("TRN2", target_bir_lowering=False)
    D = lambda n, s, dt, k: nc.dram_tensor(n, s, dt, kind=k).ap()
    hT = D("hT", [128, 8, NT], F32, "ExternalInput")
    gffn = D("gffn", [128, 8], F32, "ExternalInput")
    cmat = D("cmat", [128, 3, 128], F32, "ExternalInput")
    wg = D("wg", [F, 128, 1024], F32, "ExternalInput")
    wu = D("wu", [F, 128, 1024], F32, "ExternalInput")
    wd = D("wd", [8, 128, F * 128], F32, "ExternalInput")
    out = D("outT", [128, 8, NT], F32, "ExternalOutput")
    with ExitStack() as es:
        m = MK(nc, es)
        R = FFNRes(m, F, NB)
        cm = m.sb("cm", [128, 3, 128], BF16)
        gf = m.sb("gf", [128, 8], F32)
        epsb = m.sb("epsb", [128, 1], F32)
        hbs = [m.sb("hb%d" % i, [128, 8, NB], F32) for i in range(2)]
        uT = m.sb("uT", [128, 8, NB], BF16)
        sq = m.sb("sq", [128, 8, 512], BF16)
        rs = m.sb("rs", [128, 512], F32)
        ps_ss = m.ps("ps_ss", [128, 512])
        m.dma("pool", cm[:], cmat, writes=[cm])
        m.dma("sp", gf[:], gffn, writes=[gf])
        m.op("dve", lambda e: e.memset(epsb[:], EPS), writes=[epsb])
        ob = Buf("out")

        def load_half(i):
            for ch in range(NB // 512):
                m.dma("sp", hbs[i % 2][:, :, ch * 512:(ch + 1) * 512], hT[:, :, i * NB + ch * 512:i * NB + (ch + 1) * 512], writes=[hbs[i % 2]])

        load_half(0)
        for hb_i in range(NT // NB):
            t0 = hb_i * NB
            hb = hbs[hb_i % 2]
            if hb_i + 1 < NT // NB:
                load_half(hb_i + 1)
            for ch in range(NB // 512):
                rms_view(m, hb, ch * 512, 512, gf, cm, epsb, ps_ss, sq, rs, uT)

            def sink(d, ch, Y, hb=hb):
                m.op("dve", lambda e: e.tensor_tensor(out=hb[:, d, ch * 512:(ch + 1) * 512], in0=Y[:, :], in1=hb[:, d, ch * 512:(ch + 1) * 512], op=ALU.add),
                     reads=[Y, hb], writes=[hb])
            ffn_block(R, uT, lambda f: wg[f], lambda f: wu[f], lambda d: wd[d], sink)
            m.dma("sp", out[:, :, t0:t0 + NB], hb[:], reads=[hb], writes=[ob])
        m.final_wait("sp", [ob])
        print("P_C instrs", m.n_ins, "dmas", m.ndma)
    return nc


def rms_view(m, src, c0, n, gvec, cm, epsb, ps_ss, sq, rs, uT, u32=None):
    m.op("act", lambda e: e.activation(out=sq[:, :, 0:n], in_=src[:, :, c0:c0 + n], func=AF.Square), reads=[src], writes=[sq])
    for c in range(8):
        m.op("pe", lambda e: e.matmul(ps_ss[:, 0:n], lhsT=cm[:, 0, :], rhs=sq[:, c, 0:n], start=(c == 0), stop=(c == 7)), reads=[sq, cm], writes=[ps_ss])
    m.op("act", lambda e: e.activation(out=rs[:, 0:n], in_=ps_ss[:, 0:n], func=AF.Ln, scale=1.0 / 1024, bias=epsb[:, 0:1]), reads=[ps_ss, epsb], writes=[rs])
    m.op("act", lambda e: e.activation(out=rs[:, 0:n], in_=rs[:, 0:n], func=AF.Exp, scale=-0.5), reads=[rs], writes=[rs])
    for c in range(8):
        m.op("dve", lambda e: e.scalar_tensor_tensor(out=uT[:, c, c0:c0 + n], in0=src[:, c, c0:c0 + n], scalar=gvec[:, c:c + 1], in1=rs[:, 0:n],
                                                     op0=ALU.mult, op1=ALU.mult), reads=[src, gvec, rs], writes=[uT])
        if u32 is not None:
            m.op("dve", lambda e: e.scalar_tensor_tensor(out=u32[:, c, 0:n], in0=src[:, c, c0:c0 + n], scalar=gvec[:, c:c + 1], in1=rs[:, 0:n],
                                                         op0=ALU.mult, op1=ALU.mult), reads=[src, gvec, rs], writes=[u32])


def build_l5(NT=2048):
    nc = bass.Bass("TRN2", target_bir_lowering=False)
    D = lambda n, s, dt, k: nc.dram_tensor(n, s, dt, kind=k).ap()
    hT = D("hT", [128, 8, NT], F32, "ExternalInput")
    gffn = D("gffn", [128, 8], F32, "ExternalInput")
    cmat = D("cmat", [128, 3, 128], F32, "ExternalInput")
    wr = D("wr", [128, 8, 8], F32, "ExternalInput")
    uT_o = D("uT", [128, 8, NT], BF16, "ExternalOutput")
    g_o = D("gates", [128, NT // 128, 8], F32, "ExternalOutput")
    with ExitStack() as es:
        m = MK(nc, es)
        cm = m.sb("cm", [128, 3, 128], BF16)
        gf = m.sb("gf", [128, 8], F32)
        wrs = m.sb("wrs", [128, 8, 8], F32)
        epsb = m.sb("epsb", [128, 1], F32)
        hb = [m.sb("hb%d" % i, [128, 8, 512], F32) for i in range(2)]
        uT = [m.sb("uT%d" % i, [128, 8, 512], BF16) for i in range(2)]
        u32 = m.sb("u32", [128, 8, 512], F32)
        sq = m.sb("sq", [128, 8, 512], BF16)
        rs = m.sb("rs", [128, 512], F32)
        gates = m.sb("gates_sb", [128, NT // 128, 8], F32)
        lg = [m.sb("lg%d" % i, [128, 8], F32) for i in range(2)]
        m8 = [m.sb("m8%d" % i, [128, 8], F32) for i in range(2)]
        tt = [m.sb("tt%d" % i, [128, 4], F32) for i in range(2)]
        ga = [m.sb("ga%d" % i, [128, 8], F32) for i in range(2)]
        ps_ss = m.ps("ps_ss", [128, 512])
        ps_l = [m.ps("ps_l%d" % i, [128, 8]) for i in range(2)]
        m.dma("pool", cm[:], cmat, writes=[cm])
        m.dma("sp", gf[:], gffn, writes=[gf])
        m.dma("sp", wrs[:], wr, writes=[wrs])
        m.op("dve", lambda e: e.memset(epsb[:], EPS), writes=[epsb])
        ob = Buf("out")
        k = 0
        for t in range(NT // 512):
            b = t % 2
            m.dma("sp", hb[b][:], hT[:, :, t * 512:(t + 1) * 512], writes=[hb[b]])
            rms_view(m, hb[b], 0, 512, gf, cm, epsb, ps_ss, sq, rs, uT[b], u32=u32)
            m.dma("sp", uT_o[:, :, t * 512:(t + 1) * 512], uT[b][:], reads=[uT[b]], writes=[ob])
            for s in range(4):
                j = k % 2
                k += 1
                L, lgb, m8b, tb, gab = ps_l[j], lg[j], m8[j], tt[j], ga[j]
                for c in range(8):
                    m.op("pe", lambda e: e.matmul(L[:, :], lhsT=u32[:, c, s * 128:(s + 1) * 128], rhs=wrs[:, c, :], start=(c == 0), stop=(c == 7)),
                         reads=[u32, wrs], writes=[L])
                m.op("dve", lambda e: e.tensor_copy(out=lgb[:, :], in_=L[:, :]), reads=[L], writes=[lgb])
                m.op("dve", lambda e: e.max(out=m8b[:, :], in_=lgb[:, :]), reads=[lgb], writes=[m8b])
                m.op("dve", lambda e: e.tensor_tensor(out=tb[:, 0:1], in0=m8b[:, 1:2], in1=m8b[:, 0:1], op=ALU.subtract), reads=[m8b], writes=[tb])
                m.op("act", lambda e: e.activation(out=tb[:, 1:2], in_=tb[:, 0:1], func=AF.Exp), reads=[tb], writes=[tb])
                m.op("dve", lambda e: e.tensor_scalar(out=tb[:, 2:3], in0=tb[:, 1:2], scalar1=1.0, scalar2=None, op0=ALU.add), reads=[tb], writes=[tb])
                m.op("dve", lambda e: e.reciprocal(out=tb[:, 2:3], in_=tb[:, 2:3]), reads=[tb], writes=[tb])
                m.op("dve", lambda e: e.tensor_tensor(out=tb[:, 3:4], in0=tb[:, 1:2], in1=tb[:, 2:3], op=ALU.mult), reads=[tb], writes=[tb])
                m.op("dve", lambda e: e.tensor_scalar(out=gab[:, :], in0=lgb[:, :], scalar1=m8b[:, 0:1], scalar2=tb[:, 2:3], op0=ALU.is_equal, op1=ALU.mult),
                     reads=[lgb, m8b, tb], writes=[gab])
                m.op("dve", lambda e: e.tensor_scalar(out=lgb[:, :], in0=lgb[:, :], scalar1=m8b[:, 1:2], scalar2=tb[:, 3:4], op0=ALU.is_equal, op1=ALU.mult),
                     reads=[lgb, m8b, tb], writes=[lgb])
                m.op("dve", lambda e: e.tensor_tensor(out=gates[:, t * 4 + s, :], in0=gab[:, :], in1=lgb[:, :], op=ALU.add), reads=[gab, lgb], writes=[gates])
        ob2 = Buf("out2")
        m.dma("sp", g_o, gates[:], reads=[gates], writes=[ob2])
        m.final_wait("sp", [ob, ob2])
        print("L5 instrs", m.n_ins, "dmas", m.ndma)
    return nc


def build_l6(F=28, NTOK=16384, NB=1024):
    nc = bass.Bass("TRN2", target_bir_lowering=False)
    D = lambda n, s, dt, k: nc.dram_tensor(n, s, dt, kind=k).ap()
    uT_i = D("uT", [128, 8, NTOK], BF16, "ExternalInput")
    gbc_i = D("gbc", [128, NTOK], F32, "ExternalInput")
    wg = D("wg", [F, 128, 1024], F32, "ExternalInput")
    wu = D("wu", [F, 128, 1024], F32, "ExternalInput")
    wd = D("wd", [8, 128, F * 128], F32, "ExternalInput")
    y_o = D("yT", [128, 8, NTOK], F32, "ExternalOutput")
    with ExitStack() as es:
        m = MK(nc, es)
        R = FFNRes(m, F, NB)
        uT = [m.sb("uT%d" % i, [128, 8, NB], BF16) for i in range(2)]
        gb = [m.sb("gb%d" % i, [128, NB], F32) for i in range(2)]
        yb = m.sb("yb", [128, 8, NB], F32)
        ob = Buf("out")
        for tb in range(NTOK // NB):
            b = tb % 2
            t0 = tb * NB
            m.dma("sp", uT[b][:], uT_i[:, :, t0:t0 + NB], writes=[uT[b]])
            m.dma("sp", gb[b][:], gbc_i[:, t0:t0 + NB], writes=[gb[b]])

            def sink(d, ch, Y):
                m.op("dve", lambda e: e.tensor_tensor(out=yb[:, d, ch * 512:(ch + 1) * 512], in0=Y[:, :], in1=gb[b][:, ch * 512:(ch + 1) * 512], op=ALU.mult),
                     reads=[Y, gb[b]], writes=[yb])
            ffn_block(R, uT[b], lambda f: wg[f], lambda f: wu[f], lambda d: wd[d], sink)
            m.dma("sp", y_o[:, :, t0:t0 + NB], yb[:], reads=[yb], writes=[ob])
        m.final_wait("sp", [ob])
        print("L6 instrs", m.n_ins, "dmas", m.ndma)
    return nc


def build_l7(NT=2048):
    nc = bass.Bass("TRN2", target_bir_lowering=False)
    D = lambda n, s, dt, k: nc.dram_tensor(n, s, dt, kind=k).ap()
    hT = D("hT", [128, 8, NT], F32, "ExternalInput")
    ys = D("ys", [8, 128, 8, NT], F32, "ExternalInput")
    out = D("outT", [128, 8, NT], F32, "ExternalOutput")
    with ExitStack() as es:
        m = MK(nc, es)
        acc = [m.sb("acc%d" % i, [128, 8, 512], F32) for i in range(2)]
        yb = [m.sb("yb%d" % i, [128, 8, 512], F32) for i in range(3)]
        ob = Buf("out")
        k = 0
        for t in range(NT // 512):
            a = acc[t % 2]
            m.dma("sp", a[:], hT[:, :, t * 512:(t + 1) * 512], writes=[a])
            for e_ in range(8):
                y = yb[k % 3]
                k += 1
                m.dma("sp", y[:], ys[e_, :, :, t * 512:(t + 1) * 512], writes=[y])
                m.op("dve", lambda e: e.tensor_tensor(out=a[:], in0=a[:], in1=y[:], op=ALU.add), reads=[a, y], writes=[a])
            m.dma("sp", out[:, :, t * 512:(t + 1) * 512], a[:], reads=[a], writes=[ob])
        m.final_wait("sp", [ob])
        print("L7 instrs", m.n_ins, "dmas", m.ndma)
    return nc


U32 = mybir.dt.uint32
I32 = mybir.dt.int32
BIGPOS = 1.0e6
CB = 1024


def build_l5b():
    nc = bass.Bass("TRN2", target_bir_lowering=False)
    D = lambda n, s, dt, k: nc.dram_tensor(n, s, dt, kind=k).ap()
    gates = D("gates_all", [128, 128 * 8], F32, "ExternalInput")
    cst = D("cst", [128, 2, 128], F32, "ExternalInput")
    thr = D("thr", [128, 32], F32, "ExternalInput")
    nb_o = D("nblk", [1, 1], I32, "ExternalOutput")
    with ExitStack() as es:
        m = MK(nc, es)
        g = m.sb("g", [128, 1024], F32)
        mk_ = m.sb("mk", [128, 1024], BF16)
        cm = m.sb("cm", [128, 2, 128], BF16)
        th = m.sb("th", [128, 32], F32)
        tot = m.sb("tot", [128, 1024], F32)
        cnt = m.sb("cnt", [128, 8], F32)
        nbf = m.sb("nbf", [128, 32], F32)
        nbe = m.sb("nbe", [128, 8], F32)
        nbm = m.sb("nbm", [128, 1], F32)
        nbi = m.sb("nbi", [128, 1], I32)
        ps = [m.ps("ps%d" % i, [128, 512]) for i in range(2)]
        m.dma("sp", g[:], gates, writes=[g])
        m.dma("pool", cm[:], cst, writes=[cm])
        m.dma("sp", th[:], thr, writes=[th])
        m.op("dve", lambda e: e.tensor_scalar(out=mk_[:], in0=g[:], scalar1=0.0, scalar2=None, op0=ALU.is_gt), reads=[g], writes=[mk_])
        for h in range(2):
            m.op("pe", lambda e: e.matmul(ps[h][:, :], lhsT=cm[:, 1, :], rhs=mk_[:, h * 512:(h + 1) * 512], start=True, stop=True), reads=[cm, mk_], writes=[ps[h]])
            m.op("dve", lambda e: e.tensor_copy(out=tot[:, h * 512:(h + 1) * 512], in_=ps[h][:, :]), reads=[ps[h]], writes=[tot])
        m.op("dve", lambda e: e.tensor_reduce(out=cnt[:, :], in_=tot[:, :].rearrange("p (t e) -> p e t", e=8), axis=AX.X, op=ALU.add), reads=[tot], writes=[cnt])
        for e_ in range(8):
            m.op("dve", lambda e: e.tensor_scalar(out=nbf[:], in0=th[:], scalar1=cnt[:, e_:e_ + 1], scalar2=None, op0=ALU.is_lt), reads=[th, cnt], writes=[nbf])
            m.op("dve", lambda e: e.tensor_reduce(out=nbe[:, e_:e_ + 1], in_=nbf[:], axis=AX.X, op=ALU.add), reads=[nbf], writes=[nbe])
        m.op("dve", lambda e: e.tensor_reduce(out=nbm[:, :], in_=nbe[:, :], axis=AX.X, op=ALU.max), reads=[nbe], writes=[nbm])
        m.op("dve", lambda e: e.tensor_copy(out=nbi[:], in_=nbm[:]), reads=[nbm], writes=[nbi])
        ob = Buf("o")
        m.dma("sp", nb_o, nbi[0:1, 0:1], reads=[nbi], writes=[ob])
        m.final_wait("sp", [ob])
    return nc


def build_l6s(NBLK, F=28, NTOK=16384, skip=""):
    nc = bass.Bass("TRN2", target_bir_lowering=False)
    D = lambda n, s, dt, k: nc.dram_tensor(n, s, dt, kind=k).ap()
    NT = NTOK // 128
    NH = NBLK
    SIZES = [CB] * (NH // 2) + ([512] if NH % 2 else [])
    CAP = NH * 512
    U = D("U", [NTOK, 1024], BF16, "ExternalInput")
    gate = D("gate", [128, NT], F32, "ExternalInput")
    tokid = D("tokid", [128, NT], F32, "ExternalInput")
    rowid = D("rowid", [128, 128], F32, "ExternalInput")
    cst = D("cst", [128, 3, 128], F32, "ExternalInput")
    wg = D("wg", [F, 128, 1024], F32, "ExternalInput")
    wu = D("wu", [F, 128, 1024], F32, "ExternalInput")
    wd = D("wd", [F, 128, 1024], F32, "ExternalInput")
    y = D("y", [NTOK, 1024], F32, "ExternalOutput")
    Uc = D("Uc_i", [CAP, 1028], BF16, "Internal")
    with ExitStack() as es:
        m = MK(nc, es)
        g = m.sb("g", [128, NT], F32)
        cm = m.sb("cm", [128, 3, 128], BF16)
        mk_ = m.sb("mk", [128, NT], F32)
        mkb = m.sb("mkb", [128, NT], BF16)
        onesr = m.sb("onesr", [128, NT], F32)
        tot = m.sb("tot", [128, NT], F32)
        cum = m.sb("cum", [128, NT], F32)
        pos = m.sb("pos", [128, NT], F32)
        posu = m.sb("posu", [128, NT], U32)
        Wd = m.sb("Wd", [128, F, 1024], BF16)
        ps_a = m.ps("ps_a", [128, 512])
        ps_T = m.ps("ps_T", [128, 1024], BF16)
        ps_g = [m.ps("ps_g%d" % i, [128, 512]) for i in range(2)]
        ps_u = [m.ps("ps_u%d" % i, [128, 512]) for i in range(2)]
        ps_y = [m.ps("ps_y%d" % i, [128, 512]) for i in range(2)]

        aux = m.sb("aux", [128, NT, 2], F32)
        zt = m.sb("zt", [128, 4, 1024], F32)
        by = Buf("y")
        bZ = Buf("yzero")
        m.op("dve", lambda e: e.memset(zt[:], 0.0), writes=[zt])
        m.dma("sp", g[:], gate, writes=[g])
        rid = m.sb("rid", [128, 128], F32)
        m.dma("sp", rid[:], rowid, writes=[rid])
        m.dma("sp", aux[:, :, 0], tokid, writes=[aux], allow_slow_non_contiguous=True) if False else None
        tk = m.sb("tk", [128, NT], F32)
        m.dma("sp", tk[:], tokid, writes=[tk])
        m.op("dve", lambda e: e.tensor_copy(out=aux[:, :, 0], in_=tk[:]), reads=[tk], writes=[aux])
        m.op("dve", lambda e: e.tensor_copy(out=aux[:, :, 1], in_=g[:]), reads=[g, aux], writes=[aux])
        m.dma("pool", cm[:], cst, writes=[cm])
        m.op("dve", lambda e: e.tensor_scalar(out=mk_[:], in0=g[:], scalar1=0.0, scalar2=None, op0=ALU.is_gt), reads=[g], writes=[mk_])
        m.op("dve", lambda e: e.tensor_copy(out=mkb[:], in_=mk_[:]), reads=[mk_], writes=[mkb])
        m.op("dve", lambda e: e.memset(onesr[:], 1.0), writes=[onesr])
        m.op("pe", lambda e: e.matmul(ps_a[:, 0:NT], lhsT=cm[:, 1, :], rhs=mkb[:, :], start=True, stop=True), reads=[cm, mkb], writes=[ps_a])
        m.op("dve", lambda e: e.tensor_copy(out=tot[:], in_=ps_a[:, 0:NT]), reads=[ps_a], writes=[tot])
        m.op("pe", lambda e: e.matmul(ps_a[:, 0:NT], lhsT=cm[:, 0, :], rhs=mkb[:, :], start=True, stop=True), reads=[cm, mkb], writes=[ps_a])
        m.op("dve", lambda e: e.tensor_tensor_scan(out=cum[:], data0=onesr[:], data1=tot[:], initial=0.0, op0=ALU.mult, op1=ALU.add), reads=[onesr, tot], writes=[cum])
        m.op("dve", lambda e: e.tensor_tensor(out=pos[:], in0=cum[:], in1=tot[:], op=ALU.subtract), reads=[cum, tot], writes=[pos])
        m.op("dve", lambda e: e.tensor_tensor(out=pos[:], in0=pos[:], in1=ps_a[:, 0:NT], op=ALU.add), reads=[pos, ps_a], writes=[pos])
        m.op("dve", lambda e: e.tensor_scalar(out=pos[:], in0=pos[:], scalar1=-1.0 - BIGPOS, scalar2=None, op0=ALU.add), reads=[pos], writes=[pos])
        m.op("dve", lambda e: e.tensor_tensor(out=pos[:], in0=pos[:], in1=mk_[:], op=ALU.mult), reads=[pos, mk_], writes=[pos])
        m.op("dve", lambda e: e.tensor_scalar(out=pos[:], in0=pos[:], scalar1=BIGPOS, scalar2=None, op0=ALU.add), reads=[pos], writes=[pos])
        m.op("dve", lambda e: e.tensor_copy(out=posu[:], in_=pos[:]), reads=[pos], writes=[posu])
        bUc = Buf("Uc")
        es_main = m.es
        es_b = ExitStack()
        m.es = es_b
        NUB = 4
        utb = [m.sb("utb%d" % i, [128, 4, 1028], BF16) for i in range(NUB)]
        m.es = es_main
        for gq_ in range(1 if "B" in skip else NT // 4):
            u = utb[gq_ % NUB]
            m.dma("sp", u[:, :, 0:1024], U[gq_ * 512:(gq_ + 1) * 512, :].rearrange("(p j) n -> p j n", j=4), writes=[u])
            for j in range(4):
                m.op("dve", lambda e: e.tensor_copy(out=u[:, j, 1024:1028].bitcast(F32), in_=aux[:, 4 * gq_ + j, :]), reads=[aux, u], writes=[u])
            for j in range(4):
                m.idma(Uc, u[:, j, :], posu[:, 4 * gq_ + j:4 * gq_ + j + 1], True, CAP - 1, reads=[u, posu], writes=[bUc])
        es_b.close()
        for gq_ in range(NT // 4):
            m.dma("act", y[gq_ * 512:(gq_ + 1) * 512, :].rearrange("(p j) n -> p j n", j=4), zt[:], reads=[zt, bUc], writes=[bZ])
        for f0 in range(0, F, 4):
            m.dma("pool", Wd[:, f0:f0 + 4, :], wd[f0:f0 + 4].rearrange("f p n -> p f n"), writes=[Wd])
        es_c = ExitStack()
        m.es = es_c
        ut = [m.sb("ut%d" % i, [128, 1028], BF16) for i in range(3)]
        auxc = m.sb("auxc", [128, CB // 128, 2], F32)
        tokc = m.sb("tokc", [128, CB // 128], U32)
        tokf = m.sb("tokf", [128, CB // 128], F32)
        vmf = m.sb("vmf", [128, CB // 128], F32)
        vmu = m.sb("vmu", [128, CB // 128], U32)
        uT = m.sb("uT", [128, 8, CB], BF16)
        act = m.sb("act", [128, F, CB], BF16)
        wgb = [m.sb("wg%d" % i, [128, 8, 128], BF16) for i in range(3)]
        wub = [m.sb("wu%d" % i, [128, 8, 128], BF16) for i in range(3)]
        sg = [m.sb("sg%d" % i, [128, 512], F32) for i in range(2)]
        yb = [m.sb("yb%d" % i, [128, 1024], F32) for i in range(2)]
        m.es = es_main
        wc = 0
        gc = 0
        yc = 0
        for blk, BS in enumerate([] if "C" in skip else SIZES):
            for tt in range(BS // 128):
                u = ut[tt % 3]
                r0 = blk * CB + tt * 128
                m.dma("sp", u[:], Uc[r0:r0 + 128, :], reads=[bUc], writes=[u])
                for c in range(8):
                    m.op("pe", lambda e: e.transpose(ps_T[:, c * 128:(c + 1) * 128], u[:, c * 128:(c + 1) * 128], cm[:, 2, :]), reads=[u, cm], writes=[ps_T])
                m.op("act", lambda e: e.activation(out=uT[:, :, tt * 128:(tt + 1) * 128], in_=ps_T[:, :].rearrange("p (c t) -> p c t", t=128), func=AF.Copy),
                     reads=[ps_T], writes=[uT])
                m.op("dve", lambda e: e.tensor_copy(out=auxc[:, tt, :], in_=u[:, 1024:1028].bitcast(F32)), reads=[u, auxc], writes=[auxc])
            nt_ = CB // 128
            m.op("dve", lambda e: e.tensor_scalar(out=vmf[:, :], in0=rid[:, blk * nt_:(blk + 1) * nt_], scalar1=cum[:, NT - 1:NT], scalar2=None, op0=ALU.is_lt),
                 reads=[rid, cum], writes=[vmf])
            m.op("dve", lambda e: e.tensor_copy(out=vmu[:, :], in_=vmf[:, :]), reads=[vmf], writes=[vmu])
            m.op("dve", lambda e: e.memset(tokf[:, :], BIGPOS), writes=[tokf])
            m.op("dve", lambda e: e.copy_predicated(out=tokf[:, :], mask=vmu[:, :], data=auxc[:, :, 0]), reads=[vmu, auxc, tokf], writes=[tokf])
            m.op("dve", lambda e: e.tensor_copy(out=tokc[:, :], in_=tokf[:, :]), reads=[tokf], writes=[tokc])
            for f in range(F):
                i = wc % 3
                wc += 1
                wgt, wut = wgb[i], wub[i]
                m.dma("pool", wgt[:].rearrange("p c n -> p (c n)"), wg[f], reads=[bUc], writes=[wgt])
                m.dma("pool", wut[:].rearrange("p c n -> p (c n)"), wu[f], reads=[bUc], writes=[wut])
                for ch in range(BS // 512):
                    j = gc % 2
                    gc += 1
                    G, Uu, sgb = ps_g[j], ps_u[j], sg[j]
                    for c in range(8):
                        m.op("pe", lambda e: e.matmul(G[:, :], lhsT=wgt[:, c, :], rhs=uT[:, c, ch * 512:(ch + 1) * 512], start=(c == 0), stop=(c == 7)), reads=[wgt, uT], writes=[G])
                    for c in range(8):
                        m.op("pe", lambda e: e.matmul(Uu[:, :], lhsT=wut[:, c, :], rhs=uT[:, c, ch * 512:(ch + 1) * 512], start=(c == 0), stop=(c == 7)), reads=[wut, uT], writes=[Uu])
                    m.op("act", lambda e: e.activation(out=sgb[:, :], in_=G[:, :], func=AF.Silu), reads=[G], writes=[sgb])
                    m.op("dve", lambda e: e.tensor_tensor(out=act[:, f, ch * 512:(ch + 1) * 512], in0=Uu[:, :], in1=sgb[:, :], op=ALU.mult), reads=[Uu, sgb], writes=[act])
            for tt in range(BS // 128):
                ybb = yb[tt % 2]
                for hf in range(2):
                    Y = ps_y[yc % 2]
                    yc += 1
                    for f in range(F):
                        m.op("pe", lambda e: e.matmul(Y[:, :], lhsT=act[:, f, tt * 128:(tt + 1) * 128], rhs=Wd[:, f, hf * 512:(hf + 1) * 512], start=(f == 0), stop=(f == F - 1)),
                             reads=[act, Wd], writes=[Y])
                    m.op("act", lambda e: e.activation(out=ybb[:, hf * 512:(hf + 1) * 512], in_=Y[:, :], func=AF.Copy, scale=auxc[:, tt, 1:2]),
                         reads=[Y, auxc], writes=[ybb])
                m.idma(y, ybb[:], tokc[:, tt:tt + 1], True, NTOK - 1, reads=[ybb, tokc, bZ], writes=[by])
        es_c.close()
        m.final_wait("sp", [by, bZ])
        print("L6s instrs", m.n_ins, "dmas", m.ndma, "NBLK", NBLK)
    return nc


def build_l7t(NT=2048):
    nc = bass.Bass("TRN2", target_bir_lowering=False)
    D = lambda n, s, dt, k: nc.dram_tensor(n, s, dt, kind=k).ap()
    h = D("h", [NT, 1024], F32, "ExternalInput")
    ys = D("ys", [8, NT, 1024], F32, "ExternalInput")
    out = D("out", [NT, 1024], F32, "ExternalOutput")
    with ExitStack() as es:
        m = MK(nc, es)
        acc = [m.sb("acc%d" % i, [128, 4, 1024], F32) for i in range(2)]
        yb = [m.sb("yb%d" % i, [128, 4, 1024], F32) for i in range(3)]
        ob = Buf("out")
        k = 0
        for t in range(NT // 512):
            a = acc[t % 2]
            m.dma("sp", a[:], h[t * 512:(t + 1) * 512, :].rearrange("(j p) n -> p j n", p=128), writes=[a])
            for e_ in range(8):
                yy = yb[k % 3]
                k += 1
                m.dma("sp", yy[:], ys[e_, t * 512:(t + 1) * 512, :].rearrange("(j p) n -> p j n", p=128), writes=[yy])
                m.op("dve", lambda e: e.tensor_tensor(out=a[:], in0=a[:], in1=yy[:], op=ALU.add), reads=[a, yy], writes=[a])
            m.dma("sp", out[t * 512:(t + 1) * 512, :].rearrange("(j p) n -> p j n", p=128), a[:], reads=[a], writes=[ob])
        m.final_wait("sp", [ob])
    return nc

H_NA=6; HD=64
def fm(a):
    T = a.shape[0]
    return np.ascontiguousarray(a.reshape(T, 8, 128).transpose(2, 1, 0))
def wt(w):
    K, N = w.shape
    return np.ascontiguousarray(w.reshape(K // 128, 128, N).transpose(1, 0, 2))
def gvec(g):
    return np.ascontiguousarray(g.reshape(8, 128).T)
def cmat():
    ones = np.ones((128, 128), np.float32)
    blk = np.zeros((128, 128), np.float32); blk[:64, :64] = 1; blk[64:, 64:] = 1
    perm = np.zeros((128, 128), np.float32)
    for mm in range(128):
        k = (mm // 64) * 64 + ((mm % 64) + 32) % 64
        perm[k, mm] = 1
    return np.ascontiguousarray(np.stack([ones, blk, perm], axis=1))
def cs_table(pos):
    half = 32
    inv = np.power(np.float32(10000.0), -(2.0 / 64) * np.arange(half, dtype=np.float32)).astype(np.float32)
    ang = pos.astype(np.float32)[:, None] * inv[None, :]
    cos = np.cos(ang).astype(np.float32); sin = np.sin(ang).astype(np.float32)
    d = np.arange(128) % 64
    c = cos[:, d % 32].T
    s = np.where((d < 32)[:, None], -sin[:, d % 32].T, sin[:, d % 32].T)
    return np.ascontiguousarray(np.stack([c, s], axis=1).astype(np.float32))
def gqk_table(inp, l):
    t = np.zeros((128, 8), np.float32)
    d = np.arange(128) % 64
    t[:, 0] = inp["g_qk_na"][l, 0][d]; t[:, 1] = inp["g_qk_na"][l, 1][d]
    t[:, 2] = inp["g_qk_dil"][l, 0][d]; t[:, 3] = inp["g_qk_dil"][l, 1][d]
    t[:, 4] = inp["g_qk_mem"][l, 0][d]; t[:, 5] = inp["g_qk_mem"][l, 1][d]
    return t

BF = ml_dtypes.bfloat16
def cmask_table():
    k = np.arange(128)[:, None]; q = np.arange(128)[None, :]
    out = np.zeros((128, 17, 128), np.float32)
    for j in range(17):
        off = 128 * (j - 8) + k - q
        c = np.zeros((128, 128), np.float32)
        for w, d in ((128, 1), (512, 4), (2048, 16)):
            c += ((off % d == 0) & (np.abs(off) <= w // 2)).astype(np.float32)
        out[:, j, :] = c
    return np.ascontiguousarray(out.reshape(128, 17 * 128))
def na_bias(rpb_l, p, gb):
    ws = min(max(gb - 2, 0), 59)
    k = np.arange(128); q = np.arange(128)
    out = np.full((128, 2, 5, 128), -100.0, np.float32)
    qrow = 2 * gb + q // 64; qcol = q % 64
    rstart = np.clip(qrow - 4, 0, 120); cstart = np.clip(qcol - 8, 0, 48)
    for j in range(5):
        krow = 2 * (ws + j) + k // 64; kcol = k % 64
        inr = (krow[:, None] >= rstart[None, :]) & (krow[:, None] < rstart[None, :] + 8)
        inc = (kcol[:, None] >= cstart[None, :]) & (kcol[:, None] < cstart[None, :] + 16)
        ok = inr & inc
        ri = np.clip(krow[:, None] - qrow[None, :] + 7, 0, 14); ci = np.clip(kcol[:, None] - qcol[None, :] + 15, 0, 30)
        for hh in range(2):
            vals = rpb_l[2 * p + hh][ri, ci]
            out[:, hh, j, :] = np.where(ok, vals, np.float32(-100.0))
    return out.reshape(128, 1280)
def prep_pb(core, l, inp, pa_res, h_full_T):
    b = core // 4; ci = core % 4; s0 = ci * 2048; tile0 = ci * 16
    K = np.concatenate([np.asarray(pa_res[b * 4 + c]["kT"]) for c in range(4)], axis=2)
    V = np.concatenate([np.asarray(pa_res[b * 4 + c]["v"]) for c in range(4)], axis=1)
    Kp = np.zeros((128, 6, 8192 + 2048), K.dtype); Kp[:, :, 1024:1024 + 8192] = K
    Vp = np.zeros((128, 64 + 16, 780), V.dtype); Vp[:, 8:72] = V
    r = pa_res[core]
    d = {"qT": np.asarray(r["qT"])}
    d["kA"] = np.ascontiguousarray(Kp[:, 0:3, 1024 + s0 - 256:1024 + s0 + 2304])
    d["kB"] = np.ascontiguousarray(Kp[:, 3:6, s0:s0 + 4096])
    vA = Vp[:, 8 + tile0 - 2:8 + tile0 + 18, 0:390].reshape(128, 20, 3, 130).transpose(0, 2, 1, 3)
    vB = Vp[:, tile0:tile0 + 32, 390:780].reshape(128, 32, 3, 130).transpose(0, 2, 1, 3)
    d["vA"] = np.ascontiguousarray(vA); d["vB"] = np.ascontiguousarray(vB)
    kAe = np.zeros((128, 3, 4, 640), K.dtype); vAe = np.zeros((128, 3, 4, 650), V.dtype)
    for slot, i in enumerate((0, 1, 14, 15)):
        gb = tile0 + i; ws = min(max(gb - 2, 0), 59)
        kAe[:, :, slot, :] = K[:, 0:3, ws * 128:ws * 128 + 640]
        vv = V[:, ws:ws + 5, 0:390].reshape(128, 5, 3, 130).transpose(0, 2, 1, 3)
        vAe[:, :, slot, :] = vv.reshape(128, 3, 650)
    d["kAe"] = kAe; d["vAe"] = vAe
    d["kM"] = np.asarray(r["kmT"])
    d["vM"] = np.ascontiguousarray(np.asarray(r["vm"]).reshape(128, 2, 2, 130).transpose(0, 2, 1, 3))
    nab = np.zeros((128, 3, 5, 1280), np.float32)
    for p in range(3):
        for v, i in enumerate((0, 1, 2, 14, 15)):
            nab[:, p, v, :] = na_bias(inp["rpb_na"][l], p, tile0 + i)
    d["nab"] = nab
    d["cmask"] = cmask_table()
    d["ident"] = np.eye(128, dtype=np.float32)
    d["gout"] = np.ascontiguousarray(np.broadcast_to(inp["g_out"][l][None, :], (128, 1024))).astype(np.float32)
    d["w_out"] = wt(inp["w_out"][l])
    d["hT"] = h_full_T[core]
    return d
def prep_pa(core, l, inp, hT_core):
    b = core // 4; s0 = (core % 4) * 2048
    return {"hT": hT_core, "w_in": wt(inp["w_in"][l]), "gattn": gvec(inp["g_attn"][l]), "gqk": gqk_table(inp, l), "cmat": cmat(),
            "cs": cs_table(np.arange(s0, s0 + 2048)), "memT": fm(inp["mem"][b]), "gmem": gvec(inp["g_mem"][l]), "w_kv": wt(inp["w_mem_kv"][l])}

def wgu_t(w, F):
    return np.ascontiguousarray(w.reshape(8, 128, F, 128).transpose(2, 1, 0, 3).reshape(F, 128, 1024))
def wd_t(w, F):
    return np.ascontiguousarray(w.reshape(F, 128, 8, 128).transpose(2, 1, 0, 3).reshape(8, 128, F * 128))
def unfm(aT):
    return np.ascontiguousarray(aT.transpose(2, 1, 0).reshape(aT.shape[2], 1024))

def cst3():
    tri = (np.arange(128)[:, None] <= np.arange(128)[None, :]).astype(np.float32)
    return np.ascontiguousarray(np.stack([tri, np.ones((128, 128), np.float32), np.eye(128, dtype=np.float32)], 1))

from concourse.bass_utils import run_bass_kernel_spmd
_CORES = list(range(8))
_PROGS = {}


def _prog(name, fn):
    if name not in _PROGS:
        _PROGS[name] = fn()
    return _PROGS[name]


def _run(name, fn, ims):
    nc = _prog(name, fn)
    return run_bass_kernel_spmd(nc, ims, core_ids=_CORES).results


def _attn_layer(inp, l, hTs):
    ra = _run("pa", build_pa, [prep_pa(c, l, inp, hTs[c]) for c in range(8)])
    rb = _run("pb", build_pb, [prep_pb(c, l, inp, ra, hTs) for c in range(8)])
    return [np.asarray(rb[c]["hmidT"]) for c in range(8)]


def kernel(**inp):
    inp = {k: np.asarray(v) for k, v in inp.items()}
    x = inp["x"]
    hTs = [fm(x[c // 4, (c % 4) * 2048:(c % 4 + 1) * 2048]) for c in range(8)]
    hm = _attn_layer(inp, 0, hTs)
    wg = wgu_t(inp["w_gate_dense"][0], 22); wu = wgu_t(inp["w_up_dense"][0], 22); wd = wd_t(inp["w_down_dense"][0], 22)
    cm = cmat()
    rc = _run("pc", build_pc_dense, [{"hT": hm[c], "gffn": gvec(inp["g_ffn"][0]), "cmat": cm, "wg": wg, "wu": wu, "wd": wd} for c in range(8)])
    h1 = [np.asarray(rc[c]["outT"]) for c in range(8)]
    hm1 = _attn_layer(inp, 1, h1)
    wr = wt(inp["w_router"][0])
    r5 = _run("l5", build_l5, [{"hT": hm1[c], "gffn": gvec(inp["g_ffn"][1]), "cmat": cm, "wr": wr} for c in range(8)])
    U_all = np.ascontiguousarray(np.concatenate([np.asarray(r5[c]["uT"]).transpose(2, 1, 0).reshape(2048, 1024) for c in range(8)], axis=0))
    gates = np.concatenate([np.asarray(r5[c]["gates"]).transpose(1, 0, 2).reshape(2048, 8) for c in range(8)], axis=0)
    g_lay = np.ascontiguousarray(gates.reshape(128, 128, 8).transpose(1, 0, 2).reshape(128, 1024))
    thr = np.ascontiguousarray(np.broadcast_to((512.0 * np.arange(32, dtype=np.float32))[None, :], (128, 32)))
    c3 = cst3()
    nb = run_bass_kernel_spmd(_prog("l5b", build_l5b), [{"gates_all": g_lay, "cst": np.ascontiguousarray(c3[:, 0:2, :]), "thr": thr}], core_ids=[0]).results[0]["nblk"]
    NBLK = max(1, int(np.asarray(nb).reshape(-1)[0]))
    rowid = np.ascontiguousarray((np.arange(128)[None, :] * 128 + np.arange(128)[:, None]).astype(np.float32))
    tokid = np.ascontiguousarray(np.arange(16384, dtype=np.float32).reshape(32, 128, 4).transpose(1, 0, 2).reshape(128, 128))
    ims = []
    for e in range(8):
        ims.append({"U": U_all, "tokid": tokid, "rowid": rowid, "gate": np.ascontiguousarray(gates[:, e].reshape(32, 128, 4).transpose(1, 0, 2).reshape(128, 128)), "cst": c3,
                    "wg": wgu_t(inp["w_gate_moe"][0, e], 28), "wu": wgu_t(inp["w_up_moe"][0, e], 28),
                    "wd": np.ascontiguousarray(inp["w_down_moe"][0, e].reshape(28, 128, 1024))})
    r6 = _run("l6s_%d" % NBLK, lambda: build_l6s(NBLK), ims)
    ims = []
    for c in range(8):
        ys = np.ascontiguousarray(np.stack([np.asarray(r6[e]["y"])[c * 2048:(c + 1) * 2048] for e in range(8)]))
        ims.append({"h": unfm(hm1[c]), "ys": ys})
    r7 = _run("l7t", build_l7t, ims)
    out = np.stack([np.asarray(r7[c]["out"]) for c in range(8)]).reshape(2, 8192, 1024)
    return out.astype(np.float32)
```
